# Optimizing a Trainium2 kernel written in Bass

```python
import jax, jax.numpy as jnp
from jax import lax
import numpy as np

D_MODEL = 1024
BATCH = 4
SEQ = 4096
DEPTH = 1

GRID_W = 64
CTX_LEN = 256
HEAD_DIM = 64
NA_HEADS = 8
NA_WIN_R = 8
NA_WIN_C = 16
GQA_HEADS = 8
GQA_KV_HEADS = 2
Q_BLOCK = 128
ROPE_THETA = 10000.0
N_EXPERTS = 32
TOP_K = 4
D_FF = D_MODEL
SWIGLU_LIMIT = 7.0
SWIGLU_ALPHA = 1.702
EXPERT_BLOCK = 128
NORM_EPS = 1e-6

NA_WIDTH = NA_HEADS * HEAD_DIM
GQA_WIDTH = GQA_HEADS * HEAD_DIM
GQA_KV_WIDTH = GQA_KV_HEADS * HEAD_DIM
IN_SPLITS = (NA_WIDTH, NA_WIDTH, GQA_KV_WIDTH, GQA_KV_WIDTH, NA_WIDTH, GQA_WIDTH, D_MODEL, D_MODEL)
KV_COLS = 2 * NA_WIDTH + 2 * GQA_KV_WIDTH
IN_COLS = sum(IN_SPLITS)

kernel_name = 'hybrid_natten_gqa_moe_dit_layer'


def rmsnorm(x, g):
    xf = x.astype(jnp.float32)
    y = xf * lax.rsqrt(jnp.mean(xf * xf, axis=-1, keepdims=True) + NORM_EPS)
    return (y * g.astype(jnp.float32)).astype(x.dtype)


def split_cols(p, sizes):
    return jnp.split(p, np.cumsum(sizes)[:-1].tolist(), axis=-1)


def to_heads(t, n):
    return t.reshape(t.shape[0], t.shape[1], n, HEAD_DIM)


def axial_rope(x, row, col):
    half = HEAD_DIM // 2
    nf = half // 2
    freqs = ROPE_THETA ** (-jnp.arange(nf, dtype=jnp.float32) / nf)

    def rot(xp, pos):
        ang = pos.astype(jnp.float32)[:, None] * freqs
        cos = jnp.cos(ang)[None, :, None, :]
        sin = jnp.sin(ang)[None, :, None, :]
        x1 = xp[..., :nf].astype(jnp.float32)
        x2 = xp[..., nf:].astype(jnp.float32)
        return jnp.concatenate([x1 * cos - x2 * sin, x2 * cos + x1 * sin], axis=-1)

    return jnp.concatenate([rot(x[..., :half], row), rot(x[..., half:], col)], axis=-1).astype(x.dtype)


def gqa_attend(q, k, v):
    s = jnp.einsum('btkgd,bskd->bkgts', q, k)
    p = jax.nn.softmax(s.astype(jnp.float32), axis=-1).astype(q.dtype)
    return jnp.einsum('bkgts,bskd->btkgd', p, v)


def blocked_gqa(q, k, v):
    B, S = q.shape[0], q.shape[1]
    nb = S // Q_BLOCK
    qb = q.reshape(B, nb, Q_BLOCK, *q.shape[2:]).swapaxes(0, 1)
    out = lax.map(lambda blk: gqa_attend(blk, k, v), qb)
    return out.swapaxes(0, 1).reshape(B, S, -1)


def neighbourhood_attention(q, k, v, k_ctx, v_ctx, rpb):
    B, S, H, Dh = q.shape
    rows = S // GRID_W
    kr = min(NA_WIN_R, rows)
    kc = min(NA_WIN_C, GRID_W)
    qg = q.reshape(B, rows, GRID_W, H, Dh)
    kg = k.reshape(B, rows, GRID_W, H, Dh)
    vg = v.reshape(B, rows, GRID_W, H, Dh)
    cols = np.arange(GRID_W)
    c0 = np.clip(cols - kc // 2, 0, GRID_W - kc)
    col_idx = (c0[:, None] + np.arange(kc)[None, :]).astype(np.int32)
    col_off = (col_idx - cols[:, None] + (NA_WIN_C - 1)).astype(np.int32)

    def row_block(r):
        r0 = jnp.clip(r - kr // 2, 0, rows - kr)
        q_r = lax.dynamic_index_in_dim(qg, r, axis=1, keepdims=False)
        k_nb = lax.dynamic_slice_in_dim(kg, r0, kr, axis=1)[:, :, col_idx]
        v_nb = lax.dynamic_slice_in_dim(vg, r0, kr, axis=1)[:, :, col_idx]
        row_off = r0 + jnp.arange(kr, dtype=jnp.int32) - r + (NA_WIN_R - 1)
        bias = rpb[:, row_off[:, None, None], col_off[None, :, :]].transpose(0, 2, 1, 3)
        s_loc = jnp.einsum('bwhd,brwchd->bhwrc', q_r, k_nb) + bias.astype(q.dtype)
        s_ctx = jnp.einsum('bwhd,bnhd->bhwn', q_r, k_ctx)
        s = jnp.concatenate([s_loc.reshape(B, H, GRID_W, kr * kc), s_ctx], axis=-1)
        p = jax.nn.softmax(s.astype(jnp.float32), axis=-1).astype(q.dtype)
        p_loc = p[..., :kr * kc].reshape(B, H, GRID_W, kr, kc)
        p_ctx = p[..., kr * kc:]
        return (jnp.einsum('bhwrc,brwchd->bwhd', p_loc, v_nb)
                + jnp.einsum('bhwn,bnhd->bwhd', p_ctx, v_ctx))

    out = lax.map(row_block, jnp.arange(rows, dtype=jnp.int32))
    return out.transpose(1, 0, 2, 3, 4).reshape(B, S, H * Dh)


def merge_branches(o_a, o_b, g_a, g_b, w_out_a, w_out_b, w_o):
    return (jax.nn.sigmoid(g_a) * (o_a @ w_out_a) + jax.nn.sigmoid(g_b) * (o_b @ w_out_b)) @ w_o


def hybrid_mixer(h, h_ctx, w_in, rpb, g_qn, g_kn, w_out_a, w_out_b, w_o, row, col, update_ctx):
    B, S, _ = h.shape
    C = h_ctx.shape[1]
    scale = HEAD_DIM ** -0.5
    G = GQA_HEADS // GQA_KV_HEADS
    k_a, v_a, k_b, v_b, q_a, q_b, g_a, g_b = split_cols(h @ w_in, IN_SPLITS)
    if update_ctx:
        parts = split_cols(h_ctx @ w_in, IN_SPLITS)
    else:
        parts = split_cols(h_ctx @ w_in[:, :KV_COLS], IN_SPLITS[:4])
    k_ca = to_heads(parts[0], NA_HEADS)
    v_ca = to_heads(parts[1], NA_HEADS)
    k_cb = rmsnorm(to_heads(parts[2], GQA_KV_HEADS), g_kn)
    v_cb = to_heads(parts[3], GQA_KV_HEADS)

    o_a = neighbourhood_attention(to_heads(q_a, NA_HEADS) * scale, to_heads(k_a, NA_HEADS),
                                  to_heads(v_a, NA_HEADS), k_ca, v_ca, rpb)
    qb = axial_rope(rmsnorm(to_heads(q_b, GQA_HEADS), g_qn), row, col) * scale
    kb = axial_rope(rmsnorm(to_heads(k_b, GQA_KV_HEADS), g_kn), row, col)
    k_all = jnp.concatenate([kb, k_cb], axis=1)
    v_all = jnp.concatenate([to_heads(v_b, GQA_KV_HEADS), v_cb], axis=1)
    o_b = blocked_gqa(qb.reshape(B, S, GQA_KV_HEADS, G, HEAD_DIM), k_all, v_all)
    y = merge_branches(o_a, o_b, g_a, g_b, w_out_a, w_out_b, w_o)

    y_ctx = None
    if update_ctx:
        q_ca, q_cb, g_ca, g_cb = parts[4:]
        o_ca = gqa_attend(to_heads(q_ca, NA_HEADS)[:, :, :, None, :] * scale, k_ca, v_ca).reshape(B, C, -1)
        qcb = rmsnorm(to_heads(q_cb, GQA_HEADS), g_qn) * scale
        o_cb = gqa_attend(qcb.reshape(B, C, GQA_KV_HEADS, G, HEAD_DIM), k_cb, v_cb).reshape(B, C, -1)
        y_ctx = merge_branches(o_ca, o_cb, g_ca, g_cb, w_out_a, w_out_b, w_o)
    return y, y_ctx


def moe_ffn(h, w_router, b_router, w_gu, b_gu, w_dn, b_dn):
    T, D = h.shape
    logits = (h @ w_router + b_router).astype(jnp.float32)
    top_logits, top_e = lax.top_k(logits, TOP_K)
    top_w = jax.nn.softmax(top_logits, axis=-1).astype(h.dtype)
    n_assign = T * TOP_K
    flat_e = top_e.reshape(-1)
    flat_tok = jnp.arange(n_assign, dtype=jnp.int32) // TOP_K
    order = jnp.argsort(flat_e)
    e_sorted = flat_e[order]
    counts = jnp.bincount(flat_e, length=N_EXPERTS)
    padded = (counts + EXPERT_BLOCK - 1) // EXPERT_BLOCK * EXPERT_BLOCK
    pad_end = jnp.cumsum(padded)
    pad_start = pad_end - padded
    start = jnp.cumsum(counts) - counts
    dest = pad_start[e_sorted] + jnp.arange(n_assign, dtype=jnp.int32) - start[e_sorted]
    n_blocks = -(-n_assign // EXPERT_BLOCK) + N_EXPERTS
    n_slots = n_blocks * EXPERT_BLOCK
    slot_tok = jnp.full((n_slots,), T, dtype=jnp.int32).at[dest].set(flat_tok[order])
    slot_w = jnp.zeros((n_slots,), h.dtype).at[dest].set(top_w.reshape(-1)[order])
    block_e = jnp.minimum(jnp.searchsorted(pad_end, jnp.arange(n_blocks, dtype=jnp.int32) * EXPERT_BLOCK,
                                           side='right'), N_EXPERTS - 1)
    h_pad = jnp.concatenate([h, jnp.zeros((1, D), h.dtype)], axis=0)
    xb = h_pad[slot_tok].reshape(n_blocks, EXPERT_BLOCK, D)

    def expert_block(args):
        xe, e = args
        gu = xe @ w_gu[e] + b_gu[e]
        x_glu = jnp.minimum(gu[:, :D_FF], SWIGLU_LIMIT)
        x_lin = jnp.clip(gu[:, D_FF:], -SWIGLU_LIMIT, SWIGLU_LIMIT)
        act = (x_lin + 1) * (x_glu * jax.nn.sigmoid(SWIGLU_ALPHA * x_glu))
        return act @ w_dn[e] + b_dn[e]

    yb = lax.map(expert_block, (xb, block_e))
    y = jnp.zeros((T + 1, D), h.dtype).at[slot_tok].add(yb.reshape(n_slots, D) * slot_w[:, None])
    return y[:T]


def setup_inputs(seed: int = 0) -> dict:
    key = jax.random.key(seed)
    ks = jax.random.split(key, 23)
    f32 = jnp.float32
    L = DEPTH

    def nrm(k, shape, s):
        return jax.random.normal(k, shape, f32) * s

    def gain(k, shape):
        return 1.0 + 0.05 * jax.random.normal(k, shape, f32)

    return {
        'x': nrm(ks[0], (BATCH, SEQ, D_MODEL), 1.0),
        'c': nrm(ks[1], (BATCH, D_MODEL), 1.0),
        'ctx': nrm(ks[2], (BATCH, CTX_LEN, D_MODEL), 1.0),
        'c_ctx': nrm(ks[3], (D_MODEL,), 1.0),
        'w_mod': nrm(ks[4], (L, D_MODEL, 6 * D_MODEL), 0.5 * D_MODEL ** -0.5),
        'b_mod': nrm(ks[5], (L, 6 * D_MODEL), 0.02),
        'g_pre_mix': gain(ks[6], (L, D_MODEL)),
        'g_post_mix': gain(ks[7], (L, D_MODEL)),
        'g_pre_ffn': gain(ks[8], (L, D_MODEL)),
        'g_post_ffn': gain(ks[9], (L, D_MODEL)),
        'w_in': nrm(ks[10], (L, D_MODEL, IN_COLS), D_MODEL ** -0.5),
        'rpb': nrm(ks[11], (L, NA_HEADS, 2 * NA_WIN_R - 1, 2 * NA_WIN_C - 1), 0.1),
        'g_qnorm': gain(ks[12], (L, HEAD_DIM)),
        'g_knorm': gain(ks[13], (L, HEAD_DIM)),
        'w_out_a': nrm(ks[14], (L, NA_WIDTH, D_MODEL), NA_WIDTH ** -0.5),
        'w_out_b': nrm(ks[15], (L, GQA_WIDTH, D_MODEL), GQA_WIDTH ** -0.5),
        'w_o': nrm(ks[16], (L, D_MODEL, D_MODEL), D_MODEL ** -0.5),
        'w_router': nrm(ks[17], (L, D_MODEL, N_EXPERTS), D_MODEL ** -0.5),
        'b_router': nrm(ks[18], (L, N_EXPERTS), 0.01),
        'w_gu': nrm(ks[19], (L, N_EXPERTS, D_MODEL, 2 * D_FF), D_MODEL ** -0.5),
        'b_gu': nrm(ks[20], (L, N_EXPERTS, 2 * D_FF), 0.02),
        'w_dn': nrm(ks[21], (L, N_EXPERTS, D_FF, D_MODEL), D_FF ** -0.5),
        'b_dn': nrm(ks[22], (L, N_EXPERTS, D_MODEL), 0.02),
    }


def reference(x, c, ctx, c_ctx, w_mod, b_mod, g_pre_mix, g_post_mix, g_pre_ffn, g_post_ffn,
              w_in, rpb, g_qnorm, g_knorm, w_out_a, w_out_b, w_o,
              w_router, b_router, w_gu, b_gu, w_dn, b_dn):
    B, S, D = x.shape
    C = ctx.shape[1]
    t = jnp.arange(S, dtype=jnp.int32)
    row = t // GRID_W
    col = t % GRID_W
    silu_c = jax.nn.silu(c)
    silu_cc = jax.nn.silu(c_ctx)
    for i in range(DEPTH):
        update_ctx = i < DEPTH - 1
        sh1, sc1, gt1, sh2, sc2, gt2 = jnp.split((silu_c @ w_mod[i] + b_mod[i])[:, None, :], 6, axis=-1)
        n_mod = 6 if update_ctx else 2
        mod_c = jnp.split(silu_cc @ w_mod[i][:, :n_mod * D] + b_mod[i][:n_mod * D], n_mod)

        h = rmsnorm(x, g_pre_mix[i]) * (1 + sc1) + sh1
        h_ctx = rmsnorm(ctx, g_pre_mix[i]) * (1 + mod_c[1]) + mod_c[0]
        y, y_ctx = hybrid_mixer(h, h_ctx, w_in[i], rpb[i], g_qnorm[i], g_knorm[i],
                                w_out_a[i], w_out_b[i], w_o[i], row, col, update_ctx)
        x = x + gt1 * rmsnorm(y, g_post_mix[i])

        h2 = rmsnorm(x, g_pre_ffn[i]) * (1 + sc2) + sh2
        tokens = h2.reshape(B * S, D)
        if update_ctx:
            ctx = ctx + mod_c[2] * rmsnorm(y_ctx, g_post_mix[i])
            h2_ctx = rmsnorm(ctx, g_pre_ffn[i]) * (1 + mod_c[4]) + mod_c[3]
            tokens = jnp.concatenate([tokens, h2_ctx.reshape(B * C, D)], axis=0)
        f = moe_ffn(tokens, w_router[i], b_router[i], w_gu[i], b_gu[i], w_dn[i], b_dn[i])
        x = x + gt2 * rmsnorm(f[:B * S].reshape(B, S, D), g_post_ffn[i])
        if update_ctx:
            ctx = ctx + mod_c[5] * rmsnorm(f[B * S:].reshape(B, C, D), g_post_ffn[i])
    return x
```

```python
import numpy as np
from contextlib import ExitStack
import concourse.bass as bass
import concourse.mybir as mybir
from concourse.bass_utils import run_bass_kernel_spmd

F32 = mybir.dt.float32; BF16 = mybir.dt.bfloat16; I32 = mybir.dt.int32; U8 = mybir.dt.uint8
AF = mybir.ActivationFunctionType; ALU = mybir.AluOpType; AX = mybir.AxisListType
ENG = ('tensor', 'vector', 'scalar', 'gpsimd', 'sync')
DSZ = {F32: 4, BF16: 2, I32: 4, U8: 1}

D = 1024; NTR = 18; TOKR = 2304; NTALL = 34; NKEY = 4352; NE = 32
NBLK = 104; NSLOT = NBLK * 128
EPS = 1e-6; NEG = -30000.0; BIG = 1.0e6
STAGE = 99
SAME_ENG_SYNC = True
DEBUG = []


class Sched:
    def __init__(self, nc, stack):
        self.nc = nc; self.stack = stack
        self.ops = {e: [] for e in ENG}
        self.sems = {}; self.cnt = {}
        self.last_write = {}; self.readers = {}
        self.waited = {e: {} for e in ENG}

    def sem(self, name):
        if name not in self.sems:
            self.sems[name] = self.stack.enter_context(self.nc.semaphore(name)); self.cnt[name] = 0
        return self.sems[name]

    def op(self, eng, fn, reads=(), writes=(), dma=None):
        waits = {}
        isdma_op = dma is not None

        def need(tok):
            if tok is None:
                return
            sname, val, teng, isdma = tok
            if teng == eng and not isdma and not isdma_op and (eng == 'tensor' or not SAME_ENG_SYNC):
                return
            if self.waited[eng].get(sname, 0) >= val:
                return
            waits[sname] = max(waits.get(sname, 0), val)
        for b in reads:
            need(self.last_write.get(b))
        for b in writes:
            need(self.last_write.get(b))
            for r in self.readers.get(b, ()):
                need(r)
        for s, v in waits.items():
            self.waited[eng][s] = v
        if isdma_op:
            sname = dma; inc = 16
        else:
            sname = 'e_' + eng; inc = 1
        self.sem(sname); self.cnt[sname] += inc
        tok = (sname, self.cnt[sname], eng, isdma_op)
        for b in writes:
            self.last_write[b] = tok; self.readers[b] = []
        for b in reads:
            self.readers.setdefault(b, []).append(tok)
        self.ops[eng].append((list(waits.items()), fn, sname, inc))
        return tok

    def barrier(self):
        for e in ENG:
            waits = []
            for s, c in self.cnt.items():
                if c > 0 and self.waited[e].get(s, 0) < c and s != 'e_' + e:
                    waits.append((s, c)); self.waited[e][s] = c
            if waits:
                self.ops[e].append((waits, None, None, None))
        self.last_write = {}; self.readers = {}

    def emit(self):
        with self.nc.Block() as block:
            for eng in ENG:
                ops = self.ops[eng]
                if not ops:
                    continue

                def body(e, ops=ops):
                    for waits, fn, sname, inc in ops:
                        for s, v in waits:
                            e.wait_ge(self.sems[s], v)
                        if fn is not None:
                            fn(e).then_inc(self.sems[sname], inc)
                getattr(block, eng)(body)


class Arena:
    def __init__(self, nc, st, name, nbytes):
        self.t = st.enter_context(nc.sbuf_tensor(name, [128, nbytes], U8)); self.off = 0; self.n = nbytes; self.name = name

    def alloc(self, free_shape, dt):
        n = int(np.prod(free_shape)) * DSZ[dt]
        n_al = (n + 63) // 64 * 64
        assert self.off + n_al <= self.n, (self.name, self.off, n_al, self.n)
        ap = self.t[:, self.off:self.off + n].bitcast(dt)
        self.off += n_al
        if len(free_shape) == 2:
            ap = ap.rearrange("p (a b) -> p a b", a=free_shape[0])
        elif len(free_shape) == 3:
            ap = ap.rearrange("p (a b c) -> p a b c", a=free_shape[0], b=free_shape[1])
        return ap

    def reset(self, off=0):
        self.off = off


def build():
    nc = bass.Bass("TRN2", target_bir_lowering=False)
    dt_in = lambda name, shape, dt=F32: nc.dram_tensor(name, shape, dt, kind="ExternalInput").ap()
    xc = dt_in("xc", [NKEY, D]); cvec = dt_in("cvec", [2, D]); w_mod = dt_in("w_mod", [D, 6 * D]); b_mod = dt_in("b_mod", [6 * D])
    gvec = dt_in("gvec", [4, D]); w_in = dt_in("w_in", [D, 4352]); tbl = dt_in("tbl", [4, 128, 960]); gqk = dt_in("gqk", [2, 64])
    ropec = dt_in("ropec", [NKEY, 512]); ropes = dt_in("ropes", [NKEY, 512])
    w_oa = dt_in("w_oa", [512, D]); w_ob = dt_in("w_ob", [512, D]); w_o = dt_in("w_o", [D, D])
    w_r = dt_in("w_r", [D, NE]); b_r = dt_in("b_r", [NE])
    w_gu = dt_in("w_gu", [NE, D, 2 * D]); b_gu = dt_in("b_gu", [NE, 2 * D]); w_dn = dt_in("w_dn", [NE, D, D]); b_dn = dt_in("b_dn", [NE, D])
    out = nc.dram_tensor("out", [TOKR, D], F32, kind="ExternalOutput").ap()
    hT_scr = nc.dram_tensor("hT_scr", [128, 8, NKEY + 128], BF16, kind="Internal").ap()
    x1_scr = nc.dram_tensor("x1_scr", [TOKR, D], F32, kind="Internal").ap()
    h2_scr = nc.dram_tensor("h2_scr", [TOKR, D], BF16, kind="Internal").ap()
    xs_scr = nc.dram_tensor("xs_scr", [NSLOT, D], BF16, kind="Internal").ap()
    y_scr = nc.dram_tensor("y_scr", [NSLOT, D], F32, kind="Internal").ap()
    dbg_outs = {}
    REG = {}

    def breg(e, v):
        if v not in REG:
            REG[v] = e.to_reg(v)
        return REG[v]

    with ExitStack() as st:
        S = Sched(nc, st)
        op = S.op

        def chain(eng, fns, reads=(), writes=()):
            for fn_ in fns:
                op(eng, fn_, reads=list(reads), writes=list(writes))
        A = Arena(nc, st, "arena", 182 * 1024)
        P = Arena(nc, st, "persist", 24 * 1024)
        pq = [st.enter_context(nc.psum_tensor(f"pq{i}", [128, 1024], F32)) for i in range(3)]
        pbf = [st.enter_context(nc.psum_tensor(f"pbf{i}", [128, 1024], BF16)) for i in range(2)]
        pq = [t_[:, :] for t_ in pq]; pbf = [t_[:, :] for t_ in pbf]
        pf = [pq[i // 2][:, (i % 2) * 512:(i % 2) * 512 + 512] for i in range(6)]
        PF = [f"pf{i}" for i in range(6)]; PB = ["pb0", "pb1"]

        def dump(name, ap, shape, dt):
            if name not in DEBUG:
                return
            S.barrier()
            o = nc.dram_tensor("dbg_" + name, shape, dt, kind="ExternalOutput").ap()
            dbg_outs[name] = o
            op('sync', lambda e: e.dma_start(out=o, in_=ap), dma='dbg')

        rows_late = P.alloc([4, D], F32)
        ident = P.alloc([128], BF16)
        identf = P.alloc([128], F32)
        ones_bf = P.alloc([128], BF16)
        ones_f = P.alloc([128], F32)
        G1, A2, B2, G2 = rows_late[:, 0, :], rows_late[:, 1, :], rows_late[:, 2, :], rows_late[:, 3, :]

        chain('gpsimd', [
            lambda e: e.memset(identf, 0.0),
            lambda e: e.affine_select(out=identf, in_=identf, pattern=[[-1, 128]], compare_op=ALU.not_equal, fill=1.0, base=0, channel_multiplier=1),
            lambda e: e.memset(ones_f, 1.0),
            lambda e: e.tensor_copy(out=ones_bf, in_=ones_f),
            lambda e: e.tensor_copy(out=ident, in_=identf)], writes=['const'])

        A.reset()
        rows_early = A.alloc([4, D], F32)
        A1, B1, A1c, B1c = rows_early[:, 0, :], rows_early[:, 1, :], rows_early[:, 2, :], rows_early[:, 3, :]
        mark_p1 = A.off
        modB = A.alloc([6 * D], F32); modC = A.alloc([2 * D], F32)
        gB = A.alloc([4, D], F32)
        bmB = A.alloc([6 * D], F32)
        cT = A.alloc([2, 8], F32); sT = A.alloc([2, 8], F32)
        rep = A.alloc([2, 8, 128], BF16)
        wm = [A.alloc([8, 512], BF16) for _ in range(2)]
        op('sync', lambda e: e.dma_start(out=cT, in_=cvec.rearrange("j (k p) -> p j k", p=128), allow_slow_non_contiguous=True), writes=['cT'], dma='d_cT')
        for i in range(4):
            op('sync', lambda e, i=i: e.dma_start(out=gB[:, i, :], in_=gvec[i, :].partition_broadcast(128)), writes=['gB'], dma='d_gB')
        op('sync', lambda e: e.dma_start(out=bmB, in_=b_mod.partition_broadcast(128)), writes=['bmB'], dma='d_bmB')
        op('scalar', lambda e: e.activation(out=sT, in_=cT, func=AF.Silu), reads=['cT'], writes=['sT'])

        def mk_rep(e):
            for j in range(2):
                for k in range(8):
                    r = e.tensor_scalar(out=rep[:, j, k, :], in0=ones_f, scalar1=sT[:, j, k:k + 1], scalar2=None, op0=ALU.mult)
            return r
        op('vector', mk_rep, reads=['sT', 'const'], writes=['rep'])
        for n in range(12):
            wb = wm[n % 2]
            op('gpsimd', lambda e, n=n, wb=wb: e.dma_start(out=wb, in_=w_mod[:, n * 512:(n + 1) * 512].rearrange("(k p) c -> p k c", p=128)),
               writes=[f'wm{n % 2}'], dma=f'ld_wm{n % 2}')
            for j in range(2 if n < 4 else 1):
                bk = (2 * n + j) % 6

                def mm(e, j=j, wb=wb, bk=bk):
                    for k in range(8):
                        r = e.matmul(pf[bk], lhsT=rep[:, j, k, :], rhs=wb[:, k, :], start=(k == 0), stop=(k == 7))
                    return r
                op('tensor', mm, reads=['rep', f'wm{n % 2}'], writes=[PF[bk]])
                dst = (modB if j == 0 else modC)[:, n * 512:(n + 1) * 512]
                op('vector', lambda e, dst=dst, bk=bk, n=n: e.tensor_tensor(out=dst, in0=pf[bk], in1=bmB[:, n * 512:(n + 1) * 512], op=ALU.add),
                   reads=[PF[bk], 'bmB'], writes=['modB'])

        def mk_rows(e):
            e.scalar_tensor_tensor(out=A1, in0=modB[:, D:2 * D], scalar=1.0, in1=gB[:, 0, :], op0=ALU.add, op1=ALU.mult)
            e.tensor_copy(out=B1, in_=modB[:, 0:D])
            e.scalar_tensor_tensor(out=A1c, in0=modC[:, D:2 * D], scalar=1.0, in1=gB[:, 0, :], op0=ALU.add, op1=ALU.mult)
            e.tensor_copy(out=B1c, in_=modC[:, 0:D])
            e.tensor_tensor(out=G1, in0=modB[:, 2 * D:3 * D], in1=gB[:, 1, :], op=ALU.mult)
            e.scalar_tensor_tensor(out=A2, in0=modB[:, 4 * D:5 * D], scalar=1.0, in1=gB[:, 2, :], op0=ALU.add, op1=ALU.mult)
            e.tensor_copy(out=B2, in_=modB[:, 3 * D:4 * D])
            return e.tensor_tensor(out=G2, in0=modB[:, 5 * D:6 * D], in1=gB[:, 3, :], op=ALU.mult)
        op('vector', mk_rows, reads=['modB', 'gB'], writes=['rows'])
        dump('rows_early', rows_early, [128, 4, D], F32)
        S.barrier()

        A.reset(mark_p1)
        xt = [A.alloc([D], F32) for _ in range(2)]
        hn = [A.alloc([D], F32) for _ in range(2)]
        hb = [A.alloc([D], BF16) for _ in range(2)]
        hTt = [A.alloc([8, 128], BF16) for _ in range(2)]
        junk = A.alloc([D], F32)
        ss = A.alloc([NTALL], F32); rs = A.alloc([NTALL], F32)

        def rstd(dst, src, n, reads, key):
            op('vector', lambda e: e.tensor_scalar(out=dst, in0=src, scalar1=1.0 / n, scalar2=EPS, op0=ALU.mult, op1=ALU.add), reads=reads, writes=[key])
            op('scalar', lambda e: e.activation(out=dst, in_=dst, func=AF.Sqrt), reads=[key], writes=[key])
            op('vector', lambda e: e.reciprocal(out=dst, in_=dst), reads=[key], writes=[key])

        for t in range(NTALL):
            b = t % 2
            Ar, Br = (A1, B1) if t < 32 else (A1c, B1c)
            op('sync', lambda e, t=t, b=b: e.dma_start(out=xt[b], in_=xc[t * 128:(t + 1) * 128, :]), writes=[f'xt{b}'], dma=f'ldx{b}')
            op('scalar', lambda e, t=t, b=b: e.activation(out=junk, in_=xt[b], func=AF.Square, accum_out=ss[:, t:t + 1]), reads=[f'xt{b}'], writes=['junk', f'ss{t}'])
            rstd(rs[:, t:t + 1], ss[:, t:t + 1], D, [f'ss{t}'], f'rs{t}')
            op('vector', lambda e, t=t, b=b, Ar=Ar: e.scalar_tensor_tensor(out=hn[b], in0=xt[b], scalar=rs[:, t:t + 1], in1=Ar, op0=ALU.mult, op1=ALU.mult),
               reads=[f'xt{b}', f'rs{t}', 'rows'], writes=[f'hn{b}'])
            op('gpsimd', lambda e, b=b, Br=Br: e.tensor_tensor(out=hb[b], in0=hn[b], in1=Br, op=ALU.add), reads=[f'hn{b}', 'rows'], writes=[f'hb{b}'])

            def tr(e, b=b):
                for k in range(8):
                    r = e.transpose(pbf[b][:, k * 128:(k + 1) * 128], hb[b][:, k * 128:(k + 1) * 128], ident)
                return r
            op('tensor', tr, reads=[f'hb{b}', 'const'], writes=[PB[b]])
            op('scalar', lambda e, b=b: e.activation(out=hTt[b], in_=pbf[b].rearrange("p (k c) -> p k c", k=8), func=AF.Copy), reads=[PB[b]], writes=[f'hTt{b}'])
            op('sync', lambda e, t=t, b=b: e.dma_start(out=hT_scr[:, :, t * 128:(t + 1) * 128], in_=hTt[b]), reads=[f'hTt{b}'], writes=['hT_scr'], dma=f'sth{b}')
        S.barrier()
        if STAGE <= 1:
            return finish(nc, S, out, dbg_outs)

        A.reset()
        o_aT = A.alloc([4, TOKR], BF16)
        mark_oa = A.off
        wA = A.alloc([8, 1536], BF16)
        QaT = A.alloc([4, TOKR], BF16); KaT = A.alloc([4, TOKR], BF16)
        Va_e = A.alloc([18, 512], BF16); Va_o = A.alloc([17, 512], BF16)
        KcaT = A.alloc([4, 256], BF16); Vca = A.alloc([2, 512], BF16)
        tblS = A.alloc([4, 960], F32)
        hTg = [A.alloc([8, 576], BF16) for _ in range(2)]
        sbt = [A.alloc([768], F32) for _ in range(2)]
        pbt = [A.alloc([768], BF16) for _ in range(2)]
        pnt = [A.alloc([768], BF16) for _ in range(2)]
        pTt = [A.alloc([768], BF16) for _ in range(2)]
        sm = A.alloc([2, 4], F32)
        for i, (c0, nm) in enumerate(((0, 'ka'), (512, 'va'), (1280, 'qa'))):
            op('gpsimd', lambda e, i=i, c0=c0: e.dma_start(out=wA[:, :, i * 512:(i + 1) * 512], in_=w_in[:, c0:c0 + 512].rearrange("(k p) c -> p k c", p=128)),
               writes=['wA'], dma='d_wA')
        for p in range(4):
            op('sync', lambda e, p=p: e.dma_start(out=tblS[:, p, :], in_=tbl[p]), writes=['tbl'], dma='d_tbl')
        bkc = [0]

        def nbk():
            bkc[0] = (bkc[0] + 1) % 6
            return bkc[0]

        def proj_fm(lhs_cols, rhs_ap, ntok, dst, scale=None):
            bk = nbk()

            def mm(e):
                for k in range(8):
                    r = e.matmul(pf[bk][:, 0:ntok], lhsT=wA[:, k, lhs_cols[0]:lhs_cols[1]], rhs=rhs_ap(k), start=(k == 0), stop=(k == 7))
                return r
            op('tensor', mm, reads=['wA', 'hTg'], writes=[PF[bk]])
            if scale is None:
                op('scalar', lambda e: e.activation(out=dst, in_=pf[bk][:, 0:ntok], func=AF.Copy), reads=[PF[bk]], writes=['naprep'])
            else:
                op('scalar', lambda e: e.activation(out=dst, in_=pf[bk][:, 0:ntok], func=AF.Copy, scale=scale), reads=[PF[bk]], writes=['naprep'])

        def proj_tm(lhs_ap, dst):
            bk = nbk()

            def mm(e):
                for k in range(8):
                    r = e.matmul(pf[bk], lhsT=lhs_ap(k), rhs=wA[:, k, 512:1024], start=(k == 0), stop=(k == 7))
                return r
            op('tensor', mm, reads=['wA', 'hTg'], writes=[PF[bk]])
            op('vector', lambda e: e.tensor_copy(out=dst, in_=pf[bk]), reads=[PF[bk]], writes=['naprep'])

        for g in range(5):
            hg = hTg[g % 2]
            ntok = 512 if g < 4 else 256
            op('sync', lambda e, g=g, hg=hg: e.dma_start(out=hg, in_=hT_scr[:, :, g * 512:g * 512 + 576]), reads=['hT_scr'], writes=['hTg'], dma=f'ldh{g % 2}')
            for c in range(4):
                proj_fm((c * 128, (c + 1) * 128), lambda k, hg=hg, ntok=ntok: hg[:, k, 0:ntok], ntok, KaT[:, c, g * 512:g * 512 + ntok])
                proj_fm((1024 + c * 128, 1024 + (c + 1) * 128), lambda k, hg=hg, ntok=ntok: hg[:, k, 0:ntok], ntok, QaT[:, c, g * 512:g * 512 + ntok], scale=0.125)
            for j in range(ntok // 128):
                proj_tm(lambda k, hg=hg, j=j: hg[:, k, j * 128:(j + 1) * 128], Va_e[:, 4 * g + j, :])
                if 4 * g + j <= 16:
                    proj_tm(lambda k, hg=hg, j=j: hg[:, k, 64 + j * 128:64 + (j + 1) * 128], Va_o[:, 4 * g + j, :])
        hg = hTg[1]
        op('sync', lambda e, hg=hg: e.dma_start(out=hg[:, :, 0:256], in_=hT_scr[:, :, 4096:4352]), reads=['hT_scr'], writes=['hTg'], dma='ldh1')
        for c in range(4):
            proj_fm((c * 128, (c + 1) * 128), lambda k, hg=hg: hg[:, k, 0:256], 256, KcaT[:, c, :])
        for j in range(2):
            proj_tm(lambda k, hg=hg, j=j: hg[:, k, j * 128:(j + 1) * 128], Vca[:, j, :])
        dump('QaT', QaT, [128, 4, TOKR], BF16); dump('KaT', KaT, [128, 4, TOKR], BF16); dump('Va_e', Va_e, [128, 18, 512], BF16)

        it = 0
        for l in range(36):
            start = min(max(l - 4, 0), 28); u0 = start - l + 7; tok0 = start * 64
            for p in range(4):
                b = it % 2; it += 1
                sl, sc, po = pf[b], pf[2 + b], pf[4 + b]
                sb_, pb_, pn_, pT_ = sbt[b], pbt[b], pnt[b], pTt[b]

                def qk(e, l=l, p=p, tok0=tok0, sl=sl, sc=sc):
                    for hh in range(2):
                        ps_ = slice(hh * 64, hh * 64 + 64)
                        e.matmul(sl[ps_, :], lhsT=QaT[ps_, p, l * 64:(l + 1) * 64], rhs=KaT[ps_, p, tok0:tok0 + 512], start=True, stop=True, tile_position=(hh * 64, hh * 64))
                        r = e.matmul(sc[ps_, 0:256], lhsT=QaT[ps_, p, l * 64:(l + 1) * 64], rhs=KcaT[ps_, p, :], start=True, stop=True, tile_position=(hh * 64, hh * 64))
                    return r
                op('tensor', qk, reads=['naprep'], writes=[PF[b], PF[2 + b]])
                op('vector', lambda e, sb_=sb_, sl=sl, p=p, u0=u0: e.tensor_tensor(out=sb_[:, 0:512], in0=sl, in1=tblS[:, p, u0 * 64:u0 * 64 + 512], op=ALU.add),
                   reads=[PF[b], 'tbl'], writes=[f'sbA{b}'])
                op('scalar', lambda e, sb_=sb_, sc=sc: e.activation(out=sb_[:, 512:768], in_=sc[:, 0:256], func=AF.Copy), reads=[PF[2 + b]], writes=[f'sbB{b}'])

                chain('vector', [
                    lambda e, sb_=sb_, b=b: e.tensor_reduce(out=sm[:, b, 0:1], in_=sb_, axis=AX.X, op=ALU.max),
                    lambda e, b=b: e.tensor_scalar(out=sm[:, b, 1:2], in0=sm[:, b, 0:1], scalar1=-1.0, scalar2=None, op0=ALU.mult)],
                    reads=[f'sbA{b}', f'sbB{b}'], writes=[f'negm{b}'])
                op('scalar', lambda e, sb_=sb_, pb_=pb_, b=b: e.activation(out=pb_, in_=sb_, func=AF.Exp, bias=sm[:, b, 1:2], scale=1.0, accum_out=sm[:, b, 2:3]),
                   reads=[f'sbA{b}', f'sbB{b}', f'negm{b}'], writes=[f'pb{b}', f'sum{b}'])
                op('vector', lambda e, b=b: e.reciprocal(out=sm[:, b, 3:4], in_=sm[:, b, 2:3]), reads=[f'sum{b}'], writes=[f'rsum{b}'])
                op('gpsimd', lambda e, pn_=pn_, pb_=pb_, b=b: e.tensor_scalar(out=pn_, in0=pb_, scalar1=sm[:, b, 3:4], scalar2=None, op0=ALU.mult),
                   reads=[f'pb{b}', f'rsum{b}'], writes=[f'pn{b}'])

                def trp(e, pn_=pn_, b=b):
                    for c in range(6):
                        r = e.transpose(pbf[b][:, c * 128:(c + 1) * 128], pn_[:, c * 128:(c + 1) * 128], ident)
                    return r
                op('tensor', trp, reads=[f'pn{b}', 'const'], writes=[PB[b]])
                op('scalar', lambda e, pT_=pT_, b=b: e.activation(out=pT_, in_=pbf[b][:, 0:768], func=AF.Copy), reads=[PB[b]], writes=[f'pT{b}'])

                def pv(e, pT_=pT_, po=po, p=p, start=start):
                    for hh in range(2):
                        for c in range(6):
                            if c < 4:
                                V = Va_e[:, start // 2 + c, :] if start % 2 == 0 else Va_o[:, (start - 1) // 2 + c, :]
                            else:
                                V = Vca[:, c - 4, :]
                            r = e.matmul(po[hh * 64:hh * 64 + 64, 0:64], lhsT=V[:, p * 128 + hh * 64:p * 128 + hh * 64 + 64],
                                         rhs=pT_[:, c * 128 + hh * 64:c * 128 + hh * 64 + 64], start=(c == 0), stop=(c == 5), tile_position=(0, hh * 64))
                    return r
                op('tensor', pv, reads=[f'pT{b}', 'naprep'], writes=[PF[4 + b]])
                op('vector', lambda e, po=po, p=p, l=l: e.tensor_copy(out=o_aT[:, p, l * 64:(l + 1) * 64], in_=po[:, 0:64]), reads=[PF[4 + b]], writes=['o_aT'])
        dump('o_aT', o_aT, [128, 4, TOKR], BF16)
        S.barrier()
        if STAGE <= 2:
            return finish(nc, S, out, dbg_outs)

        A.reset(mark_oa)
        o_bT = A.alloc([8, TOKR], BF16)
        mark_ob = A.off
        wB = A.alloc([8, 768], BF16)
        QbT = A.alloc([4, TOKR], BF16); KbT = A.alloc([NKEY], BF16)
        Vb = A.alloc([NTALL, 2, 65], BF16)
        gqB = A.alloc([2, 64], F32)
        gtmp = A.alloc([2, 64], F32)
        negC = A.alloc([4], F32)
        hTg = [A.alloc([8, 512], BF16) for _ in range(2)]
        rc = [A.alloc([512], F32) for _ in range(2)]; rsn = [A.alloc([512], F32) for _ in range(2)]
        sq = A.alloc([640], F32); ssh = A.alloc([2, 16], F32)
        qn = A.alloc([640], F32); t1 = A.alloc([640], F32); t2 = A.alloc([640], F32)
        qbb = [A.alloc([640], BF16) for _ in range(2)]
        pTg = [A.alloc([512], BF16) for _ in range(4)]
        osb = [A.alloc([512], F32) for _ in range(2)]
        rec = A.alloc([512], F32)
        op('gpsimd', lambda e: e.dma_start(out=wB[:, :, 0:256], in_=w_in[:, 1024:1280].rearrange("(k p) c -> p k c", p=128)), writes=['wB'], dma='d_wB')
        op('gpsimd', lambda e: e.dma_start(out=wB[:, :, 256:768], in_=w_in[:, 1792:2304].rearrange("(k p) c -> p k c", p=128)), writes=['wB'], dma='d_wB')
        for i in range(2):
            op('sync', lambda e, i=i: e.dma_start(out=gtmp[:, i, :], in_=gqk[i, :].partition_broadcast(128)), writes=['gtmp'], dma='d_gt')

        qv = qn[:, 0:128].rearrange("p (a b) -> p a b", a=2)
        chain('vector', [
            lambda e: e.tensor_scalar(out=gqB[:, 0, :], in0=gtmp[:, 0, :], scalar1=0.125, scalar2=None, op0=ALU.mult),
            lambda e: e.tensor_copy(out=gqB[:, 1, :], in_=gtmp[:, 1, :]),
            lambda e: e.tensor_scalar(out=qv, in0=gtmp, scalar1=-1.0, scalar2=None, op0=ALU.mult),
            lambda e: e.tensor_tensor(out=gtmp, in0=gtmp, in1=qv, op=ALU.max),
            lambda e: e.tensor_reduce(out=negC[:, 0:1], in_=gtmp[:, 0, :], axis=AX.X, op=ALU.max),
            lambda e: e.tensor_reduce(out=negC[:, 1:2], in_=gtmp[:, 1, :], axis=AX.X, op=ALU.max),
            lambda e: e.tensor_tensor(out=negC[:, 2:3], in0=negC[:, 0:1], in1=negC[:, 1:2], op=ALU.mult),
            lambda e: e.tensor_scalar(out=negC[:, 3:4], in0=negC[:, 2:3], scalar1=-8.0, scalar2=None, op0=ALU.mult),
            lambda e: e.memset(Vb[:, :, :, 64:65], 1.0)], reads=['gtmp'], writes=['gtmp', 'gqB', 'negC', 'Vb', 'qn'])

        def normrope(src, H, gi, b, dst, tagr):
            W = H * 64
            op('scalar', lambda e: e.activation(out=sq[:, 0:W], in_=src, func=AF.Square), reads=tagr, writes=['sq'])

            op('vector', lambda e: e.tensor_reduce(out=ssh[:, 0, 0:H], in_=sq[:, 0:W].rearrange("p (h d) -> p h d", d=64), axis=AX.X, op=ALU.add), reads=['sq'], writes=['ssh0'])
            rstd(ssh[:, 1, 0:H], ssh[:, 0, 0:H], 64, ['ssh0'], 'ssh1')

            def n1(e):
                for h in range(H):
                    r = e.scalar_tensor_tensor(out=qn[:, h * 64:(h + 1) * 64], in0=src[:, h * 64:(h + 1) * 64], scalar=ssh[:, 1, h:h + 1], in1=gqB[:, gi, :],
                                               op0=ALU.mult, op1=ALU.mult)
                return r
            op('vector', n1, reads=['ssh1', 'gqB'] + tagr, writes=['qn'])
            op('vector', lambda e: e.tensor_tensor(out=t1[:, 0:W], in0=qn[:, 0:W], in1=rc[b][:, 0:W], op=ALU.mult), reads=['qn', f'rc{b}'], writes=['t1'])

            def r2(e):
                q4 = qn[:, 0:W].rearrange("p (a s f) -> p a s f", s=2, f=16)
                s4 = rsn[b][:, 0:W].rearrange("p (a s f) -> p a s f", s=2, f=16)
                o4 = t2[:, 0:W].rearrange("p (a s f) -> p a s f", s=2, f=16)
                e.tensor_tensor(out=o4[:, :, 0, :], in0=q4[:, :, 1, :], in1=s4[:, :, 0, :], op=ALU.mult)
                return e.tensor_tensor(out=o4[:, :, 1, :], in0=q4[:, :, 0, :], in1=s4[:, :, 1, :], op=ALU.mult)
            op('vector', r2, reads=['qn', f'rc{b}'], writes=['t2'])
            op('vector', lambda e: e.tensor_tensor(out=dst, in0=t1[:, 0:W], in1=t2[:, 0:W], op=ALU.add), reads=['t1', 't2'], writes=['qbb'])

        for t in list(range(NTALL)) + [0]:
            b = t % 2
            g = t // 4
            hg = hTg[g % 2]
            if t % 4 == 0:
                n = min(512, NKEY - g * 512)
                op('sync', lambda e, g=g, hg=hg, n=n: e.dma_start(out=hg[:, :, 0:n], in_=hT_scr[:, :, g * 512:g * 512 + n]), reads=['hT_scr'], writes=[f'hTg{g % 2}'], dma=f'ldh{g % 2}')
            j = t % 4
            op('sync', lambda e, t=t, b=b: e.dma_start(out=rc[b], in_=ropec[t * 128:(t + 1) * 128, :]), writes=[f'rc{b}'], dma=f'ldr{b}')
            op('sync', lambda e, t=t, b=b: e.dma_start(out=rsn[b], in_=ropes[t * 128:(t + 1) * 128, :]), writes=[f'rc{b}'], dma=f'ldr{b}')
            bk = nbk()

            def mmkv(e, hg=hg, j=j, bk=bk):
                for k in range(8):
                    r = e.matmul(pf[bk][:, 0:256], lhsT=hg[:, k, j * 128:(j + 1) * 128], rhs=wB[:, k, 0:256], start=(k == 0), stop=(k == 7))
                return r
            op('tensor', mmkv, reads=['wB', f'hTg{g % 2}'], writes=[PF[bk]])
            op('scalar', lambda e, t=t, bk=bk: e.activation(out=Vb[:, t, :, 0:64], in_=pf[bk][:, 128:256].rearrange("p (h d) -> p h d", d=64), func=AF.Copy),
               reads=[PF[bk]], writes=['Vb'])
            normrope(pf[bk][:, 0:128], 2, 1, b, qbb[b][:, 0:128], [PF[bk]])
            op('tensor', lambda e, b=b: e.transpose(pbf[b][:, 0:128], qbb[b][:, 0:128], ident), reads=['qbb', 'const'], writes=[PB[b]])
            op('scalar', lambda e, t=t, b=b: e.activation(out=KbT[:, t * 128:(t + 1) * 128], in_=pbf[b][:, 0:128], func=AF.Copy), reads=[PB[b]], writes=['KbT'])
            if t < NTR:
                bk2 = nbk()

                def mmq(e, hg=hg, j=j, bk2=bk2):
                    for k in range(8):
                        r = e.matmul(pf[bk2], lhsT=hg[:, k, j * 128:(j + 1) * 128], rhs=wB[:, k, 256:768], start=(k == 0), stop=(k == 7))
                    return r
                op('tensor', mmq, reads=['wB', f'hTg{g % 2}'], writes=[PF[bk2]])
                normrope(pf[bk2], 8, 0, b, qbb[b][:, 0:512], [PF[bk2]])

                def trq(e, b=b):
                    for gg in range(4):
                        r = e.transpose(pbf[b][:, 128 + gg * 128:128 + (gg + 1) * 128], qbb[b][:, gg * 128:(gg + 1) * 128], ident)
                    return r
                op('tensor', trq, reads=['qbb', 'const'], writes=[PB[b]])
                op('scalar', lambda e, t=t, b=b: e.activation(out=QbT[:, :, t * 128:(t + 1) * 128], in_=pbf[b][:, 128:640].rearrange("p (g c) -> p g c", g=4), func=AF.Copy),
                   reads=[PB[b]], writes=['QbT'])
        dump('QbT', QbT, [128, 4, TOKR], BF16); dump('KbT', KbT, [128, NKEY], BF16); dump('Vb', Vb, [128, NTALL, 2, 65], BF16)

        it = 0
        for kvh in range(2):
            pr = slice(kvh * 64, kvh * 64 + 64)
            for qt in range(NTR):
                ob = qt % 2
                po = pf[4 + ob]
                for c in range(NTALL):
                    sb_ = it % 4; it += 1
                    st_ = pf[sb_]
                    op('tensor', lambda e, c=c, qt=qt, st_=st_, pr=pr, kvh=kvh: e.matmul(st_, lhsT=KbT[pr, c * 128:(c + 1) * 128], rhs=QbT[pr, :, qt * 128:(qt + 1) * 128],
                                                                                 start=True, stop=True, tile_position=(kvh * 64, 0)),
                       reads=['KbT', 'QbT'], writes=[PF[sb_]])
                    op('scalar', lambda e, st_=st_, sb_=sb_: e.activation(out=pTg[sb_], in_=st_, func=AF.Exp, bias=negC[:, 3:4], scale=1.0), reads=[PF[sb_], 'negC'], writes=[f'pTg{sb_}'])
                    op('tensor', lambda e, c=c, sb_=sb_, po=po, kvh=kvh: e.matmul(po[0:65, :], lhsT=Vb[:, c, kvh, :], rhs=pTg[sb_], start=(c == 0), stop=(c == NTALL - 1)),
                       reads=[f'pTg{sb_}', 'Vb'], writes=[PF[4 + ob]])
                op('scalar', lambda e, po=po, ob=ob: e.activation(out=osb[ob][0:65, :], in_=po[0:65, :], func=AF.Copy), reads=[PF[4 + ob]], writes=[f'osb{ob}'])
                op('vector', lambda e, ob=ob: e.reciprocal(out=rec[64:65, :], in_=osb[ob][64:65, :]), reads=[f'osb{ob}'], writes=['rec'])
                bk = 4 + ob
                op('tensor', lambda e, po=po: e.matmul(po[0:64, :], lhsT=ones_f[64:65, 0:64], rhs=rec[64:65, :], start=True, stop=True), reads=['rec', 'const'], writes=[PF[bk]])
                op('vector', lambda e, po=po, ob=ob, kvh=kvh, qt=qt: e.tensor_tensor(out=o_bT[0:64, kvh * 4:(kvh + 1) * 4, qt * 128:(qt + 1) * 128],
                                                                                 in0=osb[ob][0:64, :].rearrange("p (g t) -> p g t", g=4),
                                                                                 in1=po[0:64, :].rearrange("p (g t) -> p g t", g=4), op=ALU.mult),
                   reads=[f'osb{ob}', PF[bk]], writes=['o_bT'])
        dump('o_bT', o_bT, [128, 8, TOKR], BF16)
        S.barrier()
        if STAGE <= 3:
            return finish(nc, S, out, dbg_outs)

        A.reset(mark_ob)
        wG = A.alloc([8, 2048], BF16); wOA = A.alloc([4, D], BF16); wOB = A.alloc([8, D], BF16); wO = A.alloc([8, D], BF16)
        wR = A.alloc([8, NE], BF16); bR = A.alloc([NE], BF16)
        hTg = [A.alloc([8, 512], BF16)] * 2
        zT = [A.alloc([8, 512], BF16) for _ in range(2)]
        sga = A.alloc([512], F32); sgb = A.alloc([512], F32)
        xt4 = A.alloc([D], F32); x1t = [A.alloc([D], F32) for _ in range(2)]; tmpf = A.alloc([D], F32)
        h2b = [A.alloc([D], BF16) for _ in range(2)]; h2T = A.alloc([8, 128], BF16)
        lg = P.alloc([NTR, NE], F32); mx8 = P.alloc([NTR, 8], F32); posA = P.alloc([NTR, NE], F32)
        wts = P.alloc([NTR, 4], F32); sloti = P.alloc([NTR * 4], I32)
        mask = A.alloc([NE], F32); maskb = A.alloc([NE], BF16); cntp = P.alloc([NE], F32)
        sms = A.alloc([NTR, 8], F32); e4 = A.alloc([4], F32)
        utri = A.alloc([128], BF16); utf = A.alloc([128], F32)
        mark_route = A.off
        for (dst, src, nm) in ((wG[:, :, 0:1024], w_in[:, 2304:3328], 0), (wG[:, :, 1024:2048], w_in[:, 3328:4352], 1), (wO, w_o, 2)):
            op('gpsimd', lambda e, dst=dst, src=src: e.dma_start(out=dst, in_=src.rearrange("(k p) c -> p k c", p=128)), writes=['wM'], dma='d_wM')
        op('gpsimd', lambda e: e.dma_start(out=wOA, in_=w_oa.rearrange("(k p) c -> p k c", p=128)), writes=['wM'], dma='d_wM')
        op('gpsimd', lambda e: e.dma_start(out=wOB[0:64], in_=w_ob.rearrange("(h d) c -> d h c", d=64)), writes=['wM'], dma='d_wM')
        op('gpsimd', lambda e: e.dma_start(out=wR, in_=w_r.rearrange("(k p) c -> p k c", p=128)), writes=['wM'], dma='d_wM')
        op('gpsimd', lambda e: e.dma_start(out=bR[0:1, :], in_=b_r.rearrange("(o n) -> o n", o=1)), writes=['wM'], dma='d_wM')

        chain('gpsimd', [
            lambda e: e.memset(utf, 1.0),
            lambda e: e.affine_select(out=utf, in_=utf, pattern=[[1, 128]], compare_op=ALU.is_gt, fill=0.0, base=0, channel_multiplier=-1),
            lambda e: e.memset(cntp, 0.0),
            lambda e: e.tensor_copy(out=utri, in_=utf)], writes=['utri', 'route'])

        for g in range(5):
            hg = hTg[g % 2]; z = zT[g % 2]
            ntok = 512 if g < 4 else 256
            tk = slice(g * 512, g * 512 + ntok)
            op('sync', lambda e, g=g, hg=hg, ntok=ntok: e.dma_start(out=hg[:, :, 0:ntok], in_=hT_scr[:, :, g * 512:g * 512 + ntok]), reads=['hT_scr'], writes=[f'hTg{g % 2}'], dma=f'ldh{g % 2}')
            for oc in range(8):
                def mm4(e, hg=hg, oc=oc, ntok=ntok, tk=tk):
                    for k in range(8):
                        e.matmul(pf[0][:, 0:ntok], lhsT=wG[:, k, oc * 128:(oc + 1) * 128], rhs=hg[:, k, 0:ntok], start=(k == 0), stop=(k == 7))
                    for k in range(8):
                        e.matmul(pf[1][:, 0:ntok], lhsT=wG[:, k, 1024 + oc * 128:1024 + (oc + 1) * 128], rhs=hg[:, k, 0:ntok], start=(k == 0), stop=(k == 7))
                    for k in range(4):
                        e.matmul(pf[2][:, 0:ntok], lhsT=wOA[:, k, oc * 128:(oc + 1) * 128], rhs=o_aT[:, k, tk], start=(k == 0), stop=(k == 3))
                    for k in range(8):
                        r = e.matmul(pf[3][:, 0:ntok], lhsT=wOB[0:64, k, oc * 128:(oc + 1) * 128], rhs=o_bT[0:64, k, tk], start=(k == 0), stop=(k == 7))
                    return r
                op('tensor', mm4, reads=['wM', f'hTg{g % 2}', 'o_aT', 'o_bT'], writes=[PF[0], PF[1], PF[2], PF[3]])

                def sg(e, ntok=ntok):
                    e.activation(out=sga[:, 0:ntok], in_=pf[0][:, 0:ntok], func=AF.Sigmoid)
                    return e.activation(out=sgb[:, 0:ntok], in_=pf[1][:, 0:ntok], func=AF.Sigmoid)
                op('scalar', sg, reads=[PF[0], PF[1]], writes=['sg'])

                def zz(e, ntok=ntok):
                    e.tensor_tensor(out=sga[:, 0:ntok], in0=sga[:, 0:ntok], in1=pf[2][:, 0:ntok], op=ALU.mult)
                    return e.tensor_tensor(out=sgb[:, 0:ntok], in0=sgb[:, 0:ntok], in1=pf[3][:, 0:ntok], op=ALU.mult)
                op('vector', zz, reads=['sg', PF[2], PF[3]], writes=['sg2'])
                op('gpsimd', lambda e, z=z, oc=oc, ntok=ntok: e.tensor_tensor(out=z[:, oc, 0:ntok], in0=sga[:, 0:ntok], in1=sgb[:, 0:ntok], op=ALU.add),
                   reads=['sg2'], writes=[f'zT{g % 2}', 'sg'])
            for j in range(ntok // 128):
                t = 4 * g + j
                yb = pq[2]
                b = t % 2

                def mmy(e, z=z, j=j, yb=yb):
                    for n in range(2):
                        for k in range(8):
                            r = e.matmul(yb[:, n * 512:(n + 1) * 512], lhsT=z[:, k, j * 128:(j + 1) * 128], rhs=wO[:, k, n * 512:(n + 1) * 512], start=(k == 0), stop=(k == 7))
                    return r
                op('tensor', mmy, reads=['wM', f'zT{g % 2}'], writes=[PF[4], PF[5]])
                op('sync', lambda e, t=t: e.dma_start(out=xt4, in_=xc[t * 128:(t + 1) * 128, :]), writes=['xt'], dma='ldx0')
                op('scalar', lambda e, yb=yb, t=t: e.activation(out=tmpf, in_=yb, func=AF.Square, accum_out=sms[:, t, 0:1]), reads=[PF[4], PF[5]], writes=['tmpf', 'ssy'])
                rstd(sms[:, t, 1:2], sms[:, t, 0:1], D, ['ssy'], 'rsy')
                op('vector', lambda e, yb=yb, t=t: e.scalar_tensor_tensor(out=tmpf, in0=yb, scalar=sms[:, t, 1:2], in1=G1, op0=ALU.mult, op1=ALU.mult),
                   reads=[PF[4], PF[5], 'rsy', 'rows'], writes=['tmpf'])
                op('gpsimd', lambda e, b=b: e.tensor_tensor(out=x1t[b], in0=tmpf, in1=xt4, op=ALU.add), reads=['tmpf', 'xt'], writes=[f'x1t{b}'])
                op('sync', lambda e, t=t, b=b: e.dma_start(out=x1_scr[t * 128:(t + 1) * 128, :], in_=x1t[b]), reads=[f'x1t{b}'], writes=['x1_scr'], dma=f'stx{b}')
                op('scalar', lambda e, t=t, b=b: e.activation(out=tmpf, in_=x1t[b], func=AF.Square, accum_out=sms[:, t, 2:3]), reads=[f'x1t{b}'], writes=['tmpf', 'ss2'])
                rstd(sms[:, t, 3:4], sms[:, t, 2:3], D, ['ss2'], 'rs2')
                op('vector', lambda e, t=t, b=b: e.scalar_tensor_tensor(out=tmpf, in0=x1t[b], scalar=sms[:, t, 3:4], in1=A2, op0=ALU.mult, op1=ALU.mult),
                   reads=[f'x1t{b}', 'rs2', 'rows'], writes=['tmpf'])
                op('gpsimd', lambda e, b=b: e.tensor_tensor(out=h2b[b], in0=tmpf, in1=B2, op=ALU.add), reads=['tmpf', 'rows'], writes=[f'h2b{b}'])
                op('sync', lambda e, t=t, b=b: e.dma_start(out=h2_scr[t * 128:(t + 1) * 128, :], in_=h2b[b]), reads=[f'h2b{b}'], writes=['h2_scr'], dma=f'sth{b}')

                def trh(e, b=b):
                    for k in range(8):
                        r = e.transpose(pbf[b][:, k * 128:(k + 1) * 128], h2b[b][:, k * 128:(k + 1) * 128], ident)
                    return r
                op('tensor', trh, reads=[f'h2b{b}', 'const'], writes=[PB[b]])
                op('scalar', lambda e, b=b: e.activation(out=h2T, in_=pbf[b].rearrange("p (k c) -> p k c", k=8), func=AF.Copy), reads=[PB[b]], writes=['h2T'])

                def mml(e):
                    for k in range(8):
                        e.matmul(pf[0][:, 0:NE], lhsT=h2T[:, k, :], rhs=wR[:, k, :], start=(k == 0), stop=False)
                    return e.matmul(pf[0][:, 0:NE], lhsT=ones_bf[0:1, :], rhs=bR[0:1, :], start=False, stop=True)
                op('tensor', mml, reads=['h2T', 'wM', 'const'], writes=[PF[0]])

                chain('vector', [
                    lambda e, t=t: e.tensor_copy(out=lg[:, t, :], in_=pf[0][:, 0:NE]),
                    lambda e, t=t: e.max(out=mx8[:, t, :], in_=lg[:, t, :]),
                    lambda e, t=t: e.tensor_scalar(out=mask, in0=lg[:, t, :], scalar1=mx8[:, t, 3:4], scalar2=None, op0=ALU.is_ge),
                    lambda e: e.tensor_copy(out=maskb, in_=mask),
                    lambda e, t=t: e.tensor_scalar(out=sms[:, t, 4:5], in0=mx8[:, t, 0:1], scalar1=-1.0, scalar2=None, op0=ALU.mult)],
                    reads=[PF[0]], writes=['lg', 'maskb', 'negmx'])
                op('scalar', lambda e, t=t: e.activation(out=e4, in_=mx8[:, t, 0:4], func=AF.Exp, bias=sms[:, t, 4:5], scale=1.0, accum_out=sms[:, t, 5:6]),
                   reads=['lg', 'negmx'], writes=['e4'])

                def mmc(e):
                    e.matmul(pf[1][:, 0:NE], lhsT=utri, rhs=maskb, start=True, stop=True)
                    return e.matmul(pf[1][:, NE:2 * NE], lhsT=ones_bf, rhs=maskb, start=True, stop=True)
                op('tensor', mmc, reads=['maskb', 'utri', 'const'], writes=[PF[1]])

                chain('vector', [
                    lambda e, t=t: e.reciprocal(out=sms[:, t, 6:7], in_=sms[:, t, 5:6]),
                    lambda e, t=t: e.tensor_scalar(out=wts[:, t, :], in0=e4, scalar1=sms[:, t, 6:7], scalar2=None, op0=ALU.mult),
                    lambda e, t=t: e.tensor_tensor(out=posA[:, t, :], in0=pf[1][:, 0:NE], in1=cntp, op=ALU.add),
                    lambda e: e.tensor_tensor(out=cntp, in0=cntp, in1=pf[1][:, NE:2 * NE], op=ALU.add)],
                    reads=['e4', PF[1]], writes=['route'])
        dump('lg', lg, [128, NTR, NE], F32); dump('posA', posA, [128, NTR, NE], F32); dump('cntp', cntp, [128, NE], F32)
        S.barrier()
        if STAGE <= 4:
            return finish(nc, S, out, dbg_outs)

        A.reset()
        ci = A.alloc([NE], I32); padf = A.alloc([NE], F32); padT = A.alloc([128], F32); ltri = A.alloc([NE], F32)
        basef = A.alloc([NE], F32); pend = A.alloc([NE], F32)
        thr = A.alloc([NBLK], F32); EB = A.alloc([NBLK], F32); skp = A.alloc([NBLK], F32)
        idxw_f = A.alloc([NBLK], F32); idxb_f = A.alloc([NBLK], F32); pidx = A.alloc([1], F32)
        idxw = A.alloc([NBLK], I32); idxb = A.alloc([NBLK], I32)
        idxw8_f = A.alloc([8, NBLK], F32); idxw8 = A.alloc([8, NBLK], I32)
        slot2 = A.alloc([NE], F32); slotf = A.alloc([NTR * 4], F32); tmp32 = A.alloc([NE], F32)
        h2l = [A.alloc([D], BF16) for _ in range(2)]

        chain('vector', [
            lambda e: e.tensor_scalar(out=padf, in0=cntp, scalar1=127.0, scalar2=None, op0=ALU.add),
            lambda e: e.tensor_copy(out=ci, in_=padf),
            lambda e: e.tensor_single_scalar(out=ci, in_=ci, scalar=7, op=ALU.arith_shift_right),
            lambda e: e.tensor_single_scalar(out=ci, in_=ci, scalar=7, op=ALU.logical_shift_left),
            lambda e: e.tensor_copy(out=padf, in_=ci)], reads=['route'], writes=['padf'])

        chain('gpsimd', [
            lambda e: e.memset(ltri, 1.0),
            lambda e: e.affine_select(out=ltri, in_=ltri, pattern=[[1, NE]], compare_op=ALU.is_gt, fill=0.0, base=0, channel_multiplier=-1),
            lambda e: e.iota(thr, pattern=[[128, NBLK]], base=0, channel_multiplier=0, allow_small_or_imprecise_dtypes=True),
            lambda e: e.iota(pidx, pattern=[[0, 1]], base=0, channel_multiplier=1, allow_small_or_imprecise_dtypes=True)], writes=['ltri'])
        op('tensor', lambda e: e.transpose(pq[0][0:NE, 0:128], padf, identf), reads=['padf', 'const'], writes=[PF[0]])
        op('vector', lambda e: e.tensor_copy(out=padT[0:NE, :], in_=pq[0][0:NE, 0:128]), reads=[PF[0]], writes=['padT'])
        op('tensor', lambda e: e.matmul(pf[1][:, 0:NE], lhsT=padT[0:NE, :], rhs=ltri[0:NE, :], start=True, stop=True), reads=['padT', 'ltri'], writes=[PF[1]])

        lay2 = [
            lambda e: e.tensor_copy(out=basef, in_=pf[1][:, 0:NE]),
            lambda e: e.tensor_tensor(out=pend, in0=basef, in1=padf, op=ALU.add),
            lambda e: e.memset(EB, 0.0)]
        for ex in range(NE):
            lay2.append(lambda e, ex=ex: e.scalar_tensor_tensor(out=EB, in0=thr, scalar=pend[:, ex:ex + 1], in1=EB, op0=ALU.is_ge, op1=ALU.add))
        lay2 += [
            lambda e: e.tensor_scalar(out=EB, in0=EB, scalar1=float(NE - 1), scalar2=None, op0=ALU.min),
            lambda e: e.memset(skp, 0.0),
            lambda e: e.tensor_tensor(out=skp[:, 1:NBLK], in0=EB[:, 1:NBLK], in1=EB[:, 0:NBLK - 1], op=ALU.is_equal),
            lambda e: e.tensor_scalar(out=skp, in0=skp, scalar1=BIG, scalar2=None, op0=ALU.mult),
            lambda e: e.scalar_tensor_tensor(out=idxw_f, in0=EB, scalar=1024.0, in1=skp, op0=ALU.mult, op1=ALU.add),
            lambda e: e.tensor_scalar(out=idxw_f, in0=idxw_f, scalar1=pidx[:, 0:1], scalar2=None, op0=ALU.add),
            lambda e: e.tensor_tensor(out=idxb_f, in0=EB, in1=skp, op=ALU.add),
            lambda e: e.tensor_copy(out=idxw, in_=idxw_f)]
        for k8 in range(8):
            lay2.append(lambda e, k8=k8: e.tensor_scalar(out=idxw8_f[:, k8, :], in0=idxw_f, scalar1=128.0 * k8, scalar2=None, op0=ALU.add))
        lay2 += [lambda e: e.tensor_copy(out=idxw8, in_=idxw8_f), lambda e: e.tensor_copy(out=idxb, in_=idxb_f)]
        chain('vector', lay2, reads=[PF[1], 'padf', 'ltri'], writes=['lay'])
        for t in range(NTR):
            b = t % 2
            op('sync', lambda e, t=t, b=b: e.dma_start(out=h2l[b], in_=h2_scr[t * 128:(t + 1) * 128, :]), reads=['h2_scr'], writes=[f'h2l{b}'], dma=f'ldx{b}')

            slf = [lambda e, t=t: e.tensor_tensor(out=slot2, in0=posA[:, t, :], in1=basef, op=ALU.add)]
            for k in range(4):
                slf.append(lambda e, t=t, k=k: e.scalar_tensor_tensor(out=tmp32, in0=lg[:, t, :], scalar=mx8[:, t, k:k + 1], in1=slot2, op0=ALU.is_equal, op1=ALU.mult,
                                                                      accum_out=slotf[:, 4 * t + k:4 * t + k + 1]))
            slf.append(lambda e, t=t: e.tensor_copy(out=sloti[:, 4 * t:4 * t + 4], in_=slotf[:, 4 * t:4 * t + 4]))
            chain('vector', slf, reads=['lay'], writes=[f'sloti{t}', 'slot2'])
            for k in range(4):
                op('gpsimd', lambda e, t=t, k=k, b=b: e.indirect_dma_start(out=xs_scr, out_offset=bass.IndirectOffsetOnAxis(ap=sloti[:, 4 * t + k:4 * t + k + 1], axis=0),
                                                                       in_=h2l[b], in_offset=None, bounds_check=breg(e, NSLOT - 1), oob_is_err=False),
                   reads=[f'sloti{t}', f'h2l{b}'], writes=['xs_scr'], dma=f'sc{b}')
        dump('sloti', sloti, [128, NTR * 4], I32); dump('idxw', idxw, [128, NBLK], I32); dump('wts', wts, [128, NTR, 4], F32)
        S.barrier()
        if STAGE <= 5:
            return finish(nc, S, out, dbg_outs)

        mark_ex = A.off
        wgu = A.alloc([8, 2 * D], BF16); wdn = A.alloc([8, D], BF16)
        bgu = A.alloc([2 * D], BF16); bdn = A.alloc([D], BF16)
        xe = [A.alloc([D], BF16) for _ in range(2)]; xT = [A.alloc([8, 128], BF16) for _ in range(2)]
        gs = A.alloc([D], F32); sg_ = A.alloc([D], F32); l1 = A.alloc([D], F32); tt = A.alloc([D], F32)
        actb = A.alloc([D], BF16); aT = A.alloc([8, 128], BF16)
        yo = [A.alloc([D], F32) for _ in range(2)]
        wgu_flat = w_gu.rearrange("e k n -> (e k) n"); wdn_flat = w_dn.rearrange("e k n -> (e k) n")
        wgu_v = bass.AP(tensor=w_gu.tensor, offset=0, ap=[[2 * D, NE * D - 896], [128 * 2 * D, 8], [1, 2 * D]])
        wdn_v = bass.AP(tensor=w_dn.tensor, offset=0, ap=[[D, NE * D - 896], [128 * D, 8], [1, D]])
        for blk in range(NBLK):
            b = blk % 2
            iw = bass.IndirectOffsetOnAxis(ap=idxw[:, blk:blk + 1], axis=0)
            ib = bass.IndirectOffsetOnAxis(ap=idxb[:, blk:blk + 1], axis=0)
            for k8 in range(8):
                op('gpsimd', lambda e, blk=blk, k8=k8: e.indirect_dma_start(out=wgu[:, k8, :], out_offset=None, in_=wgu_flat,
                                                                            in_offset=bass.IndirectOffsetOnAxis(ap=idxw8[:, k8, blk:blk + 1], axis=0),
                                                                            bounds_check=breg(e, NE * D - 1), oob_is_err=False),
                   reads=['lay'], writes=['wgu'], dma='ld_wgu')
            op('gpsimd', lambda e, ib=ib: e.indirect_dma_start(out=bgu, out_offset=None, in_=b_gu, in_offset=ib, bounds_check=breg(e, NE - 1), oob_is_err=False),
               reads=['lay'], writes=['wgu'], dma='ld_wgu')
            for k8 in range(8):
                op('gpsimd', lambda e, blk=blk, k8=k8: e.indirect_dma_start(out=wdn[:, k8, :], out_offset=None, in_=wdn_flat,
                                                                            in_offset=bass.IndirectOffsetOnAxis(ap=idxw8[:, k8, blk:blk + 1], axis=0),
                                                                            bounds_check=breg(e, NE * D - 1), oob_is_err=False),
                   reads=['lay'], writes=['wdn'], dma='ld_wdn')
            op('gpsimd', lambda e, ib=ib: e.indirect_dma_start(out=bdn, out_offset=None, in_=b_dn, in_offset=ib, bounds_check=breg(e, NE - 1), oob_is_err=False),
               reads=['lay'], writes=['wdn'], dma='ld_wdn')
            op('sync', lambda e, blk=blk, b=b: e.dma_start(out=xe[b], in_=xs_scr[blk * 128:(blk + 1) * 128, :]), reads=['xs_scr'], writes=[f'xe{b}'], dma=f'ldx{b}')

            def trx(e, b=b):
                for k in range(8):
                    r = e.transpose(pbf[0][:, k * 128:(k + 1) * 128], xe[b][:, k * 128:(k + 1) * 128], ident)
                return r
            op('tensor', trx, reads=[f'xe{b}', 'const'], writes=[PB[0]])
            op('scalar', lambda e, b=b: e.activation(out=xT[b], in_=pbf[0].rearrange("p (k c) -> p k c", k=8), func=AF.Copy), reads=[PB[0]], writes=[f'xT{b}'])

            def mgu(e, b=b):
                for k in range(8):
                    for n in range(4):
                        e.matmul(pf[n], lhsT=xT[b][:, k, :], rhs=wgu[:, k, n * 512:(n + 1) * 512], start=(k == 0), stop=False)
                for n in range(4):
                    r = e.matmul(pf[n], lhsT=ones_bf[0:1, :], rhs=bgu[0:1, n * 512:(n + 1) * 512], start=False, stop=True)
                return r
            op('tensor', mgu, reads=[f'xT{b}', 'wgu', 'const'], writes=[PF[0], PF[1], PF[2], PF[3]])
            op('vector', lambda e: e.tensor_scalar(out=gs, in0=pq[0], scalar1=7.0, scalar2=None, op0=ALU.min), reads=[PF[0], PF[1]], writes=['gs'])
            op('scalar', lambda e: e.activation(out=sg_, in_=gs, func=AF.Sigmoid, scale=1.702), reads=['gs'], writes=['sg_'])
            op('vector', lambda e: e.tensor_scalar(out=l1, in0=pq[1], scalar1=7.0, scalar2=-7.0, op0=ALU.min, op1=ALU.max), reads=[PF[2], PF[3]], writes=['l1'])
            op('gpsimd', lambda e: e.tensor_tensor(out=tt, in0=gs, in1=sg_, op=ALU.mult), reads=['gs', 'sg_'], writes=['tt'])
            op('vector', lambda e: e.scalar_tensor_tensor(out=actb, in0=l1, scalar=1.0, in1=tt, op0=ALU.add, op1=ALU.mult), reads=['l1', 'tt'], writes=['actb'])

            def tra(e):
                for k in range(8):
                    r = e.transpose(pbf[1][:, k * 128:(k + 1) * 128], actb[:, k * 128:(k + 1) * 128], ident)
                return r
            op('tensor', tra, reads=['actb', 'const'], writes=[PB[1]])
            op('scalar', lambda e: e.activation(out=aT, in_=pbf[1].rearrange("p (k c) -> p k c", k=8), func=AF.Copy), reads=[PB[1]], writes=['aT'])

            def mdn(e):
                for k in range(8):
                    for n in range(2):
                        e.matmul(pf[4 + n], lhsT=aT[:, k, :], rhs=wdn[:, k, n * 512:(n + 1) * 512], start=(k == 0), stop=False)
                for n in range(2):
                    r = e.matmul(pf[4 + n], lhsT=ones_bf[0:1, :], rhs=bdn[0:1, n * 512:(n + 1) * 512], start=False, stop=True)
                return r
            op('tensor', mdn, reads=['aT', 'wdn', 'const'], writes=[PF[4], PF[5]])
            op('scalar', lambda e, b=b: e.activation(out=yo[b], in_=pq[2], func=AF.Copy), reads=[PF[4], PF[5]], writes=[f'yo{b}'])
            op('sync', lambda e, blk=blk, b=b: e.dma_start(out=y_scr[blk * 128:(blk + 1) * 128, :], in_=yo[b]), reads=[f'yo{b}'], writes=['y_scr'], dma=f'sty{b}')
        S.barrier()

        A.reset(mark_ex)
        gk = [[A.alloc([D], F32) for _ in range(4)] for _ in range(2)]
        acc = A.alloc([D], F32); x1l = [A.alloc([D], F32) for _ in range(2)]; ot = [A.alloc([D], F32) for _ in range(2)]
        jk = A.alloc([D], F32)
        fs = A.alloc([NTR, 2], F32)
        for t in range(NTR):
            b = t % 2
            for k in range(4):
                op('gpsimd', lambda e, t=t, k=k, b=b: e.indirect_dma_start(out=gk[b][k], out_offset=None, in_=y_scr,
                                                                       in_offset=bass.IndirectOffsetOnAxis(ap=sloti[:, 4 * t + k:4 * t + k + 1], axis=0),
                                                                       bounds_check=breg(e, NSLOT - 1), oob_is_err=False),
                   reads=['y_scr'], writes=[f'gk{b}{k}'], dma=f'ga{b}')
            op('sync', lambda e, t=t, b=b: e.dma_start(out=x1l[b], in_=x1_scr[t * 128:(t + 1) * 128, :]), reads=['x1_scr'], writes=[f'x1l{b}'], dma=f'ldx{b}')

            cmb = [lambda e, t=t, b=b: e.tensor_scalar(out=acc, in0=gk[b][0], scalar1=wts[:, t, 0:1], scalar2=None, op0=ALU.mult)]
            for k in range(1, 4):
                cmb.append(lambda e, t=t, b=b, k=k: e.scalar_tensor_tensor(out=acc, in0=gk[b][k], scalar=wts[:, t, k:k + 1], in1=acc, op0=ALU.mult, op1=ALU.add))
            chain('vector', cmb, reads=[f'gk{b}{k}' for k in range(4)], writes=['acc'])
            op('scalar', lambda e, t=t: e.activation(out=jk, in_=acc, func=AF.Square, accum_out=fs[:, t, 0:1]), reads=['acc'], writes=['jk', 'fss'])
            rstd(fs[:, t, 1:2], fs[:, t, 0:1], D, ['fss'], 'fsr')
            op('vector', lambda e, t=t: e.scalar_tensor_tensor(out=acc, in0=acc, scalar=fs[:, t, 1:2], in1=G2, op0=ALU.mult, op1=ALU.mult), reads=['acc', 'fsr'], writes=['acc'])
            op('gpsimd', lambda e, b=b: e.tensor_tensor(out=ot[b], in0=acc, in1=x1l[b], op=ALU.add), reads=['acc', f'x1l{b}'], writes=[f'ot{b}'])
            op('sync', lambda e, t=t, b=b: e.dma_start(out=out[t * 128:(t + 1) * 128, :], in_=ot[b]), reads=[f'ot{b}'], writes=['out'], dma=f'sto{b}')
        return finish(nc, S, out, dbg_outs)


def finish(nc, S, out, dbg_outs):
    S.barrier()
    S.emit()
    return nc, dbg_outs


_CACHE = {}


def _host_tables():
    if 'rope' in _CACHE:
        return _CACHE['rope'], _CACHE['tblidx']
    half = 32; nf = 16
    freqs = (10000.0 ** (-np.arange(nf, dtype=np.float32) / nf)).astype(np.float32)
    rope = {}
    for hf in range(2):
        rng_rows = np.arange(28 * hf, 28 * hf + 36)
        rest = np.arange(36, 64) if hf == 0 else np.arange(0, 28)
        rows = np.concatenate([rng_rows, rest])
        tok = (rows[:, None] * 64 + np.arange(64)[None, :]).reshape(-1)
        r = (tok // 64).astype(np.float32); c = (tok % 64).astype(np.float32)
        cosT = np.ones((4352, 64), np.float32); sinT = np.zeros((4352, 64), np.float32)
        for hi, pos in enumerate((r, c)):
            ang = pos[:, None] * freqs[None, :]
            co = np.cos(ang).astype(np.float32); si = np.sin(ang).astype(np.float32)
            cosT[:4096, hi * 32:hi * 32 + 16] = co; cosT[:4096, hi * 32 + 16:hi * 32 + 32] = co
            sinT[:4096, hi * 32:hi * 32 + 16] = -si; sinT[:4096, hi * 32 + 16:hi * 32 + 32] = si
        rope[hf] = (np.ascontiguousarray(np.tile(cosT, (1, 8))), np.ascontiguousarray(np.tile(sinT, (1, 8))), tok)
    qc = np.arange(64)[:, None]; kc = np.arange(64)[None, :]
    c0 = np.clip(qc - 8, 0, 48)
    valid = (kc >= c0) & (kc < c0 + 16)
    off = np.clip(kc - qc + 15, 0, 30)
    _CACHE['rope'] = rope; _CACHE['tblidx'] = (valid, off)
    return rope, (valid, off)


def kernel(x, c, ctx, c_ctx, w_mod, b_mod, g_pre_mix, g_post_mix, g_pre_ffn, g_post_ffn, w_in, rpb, g_qnorm, g_knorm,
           w_out_a, w_out_b, w_o, w_router, b_router, w_gu, b_gu, w_dn, b_dn):
    f = lambda a: np.ascontiguousarray(np.asarray(a, dtype=np.float32))
    x = f(x); ctx = f(ctx); c = f(c); c_ctx = f(c_ctx)
    rope, (valid, off) = _host_tables()
    rp = f(rpb)[0]
    T = rp[:, :, off]
    T = np.where(valid[None, None], T, np.float32(NEG)).astype(np.float32)
    T = T.transpose(0, 2, 1, 3).reshape(4, 2 * 64, 15 * 64)
    w_in0 = f(w_in)[0]
    qb = w_in0[:, 1792:2304].reshape(1024, 2, 4, 64).transpose(0, 2, 1, 3).reshape(1024, 512)
    w_in_p = w_in0.copy(); w_in_p[:, 1792:2304] = qb
    shared = dict(w_mod=f(w_mod)[0], b_mod=f(b_mod)[0], gvec=np.stack([f(g_pre_mix)[0], f(g_post_mix)[0], f(g_pre_ffn)[0], f(g_post_ffn)[0]]),
                  w_in=w_in_p, tbl=np.ascontiguousarray(T), gqk=np.stack([f(g_qnorm)[0], f(g_knorm)[0]]),
                  w_oa=f(w_out_a)[0], w_ob=f(w_out_b)[0], w_o=f(w_o)[0], w_r=f(w_router)[0], b_r=f(b_router)[0],
                  w_gu=f(w_gu)[0], b_gu=f(b_gu)[0], w_dn=f(w_dn)[0], b_dn=f(b_dn)[0])
    in_maps = []
    for core in range(8):
        b, hf = core // 2, core % 2
        cosT, sinT, tok = rope[hf]
        xcore = np.concatenate([x[b][tok], ctx[b]], axis=0)
        m = dict(shared)
        m.update(xc=np.ascontiguousarray(xcore), cvec=np.stack([c[b], c_ctx]), ropec=cosT, ropes=sinT)
        in_maps.append(m)
    key = ('nc', STAGE, tuple(DEBUG))
    if key not in _CACHE:
        _CACHE[key] = build()
    nc, dbg = _CACHE[key]
    res = run_bass_kernel_spmd(nc, in_maps, core_ids=list(range(8)))
    _CACHE['last'] = res
    outp = np.empty((4, 4096, 1024), np.float32)
    for core in range(8):
        b, hf = core // 2, core % 2
        o = res.results[core]["out"]
        if hf == 0:
            outp[b, 0:2048] = o[0:2048]
        else:
            outp[b, 2048:4096] = o[256:2304]
    return outp
```

```python
import numpy as np
from contextlib import ExitStack
import concourse.bass as bass
import concourse.mybir as mybir
from concourse.bass_utils import run_bass_kernel_spmd

F32 = mybir.dt.float32; BF16 = mybir.dt.bfloat16; I32 = mybir.dt.int32; U8 = mybir.dt.uint8
AF = mybir.ActivationFunctionType; ALU = mybir.AluOpType; AX = mybir.AxisListType
ENG = ('tensor', 'vector', 'scalar', 'gpsimd', 'sync')
DSZ = {F32: 4, BF16: 2, I32: 4, U8: 1}

D = 1024; NTR = 18; TOKR = 2304; NTALL = 34; NKEY = 4352; NE = 32
NBLK = 104; NSLOT = NBLK * 128
EPS = 1e-6; NEG = -30000.0; BIG = 1.0e6
STAGE = 99
SAME_ENG_SYNC = True
DEBUG = []


class Sched:
    def __init__(self, nc, stack):
        self.nc = nc; self.stack = stack
        self.ops = {e: [] for e in ENG}
        self.sems = {}; self.cnt = {}
        self.last_write = {}; self.readers = {}
        self.waited = {e: {} for e in ENG}

    def sem(self, name):
        if name not in self.sems:
            self.sems[name] = self.stack.enter_context(self.nc.semaphore(name)); self.cnt[name] = 0
        return self.sems[name]

    def op(self, eng, fn, reads=(), writes=(), dma=None):
        waits = {}
        isdma_op = dma is not None

        def need(tok):
            if tok is None:
                return
            sname, val, teng, isdma = tok
            if teng == eng and not isdma and not isdma_op and (eng == 'tensor' or not SAME_ENG_SYNC):
                return
            if self.waited[eng].get(sname, 0) >= val:
                return
            waits[sname] = max(waits.get(sname, 0), val)
        for b in reads:
            need(self.last_write.get(b))
        for b in writes:
            need(self.last_write.get(b))
            for r in self.readers.get(b, ()):
                need(r)
        for s, v in waits.items():
            self.waited[eng][s] = v
        if isdma_op:
            sname = dma; inc = 16
        else:
            sname = 'e_' + eng; inc = 1
        self.sem(sname); self.cnt[sname] += inc
        tok = (sname, self.cnt[sname], eng, isdma_op)
        for b in writes:
            self.last_write[b] = tok; self.readers[b] = []
        for b in reads:
            self.readers.setdefault(b, []).append(tok)
        self.ops[eng].append((list(waits.items()), fn, sname, inc))
        return tok

    def barrier(self):
        for e in ENG:
            waits = []
            for s, c in self.cnt.items():
                if c > 0 and self.waited[e].get(s, 0) < c and s != 'e_' + e:
                    waits.append((s, c)); self.waited[e][s] = c
            if waits:
                self.ops[e].append((waits, None, None, None))
        self.last_write = {}; self.readers = {}

    def emit(self):
        with self.nc.Block() as block:
            for eng in ENG:
                ops = self.ops[eng]
                if not ops:
                    continue

                def body(e, ops=ops):
                    for waits, fn, sname, inc in ops:
                        for s, v in waits:
                            e.wait_ge(self.sems[s], v)
                        if fn is not None:
                            fn(e).then_inc(self.sems[sname], inc)
                getattr(block, eng)(body)


class Arena:
    def __init__(self, nc, st, name, nbytes):
        self.t = st.enter_context(nc.sbuf_tensor(name, [128, nbytes], U8)); self.off = 0; self.n = nbytes; self.name = name

    def alloc(self, free_shape, dt):
        n = int(np.prod(free_shape)) * DSZ[dt]
        n_al = (n + 63) // 64 * 64
        assert self.off + n_al <= self.n, (self.name, self.off, n_al, self.n)
        ap = self.t[:, self.off:self.off + n].bitcast(dt)
        self.off += n_al
        if len(free_shape) == 2:
            ap = ap.rearrange("p (a b) -> p a b", a=free_shape[0])
        elif len(free_shape) == 3:
            ap = ap.rearrange("p (a b c) -> p a b c", a=free_shape[0], b=free_shape[1])
        return ap

    def reset(self, off=0):
        self.off = off


def build():
    nc = bass.Bass("TRN2", target_bir_lowering=False)
    dt_in = lambda name, shape, dt=F32: nc.dram_tensor(name, shape, dt, kind="ExternalInput").ap()
    xc = dt_in("xc", [NKEY, D]); cvec = dt_in("cvec", [2, D]); w_mod = dt_in("w_mod", [D, 6 * D]); b_mod = dt_in("b_mod", [6 * D])
    gvec = dt_in("gvec", [4, D]); w_in = dt_in("w_in", [D, 4352]); tbl = dt_in("tbl", [4, 128, 960]); gqk = dt_in("gqk", [2, 64])
    ropec = dt_in("ropec", [NKEY, 512]); ropes = dt_in("ropes", [NKEY, 512])
    w_oa = dt_in("w_oa", [512, D]); w_ob = dt_in("w_ob", [512, D]); w_o = dt_in("w_o", [D, D])
    w_r = dt_in("w_r", [D, NE]); b_r = dt_in("b_r", [NE])
    w_gu = dt_in("w_gu", [NE, D, 2 * D]); b_gu = dt_in("b_gu", [NE, 2 * D]); w_dn = dt_in("w_dn", [NE, D, D]); b_dn = dt_in("b_dn", [NE, D])
    out = nc.dram_tensor("out", [TOKR, D], F32, kind="ExternalOutput").ap()
    hT_scr = nc.dram_tensor("hT_scr", [128, 8, NKEY + 128], BF16, kind="Internal").ap()
    x1_scr = nc.dram_tensor("x1_scr", [TOKR, D], F32, kind="Internal").ap()
    h2_scr = nc.dram_tensor("h2_scr", [TOKR, D], BF16, kind="Internal").ap()
    xs_scr = nc.dram_tensor("xs_scr", [NSLOT, D], BF16, kind="Internal").ap()
    y_scr = nc.dram_tensor("y_scr", [NSLOT, D], F32, kind="Internal").ap()
    dbg_outs = {}
    REG = {}

    def breg(e, v):
        if v not in REG:
            REG[v] = e.to_reg(v)
        return REG[v]

    with ExitStack() as st:
        S = Sched(nc, st)
        op = S.op

        def chain(eng, fns, reads=(), writes=()):
            for fn_ in fns:
                op(eng, fn_, reads=list(reads), writes=list(writes))
        A = Arena(nc, st, "arena", 182 * 1024)
        P = Arena(nc, st, "persist", 24 * 1024)
        pq = [st.enter_context(nc.psum_tensor(f"pq{i}", [128, 1024], F32)) for i in range(3)]
        pbf = [st.enter_context(nc.psum_tensor(f"pbf{i}", [128, 1024], BF16)) for i in range(2)]
        pq = [t_[:, :] for t_ in pq]; pbf = [t_[:, :] for t_ in pbf]
        pf = [pq[i // 2][:, (i % 2) * 512:(i % 2) * 512 + 512] for i in range(6)]
        PF = [f"pf{i}" for i in range(6)]; PB = ["pb0", "pb1"]

        def dump(name, ap, shape, dt):
            if name not in DEBUG:
                return
            S.barrier()
            o = nc.dram_tensor("dbg_" + name, shape, dt, kind="ExternalOutput").ap()
            dbg_outs[name] = o
            op('sync', lambda e: e.dma_start(out=o, in_=ap), dma='dbg')

        rows_late = P.alloc([4, D], F32)
        ident = P.alloc([128], BF16)
        identf = P.alloc([128], F32)
        ones_bf = P.alloc([128], BF16)
        ones_f = P.alloc([128], F32)
        G1, A2, B2, G2 = rows_late[:, 0, :], rows_late[:, 1, :], rows_late[:, 2, :], rows_late[:, 3, :]

        chain('gpsimd', [
            lambda e: e.memset(identf, 0.0),
            lambda e: e.affine_select(out=identf, in_=identf, pattern=[[-1, 128]], compare_op=ALU.not_equal, fill=1.0, base=0, channel_multiplier=1),
            lambda e: e.memset(ones_f, 1.0),
            lambda e: e.tensor_copy(out=ones_bf, in_=ones_f),
            lambda e: e.tensor_copy(out=ident, in_=identf)], writes=['const'])

        A.reset()
        rows_early = A.alloc([4, D], F32)
        A1, B1, A1c, B1c = rows_early[:, 0, :], rows_early[:, 1, :], rows_early[:, 2, :], rows_early[:, 3, :]
        mark_p1 = A.off
        modB = A.alloc([6 * D], F32); modC = A.alloc([2 * D], F32)
        gB = A.alloc([4, D], F32)
        bmB = A.alloc([6 * D], F32)
        cT = A.alloc([2, 8], F32); sT = A.alloc([2, 8], F32)
        rep = A.alloc([2, 8, 128], BF16)
        wm = [A.alloc([8, 512], BF16) for _ in range(2)]
        op('sync', lambda e: e.dma_start(out=cT, in_=cvec.rearrange("j (k p) -> p j k", p=128), allow_slow_non_contiguous=True), writes=['cT'], dma='d_cT')
        for i in range(4):
            op('sync', lambda e, i=i: e.dma_start(out=gB[:, i, :], in_=gvec[i, :].partition_broadcast(128)), writes=['gB'], dma='d_gB')
        op('sync', lambda e: e.dma_start(out=bmB, in_=b_mod.partition_broadcast(128)), writes=['bmB'], dma='d_bmB')
        op('scalar', lambda e: e.activation(out=sT, in_=cT, func=AF.Silu), reads=['cT'], writes=['sT'])

        def mk_rep(e):
            for j in range(2):
                for k in range(8):
                    r = e.tensor_scalar(out=rep[:, j, k, :], in0=ones_f, scalar1=sT[:, j, k:k + 1], scalar2=None, op0=ALU.mult)
            return r
        op('vector', mk_rep, reads=['sT', 'const'], writes=['rep'])
        for n in range(12):
            wb = wm[n % 2]
            op('gpsimd', lambda e, n=n, wb=wb: e.dma_start(out=wb, in_=w_mod[:, n * 512:(n + 1) * 512].rearrange("(k p) c -> p k c", p=128)),
               writes=[f'wm{n % 2}'], dma=f'ld_wm{n % 2}')
            for j in range(2 if n < 4 else 1):
                bk = (2 * n + j) % 6

                def mm(e, j=j, wb=wb, bk=bk):
                    for k in range(8):
                        r = e.matmul(pf[bk], lhsT=rep[:, j, k, :], rhs=wb[:, k, :], start=(k == 0), stop=(k == 7))
                    return r
                op('tensor', mm, reads=['rep', f'wm{n % 2}'], writes=[PF[bk]])
                dst = (modB if j == 0 else modC)[:, n * 512:(n + 1) * 512]
                op('vector', lambda e, dst=dst, bk=bk, n=n: e.tensor_tensor(out=dst, in0=pf[bk], in1=bmB[:, n * 512:(n + 1) * 512], op=ALU.add),
                   reads=[PF[bk], 'bmB'], writes=['modB'])

        def mk_rows(e):
            e.scalar_tensor_tensor(out=A1, in0=modB[:, D:2 * D], scalar=1.0, in1=gB[:, 0, :], op0=ALU.add, op1=ALU.mult)
            e.tensor_copy(out=B1, in_=modB[:, 0:D])
            e.scalar_tensor_tensor(out=A1c, in0=modC[:, D:2 * D], scalar=1.0, in1=gB[:, 0, :], op0=ALU.add, op1=ALU.mult)
            e.tensor_copy(out=B1c, in_=modC[:, 0:D])
            e.tensor_tensor(out=G1, in0=modB[:, 2 * D:3 * D], in1=gB[:, 1, :], op=ALU.mult)
            e.scalar_tensor_tensor(out=A2, in0=modB[:, 4 * D:5 * D], scalar=1.0, in1=gB[:, 2, :], op0=ALU.add, op1=ALU.mult)
            e.tensor_copy(out=B2, in_=modB[:, 3 * D:4 * D])
            return e.tensor_tensor(out=G2, in0=modB[:, 5 * D:6 * D], in1=gB[:, 3, :], op=ALU.mult)
        op('vector', mk_rows, reads=['modB', 'gB'], writes=['rows'])
        dump('rows_early', rows_early, [128, 4, D], F32)
        S.barrier()

        A.reset(mark_p1)
        xt = [A.alloc([D], F32) for _ in range(2)]
        hn = [A.alloc([D], F32) for _ in range(2)]
        hb = [A.alloc([D], BF16) for _ in range(2)]
        hTt = [A.alloc([8, 128], BF16) for _ in range(2)]
        junk = A.alloc([D], F32)
        ss = A.alloc([NTALL], F32); rs = A.alloc([NTALL], F32)

        def rstd(dst, src, n, reads, key):
            op('vector', lambda e: e.tensor_scalar(out=dst, in0=src, scalar1=1.0 / n, scalar2=EPS, op0=ALU.mult, op1=ALU.add), reads=reads, writes=[key])
            op('scalar', lambda e: e.activation(out=dst, in_=dst, func=AF.Sqrt), reads=[key], writes=[key])
            op('vector', lambda e: e.reciprocal(out=dst, in_=dst), reads=[key], writes=[key])

        for t in range(NTALL):
            b = t % 2
            Ar, Br = (A1, B1) if t < 32 else (A1c, B1c)
            op('sync', lambda e, t=t, b=b: e.dma_start(out=xt[b], in_=xc[t * 128:(t + 1) * 128, :]), writes=[f'xt{b}'], dma=f'ldx{b}')
            op('scalar', lambda e, t=t, b=b: e.activation(out=junk, in_=xt[b], func=AF.Square, accum_out=ss[:, t:t + 1]), reads=[f'xt{b}'], writes=['junk', f'ss{t}'])
            rstd(rs[:, t:t + 1], ss[:, t:t + 1], D, [f'ss{t}'], f'rs{t}')
            op('vector', lambda e, t=t, b=b, Ar=Ar: e.scalar_tensor_tensor(out=hn[b], in0=xt[b], scalar=rs[:, t:t + 1], in1=Ar, op0=ALU.mult, op1=ALU.mult),
               reads=[f'xt{b}', f'rs{t}', 'rows'], writes=[f'hn{b}'])
            op('gpsimd', lambda e, b=b, Br=Br: e.tensor_tensor(out=hb[b], in0=hn[b], in1=Br, op=ALU.add), reads=[f'hn{b}', 'rows'], writes=[f'hb{b}'])

            def tr(e, b=b):
                for k in range(8):
                    r = e.transpose(pbf[b][:, k * 128:(k + 1) * 128], hb[b][:, k * 128:(k + 1) * 128], ident)
                return r
            op('tensor', tr, reads=[f'hb{b}', 'const'], writes=[PB[b]])
            op('scalar', lambda e, b=b: e.activation(out=hTt[b], in_=pbf[b].rearrange("p (k c) -> p k c", k=8), func=AF.Copy), reads=[PB[b]], writes=[f'hTt{b}'])
            op('sync', lambda e, t=t, b=b: e.dma_start(out=hT_scr[:, :, t * 128:(t + 1) * 128], in_=hTt[b]), reads=[f'hTt{b}'], writes=['hT_scr'], dma=f'sth{b}')
        S.barrier()
        if STAGE <= 1:
            return finish(nc, S, out, dbg_outs)

        A.reset()
        o_aT = A.alloc([4, TOKR], BF16)
        mark_oa = A.off
        wA = A.alloc([8, 1536], BF16)
        QaT = A.alloc([4, TOKR], BF16); KaT = A.alloc([4, TOKR], BF16)
        Va_e = A.alloc([18, 512], BF16); Va_o = A.alloc([17, 512], BF16)
        KcaT = A.alloc([4, 256], BF16); Vca = A.alloc([2, 512], BF16)
        tblS = A.alloc([4, 960], F32)
        hTg = [A.alloc([8, 576], BF16) for _ in range(2)]
        sbt = [A.alloc([768], F32) for _ in range(2)]
        pbt = [A.alloc([768], BF16) for _ in range(2)]
        pnt = [A.alloc([768], BF16) for _ in range(2)]
        pTt = [A.alloc([768], BF16) for _ in range(2)]
        sm = A.alloc([2, 4], F32)
        for i, (c0, nm) in enumerate(((0, 'ka'), (512, 'va'), (1280, 'qa'))):
            op('gpsimd', lambda e, i=i, c0=c0: e.dma_start(out=wA[:, :, i * 512:(i + 1) * 512], in_=w_in[:, c0:c0 + 512].rearrange("(k p) c -> p k c", p=128)),
               writes=['wA'], dma='d_wA')
        for p in range(4):
            op('sync', lambda e, p=p: e.dma_start(out=tblS[:, p, :], in_=tbl[p]), writes=['tbl'], dma='d_tbl')
        bkc = [0]

        def nbk():
            bkc[0] = (bkc[0] + 1) % 6
            return bkc[0]

        def proj_fm(lhs_cols, rhs_ap, ntok, dst, scale=None):
            bk = nbk()

            def mm(e):
                for k in range(8):
                    r = e.matmul(pf[bk][:, 0:ntok], lhsT=wA[:, k, lhs_cols[0]:lhs_cols[1]], rhs=rhs_ap(k), start=(k == 0), stop=(k == 7))
                return r
            op('tensor', mm, reads=['wA', 'hTg'], writes=[PF[bk]])
            if scale is None:
                op('scalar', lambda e: e.activation(out=dst, in_=pf[bk][:, 0:ntok], func=AF.Copy), reads=[PF[bk]], writes=['naprep'])
            else:
                op('scalar', lambda e: e.activation(out=dst, in_=pf[bk][:, 0:ntok], func=AF.Copy, scale=scale), reads=[PF[bk]], writes=['naprep'])

        def proj_tm(lhs_ap, dst):
            bk = nbk()

            def mm(e):
                for k in range(8):
                    r = e.matmul(pf[bk], lhsT=lhs_ap(k), rhs=wA[:, k, 512:1024], start=(k == 0), stop=(k == 7))
                return r
            op('tensor', mm, reads=['wA', 'hTg'], writes=[PF[bk]])
            op('vector', lambda e: e.tensor_copy(out=dst, in_=pf[bk]), reads=[PF[bk]], writes=['naprep'])

        for g in range(5):
            hg = hTg[g % 2]
            ntok = 512 if g < 4 else 256
            op('sync', lambda e, g=g, hg=hg: e.dma_start(out=hg, in_=hT_scr[:, :, g * 512:g * 512 + 576]), reads=['hT_scr'], writes=['hTg'], dma=f'ldh{g % 2}')
            for c in range(4):
                proj_fm((c * 128, (c + 1) * 128), lambda k, hg=hg, ntok=ntok: hg[:, k, 0:ntok], ntok, KaT[:, c, g * 512:g * 512 + ntok])
                proj_fm((1024 + c * 128, 1024 + (c + 1) * 128), lambda k, hg=hg, ntok=ntok: hg[:, k, 0:ntok], ntok, QaT[:, c, g * 512:g * 512 + ntok], scale=0.125)
            for j in range(ntok // 128):
                proj_tm(lambda k, hg=hg, j=j: hg[:, k, j * 128:(j + 1) * 128], Va_e[:, 4 * g + j, :])
                if 4 * g + j <= 16:
                    proj_tm(lambda k, hg=hg, j=j: hg[:, k, 64 + j * 128:64 + (j + 1) * 128], Va_o[:, 4 * g + j, :])
        hg = hTg[1]
        op('sync', lambda e, hg=hg: e.dma_start(out=hg[:, :, 0:256], in_=hT_scr[:, :, 4096:4352]), reads=['hT_scr'], writes=['hTg'], dma='ldh1')
        for c in range(4):
            proj_fm((c * 128, (c + 1) * 128), lambda k, hg=hg: hg[:, k, 0:256], 256, KcaT[:, c, :])
        for j in range(2):
            proj_tm(lambda k, hg=hg, j=j: hg[:, k, j * 128:(j + 1) * 128], Vca[:, j, :])
        dump('QaT', QaT, [128, 4, TOKR], BF16); dump('KaT', KaT, [128, 4, TOKR], BF16); dump('Va_e', Va_e, [128, 18, 512], BF16)

        it = 0
        for l in range(36):
            start = min(max(l - 4, 0), 28); u0 = start - l + 7; tok0 = start * 64
            for p in range(4):
                b = it % 2; it += 1
                sl, sc, po = pf[b], pf[2 + b], pf[4 + b]
                sb_, pb_, pn_, pT_ = sbt[b], pbt[b], pnt[b], pTt[b]

                def qk(e, l=l, p=p, tok0=tok0, sl=sl, sc=sc):
                    for hh in range(2):
                        ps_ = slice(hh * 64, hh * 64 + 64)
                        e.matmul(sl[ps_, :], lhsT=QaT[ps_, p, l * 64:(l + 1) * 64], rhs=KaT[ps_, p, tok0:tok0 + 512], start=True, stop=True, tile_position=(hh * 64, hh * 64))
                        r = e.matmul(sc[ps_, 0:256], lhsT=QaT[ps_, p, l * 64:(l + 1) * 64], rhs=KcaT[ps_, p, :], start=True, stop=True, tile_position=(hh * 64, hh * 64))
                    return r
                op('tensor', qk, reads=['naprep'], writes=[PF[b], PF[2 + b]])
                op('vector', lambda e, sb_=sb_, sl=sl, p=p, u0=u0: e.tensor_tensor(out=sb_[:, 0:512], in0=sl, in1=tblS[:, p, u0 * 64:u0 * 64 + 512], op=ALU.add),
                   reads=[PF[b], 'tbl'], writes=[f'sbA{b}'])
                op('scalar', lambda e, sb_=sb_, sc=sc: e.activation(out=sb_[:, 512:768], in_=sc[:, 0:256], func=AF.Copy), reads=[PF[2 + b]], writes=[f'sbB{b}'])

                chain('vector', [
                    lambda e, sb_=sb_, b=b: e.tensor_reduce(out=sm[:, b, 0:1], in_=sb_, axis=AX.X, op=ALU.max),
                    lambda e, b=b: e.tensor_scalar(out=sm[:, b, 1:2], in0=sm[:, b, 0:1], scalar1=-1.0, scalar2=None, op0=ALU.mult)],
                    reads=[f'sbA{b}', f'sbB{b}'], writes=[f'negm{b}'])
                op('scalar', lambda e, sb_=sb_, pb_=pb_, b=b: e.activation(out=pb_, in_=sb_, func=AF.Exp, bias=sm[:, b, 1:2], scale=1.0, accum_out=sm[:, b, 2:3]),
                   reads=[f'sbA{b}', f'sbB{b}', f'negm{b}'], writes=[f'pb{b}', f'sum{b}'])
                op('vector', lambda e, b=b: e.reciprocal(out=sm[:, b, 3:4], in_=sm[:, b, 2:3]), reads=[f'sum{b}'], writes=[f'rsum{b}'])
                op('gpsimd', lambda e, pn_=pn_, pb_=pb_, b=b: e.tensor_scalar(out=pn_, in0=pb_, scalar1=sm[:, b, 3:4], scalar2=None, op0=ALU.mult),
                   reads=[f'pb{b}', f'rsum{b}'], writes=[f'pn{b}'])

                def trp(e, pn_=pn_, b=b):
                    for c in range(6):
                        r = e.transpose(pbf[b][:, c * 128:(c + 1) * 128], pn_[:, c * 128:(c + 1) * 128], ident)
                    return r
                op('tensor', trp, reads=[f'pn{b}', 'const'], writes=[PB[b]])
                op('scalar', lambda e, pT_=pT_, b=b: e.activation(out=pT_, in_=pbf[b][:, 0:768], func=AF.Copy), reads=[PB[b]], writes=[f'pT{b}'])

                def pv(e, pT_=pT_, po=po, p=p, start=start):
                    for hh in range(2):
                        for c in range(6):
                            if c < 4:
                                V = Va_e[:, start // 2 + c, :] if start % 2 == 0 else Va_o[:, (start - 1) // 2 + c, :]
                            else:
                                V = Vca[:, c - 4, :]
                            r = e.matmul(po[hh * 64:hh * 64 + 64, 0:64], lhsT=V[:, p * 128 + hh * 64:p * 128 + hh * 64 + 64],
                                         rhs=pT_[:, c * 128 + hh * 64:c * 128 + hh * 64 + 64], start=(c == 0), stop=(c == 5), tile_position=(0, hh * 64))
                    return r
                op('tensor', pv, reads=[f'pT{b}', 'naprep'], writes=[PF[4 + b]])
                op('vector', lambda e, po=po, p=p, l=l: e.tensor_copy(out=o_aT[:, p, l * 64:(l + 1) * 64], in_=po[:, 0:64]), reads=[PF[4 + b]], writes=['o_aT'])
        dump('o_aT', o_aT, [128, 4, TOKR], BF16)
        S.barrier()
        if STAGE <= 2:
            return finish(nc, S, out, dbg_outs)

        A.reset(mark_oa)
        o_bT = A.alloc([8, TOKR], BF16)
        mark_ob = A.off
        wB = A.alloc([8, 768], BF16)
        QbT = A.alloc([4, TOKR], BF16); KbT = A.alloc([NKEY], BF16)
        Vb = A.alloc([NTALL, 2, 65], BF16)
        gqB = A.alloc([2, 64], F32)
        gtmp = A.alloc([2, 64], F32)
        negC = A.alloc([4], F32)
        hTg = [A.alloc([8, 512], BF16) for _ in range(2)]
        rc = [A.alloc([512], F32) for _ in range(2)]; rsn = [A.alloc([512], F32) for _ in range(2)]
        sq = A.alloc([640], F32); ssh = A.alloc([2, 16], F32)
        qn = A.alloc([640], F32); t1 = A.alloc([640], F32); t2 = A.alloc([640], F32)
        qbb = [A.alloc([640], BF16) for _ in range(2)]
        pTg = [A.alloc([512], BF16) for _ in range(4)]
        osb = [A.alloc([512], F32) for _ in range(2)]
        rec = A.alloc([512], F32)
        op('gpsimd', lambda e: e.dma_start(out=wB[:, :, 0:256], in_=w_in[:, 1024:1280].rearrange("(k p) c -> p k c", p=128)), writes=['wB'], dma='d_wB')
        op('gpsimd', lambda e: e.dma_start(out=wB[:, :, 256:768], in_=w_in[:, 1792:2304].rearrange("(k p) c -> p k c", p=128)), writes=['wB'], dma='d_wB')
        for i in range(2):
            op('sync', lambda e, i=i: e.dma_start(out=gtmp[:, i, :], in_=gqk[i, :].partition_broadcast(128)), writes=['gtmp'], dma='d_gt')

        qv = qn[:, 0:128].rearrange("p (a b) -> p a b", a=2)
        chain('vector', [
            lambda e: e.tensor_scalar(out=gqB[:, 0, :], in0=gtmp[:, 0, :], scalar1=0.125, scalar2=None, op0=ALU.mult),
            lambda e: e.tensor_copy(out=gqB[:, 1, :], in_=gtmp[:, 1, :]),
            lambda e: e.tensor_scalar(out=qv, in0=gtmp, scalar1=-1.0, scalar2=None, op0=ALU.mult),
            lambda e: e.tensor_tensor(out=gtmp, in0=gtmp, in1=qv, op=ALU.max),
            lambda e: e.tensor_reduce(out=negC[:, 0:1], in_=gtmp[:, 0, :], axis=AX.X, op=ALU.max),
            lambda e: e.tensor_reduce(out=negC[:, 1:2], in_=gtmp[:, 1, :], axis=AX.X, op=ALU.max),
            lambda e: e.tensor_tensor(out=negC[:, 2:3], in0=negC[:, 0:1], in1=negC[:, 1:2], op=ALU.mult),
            lambda e: e.tensor_scalar(out=negC[:, 3:4], in0=negC[:, 2:3], scalar1=-8.0, scalar2=None, op0=ALU.mult),
            lambda e: e.memset(Vb[:, :, :, 64:65], 1.0)], reads=['gtmp'], writes=['gtmp', 'gqB', 'negC', 'Vb', 'qn'])

        def normrope(src, H, gi, b, dst, tagr):
            W = H * 64
            op('scalar', lambda e: e.activation(out=sq[:, 0:W], in_=src, func=AF.Square), reads=tagr, writes=['sq'])

            op('vector', lambda e: e.tensor_reduce(out=ssh[:, 0, 0:H], in_=sq[:, 0:W].rearrange("p (h d) -> p h d", d=64), axis=AX.X, op=ALU.add), reads=['sq'], writes=['ssh0'])
            rstd(ssh[:, 1, 0:H], ssh[:, 0, 0:H], 64, ['ssh0'], 'ssh1')

            def n1(e):
                for h in range(H):
                    r = e.scalar_tensor_tensor(out=qn[:, h * 64:(h + 1) * 64], in0=src[:, h * 64:(h + 1) * 64], scalar=ssh[:, 1, h:h + 1], in1=gqB[:, gi, :],
                                               op0=ALU.mult, op1=ALU.mult)
                return r
            op('vector', n1, reads=['ssh1', 'gqB'] + tagr, writes=['qn'])
            op('vector', lambda e: e.tensor_tensor(out=t1[:, 0:W], in0=qn[:, 0:W], in1=rc[b][:, 0:W], op=ALU.mult), reads=['qn', f'rc{b}'], writes=['t1'])

            def r2(e):
                q4 = qn[:, 0:W].rearrange("p (a s f) -> p a s f", s=2, f=16)
                s4 = rsn[b][:, 0:W].rearrange("p (a s f) -> p a s f", s=2, f=16)
                o4 = t2[:, 0:W].rearrange("p (a s f) -> p a s f", s=2, f=16)
                e.tensor_tensor(out=o4[:, :, 0, :], in0=q4[:, :, 1, :], in1=s4[:, :, 0, :], op=ALU.mult)
                return e.tensor_tensor(out=o4[:, :, 1, :], in0=q4[:, :, 0, :], in1=s4[:, :, 1, :], op=ALU.mult)
            op('vector', r2, reads=['qn', f'rc{b}'], writes=['t2'])
            op('vector', lambda e: e.tensor_tensor(out=dst, in0=t1[:, 0:W], in1=t2[:, 0:W], op=ALU.add), reads=['t1', 't2'], writes=['qbb'])

        for t in range(NTALL):
            b = t % 2
            g = t // 4
            hg = hTg[g % 2]
            if t % 4 == 0:
                n = min(512, NKEY - g * 512)
                op('sync', lambda e, g=g, hg=hg, n=n: e.dma_start(out=hg[:, :, 0:n], in_=hT_scr[:, :, g * 512:g * 512 + n]), reads=['hT_scr'], writes=[f'hTg{g % 2}'], dma=f'ldh{g % 2}')
            j = t % 4
            op('sync', lambda e, t=t, b=b: e.dma_start(out=rc[b], in_=ropec[t * 128:(t + 1) * 128, :]), writes=[f'rc{b}'], dma=f'ldr{b}')
            op('sync', lambda e, t=t, b=b: e.dma_start(out=rsn[b], in_=ropes[t * 128:(t + 1) * 128, :]), writes=[f'rc{b}'], dma=f'ldr{b}')
            bk = nbk()

            def mmkv(e, hg=hg, j=j, bk=bk):
                for k in range(8):
                    r = e.matmul(pf[bk][:, 0:256], lhsT=hg[:, k, j * 128:(j + 1) * 128], rhs=wB[:, k, 0:256], start=(k == 0), stop=(k == 7))
                return r
            op('tensor', mmkv, reads=['wB', f'hTg{g % 2}'], writes=[PF[bk]])
            op('scalar', lambda e, t=t, bk=bk: e.activation(out=Vb[:, t, :, 0:64], in_=pf[bk][:, 128:256].rearrange("p (h d) -> p h d", d=64), func=AF.Copy),
               reads=[PF[bk]], writes=['Vb'])
            normrope(pf[bk][:, 0:128], 2, 1, b, qbb[b][:, 0:128], [PF[bk]])
            op('tensor', lambda e, b=b: e.transpose(pbf[b][:, 0:128], qbb[b][:, 0:128], ident), reads=['qbb', 'const'], writes=[PB[b]])
            op('scalar', lambda e, t=t, b=b: e.activation(out=KbT[:, t * 128:(t + 1) * 128], in_=pbf[b][:, 0:128], func=AF.Copy), reads=[PB[b]], writes=['KbT'])
            if t < NTR:
                bk2 = nbk()

                def mmq(e, hg=hg, j=j, bk2=bk2):
                    for k in range(8):
                        r = e.matmul(pf[bk2], lhsT=hg[:, k, j * 128:(j + 1) * 128], rhs=wB[:, k, 256:768], start=(k == 0), stop=(k == 7))
                    return r
                op('tensor', mmq, reads=['wB', f'hTg{g % 2}'], writes=[PF[bk2]])
                normrope(pf[bk2], 8, 0, b, qbb[b][:, 0:512], [PF[bk2]])

                def trq(e, b=b):
                    for gg in range(4):
                        r = e.transpose(pbf[b][:, 128 + gg * 128:128 + (gg + 1) * 128], qbb[b][:, gg * 128:(gg + 1) * 128], ident)
                    return r
                op('tensor', trq, reads=['qbb', 'const'], writes=[PB[b]])
                op('scalar', lambda e, t=t, b=b: e.activation(out=QbT[:, :, t * 128:(t + 1) * 128], in_=pbf[b][:, 128:640].rearrange("p (g c) -> p g c", g=4), func=AF.Copy),
                   reads=[PB[b]], writes=['QbT'])
        dump('QbT', QbT, [128, 4, TOKR], BF16); dump('KbT', KbT, [128, NKEY], BF16); dump('Vb', Vb, [128, NTALL, 2, 65], BF16)

        it = 0
        for kvh in range(2):
            pr = slice(kvh * 64, kvh * 64 + 64)
            for qt in range(NTR):
                ob = qt % 2
                po = pf[4 + ob]
                for c in range(NTALL):
                    sb_ = it % 4; it += 1
                    st_ = pf[sb_]
                    op('tensor', lambda e, c=c, qt=qt, st_=st_, pr=pr, kvh=kvh: e.matmul(st_, lhsT=KbT[pr, c * 128:(c + 1) * 128], rhs=QbT[pr, :, qt * 128:(qt + 1) * 128],
                                                                                 start=True, stop=True, tile_position=(kvh * 64, 0)),
                       reads=['KbT', 'QbT'], writes=[PF[sb_]])
                    op('scalar', lambda e, st_=st_, sb_=sb_: e.activation(out=pTg[sb_], in_=st_, func=AF.Exp, bias=negC[:, 3:4], scale=1.0), reads=[PF[sb_], 'negC'], writes=[f'pTg{sb_}'])
                    op('tensor', lambda e, c=c, sb_=sb_, po=po, kvh=kvh: e.matmul(po[0:65, :], lhsT=Vb[:, c, kvh, :], rhs=pTg[sb_], start=(c == 0), stop=(c == NTALL - 1)),
                       reads=[f'pTg{sb_}', 'Vb'], writes=[PF[4 + ob]])
                op('scalar', lambda e, po=po, ob=ob: e.activation(out=osb[ob][0:65, :], in_=po[0:65, :], func=AF.Copy), reads=[PF[4 + ob]], writes=[f'osb{ob}'])
                op('vector', lambda e, ob=ob: e.reciprocal(out=rec[64:65, :], in_=osb[ob][64:65, :]), reads=[f'osb{ob}'], writes=['rec'])
                bk = 4 + ob
                op('tensor', lambda e, po=po: e.matmul(po[0:64, :], lhsT=ones_f[64:65, 0:64], rhs=rec[64:65, :], start=True, stop=True), reads=['rec', 'const'], writes=[PF[bk]])
                op('vector', lambda e, po=po, ob=ob, kvh=kvh, qt=qt: e.tensor_tensor(out=o_bT[0:64, kvh * 4:(kvh + 1) * 4, qt * 128:(qt + 1) * 128],
                                                                                 in0=osb[ob][0:64, :].rearrange("p (g t) -> p g t", g=4),
                                                                                 in1=po[0:64, :].rearrange("p (g t) -> p g t", g=4), op=ALU.mult),
                   reads=[f'osb{ob}', PF[bk]], writes=['o_bT'])
        dump('o_bT', o_bT, [128, 8, TOKR], BF16)
        S.barrier()
        if STAGE <= 3:
            return finish(nc, S, out, dbg_outs)

        A.reset(mark_ob)
        wG = A.alloc([8, 2048], BF16); wOA = A.alloc([4, D], BF16); wOB = A.alloc([8, D], BF16); wO = A.alloc([8, D], BF16)
        wR = A.alloc([8, NE], BF16); bR = A.alloc([NE], BF16)
        hTg = [A.alloc([8, 512], BF16)] * 2
        zT = [A.alloc([8, 512], BF16) for _ in range(2)]
        sga = A.alloc([512], F32); sgb = A.alloc([512], F32)
        xt4 = A.alloc([D], F32); x1t = [A.alloc([D], F32) for _ in range(2)]; tmpf = A.alloc([D], F32)
        h2b = [A.alloc([D], BF16) for _ in range(2)]; h2T = A.alloc([8, 128], BF16)
        lg = P.alloc([NTR, NE], F32); mx8 = P.alloc([NTR, 8], F32); posA = P.alloc([NTR, NE], F32)
        wts = P.alloc([NTR, 4], F32); sloti = P.alloc([NTR * 4], I32)
        mask = A.alloc([NE], F32); maskb = A.alloc([NE], BF16); cntp = P.alloc([NE], F32)
        sms = A.alloc([NTR, 8], F32); e4 = A.alloc([4], F32)
        utri = A.alloc([128], BF16); utf = A.alloc([128], F32)
        mark_route = A.off
        for (dst, src, nm) in ((wG[:, :, 0:1024], w_in[:, 2304:3328], 0), (wG[:, :, 1024:2048], w_in[:, 3328:4352], 1), (wO, w_o, 2)):
            op('gpsimd', lambda e, dst=dst, src=src: e.dma_start(out=dst, in_=src.rearrange("(k p) c -> p k c", p=128)), writes=['wM'], dma='d_wM')
        op('gpsimd', lambda e: e.dma_start(out=wOA, in_=w_oa.rearrange("(k p) c -> p k c", p=128)), writes=['wM'], dma='d_wM')
        op('gpsimd', lambda e: e.dma_start(out=wOB[0:64], in_=w_ob.rearrange("(h d) c -> d h c", d=64)), writes=['wM'], dma='d_wM')
        op('gpsimd', lambda e: e.dma_start(out=wR, in_=w_r.rearrange("(k p) c -> p k c", p=128)), writes=['wM'], dma='d_wM')
        op('gpsimd', lambda e: e.dma_start(out=bR[0:1, :], in_=b_r.rearrange("(o n) -> o n", o=1)), writes=['wM'], dma='d_wM')

        chain('gpsimd', [
            lambda e: e.memset(utf, 1.0),
            lambda e: e.affine_select(out=utf, in_=utf, pattern=[[1, 128]], compare_op=ALU.is_gt, fill=0.0, base=0, channel_multiplier=-1),
            lambda e: e.memset(cntp, 0.0),
            lambda e: e.tensor_copy(out=utri, in_=utf)], writes=['utri', 'route'])

        for g in range(5):
            hg = hTg[g % 2]; z = zT[g % 2]
            ntok = 512 if g < 4 else 256
            tk = slice(g * 512, g * 512 + ntok)
            op('sync', lambda e, g=g, hg=hg, ntok=ntok: e.dma_start(out=hg[:, :, 0:ntok], in_=hT_scr[:, :, g * 512:g * 512 + ntok]), reads=['hT_scr'], writes=[f'hTg{g % 2}'], dma=f'ldh{g % 2}')
            for oc in range(8):
                def mm4(e, hg=hg, oc=oc, ntok=ntok, tk=tk):
                    for k in range(8):
                        e.matmul(pf[0][:, 0:ntok], lhsT=wG[:, k, oc * 128:(oc + 1) * 128], rhs=hg[:, k, 0:ntok], start=(k == 0), stop=(k == 7))
                    for k in range(8):
                        e.matmul(pf[1][:, 0:ntok], lhsT=wG[:, k, 1024 + oc * 128:1024 + (oc + 1) * 128], rhs=hg[:, k, 0:ntok], start=(k == 0), stop=(k == 7))
                    for k in range(4):
                        e.matmul(pf[2][:, 0:ntok], lhsT=wOA[:, k, oc * 128:(oc + 1) * 128], rhs=o_aT[:, k, tk], start=(k == 0), stop=(k == 3))
                    for k in range(8):
                        r = e.matmul(pf[3][:, 0:ntok], lhsT=wOB[0:64, k, oc * 128:(oc + 1) * 128], rhs=o_bT[0:64, k, tk], start=(k == 0), stop=(k == 7))
                    return r
                op('tensor', mm4, reads=['wM', f'hTg{g % 2}', 'o_aT', 'o_bT'], writes=[PF[0], PF[1], PF[2], PF[3]])

                def sg(e, ntok=ntok):
                    e.activation(out=sga[:, 0:ntok], in_=pf[0][:, 0:ntok], func=AF.Sigmoid)
                    return e.activation(out=sgb[:, 0:ntok], in_=pf[1][:, 0:ntok], func=AF.Sigmoid)
                op('scalar', sg, reads=[PF[0], PF[1]], writes=['sg'])

                def zz(e, ntok=ntok):
                    e.tensor_tensor(out=sga[:, 0:ntok], in0=sga[:, 0:ntok], in1=pf[2][:, 0:ntok], op=ALU.mult)
                    return e.tensor_tensor(out=sgb[:, 0:ntok], in0=sgb[:, 0:ntok], in1=pf[3][:, 0:ntok], op=ALU.mult)
                op('vector', zz, reads=['sg', PF[2], PF[3]], writes=['sg2'])
                op('gpsimd', lambda e, z=z, oc=oc, ntok=ntok: e.tensor_tensor(out=z[:, oc, 0:ntok], in0=sga[:, 0:ntok], in1=sgb[:, 0:ntok], op=ALU.add),
                   reads=['sg2'], writes=[f'zT{g % 2}', 'sg'])
            for j in range(ntok // 128):
                t = 4 * g + j
                yb = pq[2]
                b = t % 2

                def mmy(e, z=z, j=j, yb=yb):
                    for n in range(2):
                        for k in range(8):
                            r = e.matmul(yb[:, n * 512:(n + 1) * 512], lhsT=z[:, k, j * 128:(j + 1) * 128], rhs=wO[:, k, n * 512:(n + 1) * 512], start=(k == 0), stop=(k == 7))
                    return r
                op('tensor', mmy, reads=['wM', f'zT{g % 2}'], writes=[PF[4], PF[5]])
                op('sync', lambda e, t=t: e.dma_start(out=xt4, in_=xc[t * 128:(t + 1) * 128, :]), writes=['xt'], dma='ldx0')
                op('scalar', lambda e, yb=yb, t=t: e.activation(out=tmpf, in_=yb, func=AF.Square, accum_out=sms[:, t, 0:1]), reads=[PF[4], PF[5]], writes=['tmpf', 'ssy'])
                rstd(sms[:, t, 1:2], sms[:, t, 0:1], D, ['ssy'], 'rsy')
                op('vector', lambda e, yb=yb, t=t: e.scalar_tensor_tensor(out=tmpf, in0=yb, scalar=sms[:, t, 1:2], in1=G1, op0=ALU.mult, op1=ALU.mult),
                   reads=[PF[4], PF[5], 'rsy', 'rows'], writes=['tmpf'])
                op('gpsimd', lambda e, b=b: e.tensor_tensor(out=x1t[b], in0=tmpf, in1=xt4, op=ALU.add), reads=['tmpf', 'xt'], writes=[f'x1t{b}'])
                op('sync', lambda e, t=t, b=b: e.dma_start(out=x1_scr[t * 128:(t + 1) * 128, :], in_=x1t[b]), reads=[f'x1t{b}'], writes=['x1_scr'], dma=f'stx{b}')
                op('scalar', lambda e, t=t, b=b: e.activation(out=tmpf, in_=x1t[b], func=AF.Square, accum_out=sms[:, t, 2:3]), reads=[f'x1t{b}'], writes=['tmpf', 'ss2'])
                rstd(sms[:, t, 3:4], sms[:, t, 2:3], D, ['ss2'], 'rs2')
                op('vector', lambda e, t=t, b=b: e.scalar_tensor_tensor(out=tmpf, in0=x1t[b], scalar=sms[:, t, 3:4], in1=A2, op0=ALU.mult, op1=ALU.mult),
                   reads=[f'x1t{b}', 'rs2', 'rows'], writes=['tmpf'])
                op('gpsimd', lambda e, b=b: e.tensor_tensor(out=h2b[b], in0=tmpf, in1=B2, op=ALU.add), reads=['tmpf', 'rows'], writes=[f'h2b{b}'])
                op('sync', lambda e, t=t, b=b: e.dma_start(out=h2_scr[t * 128:(t + 1) * 128, :], in_=h2b[b]), reads=[f'h2b{b}'], writes=['h2_scr'], dma=f'sth{b}')

                def trh(e, b=b):
                    for k in range(8):
                        r = e.transpose(pbf[b][:, k * 128:(k + 1) * 128], h2b[b][:, k * 128:(k + 1) * 128], ident)
                    return r
                op('tensor', trh, reads=[f'h2b{b}', 'const'], writes=[PB[b]])
                op('scalar', lambda e, b=b: e.activation(out=h2T, in_=pbf[b].rearrange("p (k c) -> p k c", k=8), func=AF.Copy), reads=[PB[b]], writes=['h2T'])

                def mml(e):
                    for k in range(8):
                        e.matmul(pf[0][:, 0:NE], lhsT=h2T[:, k, :], rhs=wR[:, k, :], start=(k == 0), stop=False)
                    return e.matmul(pf[0][:, 0:NE], lhsT=ones_bf[0:1, :], rhs=bR[0:1, :], start=False, stop=True)
                op('tensor', mml, reads=['h2T', 'wM', 'const'], writes=[PF[0]])

                chain('vector', [
                    lambda e, t=t: e.tensor_copy(out=lg[:, t, :], in_=pf[0][:, 0:NE]),
                    lambda e, t=t: e.max(out=mx8[:, t, :], in_=lg[:, t, :]),
                    lambda e, t=t: e.tensor_scalar(out=mask, in0=lg[:, t, :], scalar1=mx8[:, t, 3:4], scalar2=None, op0=ALU.is_ge),
                    lambda e: e.tensor_copy(out=maskb, in_=mask),
                    lambda e, t=t: e.tensor_scalar(out=sms[:, t, 4:5], in0=mx8[:, t, 0:1], scalar1=-1.0, scalar2=None, op0=ALU.mult)],
                    reads=[PF[0]], writes=['lg', 'maskb', 'negmx'])
                op('scalar', lambda e, t=t: e.activation(out=e4, in_=mx8[:, t, 0:4], func=AF.Exp, bias=sms[:, t, 4:5], scale=1.0, accum_out=sms[:, t, 5:6]),
                   reads=['lg', 'negmx'], writes=['e4'])

                def mmc(e):
                    e.matmul(pf[1][:, 0:NE], lhsT=utri, rhs=maskb, start=True, stop=True)
                    return e.matmul(pf[1][:, NE:2 * NE], lhsT=ones_bf, rhs=maskb, start=True, stop=True)
                op('tensor', mmc, reads=['maskb', 'utri', 'const'], writes=[PF[1]])

                chain('vector', [
                    lambda e, t=t: e.reciprocal(out=sms[:, t, 6:7], in_=sms[:, t, 5:6]),
                    lambda e, t=t: e.tensor_scalar(out=wts[:, t, :], in0=e4, scalar1=sms[:, t, 6:7], scalar2=None, op0=ALU.mult),
                    lambda e, t=t: e.tensor_tensor(out=posA[:, t, :], in0=pf[1][:, 0:NE], in1=cntp, op=ALU.add),
                    lambda e: e.tensor_tensor(out=cntp, in0=cntp, in1=pf[1][:, NE:2 * NE], op=ALU.add)],
                    reads=['e4', PF[1]], writes=['route'])
        dump('lg', lg, [128, NTR, NE], F32); dump('posA', posA, [128, NTR, NE], F32); dump('cntp', cntp, [128, NE], F32)
        S.barrier()
        if STAGE <= 4:
            return finish(nc, S, out, dbg_outs)

        A.reset()
        ci = A.alloc([NE], I32); padf = A.alloc([NE], F32); padT = A.alloc([128], F32); ltri = A.alloc([NE], F32)
        basef = A.alloc([NE], F32); pend = A.alloc([NE], F32)
        thr = A.alloc([NBLK], F32); EB = A.alloc([NBLK], F32); skp = A.alloc([NBLK], F32)
        idxw_f = A.alloc([NBLK], F32); idxb_f = A.alloc([NBLK], F32); pidx = A.alloc([1], F32)
        idxw = A.alloc([NBLK], I32); idxb = A.alloc([NBLK], I32)
        idxw8_f = A.alloc([8, NBLK], F32); idxw8 = A.alloc([8, NBLK], I32)
        idxd_f = A.alloc([4, NBLK], F32); idxd = A.alloc([4, NBLK], I32); idxd0 = A.alloc([NBLK], F32); pidx4 = A.alloc([1], F32)
        slot2 = A.alloc([NE], F32); slotf = A.alloc([NTR * 4], F32); tmp32 = A.alloc([NE], F32)
        h2l = [A.alloc([D], BF16) for _ in range(2)]

        chain('vector', [
            lambda e: e.tensor_scalar(out=padf, in0=cntp, scalar1=127.0, scalar2=None, op0=ALU.add),
            lambda e: e.tensor_copy(out=ci, in_=padf),
            lambda e: e.tensor_single_scalar(out=ci, in_=ci, scalar=7, op=ALU.arith_shift_right),
            lambda e: e.tensor_single_scalar(out=ci, in_=ci, scalar=7, op=ALU.logical_shift_left),
            lambda e: e.tensor_copy(out=padf, in_=ci)], reads=['route'], writes=['padf'])

        chain('gpsimd', [
            lambda e: e.memset(ltri, 1.0),
            lambda e: e.affine_select(out=ltri, in_=ltri, pattern=[[1, NE]], compare_op=ALU.is_gt, fill=0.0, base=0, channel_multiplier=-1),
            lambda e: e.iota(thr, pattern=[[128, NBLK]], base=0, channel_multiplier=0, allow_small_or_imprecise_dtypes=True),
            lambda e: e.iota(pidx, pattern=[[0, 1]], base=0, channel_multiplier=1, allow_small_or_imprecise_dtypes=True)], writes=['ltri'])
        op('tensor', lambda e: e.transpose(pq[0][0:NE, 0:128], padf, identf), reads=['padf', 'const'], writes=[PF[0]])
        op('vector', lambda e: e.tensor_copy(out=padT[0:NE, :], in_=pq[0][0:NE, 0:128]), reads=[PF[0]], writes=['padT'])
        op('tensor', lambda e: e.matmul(pf[1][:, 0:NE], lhsT=padT[0:NE, :], rhs=ltri[0:NE, :], start=True, stop=True), reads=['padT', 'ltri'], writes=[PF[1]])

        lay2 = [
            lambda e: e.tensor_copy(out=basef, in_=pf[1][:, 0:NE]),
            lambda e: e.tensor_tensor(out=pend, in0=basef, in1=padf, op=ALU.add),
            lambda e: e.memset(EB, 0.0)]
        for ex in range(NE):
            lay2.append(lambda e, ex=ex: e.scalar_tensor_tensor(out=EB, in0=thr, scalar=pend[:, ex:ex + 1], in1=EB, op0=ALU.is_ge, op1=ALU.add))
        lay2 += [
            lambda e: e.tensor_scalar(out=EB, in0=EB, scalar1=float(NE - 1), scalar2=None, op0=ALU.min),
            lambda e: e.memset(skp, 0.0),
            lambda e: e.tensor_tensor(out=skp[:, 1:NBLK], in0=EB[:, 1:NBLK], in1=EB[:, 0:NBLK - 1], op=ALU.is_equal),
            lambda e: e.tensor_scalar(out=skp, in0=skp, scalar1=BIG, scalar2=None, op0=ALU.mult),
            lambda e: e.scalar_tensor_tensor(out=idxw_f, in0=EB, scalar=1024.0, in1=skp, op0=ALU.mult, op1=ALU.add),
            lambda e: e.tensor_scalar(out=idxw_f, in0=idxw_f, scalar1=pidx[:, 0:1], scalar2=None, op0=ALU.add),
            lambda e: e.tensor_tensor(out=idxb_f, in0=EB, in1=skp, op=ALU.add),
            lambda e: e.tensor_copy(out=idxw, in_=idxw_f)]
        for k8 in range(8):
            lay2.append(lambda e, k8=k8: e.tensor_scalar(out=idxw8_f[:, k8, :], in0=idxw_f, scalar1=128.0 * k8, scalar2=None, op0=ALU.add))
        lay2 += [lambda e: e.tensor_copy(out=idxw8, in_=idxw8_f), lambda e: e.tensor_copy(out=idxb, in_=idxb_f)]
        lay2 += [lambda e: e.tensor_scalar(out=pidx4, in0=pidx, scalar1=4.0, scalar2=None, op0=ALU.mult),
                 lambda e: e.scalar_tensor_tensor(out=idxd0, in0=EB, scalar=512.0, in1=skp, op0=ALU.mult, op1=ALU.add),
                 lambda e: e.tensor_scalar(out=idxd0, in0=idxd0, scalar1=pidx4[:, 0:1], scalar2=None, op0=ALU.add)]
        for j4 in range(4):
            lay2.append(lambda e, j4=j4: e.tensor_scalar(out=idxd_f[:, j4, :], in0=idxd0, scalar1=float(j4), scalar2=None, op0=ALU.add))
        lay2 += [lambda e: e.tensor_copy(out=idxd, in_=idxd_f)]
        chain('vector', lay2, reads=[PF[1], 'padf', 'ltri'], writes=['lay'])
        for t in range(NTR):
            b = t % 2
            op('sync', lambda e, t=t, b=b: e.dma_start(out=h2l[b], in_=h2_scr[t * 128:(t + 1) * 128, :]), reads=['h2_scr'], writes=[f'h2l{b}'], dma=f'ldx{b}')

            slf = [lambda e, t=t: e.tensor_tensor(out=slot2, in0=posA[:, t, :], in1=basef, op=ALU.add)]
            for k in range(4):
                slf.append(lambda e, t=t, k=k: e.scalar_tensor_tensor(out=tmp32, in0=lg[:, t, :], scalar=mx8[:, t, k:k + 1], in1=slot2, op0=ALU.is_equal, op1=ALU.mult,
                                                                      accum_out=slotf[:, 4 * t + k:4 * t + k + 1]))
            slf.append(lambda e, t=t: e.tensor_copy(out=sloti[:, 4 * t:4 * t + 4], in_=slotf[:, 4 * t:4 * t + 4]))
            chain('vector', slf, reads=['lay'], writes=[f'sloti{t}', 'slot2'])
            for k in range(4):
                op('gpsimd', lambda e, t=t, k=k, b=b: e.indirect_dma_start(out=xs_scr, out_offset=bass.IndirectOffsetOnAxis(ap=sloti[:, 4 * t + k:4 * t + k + 1], axis=0),
                                                                       in_=h2l[b], in_offset=None, bounds_check=breg(e, NSLOT - 1), oob_is_err=False),
                   reads=[f'sloti{t}', f'h2l{b}'], writes=['xs_scr'], dma=f'sc{b}')
        dump('sloti', sloti, [128, NTR * 4], I32); dump('idxw', idxw, [128, NBLK], I32); dump('wts', wts, [128, NTR, 4], F32)
        S.barrier()
        if STAGE <= 5:
            return finish(nc, S, out, dbg_outs)

        mark_ex = A.off
        wgu = A.alloc([8, 2 * D], BF16); wdn4 = A.alloc([4, 2 * D], BF16)
        wdn_k = lambda k: wdn4[:, k // 2, (k % 2) * D:(k % 2 + 1) * D]
        wdn_pairs = w_dn.rearrange("e (q two) n -> (e q) (two n)", two=2)
        bgu = A.alloc([2 * D], BF16); bdn = A.alloc([D], BF16)
        xe = [A.alloc([D], BF16) for _ in range(2)]; xT = [A.alloc([8, 128], BF16) for _ in range(2)]
        gs = A.alloc([D], F32); sg_ = A.alloc([D], F32); l1 = A.alloc([D], F32); tt = A.alloc([D], F32)
        actb = A.alloc([D], BF16); aT = A.alloc([8, 128], BF16)
        yo = [A.alloc([D], F32) for _ in range(2)]
        wgu_flat = w_gu.rearrange("e k n -> (e k) n"); wdn_flat = w_dn.rearrange("e k n -> (e k) n")
        wgu_v = bass.AP(tensor=w_gu.tensor, offset=0, ap=[[2 * D, NE * D - 896], [128 * 2 * D, 8], [1, 2 * D]])
        wdn_v = bass.AP(tensor=w_dn.tensor, offset=0, ap=[[D, NE * D - 896], [128 * D, 8], [1, D]])
        for blk in range(NBLK):
            b = blk % 2
            iw = bass.IndirectOffsetOnAxis(ap=idxw[:, blk:blk + 1], axis=0)
            ib = bass.IndirectOffsetOnAxis(ap=idxb[:, blk:blk + 1], axis=0)
            for k8 in range(8):
                op('gpsimd', lambda e, blk=blk, k8=k8: e.indirect_dma_start(out=wgu[:, k8, :], out_offset=None, in_=wgu_flat,
                                                                            in_offset=bass.IndirectOffsetOnAxis(ap=idxw8[:, k8, blk:blk + 1], axis=0),
                                                                            bounds_check=breg(e, NE * D - 1), oob_is_err=False),
                   reads=['lay'], writes=['wgu'], dma='ld_wgu')
            op('gpsimd', lambda e, ib=ib: e.indirect_dma_start(out=bgu, out_offset=None, in_=b_gu, in_offset=ib, bounds_check=breg(e, NE - 1), oob_is_err=False),
               reads=['lay'], writes=['wgu'], dma='ld_wgu')
            for j4 in range(4):
                op('gpsimd', lambda e, blk=blk, j4=j4: e.indirect_dma_start(out=wdn4[:, j4, :], out_offset=None, in_=wdn_pairs,
                                                                            in_offset=bass.IndirectOffsetOnAxis(ap=idxd[:, j4, blk:blk + 1], axis=0),
                                                                            bounds_check=breg(e, NE * 512 - 1), oob_is_err=False),
                   reads=['lay'], writes=['wdn'], dma='ld_wdn')
            op('gpsimd', lambda e, ib=ib: e.indirect_dma_start(out=bdn, out_offset=None, in_=b_dn, in_offset=ib, bounds_check=breg(e, NE - 1), oob_is_err=False),
               reads=['lay'], writes=['wdn'], dma='ld_wdn')
            op('sync', lambda e, blk=blk, b=b: e.dma_start(out=xe[b], in_=xs_scr[blk * 128:(blk + 1) * 128, :]), reads=['xs_scr'], writes=[f'xe{b}'], dma=f'ldx{b}')

            def trx(e, b=b):
                for k in range(8):
                    r = e.transpose(pbf[0][:, k * 128:(k + 1) * 128], xe[b][:, k * 128:(k + 1) * 128], ident)
                return r
            op('tensor', trx, reads=[f'xe{b}', 'const'], writes=[PB[0]])
            op('scalar', lambda e, b=b: e.activation(out=xT[b], in_=pbf[0].rearrange("p (k c) -> p k c", k=8), func=AF.Copy), reads=[PB[0]], writes=[f'xT{b}'])

            def mgu(e, b=b):
                for k in range(8):
                    for n in range(4):
                        e.matmul(pf[n], lhsT=xT[b][:, k, :], rhs=wgu[:, k, n * 512:(n + 1) * 512], start=(k == 0), stop=False)
                for n in range(4):
                    r = e.matmul(pf[n], lhsT=ones_bf[0:1, :], rhs=bgu[0:1, n * 512:(n + 1) * 512], start=False, stop=True)
                return r
            op('tensor', mgu, reads=[f'xT{b}', 'wgu', 'const'], writes=[PF[0], PF[1], PF[2], PF[3]])
            op('vector', lambda e: e.tensor_scalar(out=gs, in0=pq[0], scalar1=7.0, scalar2=None, op0=ALU.min), reads=[PF[0], PF[1]], writes=['gs'])
            op('scalar', lambda e: e.activation(out=sg_, in_=gs, func=AF.Sigmoid, scale=1.702), reads=['gs'], writes=['sg_'])
            op('vector', lambda e: e.tensor_scalar(out=l1, in0=pq[1], scalar1=7.0, scalar2=-7.0, op0=ALU.min, op1=ALU.max), reads=[PF[2], PF[3]], writes=['l1'])
            op('vector', lambda e: e.tensor_tensor(out=tt, in0=gs, in1=sg_, op=ALU.mult), reads=['gs', 'sg_'], writes=['tt'])
            op('vector', lambda e: e.scalar_tensor_tensor(out=actb, in0=l1, scalar=1.0, in1=tt, op0=ALU.add, op1=ALU.mult), reads=['l1', 'tt'], writes=['actb'])

            def tra(e):
                for k in range(8):
                    r = e.transpose(pbf[1][:, k * 128:(k + 1) * 128], actb.rearrange("t (p k) -> t k p", k=8)[:, k, :], ident)
                return r
            op('tensor', tra, reads=['actb', 'const'], writes=[PB[1]])
            op('scalar', lambda e: e.activation(out=aT, in_=pbf[1].rearrange("p (k c) -> p k c", k=8), func=AF.Copy), reads=[PB[1]], writes=['aT'])

            def mdn(e):
                for k in range(8):
                    for n in range(2):
                        e.matmul(pf[4 + n], lhsT=aT[:, k, :], rhs=wdn_k(k)[:, n * 512:(n + 1) * 512], start=(k == 0), stop=False)
                for n in range(2):
                    r = e.matmul(pf[4 + n], lhsT=ones_bf[0:1, :], rhs=bdn[0:1, n * 512:(n + 1) * 512], start=False, stop=True)
                return r
            op('tensor', mdn, reads=['aT', 'wdn', 'const'], writes=[PF[4], PF[5]])
            op('scalar', lambda e, b=b: e.activation(out=yo[b], in_=pq[2], func=AF.Copy), reads=[PF[4], PF[5]], writes=[f'yo{b}'])
            op('sync', lambda e, blk=blk, b=b: e.dma_start(out=y_scr[blk * 128:(blk + 1) * 128, :], in_=yo[b]), reads=[f'yo{b}'], writes=['y_scr'], dma=f'sty{b}')
        S.barrier()

        A.reset(mark_ex)
        gk = [[A.alloc([D], F32) for _ in range(4)] for _ in range(2)]
        acc = A.alloc([D], F32); x1l = [A.alloc([D], F32) for _ in range(2)]; ot = [A.alloc([D], F32) for _ in range(2)]
        jk = A.alloc([D], F32)
        fs = A.alloc([NTR, 2], F32)
        for t in range(NTR):
            b = t % 2
            for k in range(4):
                op('gpsimd', lambda e, t=t, k=k, b=b: e.indirect_dma_start(out=gk[b][k], out_offset=None, in_=y_scr,
                                                                       in_offset=bass.IndirectOffsetOnAxis(ap=sloti[:, 4 * t + k:4 * t + k + 1], axis=0),
                                                                       bounds_check=breg(e, NSLOT - 1), oob_is_err=False),
                   reads=['y_scr'], writes=[f'gk{b}{k}'], dma=f'ga{b}')
            op('sync', lambda e, t=t, b=b: e.dma_start(out=x1l[b], in_=x1_scr[t * 128:(t + 1) * 128, :]), reads=['x1_scr'], writes=[f'x1l{b}'], dma=f'ldx{b}')

            cmb = [lambda e, t=t, b=b: e.tensor_scalar(out=acc, in0=gk[b][0], scalar1=wts[:, t, 0:1], scalar2=None, op0=ALU.mult)]
            for k in range(1, 4):
                cmb.append(lambda e, t=t, b=b, k=k: e.scalar_tensor_tensor(out=acc, in0=gk[b][k], scalar=wts[:, t, k:k + 1], in1=acc, op0=ALU.mult, op1=ALU.add))
            chain('vector', cmb, reads=[f'gk{b}{k}' for k in range(4)], writes=['acc'])
            op('scalar', lambda e, t=t: e.activation(out=jk, in_=acc, func=AF.Square, accum_out=fs[:, t, 0:1]), reads=['acc'], writes=['jk', 'fss'])
            rstd(fs[:, t, 1:2], fs[:, t, 0:1], D, ['fss'], 'fsr')
            op('vector', lambda e, t=t: e.scalar_tensor_tensor(out=acc, in0=acc, scalar=fs[:, t, 1:2], in1=G2, op0=ALU.mult, op1=ALU.mult), reads=['acc', 'fsr'], writes=['acc'])
            op('gpsimd', lambda e, b=b: e.tensor_tensor(out=ot[b], in0=acc, in1=x1l[b], op=ALU.add), reads=['acc', f'x1l{b}'], writes=[f'ot{b}'])
            op('sync', lambda e, t=t, b=b: e.dma_start(out=out[t * 128:(t + 1) * 128, :], in_=ot[b]), reads=[f'ot{b}'], writes=['out'], dma=f'sto{b}')
        return finish(nc, S, out, dbg_outs)


def finish(nc, S, out, dbg_outs):
    S.barrier()
    S.emit()
    return nc, dbg_outs


_CACHE = {}


def _host_tables():
    if 'rope' in _CACHE:
        return _CACHE['rope'], _CACHE['tblidx']
    half = 32; nf = 16
    freqs = (10000.0 ** (-np.arange(nf, dtype=np.float32) / nf)).astype(np.float32)
    rope = {}
    for hf in range(2):
        rng_rows = np.arange(28 * hf, 28 * hf + 36)
        rest = np.arange(36, 64) if hf == 0 else np.arange(0, 28)
        rows = np.concatenate([rng_rows, rest])
        tok = (rows[:, None] * 64 + np.arange(64)[None, :]).reshape(-1)
        r = (tok // 64).astype(np.float32); c = (tok % 64).astype(np.float32)
        cosT = np.ones((4352, 64), np.float32); sinT = np.zeros((4352, 64), np.float32)
        for hi, pos in enumerate((r, c)):
            ang = pos[:, None] * freqs[None, :]
            co = np.cos(ang).astype(np.float32); si = np.sin(ang).astype(np.float32)
            cosT[:4096, hi * 32:hi * 32 + 16] = co; cosT[:4096, hi * 32 + 16:hi * 32 + 32] = co
            sinT[:4096, hi * 32:hi * 32 + 16] = -si; sinT[:4096, hi * 32 + 16:hi * 32 + 32] = si
        rope[hf] = (np.ascontiguousarray(np.tile(cosT, (1, 8))), np.ascontiguousarray(np.tile(sinT, (1, 8))), tok)
    qc = np.arange(64)[:, None]; kc = np.arange(64)[None, :]
    c0 = np.clip(qc - 8, 0, 48)
    valid = (kc >= c0) & (kc < c0 + 16)
    off = np.clip(kc - qc + 15, 0, 30)
    _CACHE['rope'] = rope; _CACHE['tblidx'] = (valid, off)
    return rope, (valid, off)


def kernel(x, c, ctx, c_ctx, w_mod, b_mod, g_pre_mix, g_post_mix, g_pre_ffn, g_post_ffn, w_in, rpb, g_qnorm, g_knorm,
           w_out_a, w_out_b, w_o, w_router, b_router, w_gu, b_gu, w_dn, b_dn):
    f = lambda a: np.ascontiguousarray(np.asarray(a, dtype=np.float32))
    x = f(x); ctx = f(ctx); c = f(c); c_ctx = f(c_ctx)
    rope, (valid, off) = _host_tables()
    rp = f(rpb)[0]
    T = rp[:, :, off]
    T = np.where(valid[None, None], T, np.float32(NEG)).astype(np.float32)
    T = T.transpose(0, 2, 1, 3).reshape(4, 2 * 64, 15 * 64)
    w_in0 = f(w_in)[0]
    qb = w_in0[:, 1792:2304].reshape(1024, 2, 4, 64).transpose(0, 2, 1, 3).reshape(1024, 512)
    w_in_p = w_in0.copy(); w_in_p[:, 1792:2304] = qb
    shared = dict(w_mod=f(w_mod)[0], b_mod=f(b_mod)[0], gvec=np.stack([f(g_pre_mix)[0], f(g_post_mix)[0], f(g_pre_ffn)[0], f(g_post_ffn)[0]]),
                  w_in=w_in_p, tbl=np.ascontiguousarray(T), gqk=np.stack([f(g_qnorm)[0], f(g_knorm)[0]]),
                  w_oa=f(w_out_a)[0], w_ob=f(w_out_b)[0], w_o=f(w_o)[0], w_r=f(w_router)[0], b_r=f(b_router)[0],
                  w_gu=f(w_gu)[0], b_gu=f(b_gu)[0], w_dn=f(w_dn)[0], b_dn=f(b_dn)[0])
    in_maps = []
    for core in range(8):
        b, hf = core // 2, core % 2
        cosT, sinT, tok = rope[hf]
        xcore = np.concatenate([x[b][tok], ctx[b]], axis=0)
        m = dict(shared)
        m.update(xc=np.ascontiguousarray(xcore), cvec=np.stack([c[b], c_ctx]), ropec=cosT, ropes=sinT)
        in_maps.append(m)
    key = ('nc', STAGE, tuple(DEBUG))
    if key not in _CACHE:
        _CACHE[key] = build()
    nc, dbg = _CACHE[key]
    res = run_bass_kernel_spmd(nc, in_maps, core_ids=list(range(8)))
    _CACHE['last'] = res
    outp = np.empty((4, 4096, 1024), np.float32)
    for core in range(8):
        b, hf = core // 2, core % 2
        o = res.results[core]["out"]
        if hf == 0:
            outp[b, 0:2048] = o[0:2048]
        else:
            outp[b, 2048:4096] = o[256:2304]
    return outp
```

```python
import numpy as np
from contextlib import ExitStack
import concourse.bass as bass
import concourse.mybir as mybir
from concourse.bass_utils import run_bass_kernel_spmd

F32 = mybir.dt.float32; BF16 = mybir.dt.bfloat16; I32 = mybir.dt.int32; U8 = mybir.dt.uint8
AF = mybir.ActivationFunctionType; ALU = mybir.AluOpType; AX = mybir.AxisListType
ENG = ('tensor', 'vector', 'scalar', 'gpsimd', 'sync')
DSZ = {F32: 4, BF16: 2, I32: 4, U8: 1}

D = 1024; NTR = 18; TOKR = 2304; NTALL = 34; NKEY = 4352; NE = 32
NBLK = 104; NSLOT = NBLK * 128
EPS = 1e-6; NEG = -30000.0; BIG = 1.0e6
STAGE = 99
SAME_ENG_SYNC = True
DEBUG = []


class Sched:
    def __init__(self, nc, stack):
        self.nc = nc; self.stack = stack
        self.ops = {e: [] for e in ENG}
        self.sems = {}; self.cnt = {}
        self.last_write = {}; self.readers = {}
        self.waited = {e: {} for e in ENG}

    def sem(self, name):
        if name not in self.sems:
            self.sems[name] = self.stack.enter_context(self.nc.semaphore(name)); self.cnt[name] = 0
        return self.sems[name]

    def op(self, eng, fn, reads=(), writes=(), dma=None):
        waits = {}
        isdma_op = dma is not None

        def need(tok):
            if tok is None:
                return
            sname, val, teng, isdma = tok
            if teng == eng and not isdma and not isdma_op and (eng == 'tensor' or not SAME_ENG_SYNC):
                return
            if self.waited[eng].get(sname, 0) >= val:
                return
            waits[sname] = max(waits.get(sname, 0), val)
        for b in reads:
            need(self.last_write.get(b))
        for b in writes:
            need(self.last_write.get(b))
            for r in self.readers.get(b, ()):
                need(r)
        for s, v in waits.items():
            self.waited[eng][s] = v
        if isdma_op:
            sname = dma; inc = 16
        else:
            sname = 'e_' + eng; inc = 1
        self.sem(sname); self.cnt[sname] += inc
        tok = (sname, self.cnt[sname], eng, isdma_op)
        for b in writes:
            self.last_write[b] = tok; self.readers[b] = []
        for b in reads:
            self.readers.setdefault(b, []).append(tok)
        self.ops[eng].append((list(waits.items()), fn, sname, inc))
        return tok

    def barrier(self):
        for e in ENG:
            waits = []
            for s, c in self.cnt.items():
                if c > 0 and self.waited[e].get(s, 0) < c and s != 'e_' + e:
                    waits.append((s, c)); self.waited[e][s] = c
            if waits:
                self.ops[e].append((waits, None, None, None))
        self.last_write = {}; self.readers = {}

    def emit(self):
        with self.nc.Block() as block:
            for eng in ENG:
                ops = self.ops[eng]
                if not ops:
                    continue

                def body(e, ops=ops):
                    for waits, fn, sname, inc in ops:
                        for s, v in waits:
                            e.wait_ge(self.sems[s], v)
                        if fn is not None:
                            fn(e).then_inc(self.sems[sname], inc)
                getattr(block, eng)(body)


class Arena:
    def __init__(self, nc, st, name, nbytes):
        self.t = st.enter_context(nc.sbuf_tensor(name, [128, nbytes], U8)); self.off = 0; self.n = nbytes; self.name = name

    def alloc(self, free_shape, dt):
        n = int(np.prod(free_shape)) * DSZ[dt]
        n_al = (n + 63) // 64 * 64
        assert self.off + n_al <= self.n, (self.name, self.off, n_al, self.n)
        ap = self.t[:, self.off:self.off + n].bitcast(dt)
        self.off += n_al
        if len(free_shape) == 2:
            ap = ap.rearrange("p (a b) -> p a b", a=free_shape[0])
        elif len(free_shape) == 3:
            ap = ap.rearrange("p (a b c) -> p a b c", a=free_shape[0], b=free_shape[1])
        return ap

    def reset(self, off=0):
        self.off = off


def build():
    nc = bass.Bass("TRN2", target_bir_lowering=False)
    dt_in = lambda name, shape, dt=F32: nc.dram_tensor(name, shape, dt, kind="ExternalInput").ap()
    xc = dt_in("xc", [NKEY, D]); cvec = dt_in("cvec", [2, D]); w_mod = dt_in("w_mod", [D, 6 * D]); b_mod = dt_in("b_mod", [6 * D])
    gvec = dt_in("gvec", [4, D]); w_in = dt_in("w_in", [D, 4352]); tbl = dt_in("tbl", [4, 128, 960]); gqk = dt_in("gqk", [2, 64])
    ropec = dt_in("ropec", [NKEY, 512]); ropes = dt_in("ropes", [NKEY, 512])
    w_oa = dt_in("w_oa", [512, D]); w_ob = dt_in("w_ob", [512, D]); w_o = dt_in("w_o", [D, D])
    w_r = dt_in("w_r", [D, NE]); b_r = dt_in("b_r", [NE])
    w_gu = dt_in("w_gu", [NE, D, 2 * D]); b_gu = dt_in("b_gu", [NE, 2 * D]); w_dn = dt_in("w_dn", [NE, D, D]); b_dn = dt_in("b_dn", [NE, D])
    out = nc.dram_tensor("out", [TOKR, D], F32, kind="ExternalOutput").ap()
    hT_scr = nc.dram_tensor("hT_scr", [128, 8, NKEY + 128], BF16, kind="Internal").ap()
    x1_scr = nc.dram_tensor("x1_scr", [TOKR, D], F32, kind="Internal").ap()
    h2_scr = nc.dram_tensor("h2_scr", [TOKR, D], BF16, kind="Internal").ap()
    xs_scr = nc.dram_tensor("xs_scr", [NSLOT, D], BF16, kind="Internal").ap()
    y_scr = nc.dram_tensor("y_scr", [NSLOT, D], F32, kind="Internal").ap()
    dbg_outs = {}
    REG = {}

    def breg(e, v):
        if v not in REG:
            REG[v] = e.to_reg(v)
        return REG[v]

    with ExitStack() as st:
        S = Sched(nc, st)
        op = S.op

        def chain(eng, fns, reads=(), writes=()):
            for fn_ in fns:
                op(eng, fn_, reads=list(reads), writes=list(writes))
        A = Arena(nc, st, "arena", 182 * 1024)
        P = Arena(nc, st, "persist", 24 * 1024)
        pq = [st.enter_context(nc.psum_tensor(f"pq{i}", [128, 1024], F32)) for i in range(3)]
        pbf = [st.enter_context(nc.psum_tensor(f"pbf{i}", [128, 1024], BF16)) for i in range(2)]
        pq = [t_[:, :] for t_ in pq]; pbf = [t_[:, :] for t_ in pbf]
        pf = [pq[i // 2][:, (i % 2) * 512:(i % 2) * 512 + 512] for i in range(6)]
        PF = [f"pf{i}" for i in range(6)]; PB = ["pb0", "pb1"]

        def dump(name, ap, shape, dt):
            if name not in DEBUG:
                return
            S.barrier()
            o = nc.dram_tensor("dbg_" + name, shape, dt, kind="ExternalOutput").ap()
            dbg_outs[name] = o
            op('sync', lambda e: e.dma_start(out=o, in_=ap), dma='dbg')

        rows_late = P.alloc([4, D], F32)
        ident = P.alloc([128], BF16)
        identf = P.alloc([128], F32)
        ones_bf = P.alloc([128], BF16)
        ones_f = P.alloc([128], F32)
        G1, A2, B2, G2 = rows_late[:, 0, :], rows_late[:, 1, :], rows_late[:, 2, :], rows_late[:, 3, :]

        chain('gpsimd', [
            lambda e: e.memset(identf, 0.0),
            lambda e: e.affine_select(out=identf, in_=identf, pattern=[[-1, 128]], compare_op=ALU.not_equal, fill=1.0, base=0, channel_multiplier=1),
            lambda e: e.memset(ones_f, 1.0),
            lambda e: e.tensor_copy(out=ones_bf, in_=ones_f),
            lambda e: e.tensor_copy(out=ident, in_=identf)], writes=['const'])

        A.reset()
        rows_early = A.alloc([4, D], F32)
        A1, B1, A1c, B1c = rows_early[:, 0, :], rows_early[:, 1, :], rows_early[:, 2, :], rows_early[:, 3, :]
        mark_p1 = A.off
        modB = A.alloc([6 * D], F32); modC = A.alloc([2 * D], F32)
        gB = A.alloc([4, D], F32)
        bmB = A.alloc([6 * D], F32)
        cT = A.alloc([2, 8], F32); sT = A.alloc([2, 8], F32)
        rep = A.alloc([2, 8, 128], BF16)
        wm = [A.alloc([8, 512], BF16) for _ in range(2)]
        op('sync', lambda e: e.dma_start(out=cT, in_=cvec.rearrange("j (k p) -> p j k", p=128), allow_slow_non_contiguous=True), writes=['cT'], dma='d_cT')
        for i in range(4):
            op('sync', lambda e, i=i: e.dma_start(out=gB[:, i, :], in_=gvec[i, :].partition_broadcast(128)), writes=['gB'], dma='d_gB')
        op('sync', lambda e: e.dma_start(out=bmB, in_=b_mod.partition_broadcast(128)), writes=['bmB'], dma='d_bmB')
        op('scalar', lambda e: e.activation(out=sT, in_=cT, func=AF.Silu), reads=['cT'], writes=['sT'])

        def mk_rep(e):
            for j in range(2):
                for k in range(8):
                    r = e.tensor_scalar(out=rep[:, j, k, :], in0=ones_f, scalar1=sT[:, j, k:k + 1], scalar2=None, op0=ALU.mult)
            return r
        op('vector', mk_rep, reads=['sT', 'const'], writes=['rep'])
        for n in range(12):
            wb = wm[n % 2]
            op('gpsimd', lambda e, n=n, wb=wb: e.dma_start(out=wb, in_=w_mod[:, n * 512:(n + 1) * 512].rearrange("(k p) c -> p k c", p=128)),
               writes=[f'wm{n % 2}'], dma=f'ld_wm{n % 2}')
            for j in range(2 if n < 4 else 1):
                bk = (2 * n + j) % 6

                def mm(e, j=j, wb=wb, bk=bk):
                    for k in range(8):
                        r = e.matmul(pf[bk], lhsT=rep[:, j, k, :], rhs=wb[:, k, :], start=(k == 0), stop=(k == 7))
                    return r
                op('tensor', mm, reads=['rep', f'wm{n % 2}'], writes=[PF[bk]])
                dst = (modB if j == 0 else modC)[:, n * 512:(n + 1) * 512]
                op('vector', lambda e, dst=dst, bk=bk, n=n: e.tensor_tensor(out=dst, in0=pf[bk], in1=bmB[:, n * 512:(n + 1) * 512], op=ALU.add),
                   reads=[PF[bk], 'bmB'], writes=['modB'])

        def mk_rows(e):
            e.scalar_tensor_tensor(out=A1, in0=modB[:, D:2 * D], scalar=1.0, in1=gB[:, 0, :], op0=ALU.add, op1=ALU.mult)
            e.tensor_copy(out=B1, in_=modB[:, 0:D])
            e.scalar_tensor_tensor(out=A1c, in0=modC[:, D:2 * D], scalar=1.0, in1=gB[:, 0, :], op0=ALU.add, op1=ALU.mult)
            e.tensor_copy(out=B1c, in_=modC[:, 0:D])
            e.tensor_tensor(out=G1, in0=modB[:, 2 * D:3 * D], in1=gB[:, 1, :], op=ALU.mult)
            e.scalar_tensor_tensor(out=A2, in0=modB[:, 4 * D:5 * D], scalar=1.0, in1=gB[:, 2, :], op0=ALU.add, op1=ALU.mult)
            e.tensor_copy(out=B2, in_=modB[:, 3 * D:4 * D])
            return e.tensor_tensor(out=G2, in0=modB[:, 5 * D:6 * D], in1=gB[:, 3, :], op=ALU.mult)
        op('vector', mk_rows, reads=['modB', 'gB'], writes=['rows'])
        dump('rows_early', rows_early, [128, 4, D], F32)
        S.barrier()

        A.reset(mark_p1)
        xt = [A.alloc([D], F32) for _ in range(2)]
        hn = [A.alloc([D], F32) for _ in range(2)]
        hb = [A.alloc([D], BF16) for _ in range(2)]
        hTt = [A.alloc([8, 128], BF16) for _ in range(2)]
        junk = A.alloc([D], F32)
        ss = A.alloc([NTALL], F32); rs = A.alloc([NTALL], F32)

        def rstd(dst, src, n, reads, key):
            op('vector', lambda e: e.tensor_scalar(out=dst, in0=src, scalar1=1.0 / n, scalar2=EPS, op0=ALU.mult, op1=ALU.add), reads=reads, writes=[key])
            op('scalar', lambda e: e.activation(out=dst, in_=dst, func=AF.Sqrt), reads=[key], writes=[key])
            op('vector', lambda e: e.reciprocal(out=dst, in_=dst), reads=[key], writes=[key])

        for t in range(NTALL):
            b = t % 2
            Ar, Br = (A1, B1) if t < 32 else (A1c, B1c)
            op('sync', lambda e, t=t, b=b: e.dma_start(out=xt[b], in_=xc[t * 128:(t + 1) * 128, :]), writes=[f'xt{b}'], dma=f'ldx{b}')
            op('scalar', lambda e, t=t, b=b: e.activation(out=junk, in_=xt[b], func=AF.Square, accum_out=ss[:, t:t + 1]), reads=[f'xt{b}'], writes=['junk', f'ss{t}'])
            rstd(rs[:, t:t + 1], ss[:, t:t + 1], D, [f'ss{t}'], f'rs{t}')
            op('vector', lambda e, t=t, b=b, Ar=Ar: e.scalar_tensor_tensor(out=hn[b], in0=xt[b], scalar=rs[:, t:t + 1], in1=Ar, op0=ALU.mult, op1=ALU.mult),
               reads=[f'xt{b}', f'rs{t}', 'rows'], writes=[f'hn{b}'])
            op('gpsimd', lambda e, b=b, Br=Br: e.tensor_tensor(out=hb[b], in0=hn[b], in1=Br, op=ALU.add), reads=[f'hn{b}', 'rows'], writes=[f'hb{b}'])

            def tr(e, b=b):
                for k in range(8):
                    r = e.transpose(pbf[b][:, k * 128:(k + 1) * 128], hb[b][:, k * 128:(k + 1) * 128], ident)
                return r
            op('tensor', tr, reads=[f'hb{b}', 'const'], writes=[PB[b]])
            op('scalar', lambda e, b=b: e.activation(out=hTt[b], in_=pbf[b].rearrange("p (k c) -> p k c", k=8), func=AF.Copy), reads=[PB[b]], writes=[f'hTt{b}'])
            op('sync', lambda e, t=t, b=b: e.dma_start(out=hT_scr[:, :, t * 128:(t + 1) * 128], in_=hTt[b]), reads=[f'hTt{b}'], writes=['hT_scr'], dma=f'sth{b}')
        S.barrier()
        if STAGE <= 1:
            return finish(nc, S, out, dbg_outs)

        A.reset()
        o_aT = A.alloc([4, TOKR], BF16)
        mark_oa = A.off
        wA = A.alloc([8, 1536], BF16)
        QaT = A.alloc([4, TOKR], BF16); KaT = A.alloc([4, TOKR], BF16)
        Va_e = A.alloc([18, 512], BF16); Va_o = A.alloc([17, 512], BF16)
        KcaT = A.alloc([4, 256], BF16); Vca = A.alloc([2, 512], BF16)
        tblS = A.alloc([4, 960], F32)
        hTg = [A.alloc([8, 576], BF16) for _ in range(2)]
        sbt = [A.alloc([768], F32) for _ in range(2)]
        pbt = [A.alloc([768], BF16) for _ in range(2)]
        pnt = [A.alloc([768], BF16) for _ in range(2)]
        pTt = [A.alloc([768], BF16) for _ in range(2)]
        sm = A.alloc([2, 4], F32)
        for i, (c0, nm) in enumerate(((0, 'ka'), (512, 'va'), (1280, 'qa'))):
            op('gpsimd', lambda e, i=i, c0=c0: e.dma_start(out=wA[:, :, i * 512:(i + 1) * 512], in_=w_in[:, c0:c0 + 512].rearrange("(k p) c -> p k c", p=128)),
               writes=['wA'], dma='d_wA')
        for p in range(4):
            op('sync', lambda e, p=p: e.dma_start(out=tblS[:, p, :], in_=tbl[p]), writes=['tbl'], dma='d_tbl')
        bkc = [0]

        def nbk():
            bkc[0] = (bkc[0] + 1) % 6
            return bkc[0]

        def proj_fm(lhs_cols, rhs_ap, ntok, dst, scale=None):
            bk = nbk()

            def mm(e):
                for k in range(8):
                    r = e.matmul(pf[bk][:, 0:ntok], lhsT=wA[:, k, lhs_cols[0]:lhs_cols[1]], rhs=rhs_ap(k), start=(k == 0), stop=(k == 7))
                return r
            op('tensor', mm, reads=['wA', 'hTg'], writes=[PF[bk]])
            if scale is None:
                op('scalar', lambda e: e.activation(out=dst, in_=pf[bk][:, 0:ntok], func=AF.Copy), reads=[PF[bk]], writes=['naprep'])
            else:
                op('scalar', lambda e: e.activation(out=dst, in_=pf[bk][:, 0:ntok], func=AF.Copy, scale=scale), reads=[PF[bk]], writes=['naprep'])

        def proj_tm(lhs_ap, dst):
            bk = nbk()

            def mm(e):
                for k in range(8):
                    r = e.matmul(pf[bk], lhsT=lhs_ap(k), rhs=wA[:, k, 512:1024], start=(k == 0), stop=(k == 7))
                return r
            op('tensor', mm, reads=['wA', 'hTg'], writes=[PF[bk]])
            op('vector', lambda e: e.tensor_copy(out=dst, in_=pf[bk]), reads=[PF[bk]], writes=['naprep'])

        for g in range(5):
            hg = hTg[g % 2]
            ntok = 512 if g < 4 else 256
            op('sync', lambda e, g=g, hg=hg: e.dma_start(out=hg, in_=hT_scr[:, :, g * 512:g * 512 + 576]), reads=['hT_scr'], writes=['hTg'], dma=f'ldh{g % 2}')
            for c in range(4):
                proj_fm((c * 128, (c + 1) * 128), lambda k, hg=hg, ntok=ntok: hg[:, k, 0:ntok], ntok, KaT[:, c, g * 512:g * 512 + ntok])
                proj_fm((1024 + c * 128, 1024 + (c + 1) * 128), lambda k, hg=hg, ntok=ntok: hg[:, k, 0:ntok], ntok, QaT[:, c, g * 512:g * 512 + ntok], scale=0.125)
            for j in range(ntok // 128):
                proj_tm(lambda k, hg=hg, j=j: hg[:, k, j * 128:(j + 1) * 128], Va_e[:, 4 * g + j, :])
                if 4 * g + j <= 16:
                    proj_tm(lambda k, hg=hg, j=j: hg[:, k, 64 + j * 128:64 + (j + 1) * 128], Va_o[:, 4 * g + j, :])
        hg = hTg[1]
        op('sync', lambda e, hg=hg: e.dma_start(out=hg[:, :, 0:256], in_=hT_scr[:, :, 4096:4352]), reads=['hT_scr'], writes=['hTg'], dma='ldh1')
        for c in range(4):
            proj_fm((c * 128, (c + 1) * 128), lambda k, hg=hg: hg[:, k, 0:256], 256, KcaT[:, c, :])
        for j in range(2):
            proj_tm(lambda k, hg=hg, j=j: hg[:, k, j * 128:(j + 1) * 128], Vca[:, j, :])
        dump('QaT', QaT, [128, 4, TOKR], BF16); dump('KaT', KaT, [128, 4, TOKR], BF16); dump('Va_e', Va_e, [128, 18, 512], BF16)

        na_its = [(l, p) for l in range(36) for p in range(4)]

        def na_ctx(it):
            l, p = na_its[it]
            start = min(max(l - 4, 0), 28); u0 = start - l + 7; tok0 = start * 64
            b = it % 2
            return l, p, start, u0, tok0, b

        def na_stage1(it):
            l, p, start, u0, tok0, b = na_ctx(it)
            sl, sc, po = pf[b], pf[2 + b], pf[4 + b]
            sb_, pb_, pn_, pT_ = sbt[b], pbt[b], pnt[b], pTt[b]

            def qk(e, l=l, p=p, tok0=tok0, sl=sl, sc=sc):
                for hh in range(2):
                    ps_ = slice(hh * 64, hh * 64 + 64)
                    e.matmul(sl[ps_, :], lhsT=QaT[ps_, p, l * 64:(l + 1) * 64], rhs=KaT[ps_, p, tok0:tok0 + 512], start=True, stop=True, tile_position=(hh * 64, hh * 64))
                    r = e.matmul(sc[ps_, 0:256], lhsT=QaT[ps_, p, l * 64:(l + 1) * 64], rhs=KcaT[ps_, p, :], start=True, stop=True, tile_position=(hh * 64, hh * 64))
                return r
            op('tensor', qk, reads=['naprep'], writes=[PF[b], PF[2 + b]])
            op('vector', lambda e, sb_=sb_, sl=sl, p=p, u0=u0: e.tensor_tensor(out=sb_[:, 0:512], in0=sl, in1=tblS[:, p, u0 * 64:u0 * 64 + 512], op=ALU.add),
               reads=[PF[b], 'tbl'], writes=[f'sbA{b}'])
            op('scalar', lambda e, sb_=sb_, sc=sc: e.activation(out=sb_[:, 512:768], in_=sc[:, 0:256], func=AF.Copy), reads=[PF[2 + b]], writes=[f'sbB{b}'])

            chain('vector', [
                lambda e, sb_=sb_, b=b: e.tensor_reduce(out=sm[:, b, 0:1], in_=sb_, axis=AX.X, op=ALU.max),
                lambda e, b=b: e.tensor_scalar(out=sm[:, b, 1:2], in0=sm[:, b, 0:1], scalar1=-1.0, scalar2=None, op0=ALU.mult)],
                reads=[f'sbA{b}', f'sbB{b}'], writes=[f'negm{b}'])
            op('scalar', lambda e, sb_=sb_, pb_=pb_, b=b: e.activation(out=pb_, in_=sb_, func=AF.Exp, bias=sm[:, b, 1:2], scale=1.0, accum_out=sm[:, b, 2:3]),
               reads=[f'sbA{b}', f'sbB{b}', f'negm{b}'], writes=[f'pb{b}', f'sum{b}'])
            op('vector', lambda e, b=b: e.reciprocal(out=sm[:, b, 3:4], in_=sm[:, b, 2:3]), reads=[f'sum{b}'], writes=[f'rsum{b}'])
            op('gpsimd', lambda e, pn_=pn_, pb_=pb_, b=b: e.tensor_scalar(out=pn_, in0=pb_, scalar1=sm[:, b, 3:4], scalar2=None, op0=ALU.mult),
               reads=[f'pb{b}', f'rsum{b}'], writes=[f'pn{b}'])


        def na_stage2(it):
            l, p, start, u0, tok0, b = na_ctx(it)
            sl, sc, po = pf[b], pf[2 + b], pf[4 + b]
            sb_, pb_, pn_, pT_ = sbt[b], pbt[b], pnt[b], pTt[b]
            def trp(e, pn_=pn_, b=b):
                for c in range(6):
                    r = e.transpose(pbf[b][:, c * 128:(c + 1) * 128], pn_[:, c * 128:(c + 1) * 128], ident)
                return r
            op('tensor', trp, reads=[f'pn{b}', 'const'], writes=[PB[b]])
            op('scalar', lambda e, pT_=pT_, b=b: e.activation(out=pT_, in_=pbf[b][:, 0:768], func=AF.Copy), reads=[PB[b]], writes=[f'pT{b}'])

            def pv(e, pT_=pT_, po=po, p=p, start=start):
                for hh in range(2):
                    for c in range(6):
                        if c < 4:
                            V = Va_e[:, start // 2 + c, :] if start % 2 == 0 else Va_o[:, (start - 1) // 2 + c, :]
                        else:
                            V = Vca[:, c - 4, :]
                        r = e.matmul(po[hh * 64:hh * 64 + 64, 0:64], lhsT=V[:, p * 128 + hh * 64:p * 128 + hh * 64 + 64],
                                     rhs=pT_[:, c * 128 + hh * 64:c * 128 + hh * 64 + 64], start=(c == 0), stop=(c == 5), tile_position=(0, hh * 64))
                return r
            op('tensor', pv, reads=[f'pT{b}', 'naprep'], writes=[PF[4 + b]])
            op('vector', lambda e, po=po, p=p, l=l: e.tensor_copy(out=o_aT[:, p, l * 64:(l + 1) * 64], in_=po[:, 0:64]), reads=[PF[4 + b]], writes=['o_aT'])

        for step in range(len(na_its) + 1):
            if step < len(na_its):
                na_stage1(step)
            if step >= 1:
                na_stage2(step - 1)
        dump('o_aT', o_aT, [128, 4, TOKR], BF16)
        S.barrier()
        if STAGE <= 2:
            return finish(nc, S, out, dbg_outs)

        A.reset(mark_oa)
        o_bT = A.alloc([8, TOKR], BF16)
        mark_ob = A.off
        wB = A.alloc([8, 768], BF16)
        QbT = A.alloc([4, TOKR], BF16); KbT = A.alloc([NKEY], BF16)
        Vb = A.alloc([NTALL, 2, 65], BF16)
        gqB = A.alloc([2, 64], F32)
        gtmp = A.alloc([2, 64], F32)
        negC = A.alloc([4], F32)
        hTg = [A.alloc([8, 512], BF16) for _ in range(2)]
        rc = [A.alloc([512], F32) for _ in range(2)]; rsn = [A.alloc([512], F32) for _ in range(2)]
        sq = A.alloc([640], F32); ssh = A.alloc([2, 16], F32)
        qn = A.alloc([640], F32); t1 = A.alloc([640], F32); t2 = A.alloc([640], F32)
        qbb = [A.alloc([640], BF16) for _ in range(2)]
        pTg = [A.alloc([512], BF16) for _ in range(4)]
        osb = [A.alloc([512], F32) for _ in range(2)]
        rec = [A.alloc([512], F32) for _ in range(2)]
        op('gpsimd', lambda e: e.dma_start(out=wB[:, :, 0:256], in_=w_in[:, 1024:1280].rearrange("(k p) c -> p k c", p=128)), writes=['wB'], dma='d_wB')
        op('gpsimd', lambda e: e.dma_start(out=wB[:, :, 256:768], in_=w_in[:, 1792:2304].rearrange("(k p) c -> p k c", p=128)), writes=['wB'], dma='d_wB')
        for i in range(2):
            op('sync', lambda e, i=i: e.dma_start(out=gtmp[:, i, :], in_=gqk[i, :].partition_broadcast(128)), writes=['gtmp'], dma='d_gt')

        qv = qn[:, 0:128].rearrange("p (a b) -> p a b", a=2)
        chain('vector', [
            lambda e: e.tensor_scalar(out=gqB[:, 0, :], in0=gtmp[:, 0, :], scalar1=0.125, scalar2=None, op0=ALU.mult),
            lambda e: e.tensor_copy(out=gqB[:, 1, :], in_=gtmp[:, 1, :]),
            lambda e: e.tensor_scalar(out=qv, in0=gtmp, scalar1=-1.0, scalar2=None, op0=ALU.mult),
            lambda e: e.tensor_tensor(out=gtmp, in0=gtmp, in1=qv, op=ALU.max),
            lambda e: e.tensor_reduce(out=negC[:, 0:1], in_=gtmp[:, 0, :], axis=AX.X, op=ALU.max),
            lambda e: e.tensor_reduce(out=negC[:, 1:2], in_=gtmp[:, 1, :], axis=AX.X, op=ALU.max),
            lambda e: e.tensor_tensor(out=negC[:, 2:3], in0=negC[:, 0:1], in1=negC[:, 1:2], op=ALU.mult),
            lambda e: e.tensor_scalar(out=negC[:, 3:4], in0=negC[:, 2:3], scalar1=-8.0, scalar2=None, op0=ALU.mult),
            lambda e: e.memset(Vb[:, :, :, 64:65], 1.0)], reads=['gtmp'], writes=['gtmp', 'gqB', 'negC', 'Vb', 'qn'])

        def normrope(src, H, gi, b, dst, tagr):
            W = H * 64
            op('scalar', lambda e: e.activation(out=sq[:, 0:W], in_=src, func=AF.Square), reads=tagr, writes=['sq'])

            op('vector', lambda e: e.tensor_reduce(out=ssh[:, 0, 0:H], in_=sq[:, 0:W].rearrange("p (h d) -> p h d", d=64), axis=AX.X, op=ALU.add), reads=['sq'], writes=['ssh0'])
            rstd(ssh[:, 1, 0:H], ssh[:, 0, 0:H], 64, ['ssh0'], 'ssh1')

            def n1(e):
                for h in range(H):
                    r = e.scalar_tensor_tensor(out=qn[:, h * 64:(h + 1) * 64], in0=src[:, h * 64:(h + 1) * 64], scalar=ssh[:, 1, h:h + 1], in1=gqB[:, gi, :],
                                               op0=ALU.mult, op1=ALU.mult)
                return r
            op('vector', n1, reads=['ssh1', 'gqB'] + tagr, writes=['qn'])
            op('vector', lambda e: e.tensor_tensor(out=t1[:, 0:W], in0=qn[:, 0:W], in1=rc[b][:, 0:W], op=ALU.mult), reads=['qn', f'rc{b}'], writes=['t1'])

            def r2(e):
                q4 = qn[:, 0:W].rearrange("p (a s f) -> p a s f", s=2, f=16)
                s4 = rsn[b][:, 0:W].rearrange("p (a s f) -> p a s f", s=2, f=16)
                o4 = t2[:, 0:W].rearrange("p (a s f) -> p a s f", s=2, f=16)
                e.tensor_tensor(out=o4[:, :, 0, :], in0=q4[:, :, 1, :], in1=s4[:, :, 0, :], op=ALU.mult)
                return e.tensor_tensor(out=o4[:, :, 1, :], in0=q4[:, :, 0, :], in1=s4[:, :, 1, :], op=ALU.mult)
            op('vector', r2, reads=['qn', f'rc{b}'], writes=['t2'])
            op('vector', lambda e: e.tensor_tensor(out=dst, in0=t1[:, 0:W], in1=t2[:, 0:W], op=ALU.add), reads=['t1', 't2'], writes=['qbb'])

        for t in range(NTALL):
            b = t % 2
            g = t // 4
            hg = hTg[g % 2]
            if t % 4 == 0:
                n = min(512, NKEY - g * 512)
                op('sync', lambda e, g=g, hg=hg, n=n: e.dma_start(out=hg[:, :, 0:n], in_=hT_scr[:, :, g * 512:g * 512 + n]), reads=['hT_scr'], writes=[f'hTg{g % 2}'], dma=f'ldh{g % 2}')
            j = t % 4
            op('sync', lambda e, t=t, b=b: e.dma_start(out=rc[b], in_=ropec[t * 128:(t + 1) * 128, :]), writes=[f'rc{b}'], dma=f'ldr{b}')
            op('sync', lambda e, t=t, b=b: e.dma_start(out=rsn[b], in_=ropes[t * 128:(t + 1) * 128, :]), writes=[f'rc{b}'], dma=f'ldr{b}')
            bk = nbk()

            def mmkv(e, hg=hg, j=j, bk=bk):
                for k in range(8):
                    r = e.matmul(pf[bk][:, 0:256], lhsT=hg[:, k, j * 128:(j + 1) * 128], rhs=wB[:, k, 0:256], start=(k == 0), stop=(k == 7))
                return r
            op('tensor', mmkv, reads=['wB', f'hTg{g % 2}'], writes=[PF[bk]])
            op('scalar', lambda e, t=t, bk=bk: e.activation(out=Vb[:, t, :, 0:64], in_=pf[bk][:, 128:256].rearrange("p (h d) -> p h d", d=64), func=AF.Copy),
               reads=[PF[bk]], writes=['Vb'])
            normrope(pf[bk][:, 0:128], 2, 1, b, qbb[b][:, 0:128], [PF[bk]])
            op('tensor', lambda e, b=b: e.transpose(pbf[b][:, 0:128], qbb[b][:, 0:128], ident), reads=['qbb', 'const'], writes=[PB[b]])
            op('scalar', lambda e, t=t, b=b: e.activation(out=KbT[:, t * 128:(t + 1) * 128], in_=pbf[b][:, 0:128], func=AF.Copy), reads=[PB[b]], writes=['KbT'])
            if t < NTR:
                bk2 = nbk()

                def mmq(e, hg=hg, j=j, bk2=bk2):
                    for k in range(8):
                        r = e.matmul(pf[bk2], lhsT=hg[:, k, j * 128:(j + 1) * 128], rhs=wB[:, k, 256:768], start=(k == 0), stop=(k == 7))
                    return r
                op('tensor', mmq, reads=['wB', f'hTg{g % 2}'], writes=[PF[bk2]])
                normrope(pf[bk2], 8, 0, b, qbb[b][:, 0:512], [PF[bk2]])

                def trq(e, b=b):
                    for gg in range(4):
                        r = e.transpose(pbf[b][:, 128 + gg * 128:128 + (gg + 1) * 128], qbb[b][:, gg * 128:(gg + 1) * 128], ident)
                    return r
                op('tensor', trq, reads=['qbb', 'const'], writes=[PB[b]])
                op('scalar', lambda e, t=t, b=b: e.activation(out=QbT[:, :, t * 128:(t + 1) * 128], in_=pbf[b][:, 128:640].rearrange("p (g c) -> p g c", g=4), func=AF.Copy),
                   reads=[PB[b]], writes=['QbT'])
        dump('QbT', QbT, [128, 4, TOKR], BF16); dump('KbT', KbT, [128, NKEY], BF16); dump('Vb', Vb, [128, NTALL, 2, 65], BF16)

        chunks = [(kvh, qt, c) for kvh in range(2) for qt in range(NTR) for c in range(NTALL)]
        LA = 2
        pending = []

        def gq_S(i):
            kvh, qt, c = chunks[i]
            pr = slice(kvh * 64, kvh * 64 + 64)
            sb_ = i % 4
            st_ = pf[sb_]
            op('tensor', lambda e: e.matmul(st_, lhsT=KbT[pr, c * 128:(c + 1) * 128], rhs=QbT[pr, :, qt * 128:(qt + 1) * 128],
                                            start=True, stop=True, tile_position=(kvh * 64, 0)),
               reads=['KbT', 'QbT'], writes=[PF[sb_]])
            op('scalar', lambda e: e.activation(out=pTg[sb_], in_=st_, func=AF.Exp, bias=negC[:, 3:4], scale=1.0), reads=[PF[sb_], 'negC'], writes=[f'pTg{sb_}'])

        def gq_PV(i, step):
            kvh, qt, c = chunks[i]
            sb_ = i % 4
            ob = (kvh * NTR + qt) % 2
            po = pf[4 + ob]
            bk = 4 + ob
            op('tensor', lambda e: e.matmul(po[0:65, :], lhsT=Vb[:, c, kvh, :], rhs=pTg[sb_], start=(c == 0), stop=(c == NTALL - 1)),
               reads=[f'pTg{sb_}', 'Vb'], writes=[PF[bk]])
            if c == NTALL - 1:
                op('scalar', lambda e: e.activation(out=osb[ob][0:65, :], in_=po[0:65, :], func=AF.Copy), reads=[PF[bk]], writes=[f'osb{ob}'])
                op('vector', lambda e: e.reciprocal(out=rec[ob][64:65, :], in_=osb[ob][64:65, :]), reads=[f'osb{ob}'], writes=[f'rec{ob}'])

                def fin():
                    op('tensor', lambda e: e.matmul(po[0:64, :], lhsT=ones_f[64:65, 0:64], rhs=rec[ob][64:65, :], start=True, stop=True), reads=[f'rec{ob}', 'const'], writes=[PF[bk]])
                    op('vector', lambda e: e.tensor_tensor(out=o_bT[0:64, kvh * 4:(kvh + 1) * 4, qt * 128:(qt + 1) * 128],
                                                           in0=osb[ob][0:64, :].rearrange("p (g t) -> p g t", g=4),
                                                           in1=po[0:64, :].rearrange("p (g t) -> p g t", g=4), op=ALU.mult),
                       reads=[f'osb{ob}', PF[bk]], writes=['o_bT'])
                pending.append((step + 4, fin))

        for step in range(len(chunks) + LA + 8):
            if step < len(chunks):
                gq_S(step)
            if LA <= step < len(chunks) + LA:
                gq_PV(step - LA, step)
            for due, fn_ in list(pending):
                if due <= step:
                    fn_(); pending.remove((due, fn_))
        assert not pending
        dump('o_bT', o_bT, [128, 8, TOKR], BF16)
        S.barrier()
        if STAGE <= 3:
            return finish(nc, S, out, dbg_outs)

        A.reset(mark_ob)
        wG = A.alloc([8, 2048], BF16); wOA = A.alloc([4, D], BF16); wOB = A.alloc([8, D], BF16); wO = A.alloc([8, D], BF16)
        wR = A.alloc([8, NE], BF16); bR = A.alloc([NE], BF16)
        hTg = [A.alloc([8, 512], BF16)] * 2
        zT = [A.alloc([8, 512], BF16) for _ in range(2)]
        sga = A.alloc([512], F32); sgb = A.alloc([512], F32)
        xt4 = A.alloc([D], F32); x1t = [A.alloc([D], F32) for _ in range(2)]; tmpf = A.alloc([D], F32)
        h2b = [A.alloc([D], BF16) for _ in range(2)]; h2T = A.alloc([8, 128], BF16)
        lg = P.alloc([NTR, NE], F32); mx8 = P.alloc([NTR, 8], F32); posA = P.alloc([NTR, NE], F32)
        wts = P.alloc([NTR, 4], F32); sloti = P.alloc([NTR * 4], I32)
        mask = A.alloc([NE], F32); maskb = A.alloc([NE], BF16); cntp = P.alloc([NE], F32)
        sms = A.alloc([NTR, 8], F32); e4 = A.alloc([4], F32)
        utri = A.alloc([128], BF16); utf = A.alloc([128], F32)
        mark_route = A.off
        for (dst, src, nm) in ((wG[:, :, 0:1024], w_in[:, 2304:3328], 0), (wG[:, :, 1024:2048], w_in[:, 3328:4352], 1), (wO, w_o, 2)):
            op('gpsimd', lambda e, dst=dst, src=src: e.dma_start(out=dst, in_=src.rearrange("(k p) c -> p k c", p=128)), writes=['wM'], dma='d_wM')
        op('gpsimd', lambda e: e.dma_start(out=wOA, in_=w_oa.rearrange("(k p) c -> p k c", p=128)), writes=['wM'], dma='d_wM')
        op('gpsimd', lambda e: e.dma_start(out=wOB[0:64], in_=w_ob.rearrange("(h d) c -> d h c", d=64)), writes=['wM'], dma='d_wM')
        op('gpsimd', lambda e: e.dma_start(out=wR, in_=w_r.rearrange("(k p) c -> p k c", p=128)), writes=['wM'], dma='d_wM')
        op('gpsimd', lambda e: e.dma_start(out=bR[0:1, :], in_=b_r.rearrange("(o n) -> o n", o=1)), writes=['wM'], dma='d_wM')

        chain('gpsimd', [
            lambda e: e.memset(utf, 1.0),
            lambda e: e.affine_select(out=utf, in_=utf, pattern=[[1, 128]], compare_op=ALU.is_gt, fill=0.0, base=0, channel_multiplier=-1),
            lambda e: e.memset(cntp, 0.0),
            lambda e: e.tensor_copy(out=utri, in_=utf)], writes=['utri', 'route'])

        for g in range(5):
            hg = hTg[g % 2]; z = zT[g % 2]
            ntok = 512 if g < 4 else 256
            tk = slice(g * 512, g * 512 + ntok)
            op('sync', lambda e, g=g, hg=hg, ntok=ntok: e.dma_start(out=hg[:, :, 0:ntok], in_=hT_scr[:, :, g * 512:g * 512 + ntok]), reads=['hT_scr'], writes=[f'hTg{g % 2}'], dma=f'ldh{g % 2}')
            for oc in range(8):
                def mm4(e, hg=hg, oc=oc, ntok=ntok, tk=tk):
                    for k in range(8):
                        e.matmul(pf[0][:, 0:ntok], lhsT=wG[:, k, oc * 128:(oc + 1) * 128], rhs=hg[:, k, 0:ntok], start=(k == 0), stop=(k == 7))
                    for k in range(8):
                        e.matmul(pf[1][:, 0:ntok], lhsT=wG[:, k, 1024 + oc * 128:1024 + (oc + 1) * 128], rhs=hg[:, k, 0:ntok], start=(k == 0), stop=(k == 7))
                    for k in range(4):
                        e.matmul(pf[2][:, 0:ntok], lhsT=wOA[:, k, oc * 128:(oc + 1) * 128], rhs=o_aT[:, k, tk], start=(k == 0), stop=(k == 3))
                    for k in range(8):
                        r = e.matmul(pf[3][:, 0:ntok], lhsT=wOB[0:64, k, oc * 128:(oc + 1) * 128], rhs=o_bT[0:64, k, tk], start=(k == 0), stop=(k == 7))
                    return r
                op('tensor', mm4, reads=['wM', f'hTg{g % 2}', 'o_aT', 'o_bT'], writes=[PF[0], PF[1], PF[2], PF[3]])

                def sg(e, ntok=ntok):
                    e.activation(out=sga[:, 0:ntok], in_=pf[0][:, 0:ntok], func=AF.Sigmoid)
                    return e.activation(out=sgb[:, 0:ntok], in_=pf[1][:, 0:ntok], func=AF.Sigmoid)
                op('scalar', sg, reads=[PF[0], PF[1]], writes=['sg'])

                def zz(e, ntok=ntok):
                    e.tensor_tensor(out=sga[:, 0:ntok], in0=sga[:, 0:ntok], in1=pf[2][:, 0:ntok], op=ALU.mult)
                    return e.tensor_tensor(out=sgb[:, 0:ntok], in0=sgb[:, 0:ntok], in1=pf[3][:, 0:ntok], op=ALU.mult)
                op('vector', zz, reads=['sg', PF[2], PF[3]], writes=['sg2'])
                op('gpsimd', lambda e, z=z, oc=oc, ntok=ntok: e.tensor_tensor(out=z[:, oc, 0:ntok], in0=sga[:, 0:ntok], in1=sgb[:, 0:ntok], op=ALU.add),
                   reads=['sg2'], writes=[f'zT{g % 2}', 'sg'])
            for j in range(ntok // 128):
                t = 4 * g + j
                yb = pq[2]
                b = t % 2

                def mmy(e, z=z, j=j, yb=yb):
                    for n in range(2):
                        for k in range(8):
                            r = e.matmul(yb[:, n * 512:(n + 1) * 512], lhsT=z[:, k, j * 128:(j + 1) * 128], rhs=wO[:, k, n * 512:(n + 1) * 512], start=(k == 0), stop=(k == 7))
                    return r
                op('tensor', mmy, reads=['wM', f'zT{g % 2}'], writes=[PF[4], PF[5]])
                op('sync', lambda e, t=t: e.dma_start(out=xt4, in_=xc[t * 128:(t + 1) * 128, :]), writes=['xt'], dma='ldx0')
                op('scalar', lambda e, yb=yb, t=t: e.activation(out=tmpf, in_=yb, func=AF.Square, accum_out=sms[:, t, 0:1]), reads=[PF[4], PF[5]], writes=['tmpf', 'ssy'])
                rstd(sms[:, t, 1:2], sms[:, t, 0:1], D, ['ssy'], 'rsy')
                op('vector', lambda e, yb=yb, t=t: e.scalar_tensor_tensor(out=tmpf, in0=yb, scalar=sms[:, t, 1:2], in1=G1, op0=ALU.mult, op1=ALU.mult),
                   reads=[PF[4], PF[5], 'rsy', 'rows'], writes=['tmpf'])
                op('gpsimd', lambda e, b=b: e.tensor_tensor(out=x1t[b], in0=tmpf, in1=xt4, op=ALU.add), reads=['tmpf', 'xt'], writes=[f'x1t{b}'])
                op('sync', lambda e, t=t, b=b: e.dma_start(out=x1_scr[t * 128:(t + 1) * 128, :], in_=x1t[b]), reads=[f'x1t{b}'], writes=['x1_scr'], dma=f'stx{b}')
                op('scalar', lambda e, t=t, b=b: e.activation(out=tmpf, in_=x1t[b], func=AF.Square, accum_out=sms[:, t, 2:3]), reads=[f'x1t{b}'], writes=['tmpf', 'ss2'])
                rstd(sms[:, t, 3:4], sms[:, t, 2:3], D, ['ss2'], 'rs2')
                op('vector', lambda e, t=t, b=b: e.scalar_tensor_tensor(out=tmpf, in0=x1t[b], scalar=sms[:, t, 3:4], in1=A2, op0=ALU.mult, op1=ALU.mult),
                   reads=[f'x1t{b}', 'rs2', 'rows'], writes=['tmpf'])
                op('gpsimd', lambda e, b=b: e.tensor_tensor(out=h2b[b], in0=tmpf, in1=B2, op=ALU.add), reads=['tmpf', 'rows'], writes=[f'h2b{b}'])
                op('sync', lambda e, t=t, b=b: e.dma_start(out=h2_scr[t * 128:(t + 1) * 128, :], in_=h2b[b]), reads=[f'h2b{b}'], writes=['h2_scr'], dma=f'sth{b}')

                def trh(e, b=b):
                    for k in range(8):
                        r = e.transpose(pbf[b][:, k * 128:(k + 1) * 128], h2b[b][:, k * 128:(k + 1) * 128], ident)
                    return r
                op('tensor', trh, reads=[f'h2b{b}', 'const'], writes=[PB[b]])
                op('scalar', lambda e, b=b: e.activation(out=h2T, in_=pbf[b].rearrange("p (k c) -> p k c", k=8), func=AF.Copy), reads=[PB[b]], writes=['h2T'])

                def mml(e):
                    for k in range(8):
                        e.matmul(pf[0][:, 0:NE], lhsT=h2T[:, k, :], rhs=wR[:, k, :], start=(k == 0), stop=False)
                    return e.matmul(pf[0][:, 0:NE], lhsT=ones_bf[0:1, :], rhs=bR[0:1, :], start=False, stop=True)
                op('tensor', mml, reads=['h2T', 'wM', 'const'], writes=[PF[0]])

                chain('vector', [
                    lambda e, t=t: e.tensor_copy(out=lg[:, t, :], in_=pf[0][:, 0:NE]),
                    lambda e, t=t: e.max(out=mx8[:, t, :], in_=lg[:, t, :]),
                    lambda e, t=t: e.tensor_scalar(out=mask, in0=lg[:, t, :], scalar1=mx8[:, t, 3:4], scalar2=None, op0=ALU.is_ge),
                    lambda e: e.tensor_copy(out=maskb, in_=mask),
                    lambda e, t=t: e.tensor_scalar(out=sms[:, t, 4:5], in0=mx8[:, t, 0:1], scalar1=-1.0, scalar2=None, op0=ALU.mult)],
                    reads=[PF[0]], writes=['lg', 'maskb', 'negmx'])
                op('scalar', lambda e, t=t: e.activation(out=e4, in_=mx8[:, t, 0:4], func=AF.Exp, bias=sms[:, t, 4:5], scale=1.0, accum_out=sms[:, t, 5:6]),
                   reads=['lg', 'negmx'], writes=['e4'])

                def mmc(e):
                    e.matmul(pf[1][:, 0:NE], lhsT=utri, rhs=maskb, start=True, stop=True)
                    return e.matmul(pf[1][:, NE:2 * NE], lhsT=ones_bf, rhs=maskb, start=True, stop=True)
                op('tensor', mmc, reads=['maskb', 'utri', 'const'], writes=[PF[1]])

                chain('vector', [
                    lambda e, t=t: e.reciprocal(out=sms[:, t, 6:7], in_=sms[:, t, 5:6]),
                    lambda e, t=t: e.tensor_scalar(out=wts[:, t, :], in0=e4, scalar1=sms[:, t, 6:7], scalar2=None, op0=ALU.mult),
                    lambda e, t=t: e.tensor_tensor(out=posA[:, t, :], in0=pf[1][:, 0:NE], in1=cntp, op=ALU.add),
                    lambda e: e.tensor_tensor(out=cntp, in0=cntp, in1=pf[1][:, NE:2 * NE], op=ALU.add)],
                    reads=['e4', PF[1]], writes=['route'])
        dump('lg', lg, [128, NTR, NE], F32); dump('posA', posA, [128, NTR, NE], F32); dump('cntp', cntp, [128, NE], F32)
        S.barrier()
        if STAGE <= 4:
            return finish(nc, S, out, dbg_outs)

        A.reset()
        ci = A.alloc([NE], I32); padf = A.alloc([NE], F32); padT = A.alloc([128], F32); ltri = A.alloc([NE], F32)
        basef = A.alloc([NE], F32); pend = A.alloc([NE], F32)
        thr = A.alloc([NBLK], F32); EB = A.alloc([NBLK], F32); skp = A.alloc([NBLK], F32)
        idxw_f = A.alloc([NBLK], F32); idxb_f = A.alloc([NBLK], F32); pidx = A.alloc([1], F32)
        idxw = A.alloc([NBLK], I32); idxb = A.alloc([NBLK], I32)
        idxw8_f = A.alloc([8, NBLK], F32); idxw8 = A.alloc([8, NBLK], I32)
        idxd_f = A.alloc([4, NBLK], F32); idxd = A.alloc([4, NBLK], I32); idxd0 = A.alloc([NBLK], F32); pidx4 = A.alloc([1], F32)
        slot2 = A.alloc([NE], F32); slotf = A.alloc([NTR * 4], F32); tmp32 = A.alloc([NE], F32)
        h2l = [A.alloc([D], BF16) for _ in range(2)]

        chain('vector', [
            lambda e: e.tensor_scalar(out=padf, in0=cntp, scalar1=127.0, scalar2=None, op0=ALU.add),
            lambda e: e.tensor_copy(out=ci, in_=padf),
            lambda e: e.tensor_single_scalar(out=ci, in_=ci, scalar=7, op=ALU.arith_shift_right),
            lambda e: e.tensor_single_scalar(out=ci, in_=ci, scalar=7, op=ALU.logical_shift_left),
            lambda e: e.tensor_copy(out=padf, in_=ci)], reads=['route'], writes=['padf'])

        chain('gpsimd', [
            lambda e: e.memset(ltri, 1.0),
            lambda e: e.affine_select(out=ltri, in_=ltri, pattern=[[1, NE]], compare_op=ALU.is_gt, fill=0.0, base=0, channel_multiplier=-1),
            lambda e: e.iota(thr, pattern=[[128, NBLK]], base=0, channel_multiplier=0, allow_small_or_imprecise_dtypes=True),
            lambda e: e.iota(pidx, pattern=[[0, 1]], base=0, channel_multiplier=1, allow_small_or_imprecise_dtypes=True)], writes=['ltri'])
        op('tensor', lambda e: e.transpose(pq[0][0:NE, 0:128], padf, identf), reads=['padf', 'const'], writes=[PF[0]])
        op('vector', lambda e: e.tensor_copy(out=padT[0:NE, :], in_=pq[0][0:NE, 0:128]), reads=[PF[0]], writes=['padT'])
        op('tensor', lambda e: e.matmul(pf[1][:, 0:NE], lhsT=padT[0:NE, :], rhs=ltri[0:NE, :], start=True, stop=True), reads=['padT', 'ltri'], writes=[PF[1]])

        lay2 = [
            lambda e: e.tensor_copy(out=basef, in_=pf[1][:, 0:NE]),
            lambda e: e.tensor_tensor(out=pend, in0=basef, in1=padf, op=ALU.add),
            lambda e: e.memset(EB, 0.0)]
        for ex in range(NE):
            lay2.append(lambda e, ex=ex: e.scalar_tensor_tensor(out=EB, in0=thr, scalar=pend[:, ex:ex + 1], in1=EB, op0=ALU.is_ge, op1=ALU.add))
        lay2 += [
            lambda e: e.tensor_scalar(out=EB, in0=EB, scalar1=float(NE - 1), scalar2=None, op0=ALU.min),
            lambda e: e.memset(skp, 0.0),
            lambda e: e.tensor_tensor(out=skp[:, 1:NBLK], in0=EB[:, 1:NBLK], in1=EB[:, 0:NBLK - 1], op=ALU.is_equal),
            lambda e: e.tensor_scalar(out=skp, in0=skp, scalar1=BIG, scalar2=None, op0=ALU.mult),
            lambda e: e.scalar_tensor_tensor(out=idxw_f, in0=EB, scalar=1024.0, in1=skp, op0=ALU.mult, op1=ALU.add),
            lambda e: e.tensor_scalar(out=idxw_f, in0=idxw_f, scalar1=pidx[:, 0:1], scalar2=None, op0=ALU.add),
            lambda e: e.tensor_tensor(out=idxb_f, in0=EB, in1=skp, op=ALU.add),
            lambda e: e.tensor_copy(out=idxw, in_=idxw_f)]
        for k8 in range(8):
            lay2.append(lambda e, k8=k8: e.tensor_scalar(out=idxw8_f[:, k8, :], in0=idxw_f, scalar1=128.0 * k8, scalar2=None, op0=ALU.add))
        lay2 += [lambda e: e.tensor_copy(out=idxw8, in_=idxw8_f), lambda e: e.tensor_copy(out=idxb, in_=idxb_f)]
        lay2 += [lambda e: e.tensor_scalar(out=pidx4, in0=pidx, scalar1=4.0, scalar2=None, op0=ALU.mult),
                 lambda e: e.scalar_tensor_tensor(out=idxd0, in0=EB, scalar=512.0, in1=skp, op0=ALU.mult, op1=ALU.add),
                 lambda e: e.tensor_scalar(out=idxd0, in0=idxd0, scalar1=pidx4[:, 0:1], scalar2=None, op0=ALU.add)]
        for j4 in range(4):
            lay2.append(lambda e, j4=j4: e.tensor_scalar(out=idxd_f[:, j4, :], in0=idxd0, scalar1=float(j4), scalar2=None, op0=ALU.add))
        lay2 += [lambda e: e.tensor_copy(out=idxd, in_=idxd_f)]
        chain('vector', lay2, reads=[PF[1], 'padf', 'ltri'], writes=['lay'])
        for t in range(NTR):
            b = t % 2
            op('sync', lambda e, t=t, b=b: e.dma_start(out=h2l[b], in_=h2_scr[t * 128:(t + 1) * 128, :]), reads=['h2_scr'], writes=[f'h2l{b}'], dma=f'ldx{b}')

            slf = [lambda e, t=t: e.tensor_tensor(out=slot2, in0=posA[:, t, :], in1=basef, op=ALU.add)]
            for k in range(4):
                slf.append(lambda e, t=t, k=k: e.scalar_tensor_tensor(out=tmp32, in0=lg[:, t, :], scalar=mx8[:, t, k:k + 1], in1=slot2, op0=ALU.is_equal, op1=ALU.mult,
                                                                      accum_out=slotf[:, 4 * t + k:4 * t + k + 1]))
            slf.append(lambda e, t=t: e.tensor_copy(out=sloti[:, 4 * t:4 * t + 4], in_=slotf[:, 4 * t:4 * t + 4]))
            chain('vector', slf, reads=['lay'], writes=[f'sloti{t}', 'slot2'])
            for k in range(4):
                op('gpsimd', lambda e, t=t, k=k, b=b: e.indirect_dma_start(out=xs_scr, out_offset=bass.IndirectOffsetOnAxis(ap=sloti[:, 4 * t + k:4 * t + k + 1], axis=0),
                                                                       in_=h2l[b], in_offset=None, bounds_check=breg(e, NSLOT - 1), oob_is_err=False),
                   reads=[f'sloti{t}', f'h2l{b}'], writes=['xs_scr'], dma=f'sc{b}')
        dump('sloti', sloti, [128, NTR * 4], I32); dump('idxw', idxw, [128, NBLK], I32); dump('wts', wts, [128, NTR, 4], F32)
        S.barrier()
        if STAGE <= 5:
            return finish(nc, S, out, dbg_outs)

        mark_ex = A.off
        wgu = A.alloc([8, 2 * D], BF16); wdn4 = A.alloc([4, 2 * D], BF16)
        wdn_k = lambda k: wdn4[:, k // 2, (k % 2) * D:(k % 2 + 1) * D]
        wdn_pairs = w_dn.rearrange("e (q two) n -> (e q) (two n)", two=2)
        bgu = A.alloc([2 * D], BF16); bdn = A.alloc([D], BF16)
        xe = [A.alloc([D], BF16) for _ in range(2)]; xT = [A.alloc([8, 128], BF16) for _ in range(2)]
        gs = A.alloc([D], F32); sg_ = A.alloc([D], F32); l1 = A.alloc([D], F32); tt = A.alloc([D], F32)
        actb = A.alloc([D], BF16); aT = A.alloc([8, 128], BF16)
        yo = [A.alloc([D], F32) for _ in range(2)]
        wgu_flat = w_gu.rearrange("e k n -> (e k) n"); wdn_flat = w_dn.rearrange("e k n -> (e k) n")
        wgu_v = bass.AP(tensor=w_gu.tensor, offset=0, ap=[[2 * D, NE * D - 896], [128 * 2 * D, 8], [1, 2 * D]])
        wdn_v = bass.AP(tensor=w_dn.tensor, offset=0, ap=[[D, NE * D - 896], [128 * D, 8], [1, D]])
        for blk in range(NBLK):
            b = blk % 2
            iw = bass.IndirectOffsetOnAxis(ap=idxw[:, blk:blk + 1], axis=0)
            ib = bass.IndirectOffsetOnAxis(ap=idxb[:, blk:blk + 1], axis=0)
            for k8 in range(8):
                op('gpsimd', lambda e, blk=blk, k8=k8: e.indirect_dma_start(out=wgu[:, k8, :], out_offset=None, in_=wgu_flat,
                                                                            in_offset=bass.IndirectOffsetOnAxis(ap=idxw8[:, k8, blk:blk + 1], axis=0),
                                                                            bounds_check=breg(e, NE * D - 1), oob_is_err=False),
                   reads=['lay'], writes=['wgu'], dma='ld_wgu')
            op('gpsimd', lambda e, ib=ib: e.indirect_dma_start(out=bgu, out_offset=None, in_=b_gu, in_offset=ib, bounds_check=breg(e, NE - 1), oob_is_err=False),
               reads=['lay'], writes=['wgu'], dma='ld_wgu')
            for j4 in range(4):
                op('gpsimd', lambda e, blk=blk, j4=j4: e.indirect_dma_start(out=wdn4[:, j4, :], out_offset=None, in_=wdn_pairs,
                                                                            in_offset=bass.IndirectOffsetOnAxis(ap=idxd[:, j4, blk:blk + 1], axis=0),
                                                                            bounds_check=breg(e, NE * 512 - 1), oob_is_err=False),
                   reads=['lay'], writes=['wdn'], dma='ld_wdn')
            op('gpsimd', lambda e, ib=ib: e.indirect_dma_start(out=bdn, out_offset=None, in_=b_dn, in_offset=ib, bounds_check=breg(e, NE - 1), oob_is_err=False),
               reads=['lay'], writes=['wdn'], dma='ld_wdn')
            op('sync', lambda e, blk=blk, b=b: e.dma_start(out=xe[b], in_=xs_scr[blk * 128:(blk + 1) * 128, :]), reads=['xs_scr'], writes=[f'xe{b}'], dma=f'ldx{b}')

            def trx(e, b=b):
                for k in range(8):
                    r = e.transpose(pbf[0][:, k * 128:(k + 1) * 128], xe[b][:, k * 128:(k + 1) * 128], ident)
                return r
            op('tensor', trx, reads=[f'xe{b}', 'const'], writes=[PB[0]])
            op('scalar', lambda e, b=b: e.activation(out=xT[b], in_=pbf[0].rearrange("p (k c) -> p k c", k=8), func=AF.Copy), reads=[PB[0]], writes=[f'xT{b}'])

            def mgu(e, b=b):
                for k in range(8):
                    for n in range(4):
                        e.matmul(pf[n], lhsT=xT[b][:, k, :], rhs=wgu[:, k, n * 512:(n + 1) * 512], start=(k == 0), stop=False)
                for n in range(4):
                    r = e.matmul(pf[n], lhsT=ones_bf[0:1, :], rhs=bgu[0:1, n * 512:(n + 1) * 512], start=False, stop=True)
                return r
            op('tensor', mgu, reads=[f'xT{b}', 'wgu', 'const'], writes=[PF[0], PF[1], PF[2], PF[3]])
            op('vector', lambda e: e.tensor_scalar(out=gs, in0=pq[0], scalar1=7.0, scalar2=None, op0=ALU.min), reads=[PF[0], PF[1]], writes=['gs'])
            op('scalar', lambda e: e.activation(out=sg_, in_=gs, func=AF.Sigmoid, scale=1.702), reads=['gs'], writes=['sg_'])
            op('vector', lambda e: e.tensor_scalar(out=l1, in0=pq[1], scalar1=7.0, scalar2=-7.0, op0=ALU.min, op1=ALU.max), reads=[PF[2], PF[3]], writes=['l1'])
            op('vector', lambda e: e.tensor_tensor(out=tt, in0=gs, in1=sg_, op=ALU.mult), reads=['gs', 'sg_'], writes=['tt'])
            op('vector', lambda e: e.scalar_tensor_tensor(out=actb, in0=l1, scalar=1.0, in1=tt, op0=ALU.add, op1=ALU.mult), reads=['l1', 'tt'], writes=['actb'])

            def tra(e):
                for k in range(8):
                    r = e.transpose(pbf[1][:, k * 128:(k + 1) * 128], actb.rearrange("t (p k) -> t k p", k=8)[:, k, :], ident)
                return r
            op('tensor', tra, reads=['actb', 'const'], writes=[PB[1]])
            op('scalar', lambda e: e.activation(out=aT, in_=pbf[1].rearrange("p (k c) -> p k c", k=8), func=AF.Copy), reads=[PB[1]], writes=['aT'])

            def mdn(e):
                for k in range(8):
                    for n in range(2):
                        e.matmul(pf[4 + n], lhsT=aT[:, k, :], rhs=wdn_k(k)[:, n * 512:(n + 1) * 512], start=(k == 0), stop=False)
                for n in range(2):
                    r = e.matmul(pf[4 + n], lhsT=ones_bf[0:1, :], rhs=bdn[0:1, n * 512:(n + 1) * 512], start=False, stop=True)
                return r
            op('tensor', mdn, reads=['aT', 'wdn', 'const'], writes=[PF[4], PF[5]])
            op('scalar', lambda e, b=b: e.activation(out=yo[b], in_=pq[2], func=AF.Copy), reads=[PF[4], PF[5]], writes=[f'yo{b}'])
            op('sync', lambda e, blk=blk, b=b: e.dma_start(out=y_scr[blk * 128:(blk + 1) * 128, :], in_=yo[b]), reads=[f'yo{b}'], writes=['y_scr'], dma=f'sty{b}')
        S.barrier()

        A.reset(mark_ex)
        gk = [[A.alloc([D], F32) for _ in range(4)] for _ in range(2)]
        acc = A.alloc([D], F32); x1l = [A.alloc([D], F32) for _ in range(2)]; ot = [A.alloc([D], F32) for _ in range(2)]
        jk = A.alloc([D], F32)
        fs = A.alloc([NTR, 2], F32)
        for t in range(NTR):
            b = t % 2
            for k in range(4):
                op('gpsimd', lambda e, t=t, k=k, b=b: e.indirect_dma_start(out=gk[b][k], out_offset=None, in_=y_scr,
                                                                       in_offset=bass.IndirectOffsetOnAxis(ap=sloti[:, 4 * t + k:4 * t + k + 1], axis=0),
                                                                       bounds_check=breg(e, NSLOT - 1), oob_is_err=False),
                   reads=['y_scr'], writes=[f'gk{b}{k}'], dma=f'ga{b}')
            op('sync', lambda e, t=t, b=b: e.dma_start(out=x1l[b], in_=x1_scr[t * 128:(t + 1) * 128, :]), reads=['x1_scr'], writes=[f'x1l{b}'], dma=f'ldx{b}')

            cmb = [lambda e, t=t, b=b: e.tensor_scalar(out=acc, in0=gk[b][0], scalar1=wts[:, t, 0:1], scalar2=None, op0=ALU.mult)]
            for k in range(1, 4):
                cmb.append(lambda e, t=t, b=b, k=k: e.scalar_tensor_tensor(out=acc, in0=gk[b][k], scalar=wts[:, t, k:k + 1], in1=acc, op0=ALU.mult, op1=ALU.add))
            chain('vector', cmb, reads=[f'gk{b}{k}' for k in range(4)], writes=['acc'])
            op('scalar', lambda e, t=t: e.activation(out=jk, in_=acc, func=AF.Square, accum_out=fs[:, t, 0:1]), reads=['acc'], writes=['jk', 'fss'])
            rstd(fs[:, t, 1:2], fs[:, t, 0:1], D, ['fss'], 'fsr')
            op('vector', lambda e, t=t: e.scalar_tensor_tensor(out=acc, in0=acc, scalar=fs[:, t, 1:2], in1=G2, op0=ALU.mult, op1=ALU.mult), reads=['acc', 'fsr'], writes=['acc'])
            op('gpsimd', lambda e, b=b: e.tensor_tensor(out=ot[b], in0=acc, in1=x1l[b], op=ALU.add), reads=['acc', f'x1l{b}'], writes=[f'ot{b}'])
            op('sync', lambda e, t=t, b=b: e.dma_start(out=out[t * 128:(t + 1) * 128, :], in_=ot[b]), reads=[f'ot{b}'], writes=['out'], dma=f'sto{b}')
        return finish(nc, S, out, dbg_outs)


def finish(nc, S, out, dbg_outs):
    S.barrier()
    S.emit()
    return nc, dbg_outs


_CACHE = {}


def _host_tables():
    if 'rope' in _CACHE:
        return _CACHE['rope'], _CACHE['tblidx']
    half = 32; nf = 16
    freqs = (10000.0 ** (-np.arange(nf, dtype=np.float32) / nf)).astype(np.float32)
    rope = {}
    for hf in range(2):
        rng_rows = np.arange(28 * hf, 28 * hf + 36)
        rest = np.arange(36, 64) if hf == 0 else np.arange(0, 28)
        rows = np.concatenate([rng_rows, rest])
        tok = (rows[:, None] * 64 + np.arange(64)[None, :]).reshape(-1)
        r = (tok // 64).astype(np.float32); c = (tok % 64).astype(np.float32)
        cosT = np.ones((4352, 64), np.float32); sinT = np.zeros((4352, 64), np.float32)
        for hi, pos in enumerate((r, c)):
            ang = pos[:, None] * freqs[None, :]
            co = np.cos(ang).astype(np.float32); si = np.sin(ang).astype(np.float32)
            cosT[:4096, hi * 32:hi * 32 + 16] = co; cosT[:4096, hi * 32 + 16:hi * 32 + 32] = co
            sinT[:4096, hi * 32:hi * 32 + 16] = -si; sinT[:4096, hi * 32 + 16:hi * 32 + 32] = si
        rope[hf] = (np.ascontiguousarray(np.tile(cosT, (1, 8))), np.ascontiguousarray(np.tile(sinT, (1, 8))), tok)
    qc = np.arange(64)[:, None]; kc = np.arange(64)[None, :]
    c0 = np.clip(qc - 8, 0, 48)
    valid = (kc >= c0) & (kc < c0 + 16)
    off = np.clip(kc - qc + 15, 0, 30)
    _CACHE['rope'] = rope; _CACHE['tblidx'] = (valid, off)
    return rope, (valid, off)


def kernel(x, c, ctx, c_ctx, w_mod, b_mod, g_pre_mix, g_post_mix, g_pre_ffn, g_post_ffn, w_in, rpb, g_qnorm, g_knorm,
           w_out_a, w_out_b, w_o, w_router, b_router, w_gu, b_gu, w_dn, b_dn):
    f = lambda a: np.ascontiguousarray(np.asarray(a, dtype=np.float32))
    x = f(x); ctx = f(ctx); c = f(c); c_ctx = f(c_ctx)
    rope, (valid, off) = _host_tables()
    rp = f(rpb)[0]
    T = rp[:, :, off]
    T = np.where(valid[None, None], T, np.float32(NEG)).astype(np.float32)
    T = T.transpose(0, 2, 1, 3).reshape(4, 2 * 64, 15 * 64)
    w_in0 = f(w_in)[0]
    qb = w_in0[:, 1792:2304].reshape(1024, 2, 4, 64).transpose(0, 2, 1, 3).reshape(1024, 512)
    w_in_p = w_in0.copy(); w_in_p[:, 1792:2304] = qb
    shared = dict(w_mod=f(w_mod)[0], b_mod=f(b_mod)[0], gvec=np.stack([f(g_pre_mix)[0], f(g_post_mix)[0], f(g_pre_ffn)[0], f(g_post_ffn)[0]]),
                  w_in=w_in_p, tbl=np.ascontiguousarray(T), gqk=np.stack([f(g_qnorm)[0], f(g_knorm)[0]]),
                  w_oa=f(w_out_a)[0], w_ob=f(w_out_b)[0], w_o=f(w_o)[0], w_r=f(w_router)[0], b_r=f(b_router)[0],
                  w_gu=f(w_gu)[0], b_gu=f(b_gu)[0], w_dn=f(w_dn)[0], b_dn=f(b_dn)[0])
    in_maps = []
    for core in range(8):
        b, hf = core // 2, core % 2
        cosT, sinT, tok = rope[hf]
        xcore = np.concatenate([x[b][tok], ctx[b]], axis=0)
        m = dict(shared)
        m.update(xc=np.ascontiguousarray(xcore), cvec=np.stack([c[b], c_ctx]), ropec=cosT, ropes=sinT)
        in_maps.append(m)
    key = ('nc', STAGE, tuple(DEBUG))
    if key not in _CACHE:
        _CACHE[key] = build()
    nc, dbg = _CACHE[key]
    res = run_bass_kernel_spmd(nc, in_maps, core_ids=list(range(8)))
    _CACHE['last'] = res
    outp = np.empty((4, 4096, 1024), np.float32)
    for core in range(8):
        b, hf = core // 2, core % 2
        o = res.results[core]["out"]
        if hf == 0:
            outp[b, 0:2048] = o[0:2048]
        else:
            outp[b, 2048:4096] = o[256:2304]
    return outp
```

```python
import numpy as np
from contextlib import ExitStack
import concourse.bass as bass
import concourse.mybir as mybir
from concourse.bass_utils import run_bass_kernel_spmd

F32 = mybir.dt.float32; BF16 = mybir.dt.bfloat16; I32 = mybir.dt.int32; U8 = mybir.dt.uint8
AF = mybir.ActivationFunctionType; ALU = mybir.AluOpType; AX = mybir.AxisListType
ENG = ('tensor', 'vector', 'scalar', 'gpsimd', 'sync')
DSZ = {F32: 4, BF16: 2, I32: 4, U8: 1}

D = 1024; NTR = 18; TOKR = 2304; NTALL = 34; NKEY = 4352; NE = 32
NBLK = 104; NSLOT = NBLK * 128
EPS = 1e-6; NEG = -30000.0; BIG = 1.0e6
STAGE = 99
SAME_ENG_SYNC = True
DEBUG = []


class Sched:
    def __init__(self, nc, stack):
        self.nc = nc; self.stack = stack
        self.ops = {e: [] for e in ENG}
        self.sems = {}; self.cnt = {}
        self.last_write = {}; self.readers = {}
        self.waited = {e: {} for e in ENG}

    def sem(self, name):
        if name not in self.sems:
            self.sems[name] = self.stack.enter_context(self.nc.semaphore(name)); self.cnt[name] = 0
        return self.sems[name]

    def op(self, eng, fn, reads=(), writes=(), dma=None):
        waits = {}
        isdma_op = dma is not None

        def need(tok):
            if tok is None:
                return
            sname, val, teng, isdma = tok
            if teng == eng and not isdma and not isdma_op and (eng == 'tensor' or not SAME_ENG_SYNC):
                return
            if self.waited[eng].get(sname, 0) >= val:
                return
            waits[sname] = max(waits.get(sname, 0), val)
        for b in reads:
            need(self.last_write.get(b))
        for b in writes:
            need(self.last_write.get(b))
            for r in self.readers.get(b, ()):
                need(r)
        for s, v in waits.items():
            self.waited[eng][s] = v
        if isdma_op:
            sname = dma; inc = 16
        else:
            sname = 'e_' + eng; inc = 1
        self.sem(sname); self.cnt[sname] += inc
        tok = (sname, self.cnt[sname], eng, isdma_op)
        for b in writes:
            self.last_write[b] = tok; self.readers[b] = []
        for b in reads:
            self.readers.setdefault(b, []).append(tok)
        self.ops[eng].append((list(waits.items()), fn, sname, inc))
        return tok

    def barrier(self):
        for e in ENG:
            waits = []
            for s, c in self.cnt.items():
                if c > 0 and self.waited[e].get(s, 0) < c and s != 'e_' + e:
                    waits.append((s, c)); self.waited[e][s] = c
            if waits:
                self.ops[e].append((waits, None, None, None))
        self.last_write = {}; self.readers = {}

    def emit(self):
        with self.nc.Block() as block:
            for eng in ENG:
                ops = self.ops[eng]
                if not ops:
                    continue

                def body(e, ops=ops):
                    for waits, fn, sname, inc in ops:
                        for s, v in waits:
                            e.wait_ge(self.sems[s], v)
                        if fn is not None:
                            fn(e).then_inc(self.sems[sname], inc)
                getattr(block, eng)(body)


class Arena:
    def __init__(self, nc, st, name, nbytes):
        self.t = st.enter_context(nc.sbuf_tensor(name, [128, nbytes], U8)); self.off = 0; self.n = nbytes; self.name = name

    def alloc(self, free_shape, dt):
        n = int(np.prod(free_shape)) * DSZ[dt]
        n_al = (n + 63) // 64 * 64
        assert self.off + n_al <= self.n, (self.name, self.off, n_al, self.n)
        ap = self.t[:, self.off:self.off + n].bitcast(dt)
        self.off += n_al
        if len(free_shape) == 2:
            ap = ap.rearrange("p (a b) -> p a b", a=free_shape[0])
        elif len(free_shape) == 3:
            ap = ap.rearrange("p (a b c) -> p a b c", a=free_shape[0], b=free_shape[1])
        return ap

    def reset(self, off=0):
        self.off = off


def build():
    nc = bass.Bass("TRN2", target_bir_lowering=False)
    dt_in = lambda name, shape, dt=F32: nc.dram_tensor(name, shape, dt, kind="ExternalInput").ap()
    xc = dt_in("xc", [NKEY, D]); cvec = dt_in("cvec", [2, D]); w_mod = dt_in("w_mod", [D, 6 * D]); b_mod = dt_in("b_mod", [6 * D])
    gvec = dt_in("gvec", [4, D]); w_in = dt_in("w_in", [D, 4352]); tbl = dt_in("tbl", [4, 128, 960]); gqk = dt_in("gqk", [2, 64])
    ropec = dt_in("ropec", [NKEY, 512]); ropes = dt_in("ropes", [NKEY, 512])
    w_oa = dt_in("w_oa", [512, D]); w_ob = dt_in("w_ob", [512, D]); w_o = dt_in("w_o", [D, D])
    w_r = dt_in("w_r", [D, NE]); b_r = dt_in("b_r", [NE])
    w_gu = dt_in("w_gu", [NE, D, 2 * D]); b_gu = dt_in("b_gu", [NE, 2 * D]); w_dn = dt_in("w_dn", [NE, D, D]); b_dn = dt_in("b_dn", [NE, D])
    out = nc.dram_tensor("out", [TOKR, D], F32, kind="ExternalOutput").ap()
    hT_scr = nc.dram_tensor("hT_scr", [128, 8, NKEY + 128], BF16, kind="Internal").ap()
    x1_scr = nc.dram_tensor("x1_scr", [TOKR, D], F32, kind="Internal").ap()
    h2_scr = nc.dram_tensor("h2_scr", [TOKR, D], BF16, kind="Internal").ap()
    xs_scr = nc.dram_tensor("xs_scr", [NSLOT, D], BF16, kind="Internal").ap()
    y_scr = nc.dram_tensor("y_scr", [NSLOT, D], F32, kind="Internal").ap()
    dbg_outs = {}
    REG = {}

    def breg(e, v):
        if v not in REG:
            REG[v] = e.to_reg(v)
        return REG[v]

    with ExitStack() as st:
        S = Sched(nc, st)
        op = S.op

        def chain(eng, fns, reads=(), writes=()):
            for fn_ in fns:
                op(eng, fn_, reads=list(reads), writes=list(writes))
        A = Arena(nc, st, "arena", 182 * 1024)
        P = Arena(nc, st, "persist", 24 * 1024)
        pq = [st.enter_context(nc.psum_tensor(f"pq{i}", [128, 1024], F32)) for i in range(3)]
        pbf = [st.enter_context(nc.psum_tensor(f"pbf{i}", [128, 1024], BF16)) for i in range(2)]
        pq = [t_[:, :] for t_ in pq]; pbf = [t_[:, :] for t_ in pbf]
        pf = [pq[i // 2][:, (i % 2) * 512:(i % 2) * 512 + 512] for i in range(6)]
        PF = [f"pf{i}" for i in range(6)]; PB = ["pb0", "pb1"]

        def dump(name, ap, shape, dt):
            if name not in DEBUG:
                return
            S.barrier()
            o = nc.dram_tensor("dbg_" + name, shape, dt, kind="ExternalOutput").ap()
            dbg_outs[name] = o
            op('sync', lambda e: e.dma_start(out=o, in_=ap), dma='dbg')

        rows_late = P.alloc([4, D], F32)
        ident = P.alloc([128], BF16)
        identf = P.alloc([128], F32)
        ones_bf = P.alloc([128], BF16)
        ones_f = P.alloc([128], F32)
        G1, A2, B2, G2 = rows_late[:, 0, :], rows_late[:, 1, :], rows_late[:, 2, :], rows_late[:, 3, :]

        chain('gpsimd', [
            lambda e: e.memset(identf, 0.0),
            lambda e: e.affine_select(out=identf, in_=identf, pattern=[[-1, 128]], compare_op=ALU.not_equal, fill=1.0, base=0, channel_multiplier=1),
            lambda e: e.memset(ones_f, 1.0),
            lambda e: e.tensor_copy(out=ones_bf, in_=ones_f),
            lambda e: e.tensor_copy(out=ident, in_=identf)], writes=['const'])

        A.reset()
        rows_early = A.alloc([4, D], F32)
        A1, B1, A1c, B1c = rows_early[:, 0, :], rows_early[:, 1, :], rows_early[:, 2, :], rows_early[:, 3, :]
        mark_p1 = A.off
        modB = A.alloc([6 * D], F32); modC = A.alloc([2 * D], F32)
        gB = A.alloc([4, D], F32)
        bmB = A.alloc([6 * D], F32)
        cT = A.alloc([2, 8], F32); sT = A.alloc([2, 8], F32)
        rep = A.alloc([2, 8, 128], BF16)
        wm = [A.alloc([8, 512], BF16) for _ in range(2)]
        op('sync', lambda e: e.dma_start(out=cT, in_=cvec.rearrange("j (k p) -> p j k", p=128), allow_slow_non_contiguous=True), writes=['cT'], dma='d_cT')
        for i in range(4):
            op('sync', lambda e, i=i: e.dma_start(out=gB[:, i, :], in_=gvec[i, :].partition_broadcast(128)), writes=['gB'], dma='d_gB')
        op('sync', lambda e: e.dma_start(out=bmB, in_=b_mod.partition_broadcast(128)), writes=['bmB'], dma='d_bmB')
        op('scalar', lambda e: e.activation(out=sT, in_=cT, func=AF.Silu), reads=['cT'], writes=['sT'])

        def mk_rep(e):
            for j in range(2):
                for k in range(8):
                    r = e.tensor_scalar(out=rep[:, j, k, :], in0=ones_f, scalar1=sT[:, j, k:k + 1], scalar2=None, op0=ALU.mult)
            return r
        op('vector', mk_rep, reads=['sT', 'const'], writes=['rep'])
        for n in range(12):
            wb = wm[n % 2]
            op('gpsimd', lambda e, n=n, wb=wb: e.dma_start(out=wb, in_=w_mod[:, n * 512:(n + 1) * 512].rearrange("(k p) c -> p k c", p=128)),
               writes=[f'wm{n % 2}'], dma=f'ld_wm{n % 2}')
            for j in range(2 if n < 4 else 1):
                bk = (2 * n + j) % 6

                def mm(e, j=j, wb=wb, bk=bk):
                    for k in range(8):
                        r = e.matmul(pf[bk], lhsT=rep[:, j, k, :], rhs=wb[:, k, :], start=(k == 0), stop=(k == 7))
                    return r
                op('tensor', mm, reads=['rep', f'wm{n % 2}'], writes=[PF[bk]])
                dst = (modB if j == 0 else modC)[:, n * 512:(n + 1) * 512]
                op('vector', lambda e, dst=dst, bk=bk, n=n: e.tensor_tensor(out=dst, in0=pf[bk], in1=bmB[:, n * 512:(n + 1) * 512], op=ALU.add),
                   reads=[PF[bk], 'bmB'], writes=['modB'])

        def mk_rows(e):
            e.scalar_tensor_tensor(out=A1, in0=modB[:, D:2 * D], scalar=1.0, in1=gB[:, 0, :], op0=ALU.add, op1=ALU.mult)
            e.tensor_copy(out=B1, in_=modB[:, 0:D])
            e.scalar_tensor_tensor(out=A1c, in0=modC[:, D:2 * D], scalar=1.0, in1=gB[:, 0, :], op0=ALU.add, op1=ALU.mult)
            e.tensor_copy(out=B1c, in_=modC[:, 0:D])
            e.tensor_tensor(out=G1, in0=modB[:, 2 * D:3 * D], in1=gB[:, 1, :], op=ALU.mult)
            e.scalar_tensor_tensor(out=A2, in0=modB[:, 4 * D:5 * D], scalar=1.0, in1=gB[:, 2, :], op0=ALU.add, op1=ALU.mult)
            e.tensor_copy(out=B2, in_=modB[:, 3 * D:4 * D])
            return e.tensor_tensor(out=G2, in0=modB[:, 5 * D:6 * D], in1=gB[:, 3, :], op=ALU.mult)
        op('vector', mk_rows, reads=['modB', 'gB'], writes=['rows'])
        dump('rows_early', rows_early, [128, 4, D], F32)
        S.barrier()

        A.reset(mark_p1)
        xt = [A.alloc([D], F32) for _ in range(2)]
        hn = [A.alloc([D], F32) for _ in range(2)]
        hb = [A.alloc([D], BF16) for _ in range(2)]
        hTt = [A.alloc([8, 128], BF16) for _ in range(2)]
        junk = A.alloc([D], F32)
        ss = A.alloc([NTALL], F32); rs = A.alloc([NTALL], F32)

        def rstd(dst, src, n, reads, key):
            op('vector', lambda e: e.tensor_scalar(out=dst, in0=src, scalar1=1.0 / n, scalar2=EPS, op0=ALU.mult, op1=ALU.add), reads=reads, writes=[key])
            op('scalar', lambda e: e.activation(out=dst, in_=dst, func=AF.Sqrt), reads=[key], writes=[key])
            op('vector', lambda e: e.reciprocal(out=dst, in_=dst), reads=[key], writes=[key])

        for t in range(NTALL):
            b = t % 2
            Ar, Br = (A1, B1) if t < 32 else (A1c, B1c)
            op('sync', lambda e, t=t, b=b: e.dma_start(out=xt[b], in_=xc[t * 128:(t + 1) * 128, :]), writes=[f'xt{b}'], dma=f'ldx{b}')
            op('scalar', lambda e, t=t, b=b: e.activation(out=junk, in_=xt[b], func=AF.Square, accum_out=ss[:, t:t + 1]), reads=[f'xt{b}'], writes=['junk', f'ss{t}'])
            rstd(rs[:, t:t + 1], ss[:, t:t + 1], D, [f'ss{t}'], f'rs{t}')
            op('vector', lambda e, t=t, b=b, Ar=Ar: e.scalar_tensor_tensor(out=hn[b], in0=xt[b], scalar=rs[:, t:t + 1], in1=Ar, op0=ALU.mult, op1=ALU.mult),
               reads=[f'xt{b}', f'rs{t}', 'rows'], writes=[f'hn{b}'])
            op('gpsimd', lambda e, b=b, Br=Br: e.tensor_tensor(out=hb[b], in0=hn[b], in1=Br, op=ALU.add), reads=[f'hn{b}', 'rows'], writes=[f'hb{b}'])

            def tr(e, b=b):
                for k in range(8):
                    r = e.transpose(pbf[b][:, k * 128:(k + 1) * 128], hb[b][:, k * 128:(k + 1) * 128], ident)
                return r
            op('tensor', tr, reads=[f'hb{b}', 'const'], writes=[PB[b]])
            op('scalar', lambda e, b=b: e.activation(out=hTt[b], in_=pbf[b].rearrange("p (k c) -> p k c", k=8), func=AF.Copy), reads=[PB[b]], writes=[f'hTt{b}'])
            op('sync', lambda e, t=t, b=b: e.dma_start(out=hT_scr[:, :, t * 128:(t + 1) * 128], in_=hTt[b]), reads=[f'hTt{b}'], dma=f'sth{b}')
        S.barrier()
        if STAGE <= 1:
            return finish(nc, S, out, dbg_outs)

        A.reset()
        o_aT = A.alloc([4, TOKR], BF16)
        mark_oa = A.off
        wA = A.alloc([8, 1536], BF16)
        QaT = A.alloc([4, TOKR], BF16); KaT = A.alloc([4, TOKR], BF16)
        Va_e = A.alloc([18, 512], BF16); Va_o = A.alloc([17, 512], BF16)
        KcaT = A.alloc([4, 256], BF16); Vca = A.alloc([2, 512], BF16)
        tblS = A.alloc([4, 960], F32)
        hTg = [A.alloc([8, 576], BF16) for _ in range(2)]
        sbt = [A.alloc([768], F32) for _ in range(2)]
        pbt = [A.alloc([768], BF16) for _ in range(2)]
        pnt = [A.alloc([768], BF16) for _ in range(2)]
        pTt = [A.alloc([768], BF16) for _ in range(2)]
        sm = A.alloc([2, 4], F32)
        for i, (c0, nm) in enumerate(((0, 'ka'), (512, 'va'), (1280, 'qa'))):
            op('gpsimd', lambda e, i=i, c0=c0: e.dma_start(out=wA[:, :, i * 512:(i + 1) * 512], in_=w_in[:, c0:c0 + 512].rearrange("(k p) c -> p k c", p=128)),
               writes=['wA'], dma='d_wA')
        for p in range(4):
            op('sync', lambda e, p=p: e.dma_start(out=tblS[:, p, :], in_=tbl[p]), writes=['tbl'], dma='d_tbl')
        bkc = [0]

        def nbk():
            bkc[0] = (bkc[0] + 1) % 6
            return bkc[0]

        def proj_fm(lhs_cols, rhs_ap, ntok, dst, scale=None):
            bk = nbk()

            def mm(e):
                for k in range(8):
                    r = e.matmul(pf[bk][:, 0:ntok], lhsT=wA[:, k, lhs_cols[0]:lhs_cols[1]], rhs=rhs_ap(k), start=(k == 0), stop=(k == 7))
                return r
            op('tensor', mm, reads=['wA', 'hTg'], writes=[PF[bk]])
            if scale is None:
                op('scalar', lambda e: e.activation(out=dst, in_=pf[bk][:, 0:ntok], func=AF.Copy), reads=[PF[bk]], writes=['naprep'])
            else:
                op('scalar', lambda e: e.activation(out=dst, in_=pf[bk][:, 0:ntok], func=AF.Copy, scale=scale), reads=[PF[bk]], writes=['naprep'])

        def proj_tm(lhs_ap, dst):
            bk = nbk()

            def mm(e):
                for k in range(8):
                    r = e.matmul(pf[bk], lhsT=lhs_ap(k), rhs=wA[:, k, 512:1024], start=(k == 0), stop=(k == 7))
                return r
            op('tensor', mm, reads=['wA', 'hTg'], writes=[PF[bk]])
            op('vector', lambda e: e.tensor_copy(out=dst, in_=pf[bk]), reads=[PF[bk]], writes=['naprep'])

        for g in range(5):
            hg = hTg[g % 2]
            ntok = 512 if g < 4 else 256
            op('sync', lambda e, g=g, hg=hg: e.dma_start(out=hg, in_=hT_scr[:, :, g * 512:g * 512 + 576]), reads=['hT_scr'], writes=['hTg'], dma=f'ldh{g % 2}')
            for c in range(4):
                proj_fm((c * 128, (c + 1) * 128), lambda k, hg=hg, ntok=ntok: hg[:, k, 0:ntok], ntok, KaT[:, c, g * 512:g * 512 + ntok])
                proj_fm((1024 + c * 128, 1024 + (c + 1) * 128), lambda k, hg=hg, ntok=ntok: hg[:, k, 0:ntok], ntok, QaT[:, c, g * 512:g * 512 + ntok], scale=0.125)
            for j in range(ntok // 128):
                proj_tm(lambda k, hg=hg, j=j: hg[:, k, j * 128:(j + 1) * 128], Va_e[:, 4 * g + j, :])
                if 4 * g + j <= 16:
                    proj_tm(lambda k, hg=hg, j=j: hg[:, k, 64 + j * 128:64 + (j + 1) * 128], Va_o[:, 4 * g + j, :])
        hg = hTg[1]
        op('sync', lambda e, hg=hg: e.dma_start(out=hg[:, :, 0:256], in_=hT_scr[:, :, 4096:4352]), reads=['hT_scr'], writes=['hTg'], dma='ldh1')
        for c in range(4):
            proj_fm((c * 128, (c + 1) * 128), lambda k, hg=hg: hg[:, k, 0:256], 256, KcaT[:, c, :])
        for j in range(2):
            proj_tm(lambda k, hg=hg, j=j: hg[:, k, j * 128:(j + 1) * 128], Vca[:, j, :])
        dump('QaT', QaT, [128, 4, TOKR], BF16); dump('KaT', KaT, [128, 4, TOKR], BF16); dump('Va_e', Va_e, [128, 18, 512], BF16)

        na_its = [(l, p) for l in range(36) for p in range(4)]

        def na_ctx(it):
            l, p = na_its[it]
            start = min(max(l - 4, 0), 28); u0 = start - l + 7; tok0 = start * 64
            b = it % 2
            return l, p, start, u0, tok0, b

        def na_stage1(it):
            l, p, start, u0, tok0, b = na_ctx(it)
            sl, sc, po = pf[b], pf[2 + b], pf[4 + b]
            sb_, pb_, pn_, pT_ = sbt[b], pbt[b], pnt[b], pTt[b]

            def qk(e, l=l, p=p, tok0=tok0, sl=sl, sc=sc):
                for hh in range(2):
                    ps_ = slice(hh * 64, hh * 64 + 64)
                    e.matmul(sl[ps_, :], lhsT=QaT[ps_, p, l * 64:(l + 1) * 64], rhs=KaT[ps_, p, tok0:tok0 + 512], start=True, stop=True, tile_position=(hh * 64, hh * 64))
                    r = e.matmul(sc[ps_, 0:256], lhsT=QaT[ps_, p, l * 64:(l + 1) * 64], rhs=KcaT[ps_, p, :], start=True, stop=True, tile_position=(hh * 64, hh * 64))
                return r
            op('tensor', qk, reads=['naprep'], writes=[PF[b], PF[2 + b]])
            op('vector', lambda e, sb_=sb_, sl=sl, p=p, u0=u0: e.tensor_tensor(out=sb_[:, 0:512], in0=sl, in1=tblS[:, p, u0 * 64:u0 * 64 + 512], op=ALU.add),
               reads=[PF[b], 'tbl'], writes=[f'sbA{b}'])
            op('scalar', lambda e, sb_=sb_, sc=sc: e.activation(out=sb_[:, 512:768], in_=sc[:, 0:256], func=AF.Copy), reads=[PF[2 + b]], writes=[f'sbB{b}'])

            chain('vector', [
                lambda e, sb_=sb_, b=b: e.tensor_reduce(out=sm[:, b, 0:1], in_=sb_, axis=AX.X, op=ALU.max),
                lambda e, b=b: e.tensor_scalar(out=sm[:, b, 1:2], in0=sm[:, b, 0:1], scalar1=-1.0, scalar2=None, op0=ALU.mult)],
                reads=[f'sbA{b}', f'sbB{b}'], writes=[f'negm{b}'])
            op('scalar', lambda e, sb_=sb_, pb_=pb_, b=b: e.activation(out=pb_, in_=sb_, func=AF.Exp, bias=sm[:, b, 1:2], scale=1.0, accum_out=sm[:, b, 2:3]),
               reads=[f'sbA{b}', f'sbB{b}', f'negm{b}'], writes=[f'pb{b}', f'sum{b}'])
            op('vector', lambda e, b=b: e.reciprocal(out=sm[:, b, 3:4], in_=sm[:, b, 2:3]), reads=[f'sum{b}'], writes=[f'rsum{b}'])
            op('gpsimd', lambda e, pn_=pn_, pb_=pb_, b=b: e.tensor_scalar(out=pn_, in0=pb_, scalar1=sm[:, b, 3:4], scalar2=None, op0=ALU.mult),
               reads=[f'pb{b}', f'rsum{b}'], writes=[f'pn{b}'])


        def na_stage2(it):
            l, p, start, u0, tok0, b = na_ctx(it)
            sl, sc, po = pf[b], pf[2 + b], pf[4 + b]
            sb_, pb_, pn_, pT_ = sbt[b], pbt[b], pnt[b], pTt[b]
            def trp(e, pn_=pn_, b=b):
                for c in range(6):
                    r = e.transpose(pbf[b][:, c * 128:(c + 1) * 128], pn_[:, c * 128:(c + 1) * 128], ident)
                return r
            op('tensor', trp, reads=[f'pn{b}', 'const'], writes=[PB[b]])
            op('scalar', lambda e, pT_=pT_, b=b: e.activation(out=pT_, in_=pbf[b][:, 0:768], func=AF.Copy), reads=[PB[b]], writes=[f'pT{b}'])

            def pv(e, pT_=pT_, po=po, p=p, start=start):
                for hh in range(2):
                    for c in range(6):
                        if c < 4:
                            V = Va_e[:, start // 2 + c, :] if start % 2 == 0 else Va_o[:, (start - 1) // 2 + c, :]
                        else:
                            V = Vca[:, c - 4, :]
                        r = e.matmul(po[hh * 64:hh * 64 + 64, 0:64], lhsT=V[:, p * 128 + hh * 64:p * 128 + hh * 64 + 64],
                                     rhs=pT_[:, c * 128 + hh * 64:c * 128 + hh * 64 + 64], start=(c == 0), stop=(c == 5), tile_position=(0, hh * 64))
                return r
            op('tensor', pv, reads=[f'pT{b}', 'naprep'], writes=[PF[4 + b]])
            op('vector', lambda e, po=po, p=p, l=l: e.tensor_copy(out=o_aT[:, p, l * 64:(l + 1) * 64], in_=po[:, 0:64]), reads=[PF[4 + b]], writes=['o_aT'])

        for step in range(len(na_its) + 1):
            if step < len(na_its):
                na_stage1(step)
            if step >= 1:
                na_stage2(step - 1)
        dump('o_aT', o_aT, [128, 4, TOKR], BF16)
        S.barrier()
        if STAGE <= 2:
            return finish(nc, S, out, dbg_outs)

        A.reset(mark_oa)
        o_bT = A.alloc([8, TOKR], BF16)
        mark_ob = A.off
        wB = A.alloc([8, 768], BF16)
        QbT = A.alloc([4, TOKR], BF16); KbT = A.alloc([NKEY], BF16)
        Vb = A.alloc([NTALL, 2, 65], BF16)
        gqB = A.alloc([2, 64], F32)
        gtmp = A.alloc([2, 64], F32)
        negC = A.alloc([4], F32)
        hTg = [A.alloc([8, 512], BF16) for _ in range(2)]
        rc = [A.alloc([512], F32) for _ in range(2)]; rsn = [A.alloc([512], F32) for _ in range(2)]
        sq = A.alloc([640], F32); ssh = A.alloc([2, 16], F32)
        qn = A.alloc([640], F32); t1 = A.alloc([640], F32); t2 = A.alloc([640], F32)
        qbb = [A.alloc([640], BF16) for _ in range(2)]
        pTg = [A.alloc([512], BF16) for _ in range(4)]
        osb = [A.alloc([512], F32) for _ in range(2)]
        rec = [A.alloc([512], F32) for _ in range(2)]
        op('gpsimd', lambda e: e.dma_start(out=wB[:, :, 0:256], in_=w_in[:, 1024:1280].rearrange("(k p) c -> p k c", p=128)), writes=['wB'], dma='d_wB')
        op('gpsimd', lambda e: e.dma_start(out=wB[:, :, 256:768], in_=w_in[:, 1792:2304].rearrange("(k p) c -> p k c", p=128)), writes=['wB'], dma='d_wB')
        for i in range(2):
            op('sync', lambda e, i=i: e.dma_start(out=gtmp[:, i, :], in_=gqk[i, :].partition_broadcast(128)), writes=['gtmp'], dma='d_gt')

        qv = qn[:, 0:128].rearrange("p (a b) -> p a b", a=2)
        chain('vector', [
            lambda e: e.tensor_scalar(out=gqB[:, 0, :], in0=gtmp[:, 0, :], scalar1=0.125, scalar2=None, op0=ALU.mult),
            lambda e: e.tensor_copy(out=gqB[:, 1, :], in_=gtmp[:, 1, :]),
            lambda e: e.tensor_scalar(out=qv, in0=gtmp, scalar1=-1.0, scalar2=None, op0=ALU.mult),
            lambda e: e.tensor_tensor(out=gtmp, in0=gtmp, in1=qv, op=ALU.max),
            lambda e: e.tensor_reduce(out=negC[:, 0:1], in_=gtmp[:, 0, :], axis=AX.X, op=ALU.max),
            lambda e: e.tensor_reduce(out=negC[:, 1:2], in_=gtmp[:, 1, :], axis=AX.X, op=ALU.max),
            lambda e: e.tensor_tensor(out=negC[:, 2:3], in0=negC[:, 0:1], in1=negC[:, 1:2], op=ALU.mult),
            lambda e: e.tensor_scalar(out=negC[:, 3:4], in0=negC[:, 2:3], scalar1=-8.0, scalar2=None, op0=ALU.mult),
            lambda e: e.memset(Vb[:, :, :, 64:65], 1.0)], reads=['gtmp'], writes=['gtmp', 'gqB', 'negC', 'Vb', 'qn'])

        def normrope(src, H, gi, b, dst, tagr):
            W = H * 64
            op('scalar', lambda e: e.activation(out=sq[:, 0:W], in_=src, func=AF.Square), reads=tagr, writes=['sq'])

            op('vector', lambda e: e.tensor_reduce(out=ssh[:, 0, 0:H], in_=sq[:, 0:W].rearrange("p (h d) -> p h d", d=64), axis=AX.X, op=ALU.add), reads=['sq'], writes=['ssh0'])
            rstd(ssh[:, 1, 0:H], ssh[:, 0, 0:H], 64, ['ssh0'], 'ssh1')

            def n1(e):
                for h in range(H):
                    r = e.scalar_tensor_tensor(out=qn[:, h * 64:(h + 1) * 64], in0=src[:, h * 64:(h + 1) * 64], scalar=ssh[:, 1, h:h + 1], in1=gqB[:, gi, :],
                                               op0=ALU.mult, op1=ALU.mult)
                return r
            op('vector', n1, reads=['ssh1', 'gqB'] + tagr, writes=['qn'])
            op('vector', lambda e: e.tensor_tensor(out=t1[:, 0:W], in0=qn[:, 0:W], in1=rc[b][:, 0:W], op=ALU.mult), reads=['qn', f'rc{b}'], writes=['t1'])

            def r2(e):
                q4 = qn[:, 0:W].rearrange("p (a s f) -> p a s f", s=2, f=16)
                s4 = rsn[b][:, 0:W].rearrange("p (a s f) -> p a s f", s=2, f=16)
                o4 = t2[:, 0:W].rearrange("p (a s f) -> p a s f", s=2, f=16)
                e.tensor_tensor(out=o4[:, :, 0, :], in0=q4[:, :, 1, :], in1=s4[:, :, 0, :], op=ALU.mult)
                return e.tensor_tensor(out=o4[:, :, 1, :], in0=q4[:, :, 0, :], in1=s4[:, :, 1, :], op=ALU.mult)
            op('vector', r2, reads=['qn', f'rsn{b}'], writes=['t2'])
            op('vector', lambda e: e.tensor_tensor(out=dst, in0=t1[:, 0:W], in1=t2[:, 0:W], op=ALU.add), reads=['t1', 't2'], writes=['qbb'])

        for t in range(NTALL):
            b = t % 2
            g = t // 4
            hg = hTg[g % 2]
            if t % 4 == 0:
                n = min(512, NKEY - g * 512)
                op('sync', lambda e, g=g, hg=hg, n=n: e.dma_start(out=hg[:, :, 0:n], in_=hT_scr[:, :, g * 512:g * 512 + n]), reads=['hT_scr'], writes=[f'hTg{g % 2}'], dma=f'ldh{g % 2}')
            j = t % 4
            op('sync', lambda e, t=t, b=b: e.dma_start(out=rc[b], in_=ropec[t * 128:(t + 1) * 128, :]), writes=[f'rc{b}'], dma=f'ldr{b}')
            op('sync', lambda e, t=t, b=b: e.dma_start(out=rsn[b], in_=ropes[t * 128:(t + 1) * 128, :]), writes=[f'rsn{b}'], dma=f'lds{b}')
            bk = nbk()

            def mmkv(e, hg=hg, j=j, bk=bk):
                for k in range(8):
                    r = e.matmul(pf[bk][:, 0:256], lhsT=hg[:, k, j * 128:(j + 1) * 128], rhs=wB[:, k, 0:256], start=(k == 0), stop=(k == 7))
                return r
            op('tensor', mmkv, reads=['wB', f'hTg{g % 2}'], writes=[PF[bk]])
            op('scalar', lambda e, t=t, bk=bk: e.activation(out=Vb[:, t, :, 0:64], in_=pf[bk][:, 128:256].rearrange("p (h d) -> p h d", d=64), func=AF.Copy),
               reads=[PF[bk]], writes=['Vb'])
            normrope(pf[bk][:, 0:128], 2, 1, b, qbb[b][:, 0:128], [PF[bk]])
            op('tensor', lambda e, b=b: e.transpose(pbf[b][:, 0:128], qbb[b][:, 0:128], ident), reads=['qbb', 'const'], writes=[PB[b]])
            op('scalar', lambda e, t=t, b=b: e.activation(out=KbT[:, t * 128:(t + 1) * 128], in_=pbf[b][:, 0:128], func=AF.Copy), reads=[PB[b]], writes=['KbT'])
            if t < NTR:
                bk2 = nbk()

                def mmq(e, hg=hg, j=j, bk2=bk2):
                    for k in range(8):
                        r = e.matmul(pf[bk2], lhsT=hg[:, k, j * 128:(j + 1) * 128], rhs=wB[:, k, 256:768], start=(k == 0), stop=(k == 7))
                    return r
                op('tensor', mmq, reads=['wB', f'hTg{g % 2}'], writes=[PF[bk2]])
                normrope(pf[bk2], 8, 0, b, qbb[b][:, 0:512], [PF[bk2]])

                def trq(e, b=b):
                    for gg in range(4):
                        r = e.transpose(pbf[b][:, 128 + gg * 128:128 + (gg + 1) * 128], qbb[b][:, gg * 128:(gg + 1) * 128], ident)
                    return r
                op('tensor', trq, reads=['qbb', 'const'], writes=[PB[b]])
                op('scalar', lambda e, t=t, b=b: e.activation(out=QbT[:, :, t * 128:(t + 1) * 128], in_=pbf[b][:, 128:640].rearrange("p (g c) -> p g c", g=4), func=AF.Copy),
                   reads=[PB[b]], writes=['QbT'])
        dump('QbT', QbT, [128, 4, TOKR], BF16); dump('KbT', KbT, [128, NKEY], BF16); dump('Vb', Vb, [128, NTALL, 2, 65], BF16)

        chunks = [(kvh, qt, c) for kvh in range(2) for qt in range(NTR) for c in range(NTALL)]
        LA = 2
        pending = []

        def gq_S(i):
            kvh, qt, c = chunks[i]
            pr = slice(kvh * 64, kvh * 64 + 64)
            sb_ = i % 4
            st_ = pf[sb_]
            op('tensor', lambda e: e.matmul(st_, lhsT=KbT[pr, c * 128:(c + 1) * 128], rhs=QbT[pr, :, qt * 128:(qt + 1) * 128],
                                            start=True, stop=True, tile_position=(kvh * 64, 0)),
               reads=['KbT', 'QbT'], writes=[PF[sb_]])
            op('scalar', lambda e: e.activation(out=pTg[sb_], in_=st_, func=AF.Exp, bias=negC[:, 3:4], scale=1.0), reads=[PF[sb_], 'negC'], writes=[f'pTg{sb_}'])

        def gq_PV(i, step):
            kvh, qt, c = chunks[i]
            sb_ = i % 4
            ob = (kvh * NTR + qt) % 2
            po = pf[4 + ob]
            bk = 4 + ob
            op('tensor', lambda e: e.matmul(po[0:65, :], lhsT=Vb[:, c, kvh, :], rhs=pTg[sb_], start=(c == 0), stop=(c == NTALL - 1)),
               reads=[f'pTg{sb_}', 'Vb'], writes=[PF[bk]])
            if c == NTALL - 1:
                op('scalar', lambda e: e.activation(out=osb[ob][0:65, :], in_=po[0:65, :], func=AF.Copy), reads=[PF[bk]], writes=[f'osb{ob}'])
                op('vector', lambda e: e.reciprocal(out=rec[ob][64:65, :], in_=osb[ob][64:65, :]), reads=[f'osb{ob}'], writes=[f'rec{ob}'])

                def fin():
                    op('tensor', lambda e: e.matmul(po[0:64, :], lhsT=ones_f[64:65, 0:64], rhs=rec[ob][64:65, :], start=True, stop=True), reads=[f'rec{ob}', 'const'], writes=[PF[bk]])
                    op('vector', lambda e: e.tensor_tensor(out=o_bT[0:64, kvh * 4:(kvh + 1) * 4, qt * 128:(qt + 1) * 128],
                                                           in0=osb[ob][0:64, :].rearrange("p (g t) -> p g t", g=4),
                                                           in1=po[0:64, :].rearrange("p (g t) -> p g t", g=4), op=ALU.mult),
                       reads=[f'osb{ob}', PF[bk]], writes=['o_bT'])
                pending.append((step + 4, fin))

        for step in range(len(chunks) + LA + 8):
            if step < len(chunks):
                gq_S(step)
            if LA <= step < len(chunks) + LA:
                gq_PV(step - LA, step)
            for due, fn_ in list(pending):
                if due <= step:
                    fn_(); pending.remove((due, fn_))
        assert not pending
        dump('o_bT', o_bT, [128, 8, TOKR], BF16)
        S.barrier()
        if STAGE <= 3:
            return finish(nc, S, out, dbg_outs)

        A.reset(mark_ob)
        wG = A.alloc([8, 2048], BF16); wOA = A.alloc([4, D], BF16); wOB = A.alloc([8, D], BF16); wO = A.alloc([8, D], BF16)
        wR = A.alloc([8, NE], BF16); bR = A.alloc([NE], BF16)
        hTg = [A.alloc([8, 512], BF16)] * 2
        zT = [A.alloc([8, 512], BF16) for _ in range(2)]
        sga = A.alloc([512], F32); sgb = A.alloc([512], F32)
        xt4 = A.alloc([D], F32); x1t = [A.alloc([D], F32) for _ in range(2)]; tmpf = A.alloc([D], F32)
        h2b = [A.alloc([D], BF16) for _ in range(2)]; h2T = A.alloc([8, 128], BF16)
        lg = P.alloc([NTR, NE], F32); mx8 = P.alloc([NTR, 8], F32); posA = P.alloc([NTR, NE], F32)
        wts = P.alloc([NTR, 4], F32); sloti = P.alloc([NTR * 4], I32)
        mask = A.alloc([NE], F32); maskb = A.alloc([NE], BF16); cntp = P.alloc([NE], F32)
        sms = A.alloc([NTR, 8], F32); e4 = A.alloc([4], F32)
        utri = A.alloc([128], BF16); utf = A.alloc([128], F32)
        mark_route = A.off
        for (dst, src, nm) in ((wG[:, :, 0:1024], w_in[:, 2304:3328], 0), (wG[:, :, 1024:2048], w_in[:, 3328:4352], 1), (wO, w_o, 2)):
            op('gpsimd', lambda e, dst=dst, src=src: e.dma_start(out=dst, in_=src.rearrange("(k p) c -> p k c", p=128)), writes=['wM'], dma='d_wM')
        op('gpsimd', lambda e: e.dma_start(out=wOA, in_=w_oa.rearrange("(k p) c -> p k c", p=128)), writes=['wM'], dma='d_wM')
        op('gpsimd', lambda e: e.dma_start(out=wOB[0:64], in_=w_ob.rearrange("(h d) c -> d h c", d=64)), writes=['wM'], dma='d_wM')
        op('gpsimd', lambda e: e.dma_start(out=wR, in_=w_r.rearrange("(k p) c -> p k c", p=128)), writes=['wM'], dma='d_wM')
        op('gpsimd', lambda e: e.dma_start(out=bR[0:1, :], in_=b_r.rearrange("(o n) -> o n", o=1)), writes=['wM'], dma='d_wM')

        chain('gpsimd', [
            lambda e: e.memset(utf, 1.0),
            lambda e: e.affine_select(out=utf, in_=utf, pattern=[[1, 128]], compare_op=ALU.is_gt, fill=0.0, base=0, channel_multiplier=-1),
            lambda e: e.memset(cntp, 0.0),
            lambda e: e.tensor_copy(out=utri, in_=utf)], writes=['utri', 'route'])

        for g in range(5):
            hg = hTg[g % 2]; z = zT[g % 2]
            ntok = 512 if g < 4 else 256
            tk = slice(g * 512, g * 512 + ntok)
            op('sync', lambda e, g=g, hg=hg, ntok=ntok: e.dma_start(out=hg[:, :, 0:ntok], in_=hT_scr[:, :, g * 512:g * 512 + ntok]), reads=['hT_scr'], writes=[f'hTg{g % 2}'], dma=f'ldh{g % 2}')
            for oc in range(8):
                def mm4(e, hg=hg, oc=oc, ntok=ntok, tk=tk):
                    for k in range(8):
                        e.matmul(pf[0][:, 0:ntok], lhsT=wG[:, k, oc * 128:(oc + 1) * 128], rhs=hg[:, k, 0:ntok], start=(k == 0), stop=(k == 7))
                    for k in range(8):
                        e.matmul(pf[1][:, 0:ntok], lhsT=wG[:, k, 1024 + oc * 128:1024 + (oc + 1) * 128], rhs=hg[:, k, 0:ntok], start=(k == 0), stop=(k == 7))
                    for k in range(4):
                        e.matmul(pf[2][:, 0:ntok], lhsT=wOA[:, k, oc * 128:(oc + 1) * 128], rhs=o_aT[:, k, tk], start=(k == 0), stop=(k == 3))
                    for k in range(8):
                        r = e.matmul(pf[3][:, 0:ntok], lhsT=wOB[0:64, k, oc * 128:(oc + 1) * 128], rhs=o_bT[0:64, k, tk], start=(k == 0), stop=(k == 7))
                    return r
                op('tensor', mm4, reads=['wM', f'hTg{g % 2}', 'o_aT', 'o_bT'], writes=[PF[0], PF[1], PF[2], PF[3]])

                def sg(e, ntok=ntok):
                    e.activation(out=sga[:, 0:ntok], in_=pf[0][:, 0:ntok], func=AF.Sigmoid)
                    return e.activation(out=sgb[:, 0:ntok], in_=pf[1][:, 0:ntok], func=AF.Sigmoid)
                op('scalar', sg, reads=[PF[0], PF[1]], writes=['sg'])

                def zz(e, ntok=ntok):
                    e.tensor_tensor(out=sga[:, 0:ntok], in0=sga[:, 0:ntok], in1=pf[2][:, 0:ntok], op=ALU.mult)
                    return e.tensor_tensor(out=sgb[:, 0:ntok], in0=sgb[:, 0:ntok], in1=pf[3][:, 0:ntok], op=ALU.mult)
                op('vector', zz, reads=['sg', PF[2], PF[3]], writes=['sg2'])
                op('gpsimd', lambda e, z=z, oc=oc, ntok=ntok: e.tensor_tensor(out=z[:, oc, 0:ntok], in0=sga[:, 0:ntok], in1=sgb[:, 0:ntok], op=ALU.add),
                   reads=['sg2'], writes=[f'zT{g % 2}', 'sg'])
            for j in range(ntok // 128):
                t = 4 * g + j
                yb = pq[2]
                b = t % 2

                def mmy(e, z=z, j=j, yb=yb):
                    for n in range(2):
                        for k in range(8):
                            r = e.matmul(yb[:, n * 512:(n + 1) * 512], lhsT=z[:, k, j * 128:(j + 1) * 128], rhs=wO[:, k, n * 512:(n + 1) * 512], start=(k == 0), stop=(k == 7))
                    return r
                op('tensor', mmy, reads=['wM', f'zT{g % 2}'], writes=[PF[4], PF[5]])
                op('sync', lambda e, t=t: e.dma_start(out=xt4, in_=xc[t * 128:(t + 1) * 128, :]), writes=['xt'], dma='ldx0')
                op('scalar', lambda e, yb=yb, t=t: e.activation(out=tmpf, in_=yb, func=AF.Square, accum_out=sms[:, t, 0:1]), reads=[PF[4], PF[5]], writes=['tmpf', 'ssy'])
                rstd(sms[:, t, 1:2], sms[:, t, 0:1], D, ['ssy'], 'rsy')
                op('vector', lambda e, yb=yb, t=t: e.scalar_tensor_tensor(out=tmpf, in0=yb, scalar=sms[:, t, 1:2], in1=G1, op0=ALU.mult, op1=ALU.mult),
                   reads=[PF[4], PF[5], 'rsy', 'rows'], writes=['tmpf'])
                op('gpsimd', lambda e, b=b: e.tensor_tensor(out=x1t[b], in0=tmpf, in1=xt4, op=ALU.add), reads=['tmpf', 'xt'], writes=[f'x1t{b}'])
                op('sync', lambda e, t=t, b=b: e.dma_start(out=x1_scr[t * 128:(t + 1) * 128, :], in_=x1t[b]), reads=[f'x1t{b}'], dma=f'stx{b}')
                op('scalar', lambda e, t=t, b=b: e.activation(out=tmpf, in_=x1t[b], func=AF.Square, accum_out=sms[:, t, 2:3]), reads=[f'x1t{b}'], writes=['tmpf', 'ss2'])
                rstd(sms[:, t, 3:4], sms[:, t, 2:3], D, ['ss2'], 'rs2')
                op('vector', lambda e, t=t, b=b: e.scalar_tensor_tensor(out=tmpf, in0=x1t[b], scalar=sms[:, t, 3:4], in1=A2, op0=ALU.mult, op1=ALU.mult),
                   reads=[f'x1t{b}', 'rs2', 'rows'], writes=['tmpf'])
                op('gpsimd', lambda e, b=b: e.tensor_tensor(out=h2b[b], in0=tmpf, in1=B2, op=ALU.add), reads=['tmpf', 'rows'], writes=[f'h2b{b}'])
                op('sync', lambda e, t=t, b=b: e.dma_start(out=h2_scr[t * 128:(t + 1) * 128, :], in_=h2b[b]), reads=[f'h2b{b}'], dma=f'sth{b}')

                def trh(e, b=b):
                    for k in range(8):
                        r = e.transpose(pbf[b][:, k * 128:(k + 1) * 128], h2b[b][:, k * 128:(k + 1) * 128], ident)
                    return r
                op('tensor', trh, reads=[f'h2b{b}', 'const'], writes=[PB[b]])
                op('scalar', lambda e, b=b: e.activation(out=h2T, in_=pbf[b].rearrange("p (k c) -> p k c", k=8), func=AF.Copy), reads=[PB[b]], writes=['h2T'])

                def mml(e):
                    for k in range(8):
                        e.matmul(pf[0][:, 0:NE], lhsT=h2T[:, k, :], rhs=wR[:, k, :], start=(k == 0), stop=False)
                    return e.matmul(pf[0][:, 0:NE], lhsT=ones_bf[0:1, :], rhs=bR[0:1, :], start=False, stop=True)
                op('tensor', mml, reads=['h2T', 'wM', 'const'], writes=[PF[0]])

                chain('vector', [
                    lambda e, t=t: e.tensor_copy(out=lg[:, t, :], in_=pf[0][:, 0:NE]),
                    lambda e, t=t: e.max(out=mx8[:, t, :], in_=lg[:, t, :]),
                    lambda e, t=t: e.tensor_scalar(out=mask, in0=lg[:, t, :], scalar1=mx8[:, t, 3:4], scalar2=None, op0=ALU.is_ge),
                    lambda e: e.tensor_copy(out=maskb, in_=mask),
                    lambda e, t=t: e.tensor_scalar(out=sms[:, t, 4:5], in0=mx8[:, t, 0:1], scalar1=-1.0, scalar2=None, op0=ALU.mult)],
                    reads=[PF[0]], writes=['lg', 'maskb', 'negmx'])
                op('scalar', lambda e, t=t: e.activation(out=e4, in_=mx8[:, t, 0:4], func=AF.Exp, bias=sms[:, t, 4:5], scale=1.0, accum_out=sms[:, t, 5:6]),
                   reads=['lg', 'negmx'], writes=['e4'])

                def mmc(e):
                    e.matmul(pf[1][:, 0:NE], lhsT=utri, rhs=maskb, start=True, stop=True)
                    return e.matmul(pf[1][:, NE:2 * NE], lhsT=ones_bf, rhs=maskb, start=True, stop=True)
                op('tensor', mmc, reads=['maskb', 'utri', 'const'], writes=[PF[1]])

                chain('vector', [
                    lambda e, t=t: e.reciprocal(out=sms[:, t, 6:7], in_=sms[:, t, 5:6]),
                    lambda e, t=t: e.tensor_scalar(out=wts[:, t, :], in0=e4, scalar1=sms[:, t, 6:7], scalar2=None, op0=ALU.mult),
                    lambda e, t=t: e.tensor_tensor(out=posA[:, t, :], in0=pf[1][:, 0:NE], in1=cntp, op=ALU.add),
                    lambda e: e.tensor_tensor(out=cntp, in0=cntp, in1=pf[1][:, NE:2 * NE], op=ALU.add)],
                    reads=['e4', PF[1]], writes=['route'])
        dump('lg', lg, [128, NTR, NE], F32); dump('posA', posA, [128, NTR, NE], F32); dump('cntp', cntp, [128, NE], F32)
        S.barrier()
        if STAGE <= 4:
            return finish(nc, S, out, dbg_outs)

        A.reset()
        ci = A.alloc([NE], I32); padf = A.alloc([NE], F32); padT = A.alloc([128], F32); ltri = A.alloc([NE], F32)
        basef = A.alloc([NE], F32); pend = A.alloc([NE], F32)
        thr = A.alloc([NBLK], F32); EB = A.alloc([NBLK], F32); skp = A.alloc([NBLK], F32)
        idxw_f = A.alloc([NBLK], F32); idxb_f = A.alloc([NBLK], F32); pidx = A.alloc([1], F32)
        idxw = A.alloc([NBLK], I32); idxb = A.alloc([NBLK], I32)
        idxw8_f = A.alloc([8, NBLK], F32); idxw8 = A.alloc([8, NBLK], I32)
        idxd_f = A.alloc([4, NBLK], F32); idxd = A.alloc([4, NBLK], I32); idxd0 = A.alloc([NBLK], F32); pidx4 = A.alloc([1], F32)
        slot2 = A.alloc([NE], F32); slotf = A.alloc([NTR * 4], F32); tmp32 = A.alloc([NE], F32)
        h2l = [A.alloc([D], BF16) for _ in range(2)]

        chain('vector', [
            lambda e: e.tensor_scalar(out=padf, in0=cntp, scalar1=127.0, scalar2=None, op0=ALU.add),
            lambda e: e.tensor_copy(out=ci, in_=padf),
            lambda e: e.tensor_single_scalar(out=ci, in_=ci, scalar=7, op=ALU.arith_shift_right),
            lambda e: e.tensor_single_scalar(out=ci, in_=ci, scalar=7, op=ALU.logical_shift_left),
            lambda e: e.tensor_copy(out=padf, in_=ci)], reads=['route'], writes=['padf'])

        chain('gpsimd', [
            lambda e: e.memset(ltri, 1.0),
            lambda e: e.affine_select(out=ltri, in_=ltri, pattern=[[1, NE]], compare_op=ALU.is_gt, fill=0.0, base=0, channel_multiplier=-1),
            lambda e: e.iota(thr, pattern=[[128, NBLK]], base=0, channel_multiplier=0, allow_small_or_imprecise_dtypes=True),
            lambda e: e.iota(pidx, pattern=[[0, 1]], base=0, channel_multiplier=1, allow_small_or_imprecise_dtypes=True)], writes=['ltri'])
        op('tensor', lambda e: e.transpose(pq[0][0:NE, 0:128], padf, identf), reads=['padf', 'const'], writes=[PF[0]])
        op('vector', lambda e: e.tensor_copy(out=padT[0:NE, :], in_=pq[0][0:NE, 0:128]), reads=[PF[0]], writes=['padT'])
        op('tensor', lambda e: e.matmul(pf[1][:, 0:NE], lhsT=padT[0:NE, :], rhs=ltri[0:NE, :], start=True, stop=True), reads=['padT', 'ltri'], writes=[PF[1]])

        lay2 = [
            lambda e: e.tensor_copy(out=basef, in_=pf[1][:, 0:NE]),
            lambda e: e.tensor_tensor(out=pend, in0=basef, in1=padf, op=ALU.add),
            lambda e: e.memset(EB, 0.0)]
        for ex in range(NE):
            lay2.append(lambda e, ex=ex: e.scalar_tensor_tensor(out=EB, in0=thr, scalar=pend[:, ex:ex + 1], in1=EB, op0=ALU.is_ge, op1=ALU.add))
        lay2 += [
            lambda e: e.tensor_scalar(out=EB, in0=EB, scalar1=float(NE - 1), scalar2=None, op0=ALU.min),
            lambda e: e.memset(skp, 0.0),
            lambda e: e.tensor_tensor(out=skp[:, 1:NBLK], in0=EB[:, 1:NBLK], in1=EB[:, 0:NBLK - 1], op=ALU.is_equal),
            lambda e: e.tensor_scalar(out=skp, in0=skp, scalar1=BIG, scalar2=None, op0=ALU.mult),
            lambda e: e.scalar_tensor_tensor(out=idxw_f, in0=EB, scalar=1024.0, in1=skp, op0=ALU.mult, op1=ALU.add),
            lambda e: e.tensor_scalar(out=idxw_f, in0=idxw_f, scalar1=pidx[:, 0:1], scalar2=None, op0=ALU.add),
            lambda e: e.tensor_tensor(out=idxb_f, in0=EB, in1=skp, op=ALU.add),
            lambda e: e.tensor_copy(out=idxw, in_=idxw_f)]
        for k8 in range(8):
            lay2.append(lambda e, k8=k8: e.tensor_scalar(out=idxw8_f[:, k8, :], in0=idxw_f, scalar1=128.0 * k8, scalar2=None, op0=ALU.add))
        lay2 += [lambda e: e.tensor_copy(out=idxw8, in_=idxw8_f), lambda e: e.tensor_copy(out=idxb, in_=idxb_f)]
        lay2 += [lambda e: e.tensor_scalar(out=pidx4, in0=pidx, scalar1=4.0, scalar2=None, op0=ALU.mult),
                 lambda e: e.scalar_tensor_tensor(out=idxd0, in0=EB, scalar=512.0, in1=skp, op0=ALU.mult, op1=ALU.add),
                 lambda e: e.tensor_scalar(out=idxd0, in0=idxd0, scalar1=pidx4[:, 0:1], scalar2=None, op0=ALU.add)]
        for j4 in range(4):
            lay2.append(lambda e, j4=j4: e.tensor_scalar(out=idxd_f[:, j4, :], in0=idxd0, scalar1=float(j4), scalar2=None, op0=ALU.add))
        lay2 += [lambda e: e.tensor_copy(out=idxd, in_=idxd_f)]
        chain('vector', lay2, reads=[PF[1], 'padf', 'ltri'], writes=['lay'])
        for t in range(NTR):
            b = t % 2
            op('sync', lambda e, t=t, b=b: e.dma_start(out=h2l[b], in_=h2_scr[t * 128:(t + 1) * 128, :]), reads=['h2_scr'], writes=[f'h2l{b}'], dma=f'ldx{b}')

            slf = [lambda e, t=t: e.tensor_tensor(out=slot2, in0=posA[:, t, :], in1=basef, op=ALU.add)]
            for k in range(4):
                slf.append(lambda e, t=t, k=k: e.scalar_tensor_tensor(out=tmp32, in0=lg[:, t, :], scalar=mx8[:, t, k:k + 1], in1=slot2, op0=ALU.is_equal, op1=ALU.mult,
                                                                      accum_out=slotf[:, 4 * t + k:4 * t + k + 1]))
            slf.append(lambda e, t=t: e.tensor_copy(out=sloti[:, 4 * t:4 * t + 4], in_=slotf[:, 4 * t:4 * t + 4]))
            chain('vector', slf, reads=['lay'], writes=[f'sloti{t}', 'slot2'])
            for k in range(4):
                op('gpsimd', lambda e, t=t, k=k, b=b: e.indirect_dma_start(out=xs_scr, out_offset=bass.IndirectOffsetOnAxis(ap=sloti[:, 4 * t + k:4 * t + k + 1], axis=0),
                                                                       in_=h2l[b], in_offset=None, bounds_check=breg(e, NSLOT - 1), oob_is_err=False),
                   reads=[f'sloti{t}', f'h2l{b}'], dma=f'sc{b}')
        dump('sloti', sloti, [128, NTR * 4], I32); dump('idxw', idxw, [128, NBLK], I32); dump('wts', wts, [128, NTR, 4], F32)
        S.barrier()
        if STAGE <= 5:
            return finish(nc, S, out, dbg_outs)

        mark_ex = A.off
        wgu = A.alloc([8, 2 * D], BF16); wdn4 = A.alloc([4, 2 * D], BF16)
        wdn_k = lambda k: wdn4[:, k // 2, (k % 2) * D:(k % 2 + 1) * D]
        wdn_pairs = w_dn.rearrange("e (q two) n -> (e q) (two n)", two=2)
        bgu = A.alloc([2 * D], BF16); bdn = A.alloc([D], BF16)
        xe = [A.alloc([D], BF16) for _ in range(2)]; xT = [A.alloc([8, 128], BF16) for _ in range(2)]
        gs = A.alloc([D], F32); sg_ = A.alloc([D], F32); l1 = A.alloc([D], F32); tt = A.alloc([D], F32)
        actb = A.alloc([D], BF16); aT = A.alloc([8, 128], BF16)
        yo = [A.alloc([D], F32) for _ in range(2)]
        wgu_flat = w_gu.rearrange("e k n -> (e k) n"); wdn_flat = w_dn.rearrange("e k n -> (e k) n")
        wgu_v = bass.AP(tensor=w_gu.tensor, offset=0, ap=[[2 * D, NE * D - 896], [128 * 2 * D, 8], [1, 2 * D]])
        wdn_v = bass.AP(tensor=w_dn.tensor, offset=0, ap=[[D, NE * D - 896], [128 * D, 8], [1, D]])
        for blk in range(NBLK):
            b = blk % 2
            iw = bass.IndirectOffsetOnAxis(ap=idxw[:, blk:blk + 1], axis=0)
            ib = bass.IndirectOffsetOnAxis(ap=idxb[:, blk:blk + 1], axis=0)
            for k8 in range(8):
                op('gpsimd', lambda e, blk=blk, k8=k8: e.indirect_dma_start(out=wgu[:, k8, :], out_offset=None, in_=wgu_flat,
                                                                            in_offset=bass.IndirectOffsetOnAxis(ap=idxw8[:, k8, blk:blk + 1], axis=0),
                                                                            bounds_check=breg(e, NE * D - 1), oob_is_err=False),
                   reads=['lay'], writes=[f'wgu{k8}'], dma='ld_wgu')
            op('gpsimd', lambda e, ib=ib: e.indirect_dma_start(out=bgu, out_offset=None, in_=b_gu, in_offset=ib, bounds_check=breg(e, NE - 1), oob_is_err=False),
               reads=['lay'], writes=['bgu'], dma='ld_wgu')
            for j4 in range(4):
                op('gpsimd', lambda e, blk=blk, j4=j4: e.indirect_dma_start(out=wdn4[:, j4, :], out_offset=None, in_=wdn_pairs,
                                                                            in_offset=bass.IndirectOffsetOnAxis(ap=idxd[:, j4, blk:blk + 1], axis=0),
                                                                            bounds_check=breg(e, NE * 512 - 1), oob_is_err=False),
                   reads=['lay'], writes=[f'wdn{j4}'], dma='ld_wdn')
            op('gpsimd', lambda e, ib=ib: e.indirect_dma_start(out=bdn, out_offset=None, in_=b_dn, in_offset=ib, bounds_check=breg(e, NE - 1), oob_is_err=False),
               reads=['lay'], writes=['bdn'], dma='ld_wdn')
            op('sync', lambda e, blk=blk, b=b: e.dma_start(out=xe[b], in_=xs_scr[blk * 128:(blk + 1) * 128, :]), reads=['xs_scr'], writes=[f'xe{b}'], dma=f'ldx{b}')

            def trx(e, b=b):
                for k in range(8):
                    r = e.transpose(pbf[0][:, k * 128:(k + 1) * 128], xe[b][:, k * 128:(k + 1) * 128], ident)
                return r
            op('tensor', trx, reads=[f'xe{b}', 'const'], writes=[PB[0]])
            op('scalar', lambda e, b=b: e.activation(out=xT[b], in_=pbf[0].rearrange("p (k c) -> p k c", k=8), func=AF.Copy), reads=[PB[0]], writes=[f'xT{b}'])

            def mgu(e, b=b):
                for k in range(8):
                    for n in range(4):
                        e.matmul(pf[n], lhsT=xT[b][:, k, :], rhs=wgu[:, k, n * 512:(n + 1) * 512], start=(k == 0), stop=False)
                for n in range(4):
                    r = e.matmul(pf[n], lhsT=ones_bf[0:1, :], rhs=bgu[0:1, n * 512:(n + 1) * 512], start=False, stop=True)
                return r
            op('tensor', mgu, reads=[f'xT{b}', 'bgu', 'const'] + [f'wgu{k8}' for k8 in range(8)], writes=[PF[0], PF[1], PF[2], PF[3]])
            op('vector', lambda e: e.tensor_scalar(out=gs, in0=pq[0], scalar1=7.0, scalar2=None, op0=ALU.min), reads=[PF[0], PF[1]], writes=['gs'])
            op('scalar', lambda e: e.activation(out=sg_, in_=gs, func=AF.Sigmoid, scale=1.702), reads=['gs'], writes=['sg_'])
            op('vector', lambda e: e.tensor_scalar(out=l1, in0=pq[1], scalar1=7.0, scalar2=-7.0, op0=ALU.min, op1=ALU.max), reads=[PF[2], PF[3]], writes=['l1'])
            op('vector', lambda e: e.tensor_tensor(out=tt, in0=gs, in1=sg_, op=ALU.mult), reads=['gs', 'sg_'], writes=['tt'])
            op('vector', lambda e: e.scalar_tensor_tensor(out=actb, in0=l1, scalar=1.0, in1=tt, op0=ALU.add, op1=ALU.mult), reads=['l1', 'tt'], writes=['actb'])

            def tra(e):
                for k in range(8):
                    r = e.transpose(pbf[1][:, k * 128:(k + 1) * 128], actb.rearrange("t (p k) -> t k p", k=8)[:, k, :], ident)
                return r
            op('tensor', tra, reads=['actb', 'const'], writes=[PB[1]])
            op('scalar', lambda e: e.activation(out=aT, in_=pbf[1].rearrange("p (k c) -> p k c", k=8), func=AF.Copy), reads=[PB[1]], writes=['aT'])

            def mdn(e):
                for k in range(8):
                    for n in range(2):
                        e.matmul(pf[4 + n], lhsT=aT[:, k, :], rhs=wdn_k(k)[:, n * 512:(n + 1) * 512], start=(k == 0), stop=False)
                for n in range(2):
                    r = e.matmul(pf[4 + n], lhsT=ones_bf[0:1, :], rhs=bdn[0:1, n * 512:(n + 1) * 512], start=False, stop=True)
                return r
            op('tensor', mdn, reads=['aT', 'bdn', 'const'] + [f'wdn{j4}' for j4 in range(4)], writes=[PF[4], PF[5]])
            op('scalar', lambda e, b=b: e.activation(out=yo[b], in_=pq[2], func=AF.Copy), reads=[PF[4], PF[5]], writes=[f'yo{b}'])
            op('sync', lambda e, blk=blk, b=b: e.dma_start(out=y_scr[blk * 128:(blk + 1) * 128, :], in_=yo[b]), reads=[f'yo{b}'], dma=f'sty{b}')
        S.barrier()

        A.reset(mark_ex)
        gk = [[A.alloc([D], F32) for _ in range(4)] for _ in range(2)]
        acc = A.alloc([D], F32); x1l = [A.alloc([D], F32) for _ in range(2)]; ot = [A.alloc([D], F32) for _ in range(2)]
        jk = A.alloc([D], F32)
        fs = A.alloc([NTR, 2], F32)
        for t in range(NTR):
            b = t % 2
            for k in range(4):
                op('gpsimd', lambda e, t=t, k=k, b=b: e.indirect_dma_start(out=gk[b][k], out_offset=None, in_=y_scr,
                                                                       in_offset=bass.IndirectOffsetOnAxis(ap=sloti[:, 4 * t + k:4 * t + k + 1], axis=0),
                                                                       bounds_check=breg(e, NSLOT - 1), oob_is_err=False),
                   reads=['y_scr'], writes=[f'gk{b}{k}'], dma=f'ga{b}')
            op('sync', lambda e, t=t, b=b: e.dma_start(out=x1l[b], in_=x1_scr[t * 128:(t + 1) * 128, :]), reads=['x1_scr'], writes=[f'x1l{b}'], dma=f'ldx{b}')

            cmb = [lambda e, t=t, b=b: e.tensor_scalar(out=acc, in0=gk[b][0], scalar1=wts[:, t, 0:1], scalar2=None, op0=ALU.mult)]
            for k in range(1, 4):
                cmb.append(lambda e, t=t, b=b, k=k: e.scalar_tensor_tensor(out=acc, in0=gk[b][k], scalar=wts[:, t, k:k + 1], in1=acc, op0=ALU.mult, op1=ALU.add))
            chain('vector', cmb, reads=[f'gk{b}{k}' for k in range(4)], writes=['acc'])
            op('scalar', lambda e, t=t: e.activation(out=jk, in_=acc, func=AF.Square, accum_out=fs[:, t, 0:1]), reads=['acc'], writes=['jk', 'fss'])
            rstd(fs[:, t, 1:2], fs[:, t, 0:1], D, ['fss'], 'fsr')
            op('vector', lambda e, t=t: e.scalar_tensor_tensor(out=acc, in0=acc, scalar=fs[:, t, 1:2], in1=G2, op0=ALU.mult, op1=ALU.mult), reads=['acc', 'fsr'], writes=['acc'])
            op('gpsimd', lambda e, b=b: e.tensor_tensor(out=ot[b], in0=acc, in1=x1l[b], op=ALU.add), reads=['acc', f'x1l{b}'], writes=[f'ot{b}'])
            op('sync', lambda e, t=t, b=b: e.dma_start(out=out[t * 128:(t + 1) * 128, :], in_=ot[b]), reads=[f'ot{b}'], dma=f'sto{b}')
        return finish(nc, S, out, dbg_outs)


def finish(nc, S, out, dbg_outs):
    S.barrier()
    S.emit()
    return nc, dbg_outs


_CACHE = {}


def _host_tables():
    if 'rope' in _CACHE:
        return _CACHE['rope'], _CACHE['tblidx']
    half = 32; nf = 16
    freqs = (10000.0 ** (-np.arange(nf, dtype=np.float32) / nf)).astype(np.float32)
    rope = {}
    for hf in range(2):
        rng_rows = np.arange(28 * hf, 28 * hf + 36)
        rest = np.arange(36, 64) if hf == 0 else np.arange(0, 28)
        rows = np.concatenate([rng_rows, rest])
        tok = (rows[:, None] * 64 + np.arange(64)[None, :]).reshape(-1)
        r = (tok // 64).astype(np.float32); c = (tok % 64).astype(np.float32)
        cosT = np.ones((4352, 64), np.float32); sinT = np.zeros((4352, 64), np.float32)
        for hi, pos in enumerate((r, c)):
            ang = pos[:, None] * freqs[None, :]
            co = np.cos(ang).astype(np.float32); si = np.sin(ang).astype(np.float32)
            cosT[:4096, hi * 32:hi * 32 + 16] = co; cosT[:4096, hi * 32 + 16:hi * 32 + 32] = co
            sinT[:4096, hi * 32:hi * 32 + 16] = -si; sinT[:4096, hi * 32 + 16:hi * 32 + 32] = si
        rope[hf] = (np.ascontiguousarray(np.tile(cosT, (1, 8))), np.ascontiguousarray(np.tile(sinT, (1, 8))), tok)
    qc = np.arange(64)[:, None]; kc = np.arange(64)[None, :]
    c0 = np.clip(qc - 8, 0, 48)
    valid = (kc >= c0) & (kc < c0 + 16)
    off = np.clip(kc - qc + 15, 0, 30)
    _CACHE['rope'] = rope; _CACHE['tblidx'] = (valid, off)
    return rope, (valid, off)


def kernel(x, c, ctx, c_ctx, w_mod, b_mod, g_pre_mix, g_post_mix, g_pre_ffn, g_post_ffn, w_in, rpb, g_qnorm, g_knorm,
           w_out_a, w_out_b, w_o, w_router, b_router, w_gu, b_gu, w_dn, b_dn):
    f = lambda a: np.ascontiguousarray(np.asarray(a, dtype=np.float32))
    x = f(x); ctx = f(ctx); c = f(c); c_ctx = f(c_ctx)
    rope, (valid, off) = _host_tables()
    rp = f(rpb)[0]
    T = rp[:, :, off]
    T = np.where(valid[None, None], T, np.float32(NEG)).astype(np.float32)
    T = T.transpose(0, 2, 1, 3).reshape(4, 2 * 64, 15 * 64)
    w_in0 = f(w_in)[0]
    qb = w_in0[:, 1792:2304].reshape(1024, 2, 4, 64).transpose(0, 2, 1, 3).reshape(1024, 512)
    w_in_p = w_in0.copy(); w_in_p[:, 1792:2304] = qb
    shared = dict(w_mod=f(w_mod)[0], b_mod=f(b_mod)[0], gvec=np.stack([f(g_pre_mix)[0], f(g_post_mix)[0], f(g_pre_ffn)[0], f(g_post_ffn)[0]]),
                  w_in=w_in_p, tbl=np.ascontiguousarray(T), gqk=np.stack([f(g_qnorm)[0], f(g_knorm)[0]]),
                  w_oa=f(w_out_a)[0], w_ob=f(w_out_b)[0], w_o=f(w_o)[0], w_r=f(w_router)[0], b_r=f(b_router)[0],
                  w_gu=f(w_gu)[0], b_gu=f(b_gu)[0], w_dn=f(w_dn)[0], b_dn=f(b_dn)[0])
    in_maps = []
    for core in range(8):
        b, hf = core // 2, core % 2
        cosT, sinT, tok = rope[hf]
        xcore = np.concatenate([x[b][tok], ctx[b]], axis=0)
        m = dict(shared)
        m.update(xc=np.ascontiguousarray(xcore), cvec=np.stack([c[b], c_ctx]), ropec=cosT, ropes=sinT)
        in_maps.append(m)
    key = ('nc', STAGE, tuple(DEBUG))
    if key not in _CACHE:
        _CACHE[key] = build()
    nc, dbg = _CACHE[key]
    res = run_bass_kernel_spmd(nc, in_maps, core_ids=list(range(8)))
    _CACHE['last'] = res
    outp = np.empty((4, 4096, 1024), np.float32)
    for core in range(8):
        b, hf = core // 2, core % 2
        o = res.results[core]["out"]
        if hf == 0:
            outp[b, 0:2048] = o[0:2048]
        else:
            outp[b, 2048:4096] = o[256:2304]
    return outp
```

```python
import numpy as np
from contextlib import ExitStack
import concourse.bass as bass
import concourse.mybir as mybir
from concourse.bass_utils import run_bass_kernel_spmd

F32 = mybir.dt.float32; BF16 = mybir.dt.bfloat16; I32 = mybir.dt.int32; U8 = mybir.dt.uint8
AF = mybir.ActivationFunctionType; ALU = mybir.AluOpType; AX = mybir.AxisListType
ENG = ('tensor', 'vector', 'scalar', 'gpsimd', 'sync')
DSZ = {F32: 4, BF16: 2, I32: 4, U8: 1}

D = 1024; NTR = 18; TOKR = 2304; NTALL = 34; NKEY = 4352; NE = 32
NBLK = 104; NSLOT = NBLK * 128
EPS = 1e-6; NEG = -30000.0; BIG = 1.0e6
STAGE = 99
SAME_ENG_SYNC = True
DEBUG = []


class Sched:
    def __init__(self, nc, stack):
        self.nc = nc; self.stack = stack
        self.ops = {e: [] for e in ENG}
        self.sems = {}; self.cnt = {}
        self.last_write = {}; self.readers = {}
        self.waited = {e: {} for e in ENG}

    def sem(self, name):
        if name not in self.sems:
            self.sems[name] = self.stack.enter_context(self.nc.semaphore(name)); self.cnt[name] = 0
        return self.sems[name]

    def op(self, eng, fn, reads=(), writes=(), dma=None):
        waits = {}
        isdma_op = dma is not None

        def need(tok):
            if tok is None:
                return
            sname, val, teng, isdma = tok
            if teng == eng and not isdma and not isdma_op and (eng == 'tensor' or not SAME_ENG_SYNC):
                return
            if self.waited[eng].get(sname, 0) >= val:
                return
            waits[sname] = max(waits.get(sname, 0), val)
        for b in reads:
            need(self.last_write.get(b))
        for b in writes:
            need(self.last_write.get(b))
            for r in self.readers.get(b, ()):
                need(r)
        for s, v in waits.items():
            self.waited[eng][s] = v
        if isdma_op:
            sname = dma; inc = 16
        else:
            sname = 'e_' + eng; inc = 1
        self.sem(sname); self.cnt[sname] += inc
        tok = (sname, self.cnt[sname], eng, isdma_op)
        for b in writes:
            self.last_write[b] = tok; self.readers[b] = []
        for b in reads:
            self.readers.setdefault(b, []).append(tok)
        self.ops[eng].append((list(waits.items()), fn, sname, inc))
        return tok

    def barrier(self):
        for e in ENG:
            waits = []
            for s, c in self.cnt.items():
                if c > 0 and self.waited[e].get(s, 0) < c and s != 'e_' + e:
                    waits.append((s, c)); self.waited[e][s] = c
            if waits:
                self.ops[e].append((waits, None, None, None))
        self.last_write = {}; self.readers = {}

    def emit(self):
        with self.nc.Block() as block:
            for eng in ENG:
                ops = self.ops[eng]
                if not ops:
                    continue

                def body(e, ops=ops):
                    for waits, fn, sname, inc in ops:
                        for s, v in waits:
                            e.wait_ge(self.sems[s], v)
                        if fn is not None:
                            fn(e).then_inc(self.sems[sname], inc)
                getattr(block, eng)(body)


class Arena:
    def __init__(self, nc, st, name, nbytes):
        self.t = st.enter_context(nc.sbuf_tensor(name, [128, nbytes], U8)); self.off = 0; self.n = nbytes; self.name = name

    def alloc(self, free_shape, dt):
        n = int(np.prod(free_shape)) * DSZ[dt]
        n_al = (n + 63) // 64 * 64
        assert self.off + n_al <= self.n, (self.name, self.off, n_al, self.n)
        ap = self.t[:, self.off:self.off + n].bitcast(dt)
        self.off += n_al
        if len(free_shape) == 2:
            ap = ap.rearrange("p (a b) -> p a b", a=free_shape[0])
        elif len(free_shape) == 3:
            ap = ap.rearrange("p (a b c) -> p a b c", a=free_shape[0], b=free_shape[1])
        return ap

    def reset(self, off=0):
        self.off = off


def build():
    nc = bass.Bass("TRN2", target_bir_lowering=False)
    dt_in = lambda name, shape, dt=F32: nc.dram_tensor(name, shape, dt, kind="ExternalInput").ap()
    xc = dt_in("xc", [NKEY, D]); cvec = dt_in("cvec", [2, D]); w_mod = dt_in("w_mod", [D, 6 * D]); b_mod = dt_in("b_mod", [6 * D])
    gvec = dt_in("gvec", [4, D]); w_in = dt_in("w_in", [D, 4352]); tbl = dt_in("tbl", [4, 128, 960]); gqk = dt_in("gqk", [2, 64])
    ropec = dt_in("ropec", [NKEY, 512]); ropes = dt_in("ropes", [NKEY, 512])
    w_oa = dt_in("w_oa", [512, D]); w_ob = dt_in("w_ob", [512, D]); w_o = dt_in("w_o", [D, D])
    w_r = dt_in("w_r", [D, NE]); b_r = dt_in("b_r", [NE])
    w_gu = dt_in("w_gu", [NE, D, 2 * D]); b_gu = dt_in("b_gu", [NE, 2 * D]); w_dn = dt_in("w_dn", [NE, D, D]); b_dn = dt_in("b_dn", [NE, D])
    out = nc.dram_tensor("out", [TOKR, D], F32, kind="ExternalOutput").ap()
    hT_scr = nc.dram_tensor("hT_scr", [128, 8, NKEY + 128], BF16, kind="Internal").ap()
    x1_scr = nc.dram_tensor("x1_scr", [TOKR, D], F32, kind="Internal").ap()
    h2_scr = nc.dram_tensor("h2_scr", [TOKR, D], BF16, kind="Internal").ap()
    xs_scr = nc.dram_tensor("xs_scr", [NSLOT, D], BF16, kind="Internal").ap()
    y_scr = nc.dram_tensor("y_scr", [NSLOT, D], F32, kind="Internal").ap()
    dbg_outs = {}
    REG = {}

    def breg(e, v):
        if v not in REG:
            REG[v] = e.to_reg(v)
        return REG[v]

    with ExitStack() as st:
        S = Sched(nc, st)
        op = S.op

        def chain(eng, fns, reads=(), writes=()):
            for fn_ in fns:
                op(eng, fn_, reads=list(reads), writes=list(writes))
        A = Arena(nc, st, "arena", 182 * 1024)
        P = Arena(nc, st, "persist", 24 * 1024)
        pq = [st.enter_context(nc.psum_tensor(f"pq{i}", [128, 1024], F32)) for i in range(3)]
        pbf = [st.enter_context(nc.psum_tensor(f"pbf{i}", [128, 1024], BF16)) for i in range(2)]
        pq = [t_[:, :] for t_ in pq]; pbf = [t_[:, :] for t_ in pbf]
        pf = [pq[i // 2][:, (i % 2) * 512:(i % 2) * 512 + 512] for i in range(6)]
        PF = [f"pf{i}" for i in range(6)]; PB = ["pb0", "pb1"]

        def dump(name, ap, shape, dt):
            if name not in DEBUG:
                return
            S.barrier()
            o = nc.dram_tensor("dbg_" + name, shape, dt, kind="ExternalOutput").ap()
            dbg_outs[name] = o
            op('sync', lambda e: e.dma_start(out=o, in_=ap), dma='dbg')

        rows_late = P.alloc([4, D], F32)
        ident = P.alloc([128], BF16)
        identf = P.alloc([128], F32)
        ones_bf = P.alloc([128], BF16)
        ones_f = P.alloc([128], F32)
        G1, A2, B2, G2 = rows_late[:, 0, :], rows_late[:, 1, :], rows_late[:, 2, :], rows_late[:, 3, :]

        chain('gpsimd', [
            lambda e: e.memset(identf, 0.0),
            lambda e: e.affine_select(out=identf, in_=identf, pattern=[[-1, 128]], compare_op=ALU.not_equal, fill=1.0, base=0, channel_multiplier=1),
            lambda e: e.memset(ones_f, 1.0),
            lambda e: e.tensor_copy(out=ones_bf, in_=ones_f),
            lambda e: e.tensor_copy(out=ident, in_=identf)], writes=['const'])

        A.reset()
        rows_early = A.alloc([4, D], F32)
        A1, B1, A1c, B1c = rows_early[:, 0, :], rows_early[:, 1, :], rows_early[:, 2, :], rows_early[:, 3, :]
        mark_p1 = A.off
        modB = A.alloc([6 * D], F32); modC = A.alloc([2 * D], F32)
        gB = A.alloc([4, D], F32)
        bmB = A.alloc([6 * D], F32)
        cT = A.alloc([2, 8], F32); sT = A.alloc([2, 8], F32)
        rep = A.alloc([2, 8, 128], BF16)
        wm = [A.alloc([8, 512], BF16) for _ in range(2)]
        op('sync', lambda e: e.dma_start(out=cT, in_=cvec.rearrange("j (k p) -> p j k", p=128), allow_slow_non_contiguous=True), writes=['cT'], dma='d_cT')
        for i in range(4):
            op('sync', lambda e, i=i: e.dma_start(out=gB[:, i, :], in_=gvec[i, :].partition_broadcast(128)), writes=['gB'], dma='d_gB')
        op('sync', lambda e: e.dma_start(out=bmB, in_=b_mod.partition_broadcast(128)), writes=['bmB'], dma='d_bmB')
        op('scalar', lambda e: e.activation(out=sT, in_=cT, func=AF.Silu), reads=['cT'], writes=['sT'])

        def mk_rep(e):
            for j in range(2):
                for k in range(8):
                    r = e.tensor_scalar(out=rep[:, j, k, :], in0=ones_f, scalar1=sT[:, j, k:k + 1], scalar2=None, op0=ALU.mult)
            return r
        op('vector', mk_rep, reads=['sT', 'const'], writes=['rep'])
        for n in range(12):
            wb = wm[n % 2]
            op('gpsimd', lambda e, n=n, wb=wb: e.dma_start(out=wb, in_=w_mod[:, n * 512:(n + 1) * 512].rearrange("(k p) c -> p k c", p=128)),
               writes=[f'wm{n % 2}'], dma=f'ld_wm{n % 2}')
            for j in range(2 if n < 4 else 1):
                bk = (2 * n + j) % 6

                def mm(e, j=j, wb=wb, bk=bk):
                    for k in range(8):
                        r = e.matmul(pf[bk], lhsT=rep[:, j, k, :], rhs=wb[:, k, :], start=(k == 0), stop=(k == 7))
                    return r
                op('tensor', mm, reads=['rep', f'wm{n % 2}'], writes=[PF[bk]])
                dst = (modB if j == 0 else modC)[:, n * 512:(n + 1) * 512]
                op('vector', lambda e, dst=dst, bk=bk, n=n: e.tensor_tensor(out=dst, in0=pf[bk], in1=bmB[:, n * 512:(n + 1) * 512], op=ALU.add),
                   reads=[PF[bk], 'bmB'], writes=['modB'])

        def mk_rows(e):
            e.scalar_tensor_tensor(out=A1, in0=modB[:, D:2 * D], scalar=1.0, in1=gB[:, 0, :], op0=ALU.add, op1=ALU.mult)
            e.tensor_copy(out=B1, in_=modB[:, 0:D])
            e.scalar_tensor_tensor(out=A1c, in0=modC[:, D:2 * D], scalar=1.0, in1=gB[:, 0, :], op0=ALU.add, op1=ALU.mult)
            e.tensor_copy(out=B1c, in_=modC[:, 0:D])
            e.tensor_tensor(out=G1, in0=modB[:, 2 * D:3 * D], in1=gB[:, 1, :], op=ALU.mult)
            e.scalar_tensor_tensor(out=A2, in0=modB[:, 4 * D:5 * D], scalar=1.0, in1=gB[:, 2, :], op0=ALU.add, op1=ALU.mult)
            e.tensor_copy(out=B2, in_=modB[:, 3 * D:4 * D])
            return e.tensor_tensor(out=G2, in0=modB[:, 5 * D:6 * D], in1=gB[:, 3, :], op=ALU.mult)
        op('vector', mk_rows, reads=['modB', 'gB'], writes=['rows'])
        dump('rows_early', rows_early, [128, 4, D], F32)
        S.barrier()

        A.reset(mark_p1)
        xt = [A.alloc([D], F32) for _ in range(2)]
        hn = [A.alloc([D], F32) for _ in range(2)]
        hb = [A.alloc([D], BF16) for _ in range(2)]
        hTt = [A.alloc([8, 128], BF16) for _ in range(2)]
        junk = A.alloc([D], F32)
        ss = A.alloc([NTALL], F32); rs = A.alloc([NTALL], F32)

        def rstd(dst, src, n, reads, key):
            op('vector', lambda e: e.tensor_scalar(out=dst, in0=src, scalar1=1.0 / n, scalar2=EPS, op0=ALU.mult, op1=ALU.add), reads=reads, writes=[key])
            op('scalar', lambda e: e.activation(out=dst, in_=dst, func=AF.Sqrt), reads=[key], writes=[key])
            op('vector', lambda e: e.reciprocal(out=dst, in_=dst), reads=[key], writes=[key])

        for t in range(NTALL):
            b = t % 2
            Ar, Br = (A1, B1) if t < 32 else (A1c, B1c)
            op('sync', lambda e, t=t, b=b: e.dma_start(out=xt[b], in_=xc[t * 128:(t + 1) * 128, :]), writes=[f'xt{b}'], dma=f'ldx{b}')
            op('scalar', lambda e, t=t, b=b: e.activation(out=junk, in_=xt[b], func=AF.Square, accum_out=ss[:, t:t + 1]), reads=[f'xt{b}'], writes=['junk', f'ss{t}'])
            rstd(rs[:, t:t + 1], ss[:, t:t + 1], D, [f'ss{t}'], f'rs{t}')
            op('vector', lambda e, t=t, b=b, Ar=Ar: e.scalar_tensor_tensor(out=hn[b], in0=xt[b], scalar=rs[:, t:t + 1], in1=Ar, op0=ALU.mult, op1=ALU.mult),
               reads=[f'xt{b}', f'rs{t}', 'rows'], writes=[f'hn{b}'])
            op('gpsimd', lambda e, b=b, Br=Br: e.tensor_tensor(out=hb[b], in0=hn[b], in1=Br, op=ALU.add), reads=[f'hn{b}', 'rows'], writes=[f'hb{b}'])

            def tr(e, b=b):
                for k in range(8):
                    r = e.transpose(pbf[b][:, k * 128:(k + 1) * 128], hb[b][:, k * 128:(k + 1) * 128], ident)
                return r
            op('tensor', tr, reads=[f'hb{b}', 'const'], writes=[PB[b]])
            op('scalar', lambda e, b=b: e.activation(out=hTt[b], in_=pbf[b].rearrange("p (k c) -> p k c", k=8), func=AF.Copy), reads=[PB[b]], writes=[f'hTt{b}'])
            op('sync', lambda e, t=t, b=b: e.dma_start(out=hT_scr[:, :, t * 128:(t + 1) * 128], in_=hTt[b]), reads=[f'hTt{b}'], dma=f'sth{b}')
        S.barrier()
        if STAGE <= 1:
            return finish(nc, S, out, dbg_outs)

        A.reset()
        o_aT = A.alloc([4, TOKR], BF16)
        mark_oa = A.off
        wA = A.alloc([8, 1536], BF16)
        QaT = A.alloc([4, TOKR], BF16); KaT = A.alloc([4, TOKR], BF16)
        Va_e = A.alloc([18, 512], BF16); Va_o = A.alloc([17, 512], BF16)
        KcaT = A.alloc([4, 256], BF16); Vca = A.alloc([2, 512], BF16)
        tblS = A.alloc([4, 960], F32)
        hTg = [A.alloc([8, 576], BF16) for _ in range(2)]
        sbt = [A.alloc([768], F32) for _ in range(2)]
        pbt = [A.alloc([768], BF16) for _ in range(2)]
        pnt = [A.alloc([768], BF16) for _ in range(2)]
        pTt = [A.alloc([768], BF16) for _ in range(2)]
        sm = A.alloc([2, 4], F32)
        for i, (c0, nm) in enumerate(((0, 'ka'), (512, 'va'), (1280, 'qa'))):
            op('gpsimd', lambda e, i=i, c0=c0: e.dma_start(out=wA[:, :, i * 512:(i + 1) * 512], in_=w_in[:, c0:c0 + 512].rearrange("(k p) c -> p k c", p=128)),
               writes=['wA'], dma='d_wA')
        for p in range(4):
            op('sync', lambda e, p=p: e.dma_start(out=tblS[:, p, :], in_=tbl[p]), writes=['tbl'], dma='d_tbl')
        bkc = [0]

        def nbk():
            bkc[0] = (bkc[0] + 1) % 6
            return bkc[0]

        def proj_fm(lhs_cols, rhs_ap, ntok, dst, scale=None):
            bk = nbk()

            def mm(e):
                for k in range(8):
                    r = e.matmul(pf[bk][:, 0:ntok], lhsT=wA[:, k, lhs_cols[0]:lhs_cols[1]], rhs=rhs_ap(k), start=(k == 0), stop=(k == 7))
                return r
            op('tensor', mm, reads=['wA', 'hTg'], writes=[PF[bk]])
            if scale is None:
                op('scalar', lambda e: e.activation(out=dst, in_=pf[bk][:, 0:ntok], func=AF.Copy), reads=[PF[bk]], writes=['naprep'])
            else:
                op('scalar', lambda e: e.activation(out=dst, in_=pf[bk][:, 0:ntok], func=AF.Copy, scale=scale), reads=[PF[bk]], writes=['naprep'])

        def proj_tm(lhs_ap, dst):
            bk = nbk()

            def mm(e):
                for k in range(8):
                    r = e.matmul(pf[bk], lhsT=lhs_ap(k), rhs=wA[:, k, 512:1024], start=(k == 0), stop=(k == 7))
                return r
            op('tensor', mm, reads=['wA', 'hTg'], writes=[PF[bk]])
            op('vector', lambda e: e.tensor_copy(out=dst, in_=pf[bk]), reads=[PF[bk]], writes=['naprep'])

        for g in range(5):
            hg = hTg[g % 2]
            ntok = 512 if g < 4 else 256
            op('sync', lambda e, g=g, hg=hg: e.dma_start(out=hg, in_=hT_scr[:, :, g * 512:g * 512 + 576]), reads=['hT_scr'], writes=['hTg'], dma=f'ldh{g % 2}')
            for c in range(4):
                proj_fm((c * 128, (c + 1) * 128), lambda k, hg=hg, ntok=ntok: hg[:, k, 0:ntok], ntok, KaT[:, c, g * 512:g * 512 + ntok])
                proj_fm((1024 + c * 128, 1024 + (c + 1) * 128), lambda k, hg=hg, ntok=ntok: hg[:, k, 0:ntok], ntok, QaT[:, c, g * 512:g * 512 + ntok], scale=0.125)
            for j in range(ntok // 128):
                proj_tm(lambda k, hg=hg, j=j: hg[:, k, j * 128:(j + 1) * 128], Va_e[:, 4 * g + j, :])
                if 4 * g + j <= 16:
                    proj_tm(lambda k, hg=hg, j=j: hg[:, k, 64 + j * 128:64 + (j + 1) * 128], Va_o[:, 4 * g + j, :])
        hg = hTg[1]
        op('sync', lambda e, hg=hg: e.dma_start(out=hg[:, :, 0:256], in_=hT_scr[:, :, 4096:4352]), reads=['hT_scr'], writes=['hTg'], dma='ldh1')
        for c in range(4):
            proj_fm((c * 128, (c + 1) * 128), lambda k, hg=hg: hg[:, k, 0:256], 256, KcaT[:, c, :])
        for j in range(2):
            proj_tm(lambda k, hg=hg, j=j: hg[:, k, j * 128:(j + 1) * 128], Vca[:, j, :])
        dump('QaT', QaT, [128, 4, TOKR], BF16); dump('KaT', KaT, [128, 4, TOKR], BF16); dump('Va_e', Va_e, [128, 18, 512], BF16)

        na_its = [(l, p) for l in range(36) for p in range(4)]

        def na_ctx(it):
            l, p = na_its[it]
            start = min(max(l - 4, 0), 28); u0 = start - l + 7; tok0 = start * 64
            b = it % 2
            return l, p, start, u0, tok0, b

        def na_stage1(it):
            l, p, start, u0, tok0, b = na_ctx(it)
            sl, sc, po = pf[b], pf[2 + b], pf[4 + b]
            sb_, pb_, pn_, pT_ = sbt[b], pbt[b], pnt[b], pTt[b]

            def qk(e, l=l, p=p, tok0=tok0, sl=sl, sc=sc):
                for hh in range(2):
                    ps_ = slice(hh * 64, hh * 64 + 64)
                    e.matmul(sl[ps_, :], lhsT=QaT[ps_, p, l * 64:(l + 1) * 64], rhs=KaT[ps_, p, tok0:tok0 + 512], start=True, stop=True, tile_position=(hh * 64, hh * 64))
                    r = e.matmul(sc[ps_, 0:256], lhsT=QaT[ps_, p, l * 64:(l + 1) * 64], rhs=KcaT[ps_, p, :], start=True, stop=True, tile_position=(hh * 64, hh * 64))
                return r
            op('tensor', qk, reads=['naprep'], writes=[PF[b], PF[2 + b]])
            op('vector', lambda e, sb_=sb_, sl=sl, p=p, u0=u0: e.tensor_tensor(out=sb_[:, 0:512], in0=sl, in1=tblS[:, p, u0 * 64:u0 * 64 + 512], op=ALU.add),
               reads=[PF[b], 'tbl'], writes=[f'sbA{b}'])
            op('scalar', lambda e, sb_=sb_, sc=sc: e.activation(out=sb_[:, 512:768], in_=sc[:, 0:256], func=AF.Copy), reads=[PF[2 + b]], writes=[f'sbB{b}'])

            chain('vector', [
                lambda e, sb_=sb_, b=b: e.tensor_reduce(out=sm[:, b, 0:1], in_=sb_, axis=AX.X, op=ALU.max),
                lambda e, b=b: e.tensor_scalar(out=sm[:, b, 1:2], in0=sm[:, b, 0:1], scalar1=-1.0, scalar2=None, op0=ALU.mult)],
                reads=[f'sbA{b}', f'sbB{b}'], writes=[f'negm{b}'])
            op('scalar', lambda e, sb_=sb_, pb_=pb_, b=b: e.activation(out=pb_, in_=sb_, func=AF.Exp, bias=sm[:, b, 1:2], scale=1.0, accum_out=sm[:, b, 2:3]),
               reads=[f'sbA{b}', f'sbB{b}', f'negm{b}'], writes=[f'pb{b}', f'sum{b}'])
            op('vector', lambda e, b=b: e.reciprocal(out=sm[:, b, 3:4], in_=sm[:, b, 2:3]), reads=[f'sum{b}'], writes=[f'rsum{b}'])
            op('gpsimd', lambda e, pn_=pn_, pb_=pb_, b=b: e.tensor_scalar(out=pn_, in0=pb_, scalar1=sm[:, b, 3:4], scalar2=None, op0=ALU.mult),
               reads=[f'pb{b}', f'rsum{b}'], writes=[f'pn{b}'])


        def na_stage2(it):
            l, p, start, u0, tok0, b = na_ctx(it)
            sl, sc, po = pf[b], pf[2 + b], pf[4 + b]
            sb_, pb_, pn_, pT_ = sbt[b], pbt[b], pnt[b], pTt[b]
            def trp(e, pn_=pn_, b=b):
                for c in range(6):
                    r = e.transpose(pbf[b][:, c * 128:(c + 1) * 128], pn_[:, c * 128:(c + 1) * 128], ident)
                return r
            op('tensor', trp, reads=[f'pn{b}', 'const'], writes=[PB[b]])
            op('scalar', lambda e, pT_=pT_, b=b: e.activation(out=pT_, in_=pbf[b][:, 0:768], func=AF.Copy), reads=[PB[b]], writes=[f'pT{b}'])

            def pv(e, pT_=pT_, po=po, p=p, start=start):
                for hh in range(2):
                    for c in range(6):
                        if c < 4:
                            V = Va_e[:, start // 2 + c, :] if start % 2 == 0 else Va_o[:, (start - 1) // 2 + c, :]
                        else:
                            V = Vca[:, c - 4, :]
                        r = e.matmul(po[hh * 64:hh * 64 + 64, 0:64], lhsT=V[:, p * 128 + hh * 64:p * 128 + hh * 64 + 64],
                                     rhs=pT_[:, c * 128 + hh * 64:c * 128 + hh * 64 + 64], start=(c == 0), stop=(c == 5), tile_position=(0, hh * 64))
                return r
            op('tensor', pv, reads=[f'pT{b}', 'naprep'], writes=[PF[4 + b]])
            op('vector', lambda e, po=po, p=p, l=l: e.tensor_copy(out=o_aT[:, p, l * 64:(l + 1) * 64], in_=po[:, 0:64]), reads=[PF[4 + b]], writes=['o_aT'])

        for step in range(len(na_its) + 1):
            if step < len(na_its):
                na_stage1(step)
            if step >= 1:
                na_stage2(step - 1)
        dump('o_aT', o_aT, [128, 4, TOKR], BF16)
        S.barrier()
        if STAGE <= 2:
            return finish(nc, S, out, dbg_outs)

        A.reset(mark_oa)
        o_bT = A.alloc([8, TOKR], BF16)
        mark_ob = A.off
        wB = A.alloc([8, 768], BF16)
        QbT = A.alloc([4, TOKR], BF16); KbT = A.alloc([NKEY], BF16)
        Vb = A.alloc([NTALL, 2, 65], BF16)
        gqB = A.alloc([2, 64], F32)
        gtmp = A.alloc([2, 64], F32)
        negC = A.alloc([4], F32)
        hTg = [A.alloc([8, 512], BF16) for _ in range(2)]
        rc = [A.alloc([512], F32) for _ in range(2)]; rsn = [A.alloc([512], F32) for _ in range(2)]
        sq = A.alloc([640], F32); ssh = A.alloc([2, 16], F32)
        qn = A.alloc([640], F32); t1 = A.alloc([640], F32); t2 = A.alloc([640], F32)
        qbb = [A.alloc([640], BF16) for _ in range(2)]
        pTg = [A.alloc([512], BF16) for _ in range(4)]
        osb = [A.alloc([512], F32) for _ in range(2)]
        rec = [A.alloc([512], F32) for _ in range(2)]
        op('gpsimd', lambda e: e.dma_start(out=wB[:, :, 0:256], in_=w_in[:, 1024:1280].rearrange("(k p) c -> p k c", p=128)), writes=['wB'], dma='d_wB')
        op('gpsimd', lambda e: e.dma_start(out=wB[:, :, 256:768], in_=w_in[:, 1792:2304].rearrange("(k p) c -> p k c", p=128)), writes=['wB'], dma='d_wB')
        for i in range(2):
            op('sync', lambda e, i=i: e.dma_start(out=gtmp[:, i, :], in_=gqk[i, :].partition_broadcast(128)), writes=['gtmp'], dma='d_gt')

        qv = qn[:, 0:128].rearrange("p (a b) -> p a b", a=2)
        chain('vector', [
            lambda e: e.tensor_scalar(out=gqB[:, 0, :], in0=gtmp[:, 0, :], scalar1=0.125, scalar2=None, op0=ALU.mult),
            lambda e: e.tensor_copy(out=gqB[:, 1, :], in_=gtmp[:, 1, :]),
            lambda e: e.tensor_scalar(out=qv, in0=gtmp, scalar1=-1.0, scalar2=None, op0=ALU.mult),
            lambda e: e.tensor_tensor(out=gtmp, in0=gtmp, in1=qv, op=ALU.max),
            lambda e: e.tensor_reduce(out=negC[:, 0:1], in_=gtmp[:, 0, :], axis=AX.X, op=ALU.max),
            lambda e: e.tensor_reduce(out=negC[:, 1:2], in_=gtmp[:, 1, :], axis=AX.X, op=ALU.max),
            lambda e: e.tensor_tensor(out=negC[:, 2:3], in0=negC[:, 0:1], in1=negC[:, 1:2], op=ALU.mult),
            lambda e: e.tensor_scalar(out=negC[:, 3:4], in0=negC[:, 2:3], scalar1=-8.0, scalar2=None, op0=ALU.mult),
            lambda e: e.memset(Vb[:, :, :, 64:65], 1.0)], reads=['gtmp'], writes=['gtmp', 'gqB', 'negC', 'Vb', 'qn'])

        def normrope(src, H, gi, b, dst, tagr):
            W = H * 64
            op('scalar', lambda e: e.activation(out=sq[:, 0:W], in_=src, func=AF.Square), reads=tagr, writes=['sq'])

            op('vector', lambda e: e.tensor_reduce(out=ssh[:, 0, 0:H], in_=sq[:, 0:W].rearrange("p (h d) -> p h d", d=64), axis=AX.X, op=ALU.add), reads=['sq'], writes=['ssh0'])
            rstd(ssh[:, 1, 0:H], ssh[:, 0, 0:H], 64, ['ssh0'], 'ssh1')

            def n1(e):
                for h in range(H):
                    r = e.scalar_tensor_tensor(out=qn[:, h * 64:(h + 1) * 64], in0=src[:, h * 64:(h + 1) * 64], scalar=ssh[:, 1, h:h + 1], in1=gqB[:, gi, :],
                                               op0=ALU.mult, op1=ALU.mult)
                return r
            op('vector', n1, reads=['ssh1', 'gqB'] + tagr, writes=['qn'])
            op('vector', lambda e: e.tensor_tensor(out=t1[:, 0:W], in0=qn[:, 0:W], in1=rc[b][:, 0:W], op=ALU.mult), reads=['qn', f'rc{b}'], writes=['t1'])

            def r2(e):
                q4 = qn[:, 0:W].rearrange("p (a s f) -> p a s f", s=2, f=16)
                s4 = rsn[b][:, 0:W].rearrange("p (a s f) -> p a s f", s=2, f=16)
                o4 = t2[:, 0:W].rearrange("p (a s f) -> p a s f", s=2, f=16)
                e.tensor_tensor(out=o4[:, :, 0, :], in0=q4[:, :, 1, :], in1=s4[:, :, 0, :], op=ALU.mult)
                return e.tensor_tensor(out=o4[:, :, 1, :], in0=q4[:, :, 0, :], in1=s4[:, :, 1, :], op=ALU.mult)
            op('vector', r2, reads=['qn', f'rsn{b}'], writes=['t2'])
            op('vector', lambda e: e.tensor_tensor(out=dst, in0=t1[:, 0:W], in1=t2[:, 0:W], op=ALU.add), reads=['t1', 't2'], writes=['qbb'])

        for t in range(NTALL):
            b = t % 2
            g = t // 4
            hg = hTg[g % 2]
            if t % 4 == 0:
                n = min(512, NKEY - g * 512)
                op('sync', lambda e, g=g, hg=hg, n=n: e.dma_start(out=hg[:, :, 0:n], in_=hT_scr[:, :, g * 512:g * 512 + n]), reads=['hT_scr'], writes=[f'hTg{g % 2}'], dma=f'ldh{g % 2}')
            j = t % 4
            op('sync', lambda e, t=t, b=b: e.dma_start(out=rc[b], in_=ropec[t * 128:(t + 1) * 128, :]), writes=[f'rc{b}'], dma=f'ldr{b}')
            op('sync', lambda e, t=t, b=b: e.dma_start(out=rsn[b], in_=ropes[t * 128:(t + 1) * 128, :]), writes=[f'rsn{b}'], dma=f'lds{b}')
            bk = nbk()

            def mmkv(e, hg=hg, j=j, bk=bk):
                for k in range(8):
                    r = e.matmul(pf[bk][:, 0:256], lhsT=hg[:, k, j * 128:(j + 1) * 128], rhs=wB[:, k, 0:256], start=(k == 0), stop=(k == 7))
                return r
            op('tensor', mmkv, reads=['wB', f'hTg{g % 2}'], writes=[PF[bk]])
            op('scalar', lambda e, t=t, bk=bk: e.activation(out=Vb[:, t, :, 0:64], in_=pf[bk][:, 128:256].rearrange("p (h d) -> p h d", d=64), func=AF.Copy),
               reads=[PF[bk]], writes=['Vb'])
            normrope(pf[bk][:, 0:128], 2, 1, b, qbb[b][:, 0:128], [PF[bk]])
            op('tensor', lambda e, b=b: e.transpose(pbf[b][:, 0:128], qbb[b][:, 0:128], ident), reads=['qbb', 'const'], writes=[PB[b]])
            op('scalar', lambda e, t=t, b=b: e.activation(out=KbT[:, t * 128:(t + 1) * 128], in_=pbf[b][:, 0:128], func=AF.Copy), reads=[PB[b]], writes=['KbT'])
            if t < NTR:
                bk2 = nbk()

                def mmq(e, hg=hg, j=j, bk2=bk2):
                    for k in range(8):
                        r = e.matmul(pf[bk2], lhsT=hg[:, k, j * 128:(j + 1) * 128], rhs=wB[:, k, 256:768], start=(k == 0), stop=(k == 7))
                    return r
                op('tensor', mmq, reads=['wB', f'hTg{g % 2}'], writes=[PF[bk2]])
                normrope(pf[bk2], 8, 0, b, qbb[b][:, 0:512], [PF[bk2]])

                def trq(e, b=b):
                    for gg in range(4):
                        r = e.transpose(pbf[b][:, 128 + gg * 128:128 + (gg + 1) * 128], qbb[b][:, gg * 128:(gg + 1) * 128], ident)
                    return r
                op('tensor', trq, reads=['qbb', 'const'], writes=[PB[b]])
                op('scalar', lambda e, t=t, b=b: e.activation(out=QbT[:, :, t * 128:(t + 1) * 128], in_=pbf[b][:, 128:640].rearrange("p (g c) -> p g c", g=4), func=AF.Copy),
                   reads=[PB[b]], writes=['QbT'])
        dump('QbT', QbT, [128, 4, TOKR], BF16); dump('KbT', KbT, [128, NKEY], BF16); dump('Vb', Vb, [128, NTALL, 2, 65], BF16)

        chunks = [(kvh, qt, c) for kvh in range(2) for qt in range(NTR) for c in range(NTALL)]
        LA = 2
        pending = []

        def gq_S(i):
            kvh, qt, c = chunks[i]
            pr = slice(kvh * 64, kvh * 64 + 64)
            sb_ = i % 4
            st_ = pf[sb_]
            op('tensor', lambda e: e.matmul(st_, lhsT=KbT[pr, c * 128:(c + 1) * 128], rhs=QbT[pr, :, qt * 128:(qt + 1) * 128],
                                            start=True, stop=True, tile_position=(kvh * 64, 0)),
               reads=['KbT', 'QbT'], writes=[PF[sb_]])
            op('scalar', lambda e: e.activation(out=pTg[sb_], in_=st_, func=AF.Exp, bias=negC[:, 3:4], scale=1.0), reads=[PF[sb_], 'negC'], writes=[f'pTg{sb_}'])

        def gq_PV(i, step):
            kvh, qt, c = chunks[i]
            sb_ = i % 4
            ob = (kvh * NTR + qt) % 2
            po = pf[4 + ob]
            bk = 4 + ob
            op('tensor', lambda e: e.matmul(po[0:65, :], lhsT=Vb[:, c, kvh, :], rhs=pTg[sb_], start=(c == 0), stop=(c == NTALL - 1)),
               reads=[f'pTg{sb_}', 'Vb'], writes=[PF[bk]])
            if c == NTALL - 1:
                op('scalar', lambda e: e.activation(out=osb[ob][0:65, :], in_=po[0:65, :], func=AF.Copy), reads=[PF[bk]], writes=[f'osb{ob}'])
                op('vector', lambda e: e.reciprocal(out=rec[ob][64:65, :], in_=osb[ob][64:65, :]), reads=[f'osb{ob}'], writes=[f'rec{ob}'])

                def fin():
                    op('tensor', lambda e: e.matmul(po[0:64, :], lhsT=ones_f[64:65, 0:64], rhs=rec[ob][64:65, :], start=True, stop=True), reads=[f'rec{ob}', 'const'], writes=[PF[bk]])
                    op('vector', lambda e: e.tensor_tensor(out=o_bT[0:64, kvh * 4:(kvh + 1) * 4, qt * 128:(qt + 1) * 128],
                                                           in0=osb[ob][0:64, :].rearrange("p (g t) -> p g t", g=4),
                                                           in1=po[0:64, :].rearrange("p (g t) -> p g t", g=4), op=ALU.mult),
                       reads=[f'osb{ob}', PF[bk]], writes=['o_bT'])
                pending.append((step + 4, fin))

        for step in range(len(chunks) + LA + 8):
            if step < len(chunks):
                gq_S(step)
            if LA <= step < len(chunks) + LA:
                gq_PV(step - LA, step)
            for due, fn_ in list(pending):
                if due <= step:
                    fn_(); pending.remove((due, fn_))
        assert not pending
        dump('o_bT', o_bT, [128, 8, TOKR], BF16)
        S.barrier()
        if STAGE <= 3:
            return finish(nc, S, out, dbg_outs)

        A.reset(mark_ob)
        wG = A.alloc([8, 2048], BF16); wOA = A.alloc([4, D], BF16); wOB = A.alloc([8, D], BF16); wO = A.alloc([8, D], BF16)
        wR = A.alloc([8, NE], BF16); bR = A.alloc([NE], BF16)
        hTg = [A.alloc([8, 512], BF16)] * 2
        zT = [A.alloc([8, 512], BF16) for _ in range(2)]
        sga = A.alloc([512], F32); sgb = A.alloc([512], F32)
        xt4 = A.alloc([D], F32); x1t = [A.alloc([D], F32) for _ in range(2)]; tmpf = A.alloc([D], F32)
        h2b = [A.alloc([D], BF16) for _ in range(2)]; h2T = A.alloc([8, 128], BF16)
        lg = P.alloc([NTR, NE], F32); mx8 = P.alloc([NTR, 8], F32); posA = P.alloc([NTR, NE], F32)
        wts = P.alloc([NTR, 4], F32); sloti = P.alloc([NTR * 4], I32)
        mask = A.alloc([NE], F32); maskb = A.alloc([NE], BF16); cntp = P.alloc([NE], F32)
        sms = A.alloc([NTR, 8], F32); e4 = A.alloc([4], F32)
        utri = A.alloc([128], BF16); utf = A.alloc([128], F32)
        mark_route = A.off
        for (dst, src, nm) in ((wG[:, :, 0:1024], w_in[:, 2304:3328], 0), (wG[:, :, 1024:2048], w_in[:, 3328:4352], 1), (wO, w_o, 2)):
            op('gpsimd', lambda e, dst=dst, src=src: e.dma_start(out=dst, in_=src.rearrange("(k p) c -> p k c", p=128)), writes=['wM'], dma='d_wM')
        op('gpsimd', lambda e: e.dma_start(out=wOA, in_=w_oa.rearrange("(k p) c -> p k c", p=128)), writes=['wM'], dma='d_wM')
        op('gpsimd', lambda e: e.dma_start(out=wOB[0:64], in_=w_ob.rearrange("(h d) c -> d h c", d=64)), writes=['wM'], dma='d_wM')
        op('gpsimd', lambda e: e.dma_start(out=wR, in_=w_r.rearrange("(k p) c -> p k c", p=128)), writes=['wM'], dma='d_wM')
        op('gpsimd', lambda e: e.dma_start(out=bR[0:1, :], in_=b_r.rearrange("(o n) -> o n", o=1)), writes=['wM'], dma='d_wM')

        chain('gpsimd', [
            lambda e: e.memset(utf, 1.0),
            lambda e: e.affine_select(out=utf, in_=utf, pattern=[[1, 128]], compare_op=ALU.is_gt, fill=0.0, base=0, channel_multiplier=-1),
            lambda e: e.memset(cntp, 0.0),
            lambda e: e.tensor_copy(out=utri, in_=utf)], writes=['utri', 'route'])

        for g in range(5):
            hg = hTg[g % 2]; z = zT[g % 2]
            ntok = 512 if g < 4 else 256
            tk = slice(g * 512, g * 512 + ntok)
            op('sync', lambda e, g=g, hg=hg, ntok=ntok: e.dma_start(out=hg[:, :, 0:ntok], in_=hT_scr[:, :, g * 512:g * 512 + ntok]), reads=['hT_scr'], writes=[f'hTg{g % 2}'], dma=f'ldh{g % 2}')
            for oc in range(8):
                def mm4(e, hg=hg, oc=oc, ntok=ntok, tk=tk):
                    for k in range(8):
                        e.matmul(pf[0][:, 0:ntok], lhsT=wG[:, k, oc * 128:(oc + 1) * 128], rhs=hg[:, k, 0:ntok], start=(k == 0), stop=(k == 7))
                    for k in range(8):
                        e.matmul(pf[1][:, 0:ntok], lhsT=wG[:, k, 1024 + oc * 128:1024 + (oc + 1) * 128], rhs=hg[:, k, 0:ntok], start=(k == 0), stop=(k == 7))
                    for k in range(4):
                        e.matmul(pf[2][:, 0:ntok], lhsT=wOA[:, k, oc * 128:(oc + 1) * 128], rhs=o_aT[:, k, tk], start=(k == 0), stop=(k == 3))
                    for k in range(8):
                        r = e.matmul(pf[3][:, 0:ntok], lhsT=wOB[0:64, k, oc * 128:(oc + 1) * 128], rhs=o_bT[0:64, k, tk], start=(k == 0), stop=(k == 7))
                    return r
                op('tensor', mm4, reads=['wM', f'hTg{g % 2}', 'o_aT', 'o_bT'], writes=[PF[0], PF[1], PF[2], PF[3]])

                def sg(e, ntok=ntok):
                    e.activation(out=sga[:, 0:ntok], in_=pf[0][:, 0:ntok], func=AF.Sigmoid)
                    return e.activation(out=sgb[:, 0:ntok], in_=pf[1][:, 0:ntok], func=AF.Sigmoid)
                op('scalar', sg, reads=[PF[0], PF[1]], writes=['sg'])

                def zz(e, ntok=ntok):
                    e.tensor_tensor(out=sga[:, 0:ntok], in0=sga[:, 0:ntok], in1=pf[2][:, 0:ntok], op=ALU.mult)
                    return e.tensor_tensor(out=sgb[:, 0:ntok], in0=sgb[:, 0:ntok], in1=pf[3][:, 0:ntok], op=ALU.mult)
                op('vector', zz, reads=['sg', PF[2], PF[3]], writes=['sg2'])
                op('gpsimd', lambda e, z=z, oc=oc, ntok=ntok: e.tensor_tensor(out=z[:, oc, 0:ntok], in0=sga[:, 0:ntok], in1=sgb[:, 0:ntok], op=ALU.add),
                   reads=['sg2'], writes=[f'zT{g % 2}', 'sg'])
            for j in range(ntok // 128):
                t = 4 * g + j
                yb = pq[2]
                b = t % 2

                def mmy(e, z=z, j=j, yb=yb):
                    for n in range(2):
                        for k in range(8):
                            r = e.matmul(yb[:, n * 512:(n + 1) * 512], lhsT=z[:, k, j * 128:(j + 1) * 128], rhs=wO[:, k, n * 512:(n + 1) * 512], start=(k == 0), stop=(k == 7))
                    return r
                op('tensor', mmy, reads=['wM', f'zT{g % 2}'], writes=[PF[4], PF[5]])
                op('sync', lambda e, t=t: e.dma_start(out=xt4, in_=xc[t * 128:(t + 1) * 128, :]), writes=['xt'], dma='ldx0')
                op('scalar', lambda e, yb=yb, t=t: e.activation(out=tmpf, in_=yb, func=AF.Square, accum_out=sms[:, t, 0:1]), reads=[PF[4], PF[5]], writes=['tmpf', 'ssy'])
                rstd(sms[:, t, 1:2], sms[:, t, 0:1], D, ['ssy'], 'rsy')
                op('vector', lambda e, yb=yb, t=t: e.scalar_tensor_tensor(out=tmpf, in0=yb, scalar=sms[:, t, 1:2], in1=G1, op0=ALU.mult, op1=ALU.mult),
                   reads=[PF[4], PF[5], 'rsy', 'rows'], writes=['tmpf'])
                op('gpsimd', lambda e, b=b: e.tensor_tensor(out=x1t[b], in0=tmpf, in1=xt4, op=ALU.add), reads=['tmpf', 'xt'], writes=[f'x1t{b}'])
                op('sync', lambda e, t=t, b=b: e.dma_start(out=x1_scr[t * 128:(t + 1) * 128, :], in_=x1t[b]), reads=[f'x1t{b}'], dma=f'stx{b}')
                op('scalar', lambda e, t=t, b=b: e.activation(out=tmpf, in_=x1t[b], func=AF.Square, accum_out=sms[:, t, 2:3]), reads=[f'x1t{b}'], writes=['tmpf', 'ss2'])
                rstd(sms[:, t, 3:4], sms[:, t, 2:3], D, ['ss2'], 'rs2')
                op('vector', lambda e, t=t, b=b: e.scalar_tensor_tensor(out=tmpf, in0=x1t[b], scalar=sms[:, t, 3:4], in1=A2, op0=ALU.mult, op1=ALU.mult),
                   reads=[f'x1t{b}', 'rs2', 'rows'], writes=['tmpf'])
                op('gpsimd', lambda e, b=b: e.tensor_tensor(out=h2b[b], in0=tmpf, in1=B2, op=ALU.add), reads=['tmpf', 'rows'], writes=[f'h2b{b}'])
                op('sync', lambda e, t=t, b=b: e.dma_start(out=h2_scr[t * 128:(t + 1) * 128, :], in_=h2b[b]), reads=[f'h2b{b}'], dma=f'sth{b}')

                def trh(e, b=b):
                    for k in range(8):
                        r = e.transpose(pbf[b][:, k * 128:(k + 1) * 128], h2b[b][:, k * 128:(k + 1) * 128], ident)
                    return r
                op('tensor', trh, reads=[f'h2b{b}', 'const'], writes=[PB[b]])
                op('scalar', lambda e, b=b: e.activation(out=h2T, in_=pbf[b].rearrange("p (k c) -> p k c", k=8), func=AF.Copy), reads=[PB[b]], writes=['h2T'])

                def mml(e):
                    for k in range(8):
                        e.matmul(pf[0][:, 0:NE], lhsT=h2T[:, k, :], rhs=wR[:, k, :], start=(k == 0), stop=False)
                    return e.matmul(pf[0][:, 0:NE], lhsT=ones_bf[0:1, :], rhs=bR[0:1, :], start=False, stop=True)
                op('tensor', mml, reads=['h2T', 'wM', 'const'], writes=[PF[0]])

                chain('vector', [
                    lambda e, t=t: e.tensor_copy(out=lg[:, t, :], in_=pf[0][:, 0:NE]),
                    lambda e, t=t: e.max(out=mx8[:, t, :], in_=lg[:, t, :]),
                    lambda e, t=t: e.tensor_scalar(out=mask, in0=lg[:, t, :], scalar1=mx8[:, t, 3:4], scalar2=None, op0=ALU.is_ge),
                    lambda e: e.tensor_copy(out=maskb, in_=mask),
                    lambda e, t=t: e.tensor_scalar(out=sms[:, t, 4:5], in0=mx8[:, t, 0:1], scalar1=-1.0, scalar2=None, op0=ALU.mult)],
                    reads=[PF[0]], writes=['lg', 'maskb', 'negmx'])
                op('scalar', lambda e, t=t: e.activation(out=e4, in_=mx8[:, t, 0:4], func=AF.Exp, bias=sms[:, t, 4:5], scale=1.0, accum_out=sms[:, t, 5:6]),
                   reads=['lg', 'negmx'], writes=['e4'])

                def mmc(e):
                    e.matmul(pf[1][:, 0:NE], lhsT=utri, rhs=maskb, start=True, stop=True)
                    return e.matmul(pf[1][:, NE:2 * NE], lhsT=ones_bf, rhs=maskb, start=True, stop=True)
                op('tensor', mmc, reads=['maskb', 'utri', 'const'], writes=[PF[1]])

                chain('vector', [
                    lambda e, t=t: e.reciprocal(out=sms[:, t, 6:7], in_=sms[:, t, 5:6]),
                    lambda e, t=t: e.tensor_scalar(out=wts[:, t, :], in0=e4, scalar1=sms[:, t, 6:7], scalar2=None, op0=ALU.mult),
                    lambda e, t=t: e.tensor_tensor(out=posA[:, t, :], in0=pf[1][:, 0:NE], in1=cntp, op=ALU.add),
                    lambda e: e.tensor_tensor(out=cntp, in0=cntp, in1=pf[1][:, NE:2 * NE], op=ALU.add)],
                    reads=['e4', PF[1]], writes=['route'])
        dump('lg', lg, [128, NTR, NE], F32); dump('posA', posA, [128, NTR, NE], F32); dump('cntp', cntp, [128, NE], F32)
        S.barrier()
        if STAGE <= 4:
            return finish(nc, S, out, dbg_outs)

        A.reset()
        ci = A.alloc([NE], I32); padf = A.alloc([NE], F32); padT = A.alloc([128], F32); ltri = A.alloc([NE], F32)
        basef = A.alloc([NE], F32); pend = A.alloc([NE], F32)
        thr = A.alloc([NBLK], F32); EB = A.alloc([NBLK], F32); skp = A.alloc([NBLK], F32)
        idxw_f = A.alloc([NBLK], F32); idxb_f = A.alloc([NBLK], F32); pidx = A.alloc([1], F32)
        idxw = A.alloc([NBLK], I32); idxb = A.alloc([NBLK], I32)
        idxw8_f = A.alloc([8, NBLK], F32); idxw8 = A.alloc([8, NBLK], I32)
        idxd_f = A.alloc([4, NBLK], F32); idxd = A.alloc([4, NBLK], I32); idxd0 = A.alloc([NBLK], F32); pidx4 = A.alloc([1], F32)
        slot2 = A.alloc([NE], F32); slotf = A.alloc([NTR * 4], F32); tmp32 = A.alloc([NE], F32)
        h2l = [A.alloc([D], BF16) for _ in range(2)]

        chain('vector', [
            lambda e: e.tensor_scalar(out=padf, in0=cntp, scalar1=127.0, scalar2=None, op0=ALU.add),
            lambda e: e.tensor_copy(out=ci, in_=padf),
            lambda e: e.tensor_single_scalar(out=ci, in_=ci, scalar=7, op=ALU.arith_shift_right),
            lambda e: e.tensor_single_scalar(out=ci, in_=ci, scalar=7, op=ALU.logical_shift_left),
            lambda e: e.tensor_copy(out=padf, in_=ci)], reads=['route'], writes=['padf'])

        chain('gpsimd', [
            lambda e: e.memset(ltri, 1.0),
            lambda e: e.affine_select(out=ltri, in_=ltri, pattern=[[1, NE]], compare_op=ALU.is_gt, fill=0.0, base=0, channel_multiplier=-1),
            lambda e: e.iota(thr, pattern=[[128, NBLK]], base=0, channel_multiplier=0, allow_small_or_imprecise_dtypes=True),
            lambda e: e.iota(pidx, pattern=[[0, 1]], base=0, channel_multiplier=1, allow_small_or_imprecise_dtypes=True)], writes=['ltri'])
        op('tensor', lambda e: e.transpose(pq[0][0:NE, 0:128], padf, identf), reads=['padf', 'const'], writes=[PF[0]])
        op('vector', lambda e: e.tensor_copy(out=padT[0:NE, :], in_=pq[0][0:NE, 0:128]), reads=[PF[0]], writes=['padT'])
        op('tensor', lambda e: e.matmul(pf[1][:, 0:NE], lhsT=padT[0:NE, :], rhs=ltri[0:NE, :], start=True, stop=True), reads=['padT', 'ltri'], writes=[PF[1]])

        lay2 = [
            lambda e: e.tensor_copy(out=basef, in_=pf[1][:, 0:NE]),
            lambda e: e.tensor_tensor(out=pend, in0=basef, in1=padf, op=ALU.add),
            lambda e: e.memset(EB, 0.0)]
        for ex in range(NE):
            lay2.append(lambda e, ex=ex: e.scalar_tensor_tensor(out=EB, in0=thr, scalar=pend[:, ex:ex + 1], in1=EB, op0=ALU.is_ge, op1=ALU.add))
        lay2 += [
            lambda e: e.tensor_scalar(out=EB, in0=EB, scalar1=float(NE - 1), scalar2=None, op0=ALU.min),
            lambda e: e.memset(skp, 0.0),
            lambda e: e.tensor_tensor(out=skp[:, 1:NBLK], in0=EB[:, 1:NBLK], in1=EB[:, 0:NBLK - 1], op=ALU.is_equal),
            lambda e: e.tensor_scalar(out=skp, in0=skp, scalar1=BIG, scalar2=None, op0=ALU.mult),
            lambda e: e.scalar_tensor_tensor(out=idxw_f, in0=EB, scalar=1024.0, in1=skp, op0=ALU.mult, op1=ALU.add),
            lambda e: e.tensor_scalar(out=idxw_f, in0=idxw_f, scalar1=pidx[:, 0:1], scalar2=None, op0=ALU.add),
            lambda e: e.tensor_tensor(out=idxb_f, in0=EB, in1=skp, op=ALU.add),
            lambda e: e.tensor_copy(out=idxw, in_=idxw_f)]
        for k8 in range(8):
            lay2.append(lambda e, k8=k8: e.tensor_scalar(out=idxw8_f[:, k8, :], in0=idxw_f, scalar1=128.0 * k8, scalar2=None, op0=ALU.add))
        lay2 += [lambda e: e.tensor_copy(out=idxw8, in_=idxw8_f), lambda e: e.tensor_copy(out=idxb, in_=idxb_f)]
        lay2 += [lambda e: e.tensor_scalar(out=pidx4, in0=pidx, scalar1=4.0, scalar2=None, op0=ALU.mult),
                 lambda e: e.scalar_tensor_tensor(out=idxd0, in0=EB, scalar=512.0, in1=skp, op0=ALU.mult, op1=ALU.add),
                 lambda e: e.tensor_scalar(out=idxd0, in0=idxd0, scalar1=pidx4[:, 0:1], scalar2=None, op0=ALU.add)]
        for j4 in range(4):
            lay2.append(lambda e, j4=j4: e.tensor_scalar(out=idxd_f[:, j4, :], in0=idxd0, scalar1=float(j4), scalar2=None, op0=ALU.add))
        lay2 += [lambda e: e.tensor_copy(out=idxd, in_=idxd_f)]
        chain('vector', lay2, reads=[PF[1], 'padf', 'ltri'], writes=['lay'])
        for t in range(NTR):
            b = t % 2
            op('sync', lambda e, t=t, b=b: e.dma_start(out=h2l[b], in_=h2_scr[t * 128:(t + 1) * 128, :]), reads=['h2_scr'], writes=[f'h2l{b}'], dma=f'ldx{b}')

            slf = [lambda e, t=t: e.tensor_tensor(out=slot2, in0=posA[:, t, :], in1=basef, op=ALU.add)]
            for k in range(4):
                slf.append(lambda e, t=t, k=k: e.scalar_tensor_tensor(out=tmp32, in0=lg[:, t, :], scalar=mx8[:, t, k:k + 1], in1=slot2, op0=ALU.is_equal, op1=ALU.mult,
                                                                      accum_out=slotf[:, 4 * t + k:4 * t + k + 1]))
            slf.append(lambda e, t=t: e.tensor_copy(out=sloti[:, 4 * t:4 * t + 4], in_=slotf[:, 4 * t:4 * t + 4]))
            chain('vector', slf, reads=['lay'], writes=[f'sloti{t}', 'slot2'])
            for k in range(4):
                op('gpsimd', lambda e, t=t, k=k, b=b: e.indirect_dma_start(out=xs_scr, out_offset=bass.IndirectOffsetOnAxis(ap=sloti[:, 4 * t + k:4 * t + k + 1], axis=0),
                                                                       in_=h2l[b], in_offset=None, bounds_check=breg(e, NSLOT - 1), oob_is_err=False),
                   reads=[f'sloti{t}', f'h2l{b}'], dma=f'sc{b}')
        dump('sloti', sloti, [128, NTR * 4], I32); dump('idxw', idxw, [128, NBLK], I32); dump('wts', wts, [128, NTR, 4], F32)
        S.barrier()
        if STAGE <= 5:
            return finish(nc, S, out, dbg_outs)

        mark_ex = A.off
        wgu = A.alloc([8, 2 * D], BF16); wdn4 = A.alloc([4, 2 * D], BF16)
        wdn_k = lambda k: wdn4[:, k // 2, (k % 2) * D:(k % 2 + 1) * D]
        wdn_pairs = w_dn.rearrange("e (q two) n -> (e q) (two n)", two=2)
        bgu = A.alloc([2 * D], BF16); bdn = A.alloc([D], BF16)
        xe = [A.alloc([D], BF16) for _ in range(2)]; xT = [A.alloc([8, 128], BF16) for _ in range(2)]
        gs = A.alloc([D], F32); sg_ = A.alloc([D], F32); l1 = A.alloc([D], F32); tt = A.alloc([D], F32)
        actb = A.alloc([D], BF16); aT = A.alloc([8, 128], BF16)
        yo = [A.alloc([D], F32) for _ in range(2)]
        wgu_flat = w_gu.rearrange("e k n -> (e k) n"); wdn_flat = w_dn.rearrange("e k n -> (e k) n")
        wgu_v = bass.AP(tensor=w_gu.tensor, offset=0, ap=[[2 * D, NE * D - 896], [128 * 2 * D, 8], [1, 2 * D]])
        wdn_v = bass.AP(tensor=w_dn.tensor, offset=0, ap=[[D, NE * D - 896], [128 * D, 8], [1, D]])
        gs2 = [gs, A.alloc([D], F32)]; sg2 = [sg_, A.alloc([D], F32)]; l12 = [l1, A.alloc([D], F32)]; tt2 = [tt, A.alloc([D], F32)]
        actb2 = [actb, A.alloc([D], BF16)]; aT2 = [aT, A.alloc([8, 128], BF16)]

        def blk_ldgu(blk):
            b = blk % 2
            ib = bass.IndirectOffsetOnAxis(ap=idxb[:, blk:blk + 1], axis=0)
            for k8 in range(8):
                op('gpsimd', lambda e, k8=k8: e.indirect_dma_start(out=wgu[:, k8, :], out_offset=None, in_=wgu_flat,
                                                                   in_offset=bass.IndirectOffsetOnAxis(ap=idxw8[:, k8, blk:blk + 1], axis=0),
                                                                   bounds_check=breg(e, NE * D - 1), oob_is_err=False),
                   reads=['lay'], writes=[f'wgu{k8}'], dma='ld_wgu')
            op('gpsimd', lambda e: e.indirect_dma_start(out=bgu, out_offset=None, in_=b_gu, in_offset=ib, bounds_check=breg(e, NE - 1), oob_is_err=False),
               reads=['lay'], writes=['bgu'], dma='ld_wgu')
            op('sync', lambda e: e.dma_start(out=xe[b], in_=xs_scr[blk * 128:(blk + 1) * 128, :]), writes=[f'xe{b}'], dma=f'ldx{b}')

        def blk_lddn(blk):
            ib = bass.IndirectOffsetOnAxis(ap=idxb[:, blk:blk + 1], axis=0)
            for j4 in range(4):
                op('gpsimd', lambda e, j4=j4: e.indirect_dma_start(out=wdn4[:, j4, :], out_offset=None, in_=wdn_pairs,
                                                                   in_offset=bass.IndirectOffsetOnAxis(ap=idxd[:, j4, blk:blk + 1], axis=0),
                                                                   bounds_check=breg(e, NE * 512 - 1), oob_is_err=False),
                   reads=['lay'], writes=[f'wdn{j4}'], dma='ld_wdn')
            op('gpsimd', lambda e: e.indirect_dma_start(out=bdn, out_offset=None, in_=b_dn, in_offset=ib, bounds_check=breg(e, NE - 1), oob_is_err=False),
               reads=['lay'], writes=['bdn'], dma='ld_wdn')

        def blk_trx(blk):
            b = blk % 2

            def trx(e):
                for k in range(8):
                    r = e.transpose(pbf[0][:, k * 128:(k + 1) * 128], xe[b][:, k * 128:(k + 1) * 128], ident)
                return r
            op('tensor', trx, reads=[f'xe{b}', 'const'], writes=[PB[0]])
            op('scalar', lambda e: e.activation(out=xT[b], in_=pbf[0].rearrange("p (k c) -> p k c", k=8), func=AF.Copy), reads=[PB[0]], writes=[f'xT{b}'])

        def blk_gu(blk):
            b = blk % 2
            g_, s_, l_, t_, a_ = gs2[b], sg2[b], l12[b], tt2[b], actb2[b]

            def mgu(e):
                for k in range(8):
                    for n in range(4):
                        e.matmul(pf[n], lhsT=xT[b][:, k, :], rhs=wgu[:, k, n * 512:(n + 1) * 512], start=(k == 0), stop=False)
                for n in range(4):
                    r = e.matmul(pf[n], lhsT=ones_bf[0:1, :], rhs=bgu[0:1, n * 512:(n + 1) * 512], start=False, stop=True)
                return r
            op('tensor', mgu, reads=[f'xT{b}', 'bgu', 'const'] + [f'wgu{k8}' for k8 in range(8)], writes=[PF[0], PF[1], PF[2], PF[3]])
            op('vector', lambda e: e.tensor_scalar(out=g_, in0=pq[0], scalar1=7.0, scalar2=None, op0=ALU.min), reads=[PF[0], PF[1]], writes=[f'gs{b}'])
            op('scalar', lambda e: e.activation(out=s_, in_=g_, func=AF.Sigmoid, scale=1.702), reads=[f'gs{b}'], writes=[f'sg{b}'])
            op('vector', lambda e: e.tensor_scalar(out=l_, in0=pq[1], scalar1=7.0, scalar2=-7.0, op0=ALU.min, op1=ALU.max), reads=[PF[2], PF[3]], writes=[f'l1{b}'])
            op('vector', lambda e: e.tensor_tensor(out=t_, in0=g_, in1=s_, op=ALU.mult), reads=[f'gs{b}', f'sg{b}'], writes=[f'tt{b}'])
            op('vector', lambda e: e.scalar_tensor_tensor(out=a_, in0=l_, scalar=1.0, in1=t_, op0=ALU.add, op1=ALU.mult), reads=[f'l1{b}', f'tt{b}'], writes=[f'actb{b}'])

        def blk_tra(blk):
            b = blk % 2
            a_ = actb2[b]

            def tra(e):
                for k in range(8):
                    r = e.transpose(pbf[1][:, k * 128:(k + 1) * 128], a_.rearrange("t (p k) -> t k p", k=8)[:, k, :], ident)
                return r
            op('tensor', tra, reads=[f'actb{b}', 'const'], writes=[PB[1]])
            op('scalar', lambda e: e.activation(out=aT2[b], in_=pbf[1].rearrange("p (k c) -> p k c", k=8), func=AF.Copy), reads=[PB[1]], writes=[f'aT{b}'])

        def blk_dn(blk):
            b = blk % 2

            def mdn(e):
                for k in range(8):
                    for n in range(2):
                        e.matmul(pf[4 + n], lhsT=aT2[b][:, k, :], rhs=wdn_k(k)[:, n * 512:(n + 1) * 512], start=(k == 0), stop=False)
                for n in range(2):
                    r = e.matmul(pf[4 + n], lhsT=ones_bf[0:1, :], rhs=bdn[0:1, n * 512:(n + 1) * 512], start=False, stop=True)
                return r
            op('tensor', mdn, reads=[f'aT{b}', 'bdn', 'const'] + [f'wdn{j4}' for j4 in range(4)], writes=[PF[4], PF[5]])
            op('scalar', lambda e: e.activation(out=yo[b], in_=pq[2], func=AF.Copy), reads=[PF[4], PF[5]], writes=[f'yo{b}'])
            op('sync', lambda e: e.dma_start(out=y_scr[blk * 128:(blk + 1) * 128, :], in_=yo[b]), reads=[f'yo{b}'], dma=f'sty{b}')

        blk_ldgu(0); blk_lddn(0); blk_trx(0)
        for sblk in range(NBLK):
            blk_gu(sblk)
            if sblk >= 1:
                blk_tra(sblk - 1)
            if sblk + 1 < NBLK:
                blk_ldgu(sblk + 1)
                blk_trx(sblk + 1)
            if sblk >= 1:
                blk_dn(sblk - 1)
                blk_lddn(sblk)
        blk_tra(NBLK - 1); blk_dn(NBLK - 1)
        S.barrier()

        A.reset(mark_ex)
        gk = [[A.alloc([D], F32) for _ in range(4)] for _ in range(2)]
        acc = A.alloc([D], F32); x1l = [A.alloc([D], F32) for _ in range(2)]; ot = [A.alloc([D], F32) for _ in range(2)]
        jk = A.alloc([D], F32)
        fs = A.alloc([NTR, 2], F32)
        for t in range(NTR):
            b = t % 2
            for k in range(4):
                op('gpsimd', lambda e, t=t, k=k, b=b: e.indirect_dma_start(out=gk[b][k], out_offset=None, in_=y_scr,
                                                                       in_offset=bass.IndirectOffsetOnAxis(ap=sloti[:, 4 * t + k:4 * t + k + 1], axis=0),
                                                                       bounds_check=breg(e, NSLOT - 1), oob_is_err=False),
                   reads=['y_scr'], writes=[f'gk{b}{k}'], dma=f'ga{b}')
            op('sync', lambda e, t=t, b=b: e.dma_start(out=x1l[b], in_=x1_scr[t * 128:(t + 1) * 128, :]), reads=['x1_scr'], writes=[f'x1l{b}'], dma=f'ldx{b}')

            cmb = [lambda e, t=t, b=b: e.tensor_scalar(out=acc, in0=gk[b][0], scalar1=wts[:, t, 0:1], scalar2=None, op0=ALU.mult)]
            for k in range(1, 4):
                cmb.append(lambda e, t=t, b=b, k=k: e.scalar_tensor_tensor(out=acc, in0=gk[b][k], scalar=wts[:, t, k:k + 1], in1=acc, op0=ALU.mult, op1=ALU.add))
            chain('vector', cmb, reads=[f'gk{b}{k}' for k in range(4)], writes=['acc'])
            op('scalar', lambda e, t=t: e.activation(out=jk, in_=acc, func=AF.Square, accum_out=fs[:, t, 0:1]), reads=['acc'], writes=['jk', 'fss'])
            rstd(fs[:, t, 1:2], fs[:, t, 0:1], D, ['fss'], 'fsr')
            op('vector', lambda e, t=t: e.scalar_tensor_tensor(out=acc, in0=acc, scalar=fs[:, t, 1:2], in1=G2, op0=ALU.mult, op1=ALU.mult), reads=['acc', 'fsr'], writes=['acc'])
            op('gpsimd', lambda e, b=b: e.tensor_tensor(out=ot[b], in0=acc, in1=x1l[b], op=ALU.add), reads=['acc', f'x1l{b}'], writes=[f'ot{b}'])
            op('sync', lambda e, t=t, b=b: e.dma_start(out=out[t * 128:(t + 1) * 128, :], in_=ot[b]), reads=[f'ot{b}'], dma=f'sto{b}')
        return finish(nc, S, out, dbg_outs)


def finish(nc, S, out, dbg_outs):
    S.barrier()
    S.emit()
    return nc, dbg_outs


_CACHE = {}


def _host_tables():
    if 'rope' in _CACHE:
        return _CACHE['rope'], _CACHE['tblidx']
    half = 32; nf = 16
    freqs = (10000.0 ** (-np.arange(nf, dtype=np.float32) / nf)).astype(np.float32)
    rope = {}
    for hf in range(2):
        rng_rows = np.arange(28 * hf, 28 * hf + 36)
        rest = np.arange(36, 64) if hf == 0 else np.arange(0, 28)
        rows = np.concatenate([rng_rows, rest])
        tok = (rows[:, None] * 64 + np.arange(64)[None, :]).reshape(-1)
        r = (tok // 64).astype(np.float32); c = (tok % 64).astype(np.float32)
        cosT = np.ones((4352, 64), np.float32); sinT = np.zeros((4352, 64), np.float32)
        for hi, pos in enumerate((r, c)):
            ang = pos[:, None] * freqs[None, :]
            co = np.cos(ang).astype(np.float32); si = np.sin(ang).astype(np.float32)
            cosT[:4096, hi * 32:hi * 32 + 16] = co; cosT[:4096, hi * 32 + 16:hi * 32 + 32] = co
            sinT[:4096, hi * 32:hi * 32 + 16] = -si; sinT[:4096, hi * 32 + 16:hi * 32 + 32] = si
        rope[hf] = (np.ascontiguousarray(np.tile(cosT, (1, 8))), np.ascontiguousarray(np.tile(sinT, (1, 8))), tok)
    qc = np.arange(64)[:, None]; kc = np.arange(64)[None, :]
    c0 = np.clip(qc - 8, 0, 48)
    valid = (kc >= c0) & (kc < c0 + 16)
    off = np.clip(kc - qc + 15, 0, 30)
    _CACHE['rope'] = rope; _CACHE['tblidx'] = (valid, off)
    return rope, (valid, off)


def kernel(x, c, ctx, c_ctx, w_mod, b_mod, g_pre_mix, g_post_mix, g_pre_ffn, g_post_ffn, w_in, rpb, g_qnorm, g_knorm,
           w_out_a, w_out_b, w_o, w_router, b_router, w_gu, b_gu, w_dn, b_dn):
    f = lambda a: np.ascontiguousarray(np.asarray(a, dtype=np.float32))
    x = f(x); ctx = f(ctx); c = f(c); c_ctx = f(c_ctx)
    rope, (valid, off) = _host_tables()
    rp = f(rpb)[0]
    T = rp[:, :, off]
    T = np.where(valid[None, None], T, np.float32(NEG)).astype(np.float32)
    T = T.transpose(0, 2, 1, 3).reshape(4, 2 * 64, 15 * 64)
    w_in0 = f(w_in)[0]
    qb = w_in0[:, 1792:2304].reshape(1024, 2, 4, 64).transpose(0, 2, 1, 3).reshape(1024, 512)
    w_in_p = w_in0.copy(); w_in_p[:, 1792:2304] = qb
    shared = dict(w_mod=f(w_mod)[0], b_mod=f(b_mod)[0], gvec=np.stack([f(g_pre_mix)[0], f(g_post_mix)[0], f(g_pre_ffn)[0], f(g_post_ffn)[0]]),
                  w_in=w_in_p, tbl=np.ascontiguousarray(T), gqk=np.stack([f(g_qnorm)[0], f(g_knorm)[0]]),
                  w_oa=f(w_out_a)[0], w_ob=f(w_out_b)[0], w_o=f(w_o)[0], w_r=f(w_router)[0], b_r=f(b_router)[0],
                  w_gu=f(w_gu)[0], b_gu=f(b_gu)[0], w_dn=f(w_dn)[0], b_dn=f(b_dn)[0])
    in_maps = []
    for core in range(8):
        b, hf = core // 2, core % 2
        cosT, sinT, tok = rope[hf]
        xcore = np.concatenate([x[b][tok], ctx[b]], axis=0)
        m = dict(shared)
        m.update(xc=np.ascontiguousarray(xcore), cvec=np.stack([c[b], c_ctx]), ropec=cosT, ropes=sinT)
        in_maps.append(m)
    key = ('nc', STAGE, tuple(DEBUG))
    if key not in _CACHE:
        _CACHE[key] = build()
    nc, dbg = _CACHE[key]
    res = run_bass_kernel_spmd(nc, in_maps, core_ids=list(range(8)))
    _CACHE['last'] = res
    outp = np.empty((4, 4096, 1024), np.float32)
    for core in range(8):
        b, hf = core // 2, core % 2
        o = res.results[core]["out"]
        if hf == 0:
            outp[b, 0:2048] = o[0:2048]
        else:
            outp[b, 2048:4096] = o[256:2304]
    return outp
```

```python
import numpy as np
from contextlib import ExitStack
import concourse.bass as bass
import concourse.mybir as mybir
from concourse.bass_utils import run_bass_kernel_spmd

F32 = mybir.dt.float32; BF16 = mybir.dt.bfloat16; I32 = mybir.dt.int32; U8 = mybir.dt.uint8
AF = mybir.ActivationFunctionType; ALU = mybir.AluOpType; AX = mybir.AxisListType
ENG = ('tensor', 'vector', 'scalar', 'gpsimd', 'sync')
DSZ = {F32: 4, BF16: 2, I32: 4, U8: 1}

D = 1024; NTR = 18; TOKR = 2304; NTALL = 34; NKEY = 4352; NE = 32
NBLK = 104; NSLOT = NBLK * 128
EPS = 1e-6; NEG = -30000.0; BIG = 1.0e6
STAGE = 99
SAME_ENG_SYNC = True
DEBUG = []


class Sched:
    def __init__(self, nc, stack):
        self.nc = nc; self.stack = stack
        self.ops = {e: [] for e in ENG}
        self.sems = {}; self.cnt = {}
        self.last_write = {}; self.readers = {}
        self.waited = {e: {} for e in ENG}

    def sem(self, name):
        if name not in self.sems:
            self.sems[name] = self.stack.enter_context(self.nc.semaphore(name)); self.cnt[name] = 0
        return self.sems[name]

    def op(self, eng, fn, reads=(), writes=(), dma=None):
        waits = {}
        isdma_op = dma is not None

        def need(tok):
            if tok is None:
                return
            sname, val, teng, isdma = tok
            if teng == eng and not isdma and not isdma_op and (eng == 'tensor' or not SAME_ENG_SYNC):
                return
            if self.waited[eng].get(sname, 0) >= val:
                return
            waits[sname] = max(waits.get(sname, 0), val)
        for b in reads:
            need(self.last_write.get(b))
        for b in writes:
            need(self.last_write.get(b))
            for r in self.readers.get(b, ()):
                need(r)
        for s, v in waits.items():
            self.waited[eng][s] = v
        if isdma_op:
            sname = dma; inc = 16
        else:
            sname = 'e_' + eng; inc = 1
        self.sem(sname); self.cnt[sname] += inc
        tok = (sname, self.cnt[sname], eng, isdma_op)
        for b in writes:
            self.last_write[b] = tok; self.readers[b] = []
        for b in reads:
            self.readers.setdefault(b, []).append(tok)
        self.ops[eng].append((list(waits.items()), fn, sname, inc))
        return tok

    def barrier(self):
        for e in ENG:
            waits = []
            for s, c in self.cnt.items():
                if c > 0 and self.waited[e].get(s, 0) < c and s != 'e_' + e:
                    waits.append((s, c)); self.waited[e][s] = c
            if waits:
                self.ops[e].append((waits, None, None, None))
        self.last_write = {}; self.readers = {}

    def emit(self):
        with self.nc.Block() as block:
            for eng in ENG:
                ops = self.ops[eng]
                if not ops:
                    continue

                def body(e, ops=ops):
                    for waits, fn, sname, inc in ops:
                        for s, v in waits:
                            e.wait_ge(self.sems[s], v)
                        if fn is not None:
                            fn(e).then_inc(self.sems[sname], inc)
                getattr(block, eng)(body)


class Arena:
    def __init__(self, nc, st, name, nbytes):
        self.t = st.enter_context(nc.sbuf_tensor(name, [128, nbytes], U8)); self.off = 0; self.n = nbytes; self.name = name

    def alloc(self, free_shape, dt):
        n = int(np.prod(free_shape)) * DSZ[dt]
        n_al = (n + 63) // 64 * 64
        assert self.off + n_al <= self.n, (self.name, self.off, n_al, self.n)
        ap = self.t[:, self.off:self.off + n].bitcast(dt)
        self.off += n_al
        if len(free_shape) == 2:
            ap = ap.rearrange("p (a b) -> p a b", a=free_shape[0])
        elif len(free_shape) == 3:
            ap = ap.rearrange("p (a b c) -> p a b c", a=free_shape[0], b=free_shape[1])
        return ap

    def reset(self, off=0):
        self.off = off


def build():
    nc = bass.Bass("TRN2", target_bir_lowering=False)
    dt_in = lambda name, shape, dt=F32: nc.dram_tensor(name, shape, dt, kind="ExternalInput").ap()
    xc = dt_in("xc", [NKEY, D]); cvec = dt_in("cvec", [2, D]); w_mod = dt_in("w_mod", [D, 6 * D]); b_mod = dt_in("b_mod", [6 * D])
    gvec = dt_in("gvec", [4, D]); w_in = dt_in("w_in", [D, 4352]); tbl = dt_in("tbl", [4, 128, 960]); gqk = dt_in("gqk", [2, 64])
    ropec = dt_in("ropec", [NKEY, 512]); ropes = dt_in("ropes", [NKEY, 512])
    w_oa = dt_in("w_oa", [512, D]); w_ob = dt_in("w_ob", [512, D]); w_o = dt_in("w_o", [D, D])
    w_r = dt_in("w_r", [D, NE]); b_r = dt_in("b_r", [NE])
    w_gu = dt_in("w_gu", [NE, D, 2 * D]); b_gu = dt_in("b_gu", [NE, 2 * D]); w_dn = dt_in("w_dn", [NE, D, D]); b_dn = dt_in("b_dn", [NE, D])
    out = nc.dram_tensor("out", [TOKR, D], F32, kind="ExternalOutput").ap()
    hT_scr = nc.dram_tensor("hT_scr", [128, 8, NKEY + 128], BF16, kind="Internal").ap()
    x1_scr = nc.dram_tensor("x1_scr", [TOKR, D], F32, kind="Internal").ap()
    h2_scr = nc.dram_tensor("h2_scr", [TOKR, D], BF16, kind="Internal").ap()
    xs_scr = nc.dram_tensor("xs_scr", [NSLOT, D], BF16, kind="Internal").ap()
    y_scr = nc.dram_tensor("y_scr", [NSLOT, D], F32, kind="Internal").ap()
    dbg_outs = {}
    REG = {}

    def breg(e, v):
        if v not in REG:
            REG[v] = e.to_reg(v)
        return REG[v]

    with ExitStack() as st:
        S = Sched(nc, st)
        op = S.op

        def chain(eng, fns, reads=(), writes=()):
            for fn_ in fns:
                op(eng, fn_, reads=list(reads), writes=list(writes))
        A = Arena(nc, st, "arena", 182 * 1024)
        P = Arena(nc, st, "persist", 24 * 1024)
        pq = [st.enter_context(nc.psum_tensor(f"pq{i}", [128, 1024], F32)) for i in range(3)]
        pbf = [st.enter_context(nc.psum_tensor(f"pbf{i}", [128, 1024], BF16)) for i in range(2)]
        pq = [t_[:, :] for t_ in pq]; pbf = [t_[:, :] for t_ in pbf]
        pf = [pq[i // 2][:, (i % 2) * 512:(i % 2) * 512 + 512] for i in range(6)]
        PF = [f"pf{i}" for i in range(6)]; PB = ["pb0", "pb1"]

        def dump(name, ap, shape, dt):
            if name not in DEBUG:
                return
            S.barrier()
            o = nc.dram_tensor("dbg_" + name, shape, dt, kind="ExternalOutput").ap()
            dbg_outs[name] = o
            op('sync', lambda e: e.dma_start(out=o, in_=ap), dma='dbg')

        rows_late = P.alloc([4, D], F32)
        ident = P.alloc([128], BF16)
        identf = P.alloc([128], F32)
        ones_bf = P.alloc([128], BF16)
        ones_f = P.alloc([128], F32)
        G1, A2, B2, G2 = rows_late[:, 0, :], rows_late[:, 1, :], rows_late[:, 2, :], rows_late[:, 3, :]

        chain('gpsimd', [
            lambda e: e.memset(identf, 0.0),
            lambda e: e.affine_select(out=identf, in_=identf, pattern=[[-1, 128]], compare_op=ALU.not_equal, fill=1.0, base=0, channel_multiplier=1),
            lambda e: e.memset(ones_f, 1.0),
            lambda e: e.tensor_copy(out=ones_bf, in_=ones_f),
            lambda e: e.tensor_copy(out=ident, in_=identf)], writes=['const'])

        A.reset()
        rows_early = A.alloc([4, D], F32)
        A1, B1, A1c, B1c = rows_early[:, 0, :], rows_early[:, 1, :], rows_early[:, 2, :], rows_early[:, 3, :]
        mark_p1 = A.off
        modB = A.alloc([6 * D], F32); modC = A.alloc([2 * D], F32)
        gB = A.alloc([4, D], F32)
        bmB = A.alloc([6 * D], F32)
        cT = A.alloc([2, 8], F32); sT = A.alloc([2, 8], F32)
        rep = A.alloc([2, 8, 128], BF16)
        wm = [A.alloc([8, 512], BF16) for _ in range(2)]
        op('sync', lambda e: e.dma_start(out=cT, in_=cvec.rearrange("j (k p) -> p j k", p=128), allow_slow_non_contiguous=True), writes=['cT'], dma='d_cT')
        for i in range(4):
            op('sync', lambda e, i=i: e.dma_start(out=gB[:, i, :], in_=gvec[i, :].partition_broadcast(128)), writes=['gB'], dma='d_gB')
        op('sync', lambda e: e.dma_start(out=bmB, in_=b_mod.partition_broadcast(128)), writes=['bmB'], dma='d_bmB')
        op('scalar', lambda e: e.activation(out=sT, in_=cT, func=AF.Silu), reads=['cT'], writes=['sT'])

        def mk_rep(e):
            for j in range(2):
                for k in range(8):
                    r = e.tensor_scalar(out=rep[:, j, k, :], in0=ones_f, scalar1=sT[:, j, k:k + 1], scalar2=None, op0=ALU.mult)
            return r
        op('vector', mk_rep, reads=['sT', 'const'], writes=['rep'])
        for n in range(12):
            wb = wm[n % 2]
            op('gpsimd', lambda e, n=n, wb=wb: e.dma_start(out=wb, in_=w_mod[:, n * 512:(n + 1) * 512].rearrange("(k p) c -> p k c", p=128)),
               writes=[f'wm{n % 2}'], dma=f'ld_wm{n % 2}')
            for j in range(2 if n < 4 else 1):
                bk = (2 * n + j) % 6

                def mm(e, j=j, wb=wb, bk=bk):
                    for k in range(8):
                        r = e.matmul(pf[bk], lhsT=rep[:, j, k, :], rhs=wb[:, k, :], start=(k == 0), stop=(k == 7))
                    return r
                op('tensor', mm, reads=['rep', f'wm{n % 2}'], writes=[PF[bk]])
                dst = (modB if j == 0 else modC)[:, n * 512:(n + 1) * 512]
                op('vector', lambda e, dst=dst, bk=bk, n=n: e.tensor_tensor(out=dst, in0=pf[bk], in1=bmB[:, n * 512:(n + 1) * 512], op=ALU.add),
                   reads=[PF[bk], 'bmB'], writes=['modB'])

        def mk_rows(e):
            e.scalar_tensor_tensor(out=A1, in0=modB[:, D:2 * D], scalar=1.0, in1=gB[:, 0, :], op0=ALU.add, op1=ALU.mult)
            e.tensor_copy(out=B1, in_=modB[:, 0:D])
            e.scalar_tensor_tensor(out=A1c, in0=modC[:, D:2 * D], scalar=1.0, in1=gB[:, 0, :], op0=ALU.add, op1=ALU.mult)
            e.tensor_copy(out=B1c, in_=modC[:, 0:D])
            e.tensor_tensor(out=G1, in0=modB[:, 2 * D:3 * D], in1=gB[:, 1, :], op=ALU.mult)
            e.scalar_tensor_tensor(out=A2, in0=modB[:, 4 * D:5 * D], scalar=1.0, in1=gB[:, 2, :], op0=ALU.add, op1=ALU.mult)
            e.tensor_copy(out=B2, in_=modB[:, 3 * D:4 * D])
            return e.tensor_tensor(out=G2, in0=modB[:, 5 * D:6 * D], in1=gB[:, 3, :], op=ALU.mult)
        op('vector', mk_rows, reads=['modB', 'gB'], writes=['rows'])
        dump('rows_early', rows_early, [128, 4, D], F32)
        S.barrier()

        A.reset(mark_p1)
        xt = [A.alloc([D], F32) for _ in range(2)]
        hn = [A.alloc([D], F32) for _ in range(2)]
        hb = [A.alloc([D], BF16) for _ in range(2)]
        hTt = [A.alloc([8, 128], BF16) for _ in range(2)]
        junk = A.alloc([D], F32)
        ss = A.alloc([NTALL], F32); rs = A.alloc([NTALL], F32)

        def rstd(dst, src, n, reads, key):
            op('vector', lambda e: e.tensor_scalar(out=dst, in0=src, scalar1=1.0 / n, scalar2=EPS, op0=ALU.mult, op1=ALU.add), reads=reads, writes=[key])
            op('scalar', lambda e: e.activation(out=dst, in_=dst, func=AF.Sqrt), reads=[key], writes=[key])
            op('vector', lambda e: e.reciprocal(out=dst, in_=dst), reads=[key], writes=[key])

        for t in range(NTALL):
            b = t % 2
            Ar, Br = (A1, B1) if t < 32 else (A1c, B1c)
            op('sync', lambda e, t=t, b=b: e.dma_start(out=xt[b], in_=xc[t * 128:(t + 1) * 128, :]), writes=[f'xt{b}'], dma=f'ldx{b}')
            op('scalar', lambda e, t=t, b=b: e.activation(out=junk, in_=xt[b], func=AF.Square, accum_out=ss[:, t:t + 1]), reads=[f'xt{b}'], writes=['junk', f'ss{t}'])
            rstd(rs[:, t:t + 1], ss[:, t:t + 1], D, [f'ss{t}'], f'rs{t}')
            op('vector', lambda e, t=t, b=b, Ar=Ar: e.scalar_tensor_tensor(out=hn[b], in0=xt[b], scalar=rs[:, t:t + 1], in1=Ar, op0=ALU.mult, op1=ALU.mult),
               reads=[f'xt{b}', f'rs{t}', 'rows'], writes=[f'hn{b}'])
            op('gpsimd', lambda e, b=b, Br=Br: e.tensor_tensor(out=hb[b], in0=hn[b], in1=Br, op=ALU.add), reads=[f'hn{b}', 'rows'], writes=[f'hb{b}'])

            def tr(e, b=b):
                for k in range(8):
                    r = e.transpose(pbf[b][:, k * 128:(k + 1) * 128], hb[b][:, k * 128:(k + 1) * 128], ident)
                return r
            op('tensor', tr, reads=[f'hb{b}', 'const'], writes=[PB[b]])
            op('scalar', lambda e, b=b: e.activation(out=hTt[b], in_=pbf[b].rearrange("p (k c) -> p k c", k=8), func=AF.Copy), reads=[PB[b]], writes=[f'hTt{b}'])
            op('sync', lambda e, t=t, b=b: e.dma_start(out=hT_scr[:, :, t * 128:(t + 1) * 128], in_=hTt[b]), reads=[f'hTt{b}'], dma=f'sth{b}')
        S.barrier()
        if STAGE <= 1:
            return finish(nc, S, out, dbg_outs)

        A.reset()
        o_aT = A.alloc([4, TOKR], BF16)
        mark_oa = A.off
        wA = A.alloc([8, 1536], BF16)
        QaT = A.alloc([4, TOKR], BF16); KaT = A.alloc([4, TOKR], BF16)
        Va_e = A.alloc([18, 512], BF16); Va_o = A.alloc([17, 512], BF16)
        KcaT = A.alloc([4, 256], BF16); Vca = A.alloc([2, 512], BF16)
        tblS = A.alloc([4, 960], F32)
        hTg = [A.alloc([8, 576], BF16) for _ in range(2)]
        sbt = [A.alloc([768], F32) for _ in range(2)]
        pbt = [A.alloc([768], BF16) for _ in range(2)]
        pnt = [A.alloc([768], BF16) for _ in range(2)]
        pTt = [A.alloc([768], BF16) for _ in range(2)]
        sm = A.alloc([2, 4], F32)
        for i, (c0, nm) in enumerate(((0, 'ka'), (512, 'va'), (1280, 'qa'))):
            op('gpsimd', lambda e, i=i, c0=c0: e.dma_start(out=wA[:, :, i * 512:(i + 1) * 512], in_=w_in[:, c0:c0 + 512].rearrange("(k p) c -> p k c", p=128)),
               writes=['wA'], dma='d_wA')
        for p in range(4):
            op('sync', lambda e, p=p: e.dma_start(out=tblS[:, p, :], in_=tbl[p]), writes=['tbl'], dma='d_tbl')
        bkc = [0]

        def nbk():
            bkc[0] = (bkc[0] + 1) % 6
            return bkc[0]

        def proj_fm(lhs_cols, rhs_ap, ntok, dst, scale=None):
            bk = nbk()

            def mm(e):
                for k in range(8):
                    r = e.matmul(pf[bk][:, 0:ntok], lhsT=wA[:, k, lhs_cols[0]:lhs_cols[1]], rhs=rhs_ap(k), start=(k == 0), stop=(k == 7))
                return r
            op('tensor', mm, reads=['wA', 'hTg'], writes=[PF[bk]])
            if scale is None:
                op('scalar', lambda e: e.activation(out=dst, in_=pf[bk][:, 0:ntok], func=AF.Copy), reads=[PF[bk]], writes=['naprep'])
            else:
                op('scalar', lambda e: e.activation(out=dst, in_=pf[bk][:, 0:ntok], func=AF.Copy, scale=scale), reads=[PF[bk]], writes=['naprep'])

        def proj_tm(lhs_ap, dst):
            bk = nbk()

            def mm(e):
                for k in range(8):
                    r = e.matmul(pf[bk], lhsT=lhs_ap(k), rhs=wA[:, k, 512:1024], start=(k == 0), stop=(k == 7))
                return r
            op('tensor', mm, reads=['wA', 'hTg'], writes=[PF[bk]])
            op('vector', lambda e: e.tensor_copy(out=dst, in_=pf[bk]), reads=[PF[bk]], writes=['naprep'])

        for g in range(5):
            hg = hTg[g % 2]
            ntok = 512 if g < 4 else 256
            op('sync', lambda e, g=g, hg=hg: e.dma_start(out=hg, in_=hT_scr[:, :, g * 512:g * 512 + 576]), reads=['hT_scr'], writes=['hTg'], dma=f'ldh{g % 2}')
            for c in range(4):
                proj_fm((c * 128, (c + 1) * 128), lambda k, hg=hg, ntok=ntok: hg[:, k, 0:ntok], ntok, KaT[:, c, g * 512:g * 512 + ntok])
                proj_fm((1024 + c * 128, 1024 + (c + 1) * 128), lambda k, hg=hg, ntok=ntok: hg[:, k, 0:ntok], ntok, QaT[:, c, g * 512:g * 512 + ntok], scale=0.125)
            for j in range(ntok // 128):
                proj_tm(lambda k, hg=hg, j=j: hg[:, k, j * 128:(j + 1) * 128], Va_e[:, 4 * g + j, :])
                if 4 * g + j <= 16:
                    proj_tm(lambda k, hg=hg, j=j: hg[:, k, 64 + j * 128:64 + (j + 1) * 128], Va_o[:, 4 * g + j, :])
        hg = hTg[1]
        op('sync', lambda e, hg=hg: e.dma_start(out=hg[:, :, 0:256], in_=hT_scr[:, :, 4096:4352]), reads=['hT_scr'], writes=['hTg'], dma='ldh1')
        for c in range(4):
            proj_fm((c * 128, (c + 1) * 128), lambda k, hg=hg: hg[:, k, 0:256], 256, KcaT[:, c, :])
        for j in range(2):
            proj_tm(lambda k, hg=hg, j=j: hg[:, k, j * 128:(j + 1) * 128], Vca[:, j, :])
        dump('QaT', QaT, [128, 4, TOKR], BF16); dump('KaT', KaT, [128, 4, TOKR], BF16); dump('Va_e', Va_e, [128, 18, 512], BF16)

        na_its = [(l, p) for l in range(36) for p in range(4)]

        def na_ctx(it):
            l, p = na_its[it]
            start = min(max(l - 4, 0), 28); u0 = start - l + 7; tok0 = start * 64
            b = it % 2
            return l, p, start, u0, tok0, b

        def na_stage1(it):
            l, p, start, u0, tok0, b = na_ctx(it)
            sl, sc, po = pf[b], pf[2 + b], pf[4 + b]
            sb_, pb_, pn_, pT_ = sbt[b], pbt[b], pnt[b], pTt[b]

            def qk(e, l=l, p=p, tok0=tok0, sl=sl, sc=sc):
                for hh in range(2):
                    ps_ = slice(hh * 64, hh * 64 + 64)
                    e.matmul(sl[ps_, :], lhsT=QaT[ps_, p, l * 64:(l + 1) * 64], rhs=KaT[ps_, p, tok0:tok0 + 512], start=True, stop=True, tile_position=(hh * 64, hh * 64))
                    r = e.matmul(sc[ps_, 0:256], lhsT=QaT[ps_, p, l * 64:(l + 1) * 64], rhs=KcaT[ps_, p, :], start=True, stop=True, tile_position=(hh * 64, hh * 64))
                return r
            op('tensor', qk, reads=['naprep'], writes=[PF[b], PF[2 + b]])
            op('vector', lambda e, sb_=sb_, sl=sl, p=p, u0=u0: e.tensor_tensor(out=sb_[:, 0:512], in0=sl, in1=tblS[:, p, u0 * 64:u0 * 64 + 512], op=ALU.add),
               reads=[PF[b], 'tbl'], writes=[f'sbA{b}'])
            op('scalar', lambda e, sb_=sb_, sc=sc: e.activation(out=sb_[:, 512:768], in_=sc[:, 0:256], func=AF.Copy), reads=[PF[2 + b]], writes=[f'sbB{b}'])

            chain('vector', [
                lambda e, sb_=sb_, b=b: e.tensor_reduce(out=sm[:, b, 0:1], in_=sb_, axis=AX.X, op=ALU.max),
                lambda e, b=b: e.tensor_scalar(out=sm[:, b, 1:2], in0=sm[:, b, 0:1], scalar1=-1.0, scalar2=None, op0=ALU.mult)],
                reads=[f'sbA{b}', f'sbB{b}'], writes=[f'negm{b}'])
            op('scalar', lambda e, sb_=sb_, pb_=pb_, b=b: e.activation(out=pb_, in_=sb_, func=AF.Exp, bias=sm[:, b, 1:2], scale=1.0, accum_out=sm[:, b, 2:3]),
               reads=[f'sbA{b}', f'sbB{b}', f'negm{b}'], writes=[f'pb{b}', f'sum{b}'])
            op('vector', lambda e, b=b: e.reciprocal(out=sm[:, b, 3:4], in_=sm[:, b, 2:3]), reads=[f'sum{b}'], writes=[f'rsum{b}'])
            op('gpsimd', lambda e, pn_=pn_, pb_=pb_, b=b: e.tensor_scalar(out=pn_, in0=pb_, scalar1=sm[:, b, 3:4], scalar2=None, op0=ALU.mult),
               reads=[f'pb{b}', f'rsum{b}'], writes=[f'pn{b}'])


        def na_stage2(it):
            l, p, start, u0, tok0, b = na_ctx(it)
            sl, sc, po = pf[b], pf[2 + b], pf[4 + b]
            sb_, pb_, pn_, pT_ = sbt[b], pbt[b], pnt[b], pTt[b]
            def trp(e, pn_=pn_, b=b):
                for c in range(6):
                    r = e.transpose(pbf[b][:, c * 128:(c + 1) * 128], pn_[:, c * 128:(c + 1) * 128], ident)
                return r
            op('tensor', trp, reads=[f'pn{b}', 'const'], writes=[PB[b]])
            op('scalar', lambda e, pT_=pT_, b=b: e.activation(out=pT_, in_=pbf[b][:, 0:768], func=AF.Copy), reads=[PB[b]], writes=[f'pT{b}'])

            def pv(e, pT_=pT_, po=po, p=p, start=start):
                for hh in range(2):
                    for c in range(6):
                        if c < 4:
                            V = Va_e[:, start // 2 + c, :] if start % 2 == 0 else Va_o[:, (start - 1) // 2 + c, :]
                        else:
                            V = Vca[:, c - 4, :]
                        r = e.matmul(po[hh * 64:hh * 64 + 64, 0:64], lhsT=V[:, p * 128 + hh * 64:p * 128 + hh * 64 + 64],
                                     rhs=pT_[:, c * 128 + hh * 64:c * 128 + hh * 64 + 64], start=(c == 0), stop=(c == 5), tile_position=(0, hh * 64))
                return r
            op('tensor', pv, reads=[f'pT{b}', 'naprep'], writes=[PF[4 + b]])
            op('vector', lambda e, po=po, p=p, l=l: e.tensor_copy(out=o_aT[:, p, l * 64:(l + 1) * 64], in_=po[:, 0:64]), reads=[PF[4 + b]], writes=['o_aT'])

        for step in range(len(na_its) + 1):
            if step < len(na_its):
                na_stage1(step)
            if step >= 1:
                na_stage2(step - 1)
        dump('o_aT', o_aT, [128, 4, TOKR], BF16)
        S.barrier()
        if STAGE <= 2:
            return finish(nc, S, out, dbg_outs)

        A.reset(mark_oa)
        o_bT = A.alloc([8, TOKR], BF16)
        mark_ob = A.off
        wB = A.alloc([8, 768], BF16)
        QbT = A.alloc([4, TOKR], BF16); KbT = A.alloc([NKEY], BF16)
        Vb = A.alloc([NTALL, 2, 65], BF16)
        gqB = A.alloc([2, 64], F32)
        gtmp = A.alloc([2, 64], F32)
        negC = A.alloc([4], F32)
        hTg = [A.alloc([8, 512], BF16) for _ in range(2)]
        rc = [A.alloc([512], F32) for _ in range(2)]; rsn = [A.alloc([512], F32) for _ in range(2)]
        sq = A.alloc([640], F32); ssh = A.alloc([2, 16], F32)
        qn = A.alloc([640], F32); t1 = A.alloc([640], F32); t2 = A.alloc([640], F32)
        qbb = [A.alloc([640], BF16) for _ in range(2)]
        pTg = [A.alloc([512], BF16) for _ in range(4)]
        osb = [A.alloc([512], F32) for _ in range(2)]
        rec = [A.alloc([512], F32) for _ in range(2)]
        op('gpsimd', lambda e: e.dma_start(out=wB[:, :, 0:256], in_=w_in[:, 1024:1280].rearrange("(k p) c -> p k c", p=128)), writes=['wB'], dma='d_wB')
        op('gpsimd', lambda e: e.dma_start(out=wB[:, :, 256:768], in_=w_in[:, 1792:2304].rearrange("(k p) c -> p k c", p=128)), writes=['wB'], dma='d_wB')
        for i in range(2):
            op('sync', lambda e, i=i: e.dma_start(out=gtmp[:, i, :], in_=gqk[i, :].partition_broadcast(128)), writes=['gtmp'], dma='d_gt')

        qv = qn[:, 0:128].rearrange("p (a b) -> p a b", a=2)
        chain('vector', [
            lambda e: e.tensor_scalar(out=gqB[:, 0, :], in0=gtmp[:, 0, :], scalar1=0.125, scalar2=None, op0=ALU.mult),
            lambda e: e.tensor_copy(out=gqB[:, 1, :], in_=gtmp[:, 1, :]),
            lambda e: e.tensor_scalar(out=qv, in0=gtmp, scalar1=-1.0, scalar2=None, op0=ALU.mult),
            lambda e: e.tensor_tensor(out=gtmp, in0=gtmp, in1=qv, op=ALU.max),
            lambda e: e.tensor_reduce(out=negC[:, 0:1], in_=gtmp[:, 0, :], axis=AX.X, op=ALU.max),
            lambda e: e.tensor_reduce(out=negC[:, 1:2], in_=gtmp[:, 1, :], axis=AX.X, op=ALU.max),
            lambda e: e.tensor_tensor(out=negC[:, 2:3], in0=negC[:, 0:1], in1=negC[:, 1:2], op=ALU.mult),
            lambda e: e.tensor_scalar(out=negC[:, 3:4], in0=negC[:, 2:3], scalar1=-8.0, scalar2=None, op0=ALU.mult),
            lambda e: e.memset(Vb[:, :, :, 64:65], 1.0)], reads=['gtmp'], writes=['gtmp', 'gqB', 'negC', 'Vb', 'qn'])

        def normrope(src, H, gi, b, dst, tagr):
            W = H * 64
            op('scalar', lambda e: e.activation(out=sq[:, 0:W], in_=src, func=AF.Square), reads=tagr, writes=['sq'])

            op('vector', lambda e: e.tensor_reduce(out=ssh[:, 0, 0:H], in_=sq[:, 0:W].rearrange("p (h d) -> p h d", d=64), axis=AX.X, op=ALU.add), reads=['sq'], writes=['ssh0'])
            rstd(ssh[:, 1, 0:H], ssh[:, 0, 0:H], 64, ['ssh0'], 'ssh1')

            def n1(e):
                for h in range(H):
                    r = e.scalar_tensor_tensor(out=qn[:, h * 64:(h + 1) * 64], in0=src[:, h * 64:(h + 1) * 64], scalar=ssh[:, 1, h:h + 1], in1=gqB[:, gi, :],
                                               op0=ALU.mult, op1=ALU.mult)
                return r
            op('vector', n1, reads=['ssh1', 'gqB'] + tagr, writes=['qn'])
            op('vector', lambda e: e.tensor_tensor(out=t1[:, 0:W], in0=qn[:, 0:W], in1=rc[b][:, 0:W], op=ALU.mult), reads=['qn', f'rc{b}'], writes=['t1'])

            def r2(e):
                q4 = qn[:, 0:W].rearrange("p (a s f) -> p a s f", s=2, f=16)
                s4 = rsn[b][:, 0:W].rearrange("p (a s f) -> p a s f", s=2, f=16)
                o4 = t2[:, 0:W].rearrange("p (a s f) -> p a s f", s=2, f=16)
                e.tensor_tensor(out=o4[:, :, 0, :], in0=q4[:, :, 1, :], in1=s4[:, :, 0, :], op=ALU.mult)
                return e.tensor_tensor(out=o4[:, :, 1, :], in0=q4[:, :, 0, :], in1=s4[:, :, 1, :], op=ALU.mult)
            op('vector', r2, reads=['qn', f'rsn{b}'], writes=['t2'])
            op('vector', lambda e: e.tensor_tensor(out=dst, in0=t1[:, 0:W], in1=t2[:, 0:W], op=ALU.add), reads=['t1', 't2'], writes=['qbb'])

        for t in range(NTALL):
            b = t % 2
            g = t // 4
            hg = hTg[g % 2]
            if t % 4 == 0:
                n = min(512, NKEY - g * 512)
                op('sync', lambda e, g=g, hg=hg, n=n: e.dma_start(out=hg[:, :, 0:n], in_=hT_scr[:, :, g * 512:g * 512 + n]), reads=['hT_scr'], writes=[f'hTg{g % 2}'], dma=f'ldh{g % 2}')
            j = t % 4
            op('sync', lambda e, t=t, b=b: e.dma_start(out=rc[b], in_=ropec[t * 128:(t + 1) * 128, :]), writes=[f'rc{b}'], dma=f'ldr{b}')
            op('sync', lambda e, t=t, b=b: e.dma_start(out=rsn[b], in_=ropes[t * 128:(t + 1) * 128, :]), writes=[f'rsn{b}'], dma=f'lds{b}')
            bk = nbk()

            def mmkv(e, hg=hg, j=j, bk=bk):
                for k in range(8):
                    r = e.matmul(pf[bk][:, 0:256], lhsT=hg[:, k, j * 128:(j + 1) * 128], rhs=wB[:, k, 0:256], start=(k == 0), stop=(k == 7))
                return r
            op('tensor', mmkv, reads=['wB', f'hTg{g % 2}'], writes=[PF[bk]])
            op('scalar', lambda e, t=t, bk=bk: e.activation(out=Vb[:, t, :, 0:64], in_=pf[bk][:, 128:256].rearrange("p (h d) -> p h d", d=64), func=AF.Copy),
               reads=[PF[bk]], writes=['Vb'])
            normrope(pf[bk][:, 0:128], 2, 1, b, qbb[b][:, 0:128], [PF[bk]])
            op('tensor', lambda e, b=b: e.transpose(pbf[b][:, 0:128], qbb[b][:, 0:128], ident), reads=['qbb', 'const'], writes=[PB[b]])
            op('scalar', lambda e, t=t, b=b: e.activation(out=KbT[:, t * 128:(t + 1) * 128], in_=pbf[b][:, 0:128], func=AF.Copy), reads=[PB[b]], writes=['KbT'])
            if t < NTR:
                bk2 = nbk()

                def mmq(e, hg=hg, j=j, bk2=bk2):
                    for k in range(8):
                        r = e.matmul(pf[bk2], lhsT=hg[:, k, j * 128:(j + 1) * 128], rhs=wB[:, k, 256:768], start=(k == 0), stop=(k == 7))
                    return r
                op('tensor', mmq, reads=['wB', f'hTg{g % 2}'], writes=[PF[bk2]])
                normrope(pf[bk2], 8, 0, b, qbb[b][:, 0:512], [PF[bk2]])

                def trq(e, b=b):
                    for gg in range(4):
                        r = e.transpose(pbf[b][:, 128 + gg * 128:128 + (gg + 1) * 128], qbb[b][:, gg * 128:(gg + 1) * 128], ident)
                    return r
                op('tensor', trq, reads=['qbb', 'const'], writes=[PB[b]])
                op('scalar', lambda e, t=t, b=b: e.activation(out=QbT[:, :, t * 128:(t + 1) * 128], in_=pbf[b][:, 128:640].rearrange("p (g c) -> p g c", g=4), func=AF.Copy),
                   reads=[PB[b]], writes=['QbT'])
        dump('QbT', QbT, [128, 4, TOKR], BF16); dump('KbT', KbT, [128, NKEY], BF16); dump('Vb', Vb, [128, NTALL, 2, 65], BF16)

        chunks = [(kvh, qt, c) for kvh in range(2) for qt in range(NTR) for c in range(NTALL)]
        LA = 2
        pending = []

        def gq_S(i):
            kvh, qt, c = chunks[i]
            pr = slice(kvh * 64, kvh * 64 + 64)
            sb_ = i % 4
            st_ = pf[sb_]
            op('tensor', lambda e: e.matmul(st_, lhsT=KbT[pr, c * 128:(c + 1) * 128], rhs=QbT[pr, :, qt * 128:(qt + 1) * 128],
                                            start=True, stop=True, tile_position=(kvh * 64, 0)),
               reads=['KbT', 'QbT'], writes=[PF[sb_]])
            op('scalar', lambda e: e.activation(out=pTg[sb_], in_=st_, func=AF.Exp, bias=negC[:, 3:4], scale=1.0), reads=[PF[sb_], 'negC'], writes=[f'pTg{sb_}'])

        def gq_PV(i, step):
            kvh, qt, c = chunks[i]
            sb_ = i % 4
            ob = (kvh * NTR + qt) % 2
            po = pf[4 + ob]
            bk = 4 + ob
            op('tensor', lambda e: e.matmul(po[0:65, :], lhsT=Vb[:, c, kvh, :], rhs=pTg[sb_], start=(c == 0), stop=(c == NTALL - 1)),
               reads=[f'pTg{sb_}', 'Vb'], writes=[PF[bk]])
            if c == NTALL - 1:
                op('scalar', lambda e: e.activation(out=osb[ob][0:65, :], in_=po[0:65, :], func=AF.Copy), reads=[PF[bk]], writes=[f'osb{ob}'])
                op('vector', lambda e: e.reciprocal(out=rec[ob][64:65, :], in_=osb[ob][64:65, :]), reads=[f'osb{ob}'], writes=[f'rec{ob}'])

                def fin():
                    op('tensor', lambda e: e.matmul(po[0:64, :], lhsT=ones_f[64:65, 0:64], rhs=rec[ob][64:65, :], start=True, stop=True), reads=[f'rec{ob}', 'const'], writes=[PF[bk]])
                    op('vector', lambda e: e.tensor_tensor(out=o_bT[0:64, kvh * 4:(kvh + 1) * 4, qt * 128:(qt + 1) * 128],
                                                           in0=osb[ob][0:64, :].rearrange("p (g t) -> p g t", g=4),
                                                           in1=po[0:64, :].rearrange("p (g t) -> p g t", g=4), op=ALU.mult),
                       reads=[f'osb{ob}', PF[bk]], writes=['o_bT'])
                pending.append((step + 4, fin))

        for step in range(len(chunks) + LA + 8):
            if step < len(chunks):
                gq_S(step)
            if LA <= step < len(chunks) + LA:
                gq_PV(step - LA, step)
            for due, fn_ in list(pending):
                if due <= step:
                    fn_(); pending.remove((due, fn_))
        assert not pending
        dump('o_bT', o_bT, [128, 8, TOKR], BF16)
        S.barrier()
        if STAGE <= 3:
            return finish(nc, S, out, dbg_outs)

        A.reset(mark_ob)
        wG = A.alloc([8, 2048], BF16); wOA = A.alloc([4, D], BF16); wOB = A.alloc([8, D], BF16); wO = A.alloc([8, D], BF16)
        wR = A.alloc([8, NE], BF16); bR = A.alloc([NE], BF16)
        hTg = [A.alloc([8, 512], BF16)] * 2
        zT = [A.alloc([8, 512], BF16) for _ in range(2)]
        sga = A.alloc([512], F32); sgb = A.alloc([512], F32)
        xt4 = A.alloc([D], F32); x1t = [A.alloc([D], F32) for _ in range(2)]; tmpf = A.alloc([D], F32)
        h2b = [A.alloc([D], BF16) for _ in range(2)]; h2T = A.alloc([8, 128], BF16)
        lg = P.alloc([NTR, NE], F32); mx8 = P.alloc([NTR, 8], F32); posA = P.alloc([NTR, NE], F32)
        wts = P.alloc([NTR, 4], F32); sloti = P.alloc([NTR * 4], I32)
        mask = A.alloc([NE], F32); maskb = A.alloc([NE], BF16); cntp = P.alloc([NE], F32)
        sms = A.alloc([NTR, 8], F32); e4 = A.alloc([4], F32)
        utri = A.alloc([128], BF16); utf = A.alloc([128], F32)
        mark_route = A.off
        for (dst, src, nm) in ((wG[:, :, 0:1024], w_in[:, 2304:3328], 0), (wG[:, :, 1024:2048], w_in[:, 3328:4352], 1), (wO, w_o, 2)):
            op('gpsimd', lambda e, dst=dst, src=src: e.dma_start(out=dst, in_=src.rearrange("(k p) c -> p k c", p=128)), writes=['wM'], dma='d_wM')
        op('gpsimd', lambda e: e.dma_start(out=wOA, in_=w_oa.rearrange("(k p) c -> p k c", p=128)), writes=['wM'], dma='d_wM')
        op('gpsimd', lambda e: e.dma_start(out=wOB[0:64], in_=w_ob.rearrange("(h d) c -> d h c", d=64)), writes=['wM'], dma='d_wM')
        op('gpsimd', lambda e: e.dma_start(out=wR, in_=w_r.rearrange("(k p) c -> p k c", p=128)), writes=['wM'], dma='d_wM')
        op('gpsimd', lambda e: e.dma_start(out=bR[0:1, :], in_=b_r.rearrange("(o n) -> o n", o=1)), writes=['wM'], dma='d_wM')

        chain('gpsimd', [
            lambda e: e.memset(utf, 1.0),
            lambda e: e.affine_select(out=utf, in_=utf, pattern=[[1, 128]], compare_op=ALU.is_gt, fill=0.0, base=0, channel_multiplier=-1),
            lambda e: e.memset(cntp, 0.0),
            lambda e: e.tensor_copy(out=utri, in_=utf)], writes=['utri', 'route'])

        for g in range(5):
            hg = hTg[g % 2]; z = zT[g % 2]
            ntok = 512 if g < 4 else 256
            tk = slice(g * 512, g * 512 + ntok)
            op('sync', lambda e, g=g, hg=hg, ntok=ntok: e.dma_start(out=hg[:, :, 0:ntok], in_=hT_scr[:, :, g * 512:g * 512 + ntok]), reads=['hT_scr'], writes=[f'hTg{g % 2}'], dma=f'ldh{g % 2}')
            for oc in range(8):
                def mm4(e, hg=hg, oc=oc, ntok=ntok, tk=tk):
                    for k in range(8):
                        e.matmul(pf[0][:, 0:ntok], lhsT=wG[:, k, oc * 128:(oc + 1) * 128], rhs=hg[:, k, 0:ntok], start=(k == 0), stop=(k == 7))
                    for k in range(8):
                        e.matmul(pf[1][:, 0:ntok], lhsT=wG[:, k, 1024 + oc * 128:1024 + (oc + 1) * 128], rhs=hg[:, k, 0:ntok], start=(k == 0), stop=(k == 7))
                    for k in range(4):
                        e.matmul(pf[2][:, 0:ntok], lhsT=wOA[:, k, oc * 128:(oc + 1) * 128], rhs=o_aT[:, k, tk], start=(k == 0), stop=(k == 3))
                    for k in range(8):
                        r = e.matmul(pf[3][:, 0:ntok], lhsT=wOB[0:64, k, oc * 128:(oc + 1) * 128], rhs=o_bT[0:64, k, tk], start=(k == 0), stop=(k == 7))
                    return r
                op('tensor', mm4, reads=['wM', f'hTg{g % 2}', 'o_aT', 'o_bT'], writes=[PF[0], PF[1], PF[2], PF[3]])

                def sg(e, ntok=ntok):
                    e.activation(out=sga[:, 0:ntok], in_=pf[0][:, 0:ntok], func=AF.Sigmoid)
                    return e.activation(out=sgb[:, 0:ntok], in_=pf[1][:, 0:ntok], func=AF.Sigmoid)
                op('scalar', sg, reads=[PF[0], PF[1]], writes=['sg'])

                def zz(e, ntok=ntok):
                    e.tensor_tensor(out=sga[:, 0:ntok], in0=sga[:, 0:ntok], in1=pf[2][:, 0:ntok], op=ALU.mult)
                    return e.tensor_tensor(out=sgb[:, 0:ntok], in0=sgb[:, 0:ntok], in1=pf[3][:, 0:ntok], op=ALU.mult)
                op('vector', zz, reads=['sg', PF[2], PF[3]], writes=['sg2'])
                op('gpsimd', lambda e, z=z, oc=oc, ntok=ntok: e.tensor_tensor(out=z[:, oc, 0:ntok], in0=sga[:, 0:ntok], in1=sgb[:, 0:ntok], op=ALU.add),
                   reads=['sg2'], writes=[f'zT{g % 2}', 'sg'])
            for j in range(ntok // 128):
                t = 4 * g + j
                yb = pq[2]
                b = t % 2

                def mmy(e, z=z, j=j, yb=yb):
                    for n in range(2):
                        for k in range(8):
                            r = e.matmul(yb[:, n * 512:(n + 1) * 512], lhsT=z[:, k, j * 128:(j + 1) * 128], rhs=wO[:, k, n * 512:(n + 1) * 512], start=(k == 0), stop=(k == 7))
                    return r
                op('tensor', mmy, reads=['wM', f'zT{g % 2}'], writes=[PF[4], PF[5]])
                op('sync', lambda e, t=t: e.dma_start(out=xt4, in_=xc[t * 128:(t + 1) * 128, :]), writes=['xt'], dma='ldx0')
                op('scalar', lambda e, yb=yb, t=t: e.activation(out=tmpf, in_=yb, func=AF.Square, accum_out=sms[:, t, 0:1]), reads=[PF[4], PF[5]], writes=['tmpf', 'ssy'])
                rstd(sms[:, t, 1:2], sms[:, t, 0:1], D, ['ssy'], 'rsy')
                op('vector', lambda e, yb=yb, t=t: e.scalar_tensor_tensor(out=tmpf, in0=yb, scalar=sms[:, t, 1:2], in1=G1, op0=ALU.mult, op1=ALU.mult),
                   reads=[PF[4], PF[5], 'rsy', 'rows'], writes=['tmpf'])
                op('gpsimd', lambda e, b=b: e.tensor_tensor(out=x1t[b], in0=tmpf, in1=xt4, op=ALU.add), reads=['tmpf', 'xt'], writes=[f'x1t{b}'])
                op('sync', lambda e, t=t, b=b: e.dma_start(out=x1_scr[t * 128:(t + 1) * 128, :], in_=x1t[b]), reads=[f'x1t{b}'], dma=f'stx{b}')
                op('scalar', lambda e, t=t, b=b: e.activation(out=tmpf, in_=x1t[b], func=AF.Square, accum_out=sms[:, t, 2:3]), reads=[f'x1t{b}'], writes=['tmpf', 'ss2'])
                rstd(sms[:, t, 3:4], sms[:, t, 2:3], D, ['ss2'], 'rs2')
                op('vector', lambda e, t=t, b=b: e.scalar_tensor_tensor(out=tmpf, in0=x1t[b], scalar=sms[:, t, 3:4], in1=A2, op0=ALU.mult, op1=ALU.mult),
                   reads=[f'x1t{b}', 'rs2', 'rows'], writes=['tmpf'])
                op('gpsimd', lambda e, b=b: e.tensor_tensor(out=h2b[b], in0=tmpf, in1=B2, op=ALU.add), reads=['tmpf', 'rows'], writes=[f'h2b{b}'])
                op('sync', lambda e, t=t, b=b: e.dma_start(out=h2_scr[t * 128:(t + 1) * 128, :], in_=h2b[b]), reads=[f'h2b{b}'], dma=f'sth{b}')

                def trh(e, b=b):
                    for k in range(8):
                        r = e.transpose(pbf[b][:, k * 128:(k + 1) * 128], h2b[b][:, k * 128:(k + 1) * 128], ident)
                    return r
                op('tensor', trh, reads=[f'h2b{b}', 'const'], writes=[PB[b]])
                op('scalar', lambda e, b=b: e.activation(out=h2T, in_=pbf[b].rearrange("p (k c) -> p k c", k=8), func=AF.Copy), reads=[PB[b]], writes=['h2T'])

                def mml(e):
                    for k in range(8):
                        e.matmul(pf[0][:, 0:NE], lhsT=h2T[:, k, :], rhs=wR[:, k, :], start=(k == 0), stop=False)
                    return e.matmul(pf[0][:, 0:NE], lhsT=ones_bf[0:1, :], rhs=bR[0:1, :], start=False, stop=True)
                op('tensor', mml, reads=['h2T', 'wM', 'const'], writes=[PF[0]])

                chain('vector', [
                    lambda e, t=t: e.tensor_copy(out=lg[:, t, :], in_=pf[0][:, 0:NE]),
                    lambda e, t=t: e.max(out=mx8[:, t, :], in_=lg[:, t, :]),
                    lambda e, t=t: e.tensor_scalar(out=mask, in0=lg[:, t, :], scalar1=mx8[:, t, 3:4], scalar2=None, op0=ALU.is_ge),
                    lambda e: e.tensor_copy(out=maskb, in_=mask),
                    lambda e, t=t: e.tensor_scalar(out=sms[:, t, 4:5], in0=mx8[:, t, 0:1], scalar1=-1.0, scalar2=None, op0=ALU.mult)],
                    reads=[PF[0]], writes=['lg', 'maskb', 'negmx'])
                op('scalar', lambda e, t=t: e.activation(out=e4, in_=mx8[:, t, 0:4], func=AF.Exp, bias=sms[:, t, 4:5], scale=1.0, accum_out=sms[:, t, 5:6]),
                   reads=['lg', 'negmx'], writes=['e4'])

                def mmc(e):
                    e.matmul(pf[1][:, 0:NE], lhsT=utri, rhs=maskb, start=True, stop=True)
                    return e.matmul(pf[1][:, NE:2 * NE], lhsT=ones_bf, rhs=maskb, start=True, stop=True)
                op('tensor', mmc, reads=['maskb', 'utri', 'const'], writes=[PF[1]])

                chain('vector', [
                    lambda e, t=t: e.reciprocal(out=sms[:, t, 6:7], in_=sms[:, t, 5:6]),
                    lambda e, t=t: e.tensor_scalar(out=wts[:, t, :], in0=e4, scalar1=sms[:, t, 6:7], scalar2=None, op0=ALU.mult),
                    lambda e, t=t: e.tensor_tensor(out=posA[:, t, :], in0=pf[1][:, 0:NE], in1=cntp, op=ALU.add),
                    lambda e: e.tensor_tensor(out=cntp, in0=cntp, in1=pf[1][:, NE:2 * NE], op=ALU.add)],
                    reads=['e4', PF[1]], writes=['route'])
        dump('lg', lg, [128, NTR, NE], F32); dump('posA', posA, [128, NTR, NE], F32); dump('cntp', cntp, [128, NE], F32)
        S.barrier()
        if STAGE <= 4:
            return finish(nc, S, out, dbg_outs)

        A.reset()
        ci = A.alloc([NE], I32); padf = A.alloc([NE], F32); padT = A.alloc([128], F32); ltri = A.alloc([NE], F32)
        basef = A.alloc([NE], F32); pend = A.alloc([NE], F32)
        thr = A.alloc([NBLK], F32); EB = A.alloc([NBLK], F32); skp = A.alloc([NBLK], F32)
        idxw_f = A.alloc([NBLK], F32); idxb_f = A.alloc([NBLK], F32); pidx = A.alloc([1], F32)
        idxw = A.alloc([NBLK], I32); idxb = A.alloc([NBLK], I32)
        idxw8_f = A.alloc([8, NBLK], F32); idxw8 = A.alloc([8, NBLK], I32)
        idxd_f = A.alloc([4, NBLK], F32); idxd = A.alloc([4, NBLK], I32); idxd0 = A.alloc([NBLK], F32); pidx4 = A.alloc([1], F32)
        slot2 = A.alloc([NE], F32); slotf = A.alloc([NTR * 4], F32); tmp32 = A.alloc([NE], F32)
        h2l = [A.alloc([D], BF16) for _ in range(2)]

        chain('vector', [
            lambda e: e.tensor_scalar(out=padf, in0=cntp, scalar1=127.0, scalar2=None, op0=ALU.add),
            lambda e: e.tensor_copy(out=ci, in_=padf),
            lambda e: e.tensor_single_scalar(out=ci, in_=ci, scalar=7, op=ALU.arith_shift_right),
            lambda e: e.tensor_single_scalar(out=ci, in_=ci, scalar=7, op=ALU.logical_shift_left),
            lambda e: e.tensor_copy(out=padf, in_=ci)], reads=['route'], writes=['padf'])

        chain('gpsimd', [
            lambda e: e.memset(ltri, 1.0),
            lambda e: e.affine_select(out=ltri, in_=ltri, pattern=[[1, NE]], compare_op=ALU.is_gt, fill=0.0, base=0, channel_multiplier=-1),
            lambda e: e.iota(thr, pattern=[[128, NBLK]], base=0, channel_multiplier=0, allow_small_or_imprecise_dtypes=True),
            lambda e: e.iota(pidx, pattern=[[0, 1]], base=0, channel_multiplier=1, allow_small_or_imprecise_dtypes=True)], writes=['ltri'])
        op('tensor', lambda e: e.transpose(pq[0][0:NE, 0:128], padf, identf), reads=['padf', 'const'], writes=[PF[0]])
        op('vector', lambda e: e.tensor_copy(out=padT[0:NE, :], in_=pq[0][0:NE, 0:128]), reads=[PF[0]], writes=['padT'])
        op('tensor', lambda e: e.matmul(pf[1][:, 0:NE], lhsT=padT[0:NE, :], rhs=ltri[0:NE, :], start=True, stop=True), reads=['padT', 'ltri'], writes=[PF[1]])

        lay2 = [
            lambda e: e.tensor_copy(out=basef, in_=pf[1][:, 0:NE]),
            lambda e: e.tensor_tensor(out=pend, in0=basef, in1=padf, op=ALU.add),
            lambda e: e.memset(EB, 0.0)]
        for ex in range(NE):
            lay2.append(lambda e, ex=ex: e.scalar_tensor_tensor(out=EB, in0=thr, scalar=pend[:, ex:ex + 1], in1=EB, op0=ALU.is_ge, op1=ALU.add))
        lay2 += [
            lambda e: e.tensor_scalar(out=EB, in0=EB, scalar1=float(NE - 1), scalar2=None, op0=ALU.min),
            lambda e: e.memset(skp, 0.0),
            lambda e: e.tensor_tensor(out=skp[:, 1:NBLK], in0=EB[:, 1:NBLK], in1=EB[:, 0:NBLK - 1], op=ALU.is_equal),
            lambda e: e.tensor_scalar(out=skp, in0=skp, scalar1=BIG, scalar2=None, op0=ALU.mult),
            lambda e: e.scalar_tensor_tensor(out=idxw_f, in0=EB, scalar=1024.0, in1=skp, op0=ALU.mult, op1=ALU.add),
            lambda e: e.tensor_scalar(out=idxw_f, in0=idxw_f, scalar1=pidx[:, 0:1], scalar2=None, op0=ALU.add),
            lambda e: e.tensor_tensor(out=idxb_f, in0=EB, in1=skp, op=ALU.add),
            lambda e: e.tensor_copy(out=idxw, in_=idxw_f)]
        for k8 in range(8):
            lay2.append(lambda e, k8=k8: e.tensor_scalar(out=idxw8_f[:, k8, :], in0=idxw_f, scalar1=128.0 * k8, scalar2=None, op0=ALU.add))
        lay2 += [lambda e: e.tensor_copy(out=idxw8, in_=idxw8_f), lambda e: e.tensor_copy(out=idxb, in_=idxb_f)]
        lay2 += [lambda e: e.tensor_scalar(out=pidx4, in0=pidx, scalar1=4.0, scalar2=None, op0=ALU.mult),
                 lambda e: e.scalar_tensor_tensor(out=idxd0, in0=EB, scalar=512.0, in1=skp, op0=ALU.mult, op1=ALU.add),
                 lambda e: e.tensor_scalar(out=idxd0, in0=idxd0, scalar1=pidx4[:, 0:1], scalar2=None, op0=ALU.add)]
        for j4 in range(4):
            lay2.append(lambda e, j4=j4: e.tensor_scalar(out=idxd_f[:, j4, :], in0=idxd0, scalar1=float(j4), scalar2=None, op0=ALU.add))
        lay2 += [lambda e: e.tensor_copy(out=idxd, in_=idxd_f)]
        chain('vector', lay2, reads=[PF[1], 'padf', 'ltri'], writes=['lay'])
        for t in range(NTR):
            b = t % 2
            op('sync', lambda e, t=t, b=b: e.dma_start(out=h2l[b], in_=h2_scr[t * 128:(t + 1) * 128, :]), reads=['h2_scr'], writes=[f'h2l{b}'], dma=f'ldx{b}')

            slf = [lambda e, t=t: e.tensor_tensor(out=slot2, in0=posA[:, t, :], in1=basef, op=ALU.add)]
            for k in range(4):
                slf.append(lambda e, t=t, k=k: e.scalar_tensor_tensor(out=tmp32, in0=lg[:, t, :], scalar=mx8[:, t, k:k + 1], in1=slot2, op0=ALU.is_equal, op1=ALU.mult,
                                                                      accum_out=slotf[:, 4 * t + k:4 * t + k + 1]))
            slf.append(lambda e, t=t: e.tensor_copy(out=sloti[:, 4 * t:4 * t + 4], in_=slotf[:, 4 * t:4 * t + 4]))
            chain('vector', slf, reads=['lay'], writes=[f'sloti{t}', 'slot2'])
            for k in range(4):
                op('gpsimd', lambda e, t=t, k=k, b=b: e.indirect_dma_start(out=xs_scr, out_offset=bass.IndirectOffsetOnAxis(ap=sloti[:, 4 * t + k:4 * t + k + 1], axis=0),
                                                                       in_=h2l[b], in_offset=None, bounds_check=breg(e, NSLOT - 1), oob_is_err=False),
                   reads=[f'sloti{t}', f'h2l{b}'], dma=f'sc{b}')
        dump('sloti', sloti, [128, NTR * 4], I32); dump('idxw', idxw, [128, NBLK], I32); dump('wts', wts, [128, NTR, 4], F32)
        S.barrier()
        if STAGE <= 5:
            return finish(nc, S, out, dbg_outs)

        mark_ex = A.off
        wgu = A.alloc([8, 2 * D], BF16); wdn4 = A.alloc([4, 2 * D], BF16)
        wdn_k = lambda k: wdn4[:, k // 2, (k % 2) * D:(k % 2 + 1) * D]
        wdn_pairs = w_dn.rearrange("e (q two) n -> (e q) (two n)", two=2)
        bgu = A.alloc([2 * D], BF16); bdn = A.alloc([D], BF16)
        xe = [A.alloc([D], BF16) for _ in range(2)]; xT = [A.alloc([8, 128], BF16) for _ in range(2)]
        gs = A.alloc([D], F32); sg_ = A.alloc([D], F32); l1 = A.alloc([D], F32); tt = A.alloc([D], F32)
        actb = A.alloc([D], BF16); aT = A.alloc([8, 128], BF16)
        yo = [A.alloc([D], F32) for _ in range(2)]
        wgu_flat = w_gu.rearrange("e k n -> (e k) n"); wdn_flat = w_dn.rearrange("e k n -> (e k) n")
        wgu_v = bass.AP(tensor=w_gu.tensor, offset=0, ap=[[2 * D, NE * D - 896], [128 * 2 * D, 8], [1, 2 * D]])
        wdn_v = bass.AP(tensor=w_dn.tensor, offset=0, ap=[[D, NE * D - 896], [128 * D, 8], [1, D]])
        gs2 = [gs, A.alloc([D], F32)]; sg2 = [sg_, A.alloc([D], F32)]; l12 = [l1, A.alloc([D], F32)]; tt2 = [tt, A.alloc([D], F32)]
        actb2 = [actb, A.alloc([D], BF16)]; aT2 = [aT, A.alloc([8, 128], BF16)]

        def blk_ldgu(blk):
            b = blk % 2
            ib = bass.IndirectOffsetOnAxis(ap=idxb[:, blk:blk + 1], axis=0)
            for k8 in range(8):
                op('gpsimd', lambda e, k8=k8: e.indirect_dma_start(out=wgu[:, k8, :], out_offset=None, in_=wgu_flat,
                                                                   in_offset=bass.IndirectOffsetOnAxis(ap=idxw8[:, k8, blk:blk + 1], axis=0),
                                                                   bounds_check=breg(e, NE * D - 1), oob_is_err=False),
                   reads=['lay'], writes=[f'wgu{k8}'], dma='ld_wgu')
            op('gpsimd', lambda e: e.indirect_dma_start(out=bgu, out_offset=None, in_=b_gu, in_offset=ib, bounds_check=breg(e, NE - 1), oob_is_err=False),
               reads=['lay'], writes=['bgu'], dma='ld_wgu')
            op('sync', lambda e: e.dma_start(out=xe[b], in_=xs_scr[blk * 128:(blk + 1) * 128, :]), writes=[f'xe{b}'], dma=f'ldx{b}')

        def blk_lddn(blk):
            ib = bass.IndirectOffsetOnAxis(ap=idxb[:, blk:blk + 1], axis=0)
            for j4 in range(4):
                op('gpsimd', lambda e, j4=j4: e.indirect_dma_start(out=wdn4[:, j4, :], out_offset=None, in_=wdn_pairs,
                                                                   in_offset=bass.IndirectOffsetOnAxis(ap=idxd[:, j4, blk:blk + 1], axis=0),
                                                                   bounds_check=breg(e, NE * 512 - 1), oob_is_err=False),
                   reads=['lay'], writes=[f'wdn{j4}'], dma='ld_wdn')
            op('gpsimd', lambda e: e.indirect_dma_start(out=bdn, out_offset=None, in_=b_dn, in_offset=ib, bounds_check=breg(e, NE - 1), oob_is_err=False),
               reads=['lay'], writes=['bdn'], dma='ld_wdn')

        def blk_trx(blk):
            b = blk % 2

            def trx(e):
                for k in range(8):
                    r = e.transpose(pbf[0][:, k * 128:(k + 1) * 128], xe[b][:, k * 128:(k + 1) * 128], ident)
                return r
            op('tensor', trx, reads=[f'xe{b}', 'const'], writes=[PB[0]])
            op('scalar', lambda e: e.activation(out=xT[b], in_=pbf[0].rearrange("p (k c) -> p k c", k=8), func=AF.Copy), reads=[PB[0]], writes=[f'xT{b}'])

        def blk_gu(blk):
            b = blk % 2
            g_, s_, l_, t_, a_ = gs2[b], sg2[b], l12[b], tt2[b], actb2[b]

            for k in range(8):
                def mguk(e, k=k):
                    for n in range(4):
                        r = e.matmul(pf[n], lhsT=xT[b][:, k, :], rhs=wgu[:, k, n * 512:(n + 1) * 512], start=(k == 0), stop=False)
                    return r
                op('tensor', mguk, reads=[f'xT{b}', f'wgu{k}'], writes=[PF[0], PF[1], PF[2], PF[3]])

            def mgub(e):
                for n in range(4):
                    r = e.matmul(pf[n], lhsT=ones_bf[0:1, :], rhs=bgu[0:1, n * 512:(n + 1) * 512], start=False, stop=True)
                return r
            op('tensor', mgub, reads=['bgu', 'const'], writes=[PF[0], PF[1], PF[2], PF[3]])
            op('vector', lambda e: e.tensor_scalar(out=g_, in0=pq[0], scalar1=7.0, scalar2=None, op0=ALU.min), reads=[PF[0], PF[1]], writes=[f'gs{b}'])
            op('scalar', lambda e: e.activation(out=s_, in_=g_, func=AF.Sigmoid, scale=1.702), reads=[f'gs{b}'], writes=[f'sg{b}'])
            op('vector', lambda e: e.tensor_scalar(out=l_, in0=pq[1], scalar1=7.0, scalar2=-7.0, op0=ALU.min, op1=ALU.max), reads=[PF[2], PF[3]], writes=[f'l1{b}'])
            op('vector', lambda e: e.tensor_tensor(out=t_, in0=g_, in1=s_, op=ALU.mult), reads=[f'gs{b}', f'sg{b}'], writes=[f'tt{b}'])
            op('vector', lambda e: e.scalar_tensor_tensor(out=a_, in0=l_, scalar=1.0, in1=t_, op0=ALU.add, op1=ALU.mult), reads=[f'l1{b}', f'tt{b}'], writes=[f'actb{b}'])

        def blk_tra(blk):
            b = blk % 2
            a_ = actb2[b]

            def tra(e):
                for k in range(8):
                    r = e.transpose(pbf[1][:, k * 128:(k + 1) * 128], a_.rearrange("t (p k) -> t k p", k=8)[:, k, :], ident)
                return r
            op('tensor', tra, reads=[f'actb{b}', 'const'], writes=[PB[1]])
            op('scalar', lambda e: e.activation(out=aT2[b], in_=pbf[1].rearrange("p (k c) -> p k c", k=8), func=AF.Copy), reads=[PB[1]], writes=[f'aT{b}'])

        def blk_dn(blk):
            b = blk % 2

            for j4 in range(4):
                def mdnj(e, j4=j4):
                    for k in (2 * j4, 2 * j4 + 1):
                        for n in range(2):
                            r = e.matmul(pf[4 + n], lhsT=aT2[b][:, k, :], rhs=wdn_k(k)[:, n * 512:(n + 1) * 512], start=(k == 0), stop=False)
                    return r
                op('tensor', mdnj, reads=[f'aT{b}', f'wdn{j4}'], writes=[PF[4], PF[5]])

            def mdnb(e):
                for n in range(2):
                    r = e.matmul(pf[4 + n], lhsT=ones_bf[0:1, :], rhs=bdn[0:1, n * 512:(n + 1) * 512], start=False, stop=True)
                return r
            op('tensor', mdnb, reads=['bdn', 'const'], writes=[PF[4], PF[5]])
            op('scalar', lambda e: e.activation(out=yo[b], in_=pq[2], func=AF.Copy), reads=[PF[4], PF[5]], writes=[f'yo{b}'])
            op('sync', lambda e: e.dma_start(out=y_scr[blk * 128:(blk + 1) * 128, :], in_=yo[b]), reads=[f'yo{b}'], dma=f'sty{b}')

        blk_ldgu(0); blk_lddn(0); blk_trx(0)
        for sblk in range(NBLK):
            blk_gu(sblk)
            if sblk >= 1:
                blk_tra(sblk - 1)
            if sblk + 1 < NBLK:
                blk_ldgu(sblk + 1)
                blk_trx(sblk + 1)
            if sblk >= 1:
                blk_dn(sblk - 1)
                blk_lddn(sblk)
        blk_tra(NBLK - 1); blk_dn(NBLK - 1)
        S.barrier()

        A.reset(mark_ex)
        gk = [[A.alloc([D], F32) for _ in range(4)] for _ in range(2)]
        acc = A.alloc([D], F32); x1l = [A.alloc([D], F32) for _ in range(2)]; ot = [A.alloc([D], F32) for _ in range(2)]
        jk = A.alloc([D], F32)
        fs = A.alloc([NTR, 2], F32)
        for t in range(NTR):
            b = t % 2
            for k in range(4):
                op('gpsimd', lambda e, t=t, k=k, b=b: e.indirect_dma_start(out=gk[b][k], out_offset=None, in_=y_scr,
                                                                       in_offset=bass.IndirectOffsetOnAxis(ap=sloti[:, 4 * t + k:4 * t + k + 1], axis=0),
                                                                       bounds_check=breg(e, NSLOT - 1), oob_is_err=False),
                   reads=['y_scr'], writes=[f'gk{b}{k}'], dma=f'ga{b}')
            op('sync', lambda e, t=t, b=b: e.dma_start(out=x1l[b], in_=x1_scr[t * 128:(t + 1) * 128, :]), reads=['x1_scr'], writes=[f'x1l{b}'], dma=f'ldx{b}')

            cmb = [lambda e, t=t, b=b: e.tensor_scalar(out=acc, in0=gk[b][0], scalar1=wts[:, t, 0:1], scalar2=None, op0=ALU.mult)]
            for k in range(1, 4):
                cmb.append(lambda e, t=t, b=b, k=k: e.scalar_tensor_tensor(out=acc, in0=gk[b][k], scalar=wts[:, t, k:k + 1], in1=acc, op0=ALU.mult, op1=ALU.add))
            chain('vector', cmb, reads=[f'gk{b}{k}' for k in range(4)], writes=['acc'])
            op('scalar', lambda e, t=t: e.activation(out=jk, in_=acc, func=AF.Square, accum_out=fs[:, t, 0:1]), reads=['acc'], writes=['jk', 'fss'])
            rstd(fs[:, t, 1:2], fs[:, t, 0:1], D, ['fss'], 'fsr')
            op('vector', lambda e, t=t: e.scalar_tensor_tensor(out=acc, in0=acc, scalar=fs[:, t, 1:2], in1=G2, op0=ALU.mult, op1=ALU.mult), reads=['acc', 'fsr'], writes=['acc'])
            op('gpsimd', lambda e, b=b: e.tensor_tensor(out=ot[b], in0=acc, in1=x1l[b], op=ALU.add), reads=['acc', f'x1l{b}'], writes=[f'ot{b}'])
            op('sync', lambda e, t=t, b=b: e.dma_start(out=out[t * 128:(t + 1) * 128, :], in_=ot[b]), reads=[f'ot{b}'], dma=f'sto{b}')
        return finish(nc, S, out, dbg_outs)


def finish(nc, S, out, dbg_outs):
    S.barrier()
    S.emit()
    return nc, dbg_outs


_CACHE = {}


def _host_tables():
    if 'rope' in _CACHE:
        return _CACHE['rope'], _CACHE['tblidx']
    half = 32; nf = 16
    freqs = (10000.0 ** (-np.arange(nf, dtype=np.float32) / nf)).astype(np.float32)
    rope = {}
    for hf in range(2):
        rng_rows = np.arange(28 * hf, 28 * hf + 36)
        rest = np.arange(36, 64) if hf == 0 else np.arange(0, 28)
        rows = np.concatenate([rng_rows, rest])
        tok = (rows[:, None] * 64 + np.arange(64)[None, :]).reshape(-1)
        r = (tok // 64).astype(np.float32); c = (tok % 64).astype(np.float32)
        cosT = np.ones((4352, 64), np.float32); sinT = np.zeros((4352, 64), np.float32)
        for hi, pos in enumerate((r, c)):
            ang = pos[:, None] * freqs[None, :]
            co = np.cos(ang).astype(np.float32); si = np.sin(ang).astype(np.float32)
            cosT[:4096, hi * 32:hi * 32 + 16] = co; cosT[:4096, hi * 32 + 16:hi * 32 + 32] = co
            sinT[:4096, hi * 32:hi * 32 + 16] = -si; sinT[:4096, hi * 32 + 16:hi * 32 + 32] = si
        rope[hf] = (np.ascontiguousarray(np.tile(cosT, (1, 8))), np.ascontiguousarray(np.tile(sinT, (1, 8))), tok)
    qc = np.arange(64)[:, None]; kc = np.arange(64)[None, :]
    c0 = np.clip(qc - 8, 0, 48)
    valid = (kc >= c0) & (kc < c0 + 16)
    off = np.clip(kc - qc + 15, 0, 30)
    _CACHE['rope'] = rope; _CACHE['tblidx'] = (valid, off)
    return rope, (valid, off)


def kernel(x, c, ctx, c_ctx, w_mod, b_mod, g_pre_mix, g_post_mix, g_pre_ffn, g_post_ffn, w_in, rpb, g_qnorm, g_knorm,
           w_out_a, w_out_b, w_o, w_router, b_router, w_gu, b_gu, w_dn, b_dn):
    f = lambda a: np.ascontiguousarray(np.asarray(a, dtype=np.float32))
    x = f(x); ctx = f(ctx); c = f(c); c_ctx = f(c_ctx)
    rope, (valid, off) = _host_tables()
    rp = f(rpb)[0]
    T = rp[:, :, off]
    T = np.where(valid[None, None], T, np.float32(NEG)).astype(np.float32)
    T = T.transpose(0, 2, 1, 3).reshape(4, 2 * 64, 15 * 64)
    w_in0 = f(w_in)[0]
    qb = w_in0[:, 1792:2304].reshape(1024, 2, 4, 64).transpose(0, 2, 1, 3).reshape(1024, 512)
    w_in_p = w_in0.copy(); w_in_p[:, 1792:2304] = qb
    shared = dict(w_mod=f(w_mod)[0], b_mod=f(b_mod)[0], gvec=np.stack([f(g_pre_mix)[0], f(g_post_mix)[0], f(g_pre_ffn)[0], f(g_post_ffn)[0]]),
                  w_in=w_in_p, tbl=np.ascontiguousarray(T), gqk=np.stack([f(g_qnorm)[0], f(g_knorm)[0]]),
                  w_oa=f(w_out_a)[0], w_ob=f(w_out_b)[0], w_o=f(w_o)[0], w_r=f(w_router)[0], b_r=f(b_router)[0],
                  w_gu=f(w_gu)[0], b_gu=f(b_gu)[0], w_dn=f(w_dn)[0], b_dn=f(b_dn)[0])
    in_maps = []
    for core in range(8):
        b, hf = core // 2, core % 2
        cosT, sinT, tok = rope[hf]
        xcore = np.concatenate([x[b][tok], ctx[b]], axis=0)
        m = dict(shared)
        m.update(xc=np.ascontiguousarray(xcore), cvec=np.stack([c[b], c_ctx]), ropec=cosT, ropes=sinT)
        in_maps.append(m)
    key = ('nc', STAGE, tuple(DEBUG))
    if key not in _CACHE:
        _CACHE[key] = build()
    nc, dbg = _CACHE[key]
    res = run_bass_kernel_spmd(nc, in_maps, core_ids=list(range(8)))
    _CACHE['last'] = res
    outp = np.empty((4, 4096, 1024), np.float32)
    for core in range(8):
        b, hf = core // 2, core % 2
        o = res.results[core]["out"]
        if hf == 0:
            outp[b, 0:2048] = o[0:2048]
        else:
            outp[b, 2048:4096] = o[256:2304]
    return outp
```

```python
import numpy as np
from contextlib import ExitStack
import concourse.bass as bass
import concourse.mybir as mybir
from concourse.bass_utils import run_bass_kernel_spmd

F32 = mybir.dt.float32; BF16 = mybir.dt.bfloat16; I32 = mybir.dt.int32; U8 = mybir.dt.uint8
AF = mybir.ActivationFunctionType; ALU = mybir.AluOpType; AX = mybir.AxisListType
ENG = ('tensor', 'vector', 'scalar', 'gpsimd', 'sync')
DSZ = {F32: 4, BF16: 2, I32: 4, U8: 1}

D = 1024; NTR = 18; TOKR = 2304; NTALL = 34; NKEY = 4352; NE = 32
NBLK = 104; NSLOT = NBLK * 128
EPS = 1e-6; NEG = -30000.0; BIG = 1.0e6
STAGE = 99
SAME_ENG_SYNC = True
DEBUG = []


class Sched:
    def __init__(self, nc, stack):
        self.nc = nc; self.stack = stack
        self.ops = {e: [] for e in ENG}
        self.sems = {}; self.cnt = {}
        self.last_write = {}; self.readers = {}
        self.waited = {e: {} for e in ENG}

    def sem(self, name):
        if name not in self.sems:
            self.sems[name] = self.stack.enter_context(self.nc.semaphore(name)); self.cnt[name] = 0
        return self.sems[name]

    def op(self, eng, fn, reads=(), writes=(), dma=None):
        waits = {}
        isdma_op = dma is not None

        def need(tok):
            if tok is None:
                return
            sname, val, teng, isdma = tok
            if teng == eng and not isdma and not isdma_op and (eng == 'tensor' or not SAME_ENG_SYNC):
                return
            if self.waited[eng].get(sname, 0) >= val:
                return
            waits[sname] = max(waits.get(sname, 0), val)
        for b in reads:
            need(self.last_write.get(b))
        for b in writes:
            need(self.last_write.get(b))
            for r in self.readers.get(b, ()):
                need(r)
        for s, v in waits.items():
            self.waited[eng][s] = v
        if isdma_op:
            sname = dma; inc = 16
        else:
            sname = 'e_' + eng; inc = 1
        self.sem(sname); self.cnt[sname] += inc
        tok = (sname, self.cnt[sname], eng, isdma_op)
        for b in writes:
            self.last_write[b] = tok; self.readers[b] = []
        for b in reads:
            self.readers.setdefault(b, []).append(tok)
        self.ops[eng].append((list(waits.items()), fn, sname, inc))
        return tok

    def barrier(self):
        for e in ENG:
            waits = []
            for s, c in self.cnt.items():
                if c > 0 and self.waited[e].get(s, 0) < c and s != 'e_' + e:
                    waits.append((s, c)); self.waited[e][s] = c
            if waits:
                self.ops[e].append((waits, None, None, None))
        self.last_write = {}; self.readers = {}

    def emit(self):
        with self.nc.Block() as block:
            for eng in ENG:
                ops = self.ops[eng]
                if not ops:
                    continue

                def body(e, ops=ops):
                    for waits, fn, sname, inc in ops:
                        for s, v in waits:
                            e.wait_ge(self.sems[s], v)
                        if fn is not None:
                            fn(e).then_inc(self.sems[sname], inc)
                getattr(block, eng)(body)


class Arena:
    def __init__(self, nc, st, name, nbytes):
        self.t = st.enter_context(nc.sbuf_tensor(name, [128, nbytes], U8)); self.off = 0; self.n = nbytes; self.name = name

    def alloc(self, free_shape, dt):
        n = int(np.prod(free_shape)) * DSZ[dt]
        n_al = (n + 63) // 64 * 64
        assert self.off + n_al <= self.n, (self.name, self.off, n_al, self.n)
        ap = self.t[:, self.off:self.off + n].bitcast(dt)
        self.off += n_al
        if len(free_shape) == 2:
            ap = ap.rearrange("p (a b) -> p a b", a=free_shape[0])
        elif len(free_shape) == 3:
            ap = ap.rearrange("p (a b c) -> p a b c", a=free_shape[0], b=free_shape[1])
        return ap

    def reset(self, off=0):
        self.off = off


def build():
    nc = bass.Bass("TRN2", target_bir_lowering=False)
    dt_in = lambda name, shape, dt=F32: nc.dram_tensor(name, shape, dt, kind="ExternalInput").ap()
    xc = dt_in("xc", [NKEY, D]); cvec = dt_in("cvec", [2, D]); w_mod = dt_in("w_mod", [D, 6 * D]); b_mod = dt_in("b_mod", [6 * D])
    gvec = dt_in("gvec", [4, D]); w_in = dt_in("w_in", [D, 4352]); tbl = dt_in("tbl", [4, 128, 960]); gqk = dt_in("gqk", [2, 64])
    ropec = dt_in("ropec", [NKEY, 512]); ropes = dt_in("ropes", [NKEY, 512])
    w_oa = dt_in("w_oa", [512, D]); w_ob = dt_in("w_ob", [512, D]); w_o = dt_in("w_o", [D, D])
    w_r = dt_in("w_r", [D, NE]); b_r = dt_in("b_r", [NE])
    w_gu = dt_in("w_gu", [NE, D, 2 * D]); b_gu = dt_in("b_gu", [NE, 2 * D]); w_dn = dt_in("w_dn", [NE, D, D]); b_dn = dt_in("b_dn", [NE, D])
    out = nc.dram_tensor("out", [TOKR, D], F32, kind="ExternalOutput").ap()
    hT_scr = nc.dram_tensor("hT_scr", [128, 8, NKEY + 128], BF16, kind="Internal").ap()
    x1_scr = nc.dram_tensor("x1_scr", [TOKR, D], F32, kind="Internal").ap()
    h2_scr = nc.dram_tensor("h2_scr", [TOKR, D], BF16, kind="Internal").ap()
    xs_scr = nc.dram_tensor("xs_scr", [NSLOT, D], BF16, kind="Internal").ap()
    y_scr = nc.dram_tensor("y_scr", [NSLOT, D], F32, kind="Internal").ap()
    dbg_outs = {}
    REG = {}

    def breg(e, v):
        if v not in REG:
            REG[v] = e.to_reg(v)
        return REG[v]

    with ExitStack() as st:
        S = Sched(nc, st)
        op = S.op

        def chain(eng, fns, reads=(), writes=()):
            for fn_ in fns:
                op(eng, fn_, reads=list(reads), writes=list(writes))
        A = Arena(nc, st, "arena", 182 * 1024)
        P = Arena(nc, st, "persist", 24 * 1024)
        pq = [st.enter_context(nc.psum_tensor(f"pq{i}", [128, 1024], F32)) for i in range(3)]
        pbf = [st.enter_context(nc.psum_tensor(f"pbf{i}", [128, 1024], BF16)) for i in range(2)]
        pq = [t_[:, :] for t_ in pq]; pbf = [t_[:, :] for t_ in pbf]
        pf = [pq[i // 2][:, (i % 2) * 512:(i % 2) * 512 + 512] for i in range(6)]
        PF = [f"pf{i}" for i in range(6)]; PB = ["pb0", "pb1"]

        def dump(name, ap, shape, dt):
            if name not in DEBUG:
                return
            S.barrier()
            o = nc.dram_tensor("dbg_" + name, shape, dt, kind="ExternalOutput").ap()
            dbg_outs[name] = o
            op('sync', lambda e: e.dma_start(out=o, in_=ap), dma='dbg')

        rows_late = P.alloc([4, D], F32)
        ident = P.alloc([128], BF16)
        identf = P.alloc([128], F32)
        ones_bf = P.alloc([128], BF16)
        ones_f = P.alloc([128], F32)
        G1, A2, B2, G2 = rows_late[:, 0, :], rows_late[:, 1, :], rows_late[:, 2, :], rows_late[:, 3, :]

        chain('gpsimd', [
            lambda e: e.memset(identf, 0.0),
            lambda e: e.affine_select(out=identf, in_=identf, pattern=[[-1, 128]], compare_op=ALU.not_equal, fill=1.0, base=0, channel_multiplier=1),
            lambda e: e.memset(ones_f, 1.0),
            lambda e: e.tensor_copy(out=ones_bf, in_=ones_f),
            lambda e: e.tensor_copy(out=ident, in_=identf)], writes=['const'])

        A.reset()
        rows_early = A.alloc([4, D], F32)
        A1, B1, A1c, B1c = rows_early[:, 0, :], rows_early[:, 1, :], rows_early[:, 2, :], rows_early[:, 3, :]
        mark_p1 = A.off
        modB = A.alloc([6 * D], F32); modC = A.alloc([2 * D], F32)
        gB = A.alloc([4, D], F32)
        bmB = A.alloc([6 * D], F32)
        cT = A.alloc([2, 8], F32); sT = A.alloc([2, 8], F32)
        rep = A.alloc([2, 8, 128], BF16)
        wm = [A.alloc([8, 512], BF16) for _ in range(2)]
        op('sync', lambda e: e.dma_start(out=cT, in_=cvec.rearrange("j (k p) -> p j k", p=128), allow_slow_non_contiguous=True), writes=['cT'], dma='d_cT')
        for i in range(4):
            op('sync', lambda e, i=i: e.dma_start(out=gB[:, i, :], in_=gvec[i, :].partition_broadcast(128)), writes=['gB'], dma='d_gB')
        op('sync', lambda e: e.dma_start(out=bmB, in_=b_mod.partition_broadcast(128)), writes=['bmB'], dma='d_bmB')
        op('scalar', lambda e: e.activation(out=sT, in_=cT, func=AF.Silu), reads=['cT'], writes=['sT'])

        def mk_rep(e):
            for j in range(2):
                for k in range(8):
                    r = e.tensor_scalar(out=rep[:, j, k, :], in0=ones_f, scalar1=sT[:, j, k:k + 1], scalar2=None, op0=ALU.mult)
            return r
        op('vector', mk_rep, reads=['sT', 'const'], writes=['rep'])
        for n in range(12):
            wb = wm[n % 2]
            op('gpsimd', lambda e, n=n, wb=wb: e.dma_start(out=wb, in_=w_mod[:, n * 512:(n + 1) * 512].rearrange("(k p) c -> p k c", p=128)),
               writes=[f'wm{n % 2}'], dma=f'ld_wm{n % 2}')
            for j in range(2 if n < 4 else 1):
                bk = (2 * n + j) % 6

                def mm(e, j=j, wb=wb, bk=bk):
                    for k in range(8):
                        r = e.matmul(pf[bk], lhsT=rep[:, j, k, :], rhs=wb[:, k, :], start=(k == 0), stop=(k == 7))
                    return r
                op('tensor', mm, reads=['rep', f'wm{n % 2}'], writes=[PF[bk]])
                dst = (modB if j == 0 else modC)[:, n * 512:(n + 1) * 512]
                op('vector', lambda e, dst=dst, bk=bk, n=n: e.tensor_tensor(out=dst, in0=pf[bk], in1=bmB[:, n * 512:(n + 1) * 512], op=ALU.add),
                   reads=[PF[bk], 'bmB'], writes=['modB'])

        def mk_rows(e):
            e.scalar_tensor_tensor(out=A1, in0=modB[:, D:2 * D], scalar=1.0, in1=gB[:, 0, :], op0=ALU.add, op1=ALU.mult)
            e.tensor_copy(out=B1, in_=modB[:, 0:D])
            e.scalar_tensor_tensor(out=A1c, in0=modC[:, D:2 * D], scalar=1.0, in1=gB[:, 0, :], op0=ALU.add, op1=ALU.mult)
            e.tensor_copy(out=B1c, in_=modC[:, 0:D])
            e.tensor_tensor(out=G1, in0=modB[:, 2 * D:3 * D], in1=gB[:, 1, :], op=ALU.mult)
            e.scalar_tensor_tensor(out=A2, in0=modB[:, 4 * D:5 * D], scalar=1.0, in1=gB[:, 2, :], op0=ALU.add, op1=ALU.mult)
            e.tensor_copy(out=B2, in_=modB[:, 3 * D:4 * D])
            return e.tensor_tensor(out=G2, in0=modB[:, 5 * D:6 * D], in1=gB[:, 3, :], op=ALU.mult)
        op('vector', mk_rows, reads=['modB', 'gB'], writes=['rows'])
        dump('rows_early', rows_early, [128, 4, D], F32)
        S.barrier()

        A.reset(mark_p1)
        xt = [A.alloc([D], F32) for _ in range(2)]
        hn = [A.alloc([D], F32) for _ in range(2)]
        hb = [A.alloc([D], BF16) for _ in range(2)]
        hTt = [A.alloc([8, 128], BF16) for _ in range(2)]
        junk = A.alloc([D], F32)
        ss = A.alloc([NTALL], F32); rs = A.alloc([NTALL], F32)

        def rstd(dst, src, n, reads, key):
            op('vector', lambda e: e.tensor_scalar(out=dst, in0=src, scalar1=1.0 / n, scalar2=EPS, op0=ALU.mult, op1=ALU.add), reads=reads, writes=[key])
            op('scalar', lambda e: e.activation(out=dst, in_=dst, func=AF.Sqrt), reads=[key], writes=[key])
            op('vector', lambda e: e.reciprocal(out=dst, in_=dst), reads=[key], writes=[key])

        for t in range(NTALL):
            b = t % 2
            Ar, Br = (A1, B1) if t < 32 else (A1c, B1c)
            op('sync', lambda e, t=t, b=b: e.dma_start(out=xt[b], in_=xc[t * 128:(t + 1) * 128, :]), writes=[f'xt{b}'], dma=f'ldx{b}')
            op('scalar', lambda e, t=t, b=b: e.activation(out=junk, in_=xt[b], func=AF.Square, accum_out=ss[:, t:t + 1]), reads=[f'xt{b}'], writes=['junk', f'ss{t}'])
            rstd(rs[:, t:t + 1], ss[:, t:t + 1], D, [f'ss{t}'], f'rs{t}')
            op('vector', lambda e, t=t, b=b, Ar=Ar: e.scalar_tensor_tensor(out=hn[b], in0=xt[b], scalar=rs[:, t:t + 1], in1=Ar, op0=ALU.mult, op1=ALU.mult),
               reads=[f'xt{b}', f'rs{t}', 'rows'], writes=[f'hn{b}'])
            op('gpsimd', lambda e, b=b, Br=Br: e.tensor_tensor(out=hb[b], in0=hn[b], in1=Br, op=ALU.add), reads=[f'hn{b}', 'rows'], writes=[f'hb{b}'])

            def tr(e, b=b):
                for k in range(8):
                    r = e.transpose(pbf[b][:, k * 128:(k + 1) * 128], hb[b][:, k * 128:(k + 1) * 128], ident)
                return r
            op('tensor', tr, reads=[f'hb{b}', 'const'], writes=[PB[b]])
            op('scalar', lambda e, b=b: e.activation(out=hTt[b], in_=pbf[b].rearrange("p (k c) -> p k c", k=8), func=AF.Copy), reads=[PB[b]], writes=[f'hTt{b}'])
            op('sync', lambda e, t=t, b=b: e.dma_start(out=hT_scr[:, :, t * 128:(t + 1) * 128], in_=hTt[b]), reads=[f'hTt{b}'], dma=f'sth{b}')
        S.barrier()
        if STAGE <= 1:
            return finish(nc, S, out, dbg_outs)

        A.reset()
        o_aT = A.alloc([4, TOKR], BF16)
        mark_oa = A.off
        wA = A.alloc([8, 1536], BF16)
        QaT = A.alloc([4, TOKR], BF16); KaT = A.alloc([4, TOKR], BF16)
        Va_e = A.alloc([18, 512], BF16); Va_o = A.alloc([17, 512], BF16)
        KcaT = A.alloc([4, 256], BF16); Vca = A.alloc([2, 512], BF16)
        tblS = A.alloc([4, 960], F32)
        hTg = [A.alloc([8, 576], BF16) for _ in range(2)]
        sbt = [A.alloc([768], F32) for _ in range(2)]
        pbt = [A.alloc([768], BF16) for _ in range(2)]
        pnt = [A.alloc([768], BF16) for _ in range(2)]
        pTt = [A.alloc([768], BF16) for _ in range(2)]
        sm = A.alloc([2, 4], F32)
        for i, (c0, nm) in enumerate(((0, 'ka'), (512, 'va'), (1280, 'qa'))):
            op('gpsimd', lambda e, i=i, c0=c0: e.dma_start(out=wA[:, :, i * 512:(i + 1) * 512], in_=w_in[:, c0:c0 + 512].rearrange("(k p) c -> p k c", p=128)),
               writes=['wA'], dma='d_wA')
        for p in range(4):
            op('sync', lambda e, p=p: e.dma_start(out=tblS[:, p, :], in_=tbl[p]), writes=['tbl'], dma='d_tbl')
        bkc = [0]

        def nbk():
            bkc[0] = (bkc[0] + 1) % 6
            return bkc[0]

        def proj_fm(lhs_cols, rhs_ap, ntok, dst, scale=None):
            bk = nbk()

            def mm(e):
                for k in range(8):
                    r = e.matmul(pf[bk][:, 0:ntok], lhsT=wA[:, k, lhs_cols[0]:lhs_cols[1]], rhs=rhs_ap(k), start=(k == 0), stop=(k == 7))
                return r
            op('tensor', mm, reads=['wA', 'hTg'], writes=[PF[bk]])
            if scale is None:
                op('scalar', lambda e: e.activation(out=dst, in_=pf[bk][:, 0:ntok], func=AF.Copy), reads=[PF[bk]], writes=['naprep'])
            else:
                op('scalar', lambda e: e.activation(out=dst, in_=pf[bk][:, 0:ntok], func=AF.Copy, scale=scale), reads=[PF[bk]], writes=['naprep'])

        def proj_tm(lhs_ap, dst):
            bk = nbk()

            def mm(e):
                for k in range(8):
                    r = e.matmul(pf[bk], lhsT=lhs_ap(k), rhs=wA[:, k, 512:1024], start=(k == 0), stop=(k == 7))
                return r
            op('tensor', mm, reads=['wA', 'hTg'], writes=[PF[bk]])
            op('vector', lambda e: e.tensor_copy(out=dst, in_=pf[bk]), reads=[PF[bk]], writes=['naprep'])

        for g in range(5):
            hg = hTg[g % 2]
            ntok = 512 if g < 4 else 256
            op('sync', lambda e, g=g, hg=hg: e.dma_start(out=hg, in_=hT_scr[:, :, g * 512:g * 512 + 576]), reads=['hT_scr'], writes=['hTg'], dma=f'ldh{g % 2}')
            for c in range(4):
                proj_fm((c * 128, (c + 1) * 128), lambda k, hg=hg, ntok=ntok: hg[:, k, 0:ntok], ntok, KaT[:, c, g * 512:g * 512 + ntok])
                proj_fm((1024 + c * 128, 1024 + (c + 1) * 128), lambda k, hg=hg, ntok=ntok: hg[:, k, 0:ntok], ntok, QaT[:, c, g * 512:g * 512 + ntok], scale=0.125)
            for j in range(ntok // 128):
                proj_tm(lambda k, hg=hg, j=j: hg[:, k, j * 128:(j + 1) * 128], Va_e[:, 4 * g + j, :])
                if 4 * g + j <= 16:
                    proj_tm(lambda k, hg=hg, j=j: hg[:, k, 64 + j * 128:64 + (j + 1) * 128], Va_o[:, 4 * g + j, :])
        hg = hTg[1]
        op('sync', lambda e, hg=hg: e.dma_start(out=hg[:, :, 0:256], in_=hT_scr[:, :, 4096:4352]), reads=['hT_scr'], writes=['hTg'], dma='ldh1')
        for c in range(4):
            proj_fm((c * 128, (c + 1) * 128), lambda k, hg=hg: hg[:, k, 0:256], 256, KcaT[:, c, :])
        for j in range(2):
            proj_tm(lambda k, hg=hg, j=j: hg[:, k, j * 128:(j + 1) * 128], Vca[:, j, :])
        dump('QaT', QaT, [128, 4, TOKR], BF16); dump('KaT', KaT, [128, 4, TOKR], BF16); dump('Va_e', Va_e, [128, 18, 512], BF16)

        na_its = [(l, p) for l in range(36) for p in range(4)]

        def na_ctx(it):
            l, p = na_its[it]
            start = min(max(l - 4, 0), 28); u0 = start - l + 7; tok0 = start * 64
            b = it % 2
            return l, p, start, u0, tok0, b

        def na_stage1(it):
            l, p, start, u0, tok0, b = na_ctx(it)
            sl, sc, po = pf[b], pf[2 + b], pf[4 + b]
            sb_, pb_, pn_, pT_ = sbt[b], pbt[b], pnt[b], pTt[b]

            def qk(e, l=l, p=p, tok0=tok0, sl=sl, sc=sc):
                for hh in range(2):
                    ps_ = slice(hh * 64, hh * 64 + 64)
                    e.matmul(sl[ps_, :], lhsT=QaT[ps_, p, l * 64:(l + 1) * 64], rhs=KaT[ps_, p, tok0:tok0 + 512], start=True, stop=True, tile_position=(hh * 64, hh * 64))
                    r = e.matmul(sc[ps_, 0:256], lhsT=QaT[ps_, p, l * 64:(l + 1) * 64], rhs=KcaT[ps_, p, :], start=True, stop=True, tile_position=(hh * 64, hh * 64))
                return r
            op('tensor', qk, reads=['naprep'], writes=[PF[b], PF[2 + b]])
            op('vector', lambda e, sb_=sb_, sl=sl, p=p, u0=u0: e.tensor_tensor(out=sb_[:, 0:512], in0=sl, in1=tblS[:, p, u0 * 64:u0 * 64 + 512], op=ALU.add),
               reads=[PF[b], 'tbl'], writes=[f'sbA{b}'])
            op('scalar', lambda e, sb_=sb_, sc=sc: e.activation(out=sb_[:, 512:768], in_=sc[:, 0:256], func=AF.Copy), reads=[PF[2 + b]], writes=[f'sbB{b}'])

            chain('vector', [
                lambda e, sb_=sb_, b=b: e.tensor_reduce(out=sm[:, b, 0:1], in_=sb_, axis=AX.X, op=ALU.max),
                lambda e, b=b: e.tensor_scalar(out=sm[:, b, 1:2], in0=sm[:, b, 0:1], scalar1=-1.0, scalar2=None, op0=ALU.mult)],
                reads=[f'sbA{b}', f'sbB{b}'], writes=[f'negm{b}'])
            op('scalar', lambda e, sb_=sb_, pb_=pb_, b=b: e.activation(out=pb_, in_=sb_, func=AF.Exp, bias=sm[:, b, 1:2], scale=1.0, accum_out=sm[:, b, 2:3]),
               reads=[f'sbA{b}', f'sbB{b}', f'negm{b}'], writes=[f'pb{b}', f'sum{b}'])
            op('vector', lambda e, b=b: e.reciprocal(out=sm[:, b, 3:4], in_=sm[:, b, 2:3]), reads=[f'sum{b}'], writes=[f'rsum{b}'])
            op('gpsimd', lambda e, pn_=pn_, pb_=pb_, b=b: e.tensor_scalar(out=pn_, in0=pb_, scalar1=sm[:, b, 3:4], scalar2=None, op0=ALU.mult),
               reads=[f'pb{b}', f'rsum{b}'], writes=[f'pn{b}'])


        def na_stage2(it):
            l, p, start, u0, tok0, b = na_ctx(it)
            sl, sc, po = pf[b], pf[2 + b], pf[4 + b]
            sb_, pb_, pn_, pT_ = sbt[b], pbt[b], pnt[b], pTt[b]
            def trp(e, pn_=pn_, b=b):
                for c in range(6):
                    r = e.transpose(pbf[b][:, c * 128:(c + 1) * 128], pn_[:, c * 128:(c + 1) * 128], ident)
                return r
            op('tensor', trp, reads=[f'pn{b}', 'const'], writes=[PB[b]])
            op('scalar', lambda e, pT_=pT_, b=b: e.activation(out=pT_, in_=pbf[b][:, 0:768], func=AF.Copy), reads=[PB[b]], writes=[f'pT{b}'])

            def pv(e, pT_=pT_, po=po, p=p, start=start):
                for hh in range(2):
                    for c in range(6):
                        if c < 4:
                            V = Va_e[:, start // 2 + c, :] if start % 2 == 0 else Va_o[:, (start - 1) // 2 + c, :]
                        else:
                            V = Vca[:, c - 4, :]
                        r = e.matmul(po[hh * 64:hh * 64 + 64, 0:64], lhsT=V[:, p * 128 + hh * 64:p * 128 + hh * 64 + 64],
                                     rhs=pT_[:, c * 128 + hh * 64:c * 128 + hh * 64 + 64], start=(c == 0), stop=(c == 5), tile_position=(0, hh * 64))
                return r
            op('tensor', pv, reads=[f'pT{b}', 'naprep'], writes=[PF[4 + b]])
            op('vector', lambda e, po=po, p=p, l=l: e.tensor_copy(out=o_aT[:, p, l * 64:(l + 1) * 64], in_=po[:, 0:64]), reads=[PF[4 + b]], writes=['o_aT'])

        for step in range(len(na_its) + 1):
            if step < len(na_its):
                na_stage1(step)
            if step >= 1:
                na_stage2(step - 1)
        dump('o_aT', o_aT, [128, 4, TOKR], BF16)
        S.barrier()
        if STAGE <= 2:
            return finish(nc, S, out, dbg_outs)

        A.reset(mark_oa)
        o_bT = A.alloc([8, TOKR], BF16)
        mark_ob = A.off
        wB = A.alloc([8, 768], BF16)
        QbT = A.alloc([4, TOKR], BF16); KbT = A.alloc([NKEY], BF16)
        Vb = A.alloc([NTALL, 2, 65], BF16)
        gqB = A.alloc([2, 64], F32)
        gtmp = A.alloc([2, 64], F32)
        negC = A.alloc([4], F32)
        hTg = [A.alloc([8, 512], BF16) for _ in range(2)]
        rc = [A.alloc([512], F32) for _ in range(2)]; rsn = [A.alloc([512], F32) for _ in range(2)]
        sq = A.alloc([640], F32); ssh = A.alloc([2, 16], F32)
        qn = A.alloc([640], F32); t1 = A.alloc([640], F32); t2 = A.alloc([640], F32)
        qbb = [A.alloc([640], BF16) for _ in range(2)]
        pTg = [A.alloc([512], BF16) for _ in range(4)]
        osb = [A.alloc([512], F32) for _ in range(2)]
        rec = [A.alloc([512], F32) for _ in range(2)]
        op('gpsimd', lambda e: e.dma_start(out=wB[:, :, 0:256], in_=w_in[:, 1024:1280].rearrange("(k p) c -> p k c", p=128)), writes=['wB'], dma='d_wB')
        op('gpsimd', lambda e: e.dma_start(out=wB[:, :, 256:768], in_=w_in[:, 1792:2304].rearrange("(k p) c -> p k c", p=128)), writes=['wB'], dma='d_wB')
        for i in range(2):
            op('sync', lambda e, i=i: e.dma_start(out=gtmp[:, i, :], in_=gqk[i, :].partition_broadcast(128)), writes=['gtmp'], dma='d_gt')

        qv = qn[:, 0:128].rearrange("p (a b) -> p a b", a=2)
        chain('vector', [
            lambda e: e.tensor_scalar(out=gqB[:, 0, :], in0=gtmp[:, 0, :], scalar1=0.125, scalar2=None, op0=ALU.mult),
            lambda e: e.tensor_copy(out=gqB[:, 1, :], in_=gtmp[:, 1, :]),
            lambda e: e.tensor_scalar(out=qv, in0=gtmp, scalar1=-1.0, scalar2=None, op0=ALU.mult),
            lambda e: e.tensor_tensor(out=gtmp, in0=gtmp, in1=qv, op=ALU.max),
            lambda e: e.tensor_reduce(out=negC[:, 0:1], in_=gtmp[:, 0, :], axis=AX.X, op=ALU.max),
            lambda e: e.tensor_reduce(out=negC[:, 1:2], in_=gtmp[:, 1, :], axis=AX.X, op=ALU.max),
            lambda e: e.tensor_tensor(out=negC[:, 2:3], in0=negC[:, 0:1], in1=negC[:, 1:2], op=ALU.mult),
            lambda e: e.tensor_scalar(out=negC[:, 3:4], in0=negC[:, 2:3], scalar1=-8.0, scalar2=None, op0=ALU.mult),
            lambda e: e.memset(Vb[:, :, :, 64:65], 1.0)], reads=['gtmp'], writes=['gtmp', 'gqB', 'negC', 'Vb', 'qn'])

        def normrope(src, H, gi, b, dst, tagr):
            W = H * 64
            op('scalar', lambda e: e.activation(out=sq[:, 0:W], in_=src, func=AF.Square), reads=tagr, writes=['sq'])

            op('vector', lambda e: e.tensor_reduce(out=ssh[:, 0, 0:H], in_=sq[:, 0:W].rearrange("p (h d) -> p h d", d=64), axis=AX.X, op=ALU.add), reads=['sq'], writes=['ssh0'])
            rstd(ssh[:, 1, 0:H], ssh[:, 0, 0:H], 64, ['ssh0'], 'ssh1')

            def n1(e):
                for h in range(H):
                    r = e.scalar_tensor_tensor(out=qn[:, h * 64:(h + 1) * 64], in0=src[:, h * 64:(h + 1) * 64], scalar=ssh[:, 1, h:h + 1], in1=gqB[:, gi, :],
                                               op0=ALU.mult, op1=ALU.mult)
                return r
            op('vector', n1, reads=['ssh1', 'gqB'] + tagr, writes=['qn'])
            op('vector', lambda e: e.tensor_tensor(out=t1[:, 0:W], in0=qn[:, 0:W], in1=rc[b][:, 0:W], op=ALU.mult), reads=['qn', f'rc{b}'], writes=['t1'])

            def r2(e):
                q4 = qn[:, 0:W].rearrange("p (a s f) -> p a s f", s=2, f=16)
                s4 = rsn[b][:, 0:W].rearrange("p (a s f) -> p a s f", s=2, f=16)
                o4 = t2[:, 0:W].rearrange("p (a s f) -> p a s f", s=2, f=16)
                e.tensor_tensor(out=o4[:, :, 0, :], in0=q4[:, :, 1, :], in1=s4[:, :, 0, :], op=ALU.mult)
                return e.tensor_tensor(out=o4[:, :, 1, :], in0=q4[:, :, 0, :], in1=s4[:, :, 1, :], op=ALU.mult)
            op('vector', r2, reads=['qn', f'rsn{b}'], writes=['t2'])
            op('vector', lambda e: e.tensor_tensor(out=dst, in0=t1[:, 0:W], in1=t2[:, 0:W], op=ALU.add), reads=['t1', 't2'], writes=['qbb'])

        for t in range(NTALL):
            b = t % 2
            g = t // 4
            hg = hTg[g % 2]
            if t % 4 == 0:
                n = min(512, NKEY - g * 512)
                op('sync', lambda e, g=g, hg=hg, n=n: e.dma_start(out=hg[:, :, 0:n], in_=hT_scr[:, :, g * 512:g * 512 + n]), reads=['hT_scr'], writes=[f'hTg{g % 2}'], dma=f'ldh{g % 2}')
            j = t % 4
            op('sync', lambda e, t=t, b=b: e.dma_start(out=rc[b], in_=ropec[t * 128:(t + 1) * 128, :]), writes=[f'rc{b}'], dma=f'ldr{b}')
            op('sync', lambda e, t=t, b=b: e.dma_start(out=rsn[b], in_=ropes[t * 128:(t + 1) * 128, :]), writes=[f'rsn{b}'], dma=f'lds{b}')
            bk = nbk()

            def mmkv(e, hg=hg, j=j, bk=bk):
                for k in range(8):
                    r = e.matmul(pf[bk][:, 0:256], lhsT=hg[:, k, j * 128:(j + 1) * 128], rhs=wB[:, k, 0:256], start=(k == 0), stop=(k == 7))
                return r
            op('tensor', mmkv, reads=['wB', f'hTg{g % 2}'], writes=[PF[bk]])
            op('scalar', lambda e, t=t, bk=bk: e.activation(out=Vb[:, t, :, 0:64], in_=pf[bk][:, 128:256].rearrange("p (h d) -> p h d", d=64), func=AF.Copy),
               reads=[PF[bk]], writes=['Vb'])
            normrope(pf[bk][:, 0:128], 2, 1, b, qbb[b][:, 0:128], [PF[bk]])
            op('tensor', lambda e, b=b: e.transpose(pbf[b][:, 0:128], qbb[b][:, 0:128], ident), reads=['qbb', 'const'], writes=[PB[b]])
            op('scalar', lambda e, t=t, b=b: e.activation(out=KbT[:, t * 128:(t + 1) * 128], in_=pbf[b][:, 0:128], func=AF.Copy), reads=[PB[b]], writes=['KbT'])
            if t < NTR:
                bk2 = nbk()

                def mmq(e, hg=hg, j=j, bk2=bk2):
                    for k in range(8):
                        r = e.matmul(pf[bk2], lhsT=hg[:, k, j * 128:(j + 1) * 128], rhs=wB[:, k, 256:768], start=(k == 0), stop=(k == 7))
                    return r
                op('tensor', mmq, reads=['wB', f'hTg{g % 2}'], writes=[PF[bk2]])
                normrope(pf[bk2], 8, 0, b, qbb[b][:, 0:512], [PF[bk2]])

                def trq(e, b=b):
                    for gg in range(4):
                        r = e.transpose(pbf[b][:, 128 + gg * 128:128 + (gg + 1) * 128], qbb[b][:, gg * 128:(gg + 1) * 128], ident)
                    return r
                op('tensor', trq, reads=['qbb', 'const'], writes=[PB[b]])
                op('scalar', lambda e, t=t, b=b: e.activation(out=QbT[:, :, t * 128:(t + 1) * 128], in_=pbf[b][:, 128:640].rearrange("p (g c) -> p g c", g=4), func=AF.Copy),
                   reads=[PB[b]], writes=['QbT'])
        dump('QbT', QbT, [128, 4, TOKR], BF16); dump('KbT', KbT, [128, NKEY], BF16); dump('Vb', Vb, [128, NTALL, 2, 65], BF16)

        chunks = [(kvh, qt, c) for kvh in range(2) for qt in range(NTR) for c in range(NTALL)]
        LA = 2
        pending = []

        def gq_S(i):
            kvh, qt, c = chunks[i]
            pr = slice(kvh * 64, kvh * 64 + 64)
            sb_ = i % 4
            st_ = pf[sb_]
            op('tensor', lambda e: e.matmul(st_, lhsT=KbT[pr, c * 128:(c + 1) * 128], rhs=QbT[pr, :, qt * 128:(qt + 1) * 128],
                                            start=True, stop=True, tile_position=(kvh * 64, 0)),
               reads=['KbT', 'QbT'], writes=[PF[sb_]])
            op('scalar', lambda e: e.activation(out=pTg[sb_], in_=st_, func=AF.Exp, bias=negC[:, 3:4], scale=1.0), reads=[PF[sb_], 'negC'], writes=[f'pTg{sb_}'])

        def gq_PV(i, step):
            kvh, qt, c = chunks[i]
            sb_ = i % 4
            ob = (kvh * NTR + qt) % 2
            po = pf[4 + ob]
            bk = 4 + ob
            op('tensor', lambda e: e.matmul(po[0:65, :], lhsT=Vb[:, c, kvh, :], rhs=pTg[sb_], start=(c == 0), stop=(c == NTALL - 1)),
               reads=[f'pTg{sb_}', 'Vb'], writes=[PF[bk]])
            if c == NTALL - 1:
                op('scalar', lambda e: e.activation(out=osb[ob][0:65, :], in_=po[0:65, :], func=AF.Copy), reads=[PF[bk]], writes=[f'osb{ob}'])
                op('vector', lambda e: e.reciprocal(out=rec[ob][64:65, :], in_=osb[ob][64:65, :]), reads=[f'osb{ob}'], writes=[f'rec{ob}'])

                def fin():
                    op('tensor', lambda e: e.matmul(po[0:64, :], lhsT=ones_f[64:65, 0:64], rhs=rec[ob][64:65, :], start=True, stop=True), reads=[f'rec{ob}', 'const'], writes=[PF[bk]])
                    op('vector', lambda e: e.tensor_tensor(out=o_bT[0:64, kvh * 4:(kvh + 1) * 4, qt * 128:(qt + 1) * 128],
                                                           in0=osb[ob][0:64, :].rearrange("p (g t) -> p g t", g=4),
                                                           in1=po[0:64, :].rearrange("p (g t) -> p g t", g=4), op=ALU.mult),
                       reads=[f'osb{ob}', PF[bk]], writes=['o_bT'])
                pending.append((step + 4, fin))

        for step in range(len(chunks) + LA + 8):
            if step < len(chunks):
                gq_S(step)
            if LA <= step < len(chunks) + LA:
                gq_PV(step - LA, step)
            for due, fn_ in list(pending):
                if due <= step:
                    fn_(); pending.remove((due, fn_))
        assert not pending
        dump('o_bT', o_bT, [128, 8, TOKR], BF16)
        S.barrier()
        if STAGE <= 3:
            return finish(nc, S, out, dbg_outs)

        A.reset(mark_ob)
        wG = A.alloc([8, 2048], BF16); wOA = A.alloc([4, D], BF16); wOB = A.alloc([8, D], BF16); wO = A.alloc([8, D], BF16)
        wR = A.alloc([8, NE], BF16); bR = A.alloc([NE], BF16)
        hTg = [A.alloc([8, 512], BF16)] * 2
        zT = [A.alloc([8, 512], BF16) for _ in range(2)]
        sga = A.alloc([512], F32); sgb = A.alloc([512], F32)
        xt4 = A.alloc([D], F32); x1t = [A.alloc([D], F32) for _ in range(2)]; tmpf = A.alloc([D], F32)
        h2b = [A.alloc([D], BF16) for _ in range(2)]; h2T = A.alloc([8, 128], BF16)
        lg = P.alloc([NTR, NE], F32); mx8 = P.alloc([NTR, 8], F32); posA = P.alloc([NTR, NE], F32)
        wts = P.alloc([NTR, 4], F32); sloti = P.alloc([NTR * 4], I32)
        mask = A.alloc([NE], F32); maskb = A.alloc([NE], BF16); cntp = P.alloc([NE], F32)
        sms = A.alloc([NTR, 8], F32); e4 = A.alloc([4], F32)
        utri = A.alloc([128], BF16); utf = A.alloc([128], F32)
        mark_route = A.off
        for (dst, src, nm) in ((wG[:, :, 0:1024], w_in[:, 2304:3328], 0), (wG[:, :, 1024:2048], w_in[:, 3328:4352], 1), (wO, w_o, 2)):
            op('gpsimd', lambda e, dst=dst, src=src: e.dma_start(out=dst, in_=src.rearrange("(k p) c -> p k c", p=128)), writes=['wM'], dma='d_wM')
        op('gpsimd', lambda e: e.dma_start(out=wOA, in_=w_oa.rearrange("(k p) c -> p k c", p=128)), writes=['wM'], dma='d_wM')
        op('gpsimd', lambda e: e.dma_start(out=wOB[0:64], in_=w_ob.rearrange("(h d) c -> d h c", d=64)), writes=['wM'], dma='d_wM')
        op('gpsimd', lambda e: e.dma_start(out=wR, in_=w_r.rearrange("(k p) c -> p k c", p=128)), writes=['wM'], dma='d_wM')
        op('gpsimd', lambda e: e.dma_start(out=bR[0:1, :], in_=b_r.rearrange("(o n) -> o n", o=1)), writes=['wM'], dma='d_wM')

        chain('gpsimd', [
            lambda e: e.memset(utf, 1.0),
            lambda e: e.affine_select(out=utf, in_=utf, pattern=[[1, 128]], compare_op=ALU.is_gt, fill=0.0, base=0, channel_multiplier=-1),
            lambda e: e.memset(cntp, 0.0),
            lambda e: e.tensor_copy(out=utri, in_=utf)], writes=['utri', 'route'])

        for g in range(5):
            hg = hTg[g % 2]; z = zT[g % 2]
            ntok = 512 if g < 4 else 256
            tk = slice(g * 512, g * 512 + ntok)
            op('sync', lambda e, g=g, hg=hg, ntok=ntok: e.dma_start(out=hg[:, :, 0:ntok], in_=hT_scr[:, :, g * 512:g * 512 + ntok]), reads=['hT_scr'], writes=[f'hTg{g % 2}'], dma=f'ldh{g % 2}')
            for oc in range(8):
                def mm4(e, hg=hg, oc=oc, ntok=ntok, tk=tk):
                    for k in range(8):
                        e.matmul(pf[0][:, 0:ntok], lhsT=wG[:, k, oc * 128:(oc + 1) * 128], rhs=hg[:, k, 0:ntok], start=(k == 0), stop=(k == 7))
                    for k in range(8):
                        e.matmul(pf[1][:, 0:ntok], lhsT=wG[:, k, 1024 + oc * 128:1024 + (oc + 1) * 128], rhs=hg[:, k, 0:ntok], start=(k == 0), stop=(k == 7))
                    for k in range(4):
                        e.matmul(pf[2][:, 0:ntok], lhsT=wOA[:, k, oc * 128:(oc + 1) * 128], rhs=o_aT[:, k, tk], start=(k == 0), stop=(k == 3))
                    for k in range(8):
                        r = e.matmul(pf[3][:, 0:ntok], lhsT=wOB[0:64, k, oc * 128:(oc + 1) * 128], rhs=o_bT[0:64, k, tk], start=(k == 0), stop=(k == 7))
                    return r
                op('tensor', mm4, reads=['wM', f'hTg{g % 2}', 'o_aT', 'o_bT'], writes=[PF[0], PF[1], PF[2], PF[3]])

                def sg(e, ntok=ntok):
                    e.activation(out=sga[:, 0:ntok], in_=pf[0][:, 0:ntok], func=AF.Sigmoid)
                    return e.activation(out=sgb[:, 0:ntok], in_=pf[1][:, 0:ntok], func=AF.Sigmoid)
                op('scalar', sg, reads=[PF[0], PF[1]], writes=['sg'])

                def zz(e, ntok=ntok):
                    e.tensor_tensor(out=sga[:, 0:ntok], in0=sga[:, 0:ntok], in1=pf[2][:, 0:ntok], op=ALU.mult)
                    return e.tensor_tensor(out=sgb[:, 0:ntok], in0=sgb[:, 0:ntok], in1=pf[3][:, 0:ntok], op=ALU.mult)
                op('vector', zz, reads=['sg', PF[2], PF[3]], writes=['sg2'])
                op('gpsimd', lambda e, z=z, oc=oc, ntok=ntok: e.tensor_tensor(out=z[:, oc, 0:ntok], in0=sga[:, 0:ntok], in1=sgb[:, 0:ntok], op=ALU.add),
                   reads=['sg2'], writes=[f'zT{g % 2}', 'sg'])
            for j in range(ntok // 128):
                t = 4 * g + j
                yb = pq[2]
                b = t % 2

                def mmy(e, z=z, j=j, yb=yb):
                    for n in range(2):
                        for k in range(8):
                            r = e.matmul(yb[:, n * 512:(n + 1) * 512], lhsT=z[:, k, j * 128:(j + 1) * 128], rhs=wO[:, k, n * 512:(n + 1) * 512], start=(k == 0), stop=(k == 7))
                    return r
                op('tensor', mmy, reads=['wM', f'zT{g % 2}'], writes=[PF[4], PF[5]])
                op('sync', lambda e, t=t: e.dma_start(out=xt4, in_=xc[t * 128:(t + 1) * 128, :]), writes=['xt'], dma='ldx0')
                op('scalar', lambda e, yb=yb, t=t: e.activation(out=tmpf, in_=yb, func=AF.Square, accum_out=sms[:, t, 0:1]), reads=[PF[4], PF[5]], writes=['tmpf', 'ssy'])
                rstd(sms[:, t, 1:2], sms[:, t, 0:1], D, ['ssy'], 'rsy')
                op('vector', lambda e, yb=yb, t=t: e.scalar_tensor_tensor(out=tmpf, in0=yb, scalar=sms[:, t, 1:2], in1=G1, op0=ALU.mult, op1=ALU.mult),
                   reads=[PF[4], PF[5], 'rsy', 'rows'], writes=['tmpf'])
                op('gpsimd', lambda e, b=b: e.tensor_tensor(out=x1t[b], in0=tmpf, in1=xt4, op=ALU.add), reads=['tmpf', 'xt'], writes=[f'x1t{b}'])
                op('sync', lambda e, t=t, b=b: e.dma_start(out=x1_scr[t * 128:(t + 1) * 128, :], in_=x1t[b]), reads=[f'x1t{b}'], dma=f'stx{b}')
                op('scalar', lambda e, t=t, b=b: e.activation(out=tmpf, in_=x1t[b], func=AF.Square, accum_out=sms[:, t, 2:3]), reads=[f'x1t{b}'], writes=['tmpf', 'ss2'])
                rstd(sms[:, t, 3:4], sms[:, t, 2:3], D, ['ss2'], 'rs2')
                op('vector', lambda e, t=t, b=b: e.scalar_tensor_tensor(out=tmpf, in0=x1t[b], scalar=sms[:, t, 3:4], in1=A2, op0=ALU.mult, op1=ALU.mult),
                   reads=[f'x1t{b}', 'rs2', 'rows'], writes=['tmpf'])
                op('gpsimd', lambda e, b=b: e.tensor_tensor(out=h2b[b], in0=tmpf, in1=B2, op=ALU.add), reads=['tmpf', 'rows'], writes=[f'h2b{b}'])
                op('sync', lambda e, t=t, b=b: e.dma_start(out=h2_scr[t * 128:(t + 1) * 128, :], in_=h2b[b]), reads=[f'h2b{b}'], dma=f'sth{b}')

                def trh(e, b=b):
                    for k in range(8):
                        r = e.transpose(pbf[b][:, k * 128:(k + 1) * 128], h2b[b][:, k * 128:(k + 1) * 128], ident)
                    return r
                op('tensor', trh, reads=[f'h2b{b}', 'const'], writes=[PB[b]])
                op('scalar', lambda e, b=b: e.activation(out=h2T, in_=pbf[b].rearrange("p (k c) -> p k c", k=8), func=AF.Copy), reads=[PB[b]], writes=['h2T'])

                def mml(e):
                    for k in range(8):
                        e.matmul(pf[0][:, 0:NE], lhsT=h2T[:, k, :], rhs=wR[:, k, :], start=(k == 0), stop=False)
                    return e.matmul(pf[0][:, 0:NE], lhsT=ones_bf[0:1, :], rhs=bR[0:1, :], start=False, stop=True)
                op('tensor', mml, reads=['h2T', 'wM', 'const'], writes=[PF[0]])

                chain('vector', [
                    lambda e, t=t: e.tensor_copy(out=lg[:, t, :], in_=pf[0][:, 0:NE]),
                    lambda e, t=t: e.max(out=mx8[:, t, :], in_=lg[:, t, :]),
                    lambda e, t=t: e.tensor_scalar(out=mask, in0=lg[:, t, :], scalar1=mx8[:, t, 3:4], scalar2=None, op0=ALU.is_ge),
                    lambda e: e.tensor_copy(out=maskb, in_=mask),
                    lambda e, t=t: e.tensor_scalar(out=sms[:, t, 4:5], in0=mx8[:, t, 0:1], scalar1=-1.0, scalar2=None, op0=ALU.mult)],
                    reads=[PF[0]], writes=['lg', 'maskb', 'negmx'])
                op('scalar', lambda e, t=t: e.activation(out=e4, in_=mx8[:, t, 0:4], func=AF.Exp, bias=sms[:, t, 4:5], scale=1.0, accum_out=sms[:, t, 5:6]),
                   reads=['lg', 'negmx'], writes=['e4'])

                def mmc(e):
                    e.matmul(pf[1][:, 0:NE], lhsT=utri, rhs=maskb, start=True, stop=True)
                    return e.matmul(pf[1][:, NE:2 * NE], lhsT=ones_bf, rhs=maskb, start=True, stop=True)
                op('tensor', mmc, reads=['maskb', 'utri', 'const'], writes=[PF[1]])

                chain('vector', [
                    lambda e, t=t: e.reciprocal(out=sms[:, t, 6:7], in_=sms[:, t, 5:6]),
                    lambda e, t=t: e.tensor_scalar(out=wts[:, t, :], in0=e4, scalar1=sms[:, t, 6:7], scalar2=None, op0=ALU.mult),
                    lambda e, t=t: e.tensor_tensor(out=posA[:, t, :], in0=pf[1][:, 0:NE], in1=cntp, op=ALU.add),
                    lambda e: e.tensor_tensor(out=cntp, in0=cntp, in1=pf[1][:, NE:2 * NE], op=ALU.add)],
                    reads=['e4', PF[1]], writes=['route'])
        dump('lg', lg, [128, NTR, NE], F32); dump('posA', posA, [128, NTR, NE], F32); dump('cntp', cntp, [128, NE], F32)
        S.barrier()
        if STAGE <= 4:
            return finish(nc, S, out, dbg_outs)

        A.reset()
        ci = A.alloc([NE], I32); padf = A.alloc([NE], F32); padT = A.alloc([128], F32); ltri = A.alloc([NE], F32)
        basef = A.alloc([NE], F32); pend = A.alloc([NE], F32)
        thr = A.alloc([NBLK], F32); EB = A.alloc([NBLK], F32); skp = A.alloc([NBLK], F32)
        idxw_f = A.alloc([NBLK], F32); idxb_f = A.alloc([NBLK], F32); pidx = A.alloc([1], F32)
        idxw = A.alloc([NBLK], I32); idxb = A.alloc([NBLK], I32)
        idxw8_f = A.alloc([8, NBLK], F32); idxw8 = A.alloc([8, NBLK], I32)
        idxd_f = A.alloc([4, NBLK], F32); idxd = A.alloc([4, NBLK], I32); idxd0 = A.alloc([NBLK], F32); pidx4 = A.alloc([1], F32)
        slot2 = A.alloc([NE], F32); slotf = A.alloc([NTR * 4], F32); tmp32 = A.alloc([NE], F32)
        h2l = [A.alloc([D], BF16) for _ in range(2)]

        chain('vector', [
            lambda e: e.tensor_scalar(out=padf, in0=cntp, scalar1=127.0, scalar2=None, op0=ALU.add),
            lambda e: e.tensor_copy(out=ci, in_=padf),
            lambda e: e.tensor_single_scalar(out=ci, in_=ci, scalar=7, op=ALU.arith_shift_right),
            lambda e: e.tensor_single_scalar(out=ci, in_=ci, scalar=7, op=ALU.logical_shift_left),
            lambda e: e.tensor_copy(out=padf, in_=ci)], reads=['route'], writes=['padf'])

        chain('gpsimd', [
            lambda e: e.memset(ltri, 1.0),
            lambda e: e.affine_select(out=ltri, in_=ltri, pattern=[[1, NE]], compare_op=ALU.is_gt, fill=0.0, base=0, channel_multiplier=-1),
            lambda e: e.iota(thr, pattern=[[128, NBLK]], base=0, channel_multiplier=0, allow_small_or_imprecise_dtypes=True),
            lambda e: e.iota(pidx, pattern=[[0, 1]], base=0, channel_multiplier=1, allow_small_or_imprecise_dtypes=True)], writes=['ltri'])
        op('tensor', lambda e: e.transpose(pq[0][0:NE, 0:128], padf, identf), reads=['padf', 'const'], writes=[PF[0]])
        op('vector', lambda e: e.tensor_copy(out=padT[0:NE, :], in_=pq[0][0:NE, 0:128]), reads=[PF[0]], writes=['padT'])
        op('tensor', lambda e: e.matmul(pf[1][:, 0:NE], lhsT=padT[0:NE, :], rhs=ltri[0:NE, :], start=True, stop=True), reads=['padT', 'ltri'], writes=[PF[1]])

        lay2 = [
            lambda e: e.tensor_copy(out=basef, in_=pf[1][:, 0:NE]),
            lambda e: e.tensor_tensor(out=pend, in0=basef, in1=padf, op=ALU.add),
            lambda e: e.memset(EB, 0.0)]
        for ex in range(NE):
            lay2.append(lambda e, ex=ex: e.scalar_tensor_tensor(out=EB, in0=thr, scalar=pend[:, ex:ex + 1], in1=EB, op0=ALU.is_ge, op1=ALU.add))
        lay2 += [
            lambda e: e.tensor_scalar(out=EB, in0=EB, scalar1=float(NE - 1), scalar2=None, op0=ALU.min),
            lambda e: e.memset(skp, 0.0),
            lambda e: e.tensor_tensor(out=skp[:, 1:NBLK], in0=EB[:, 1:NBLK], in1=EB[:, 0:NBLK - 1], op=ALU.is_equal),
            lambda e: e.tensor_scalar(out=skp, in0=skp, scalar1=BIG, scalar2=None, op0=ALU.mult),
            lambda e: e.scalar_tensor_tensor(out=idxw_f, in0=EB, scalar=1024.0, in1=skp, op0=ALU.mult, op1=ALU.add),
            lambda e: e.tensor_scalar(out=idxw_f, in0=idxw_f, scalar1=pidx[:, 0:1], scalar2=None, op0=ALU.add),
            lambda e: e.tensor_tensor(out=idxb_f, in0=EB, in1=skp, op=ALU.add),
            lambda e: e.tensor_copy(out=idxw, in_=idxw_f)]
        for k8 in range(8):
            lay2.append(lambda e, k8=k8: e.tensor_scalar(out=idxw8_f[:, k8, :], in0=idxw_f, scalar1=128.0 * k8, scalar2=None, op0=ALU.add))
        lay2 += [lambda e: e.tensor_copy(out=idxw8, in_=idxw8_f), lambda e: e.tensor_copy(out=idxb, in_=idxb_f)]
        lay2 += [lambda e: e.tensor_scalar(out=pidx4, in0=pidx, scalar1=4.0, scalar2=None, op0=ALU.mult),
                 lambda e: e.scalar_tensor_tensor(out=idxd0, in0=EB, scalar=512.0, in1=skp, op0=ALU.mult, op1=ALU.add),
                 lambda e: e.tensor_scalar(out=idxd0, in0=idxd0, scalar1=pidx4[:, 0:1], scalar2=None, op0=ALU.add)]
        for j4 in range(4):
            lay2.append(lambda e, j4=j4: e.tensor_scalar(out=idxd_f[:, j4, :], in0=idxd0, scalar1=float(j4), scalar2=None, op0=ALU.add))
        lay2 += [lambda e: e.tensor_copy(out=idxd, in_=idxd_f)]
        chain('vector', lay2, reads=[PF[1], 'padf', 'ltri'], writes=['lay'])
        for t in range(NTR):
            b = t % 2
            op('sync', lambda e, t=t, b=b: e.dma_start(out=h2l[b], in_=h2_scr[t * 128:(t + 1) * 128, :]), reads=['h2_scr'], writes=[f'h2l{b}'], dma=f'ldx{b}')

            slf = [lambda e, t=t: e.tensor_tensor(out=slot2, in0=posA[:, t, :], in1=basef, op=ALU.add)]
            for k in range(4):
                slf.append(lambda e, t=t, k=k: e.scalar_tensor_tensor(out=tmp32, in0=lg[:, t, :], scalar=mx8[:, t, k:k + 1], in1=slot2, op0=ALU.is_equal, op1=ALU.mult,
                                                                      accum_out=slotf[:, 4 * t + k:4 * t + k + 1]))
            slf.append(lambda e, t=t: e.tensor_copy(out=sloti[:, 4 * t:4 * t + 4], in_=slotf[:, 4 * t:4 * t + 4]))
            chain('vector', slf, reads=['lay'], writes=[f'sloti{t}', 'slot2'])
            for k in range(4):
                op('gpsimd', lambda e, t=t, k=k, b=b: e.indirect_dma_start(out=xs_scr, out_offset=bass.IndirectOffsetOnAxis(ap=sloti[:, 4 * t + k:4 * t + k + 1], axis=0),
                                                                       in_=h2l[b], in_offset=None, bounds_check=breg(e, NSLOT - 1), oob_is_err=False),
                   reads=[f'sloti{t}', f'h2l{b}'], dma=f'sc{b}')
        dump('sloti', sloti, [128, NTR * 4], I32); dump('idxw', idxw, [128, NBLK], I32); dump('wts', wts, [128, NTR, 4], F32)
        S.barrier()
        if STAGE <= 5:
            return finish(nc, S, out, dbg_outs)

        mark_ex = A.off
        wgu = A.alloc([8, 2 * D], BF16); wdn4 = A.alloc([4, 2 * D], BF16)
        wdn_k = lambda k: wdn4[:, k // 2, (k % 2) * D:(k % 2 + 1) * D]
        wdn_pairs = w_dn.rearrange("e (q two) n -> (e q) (two n)", two=2)
        bgu = A.alloc([2 * D], BF16); bdn = A.alloc([D], BF16)
        xe = [A.alloc([D], BF16) for _ in range(2)]; xT = [A.alloc([8, 128], BF16) for _ in range(2)]
        gs = A.alloc([D], F32); sg_ = A.alloc([D], F32); l1 = A.alloc([D], F32); tt = A.alloc([D], F32)
        actb = A.alloc([D], BF16); aT = A.alloc([8, 128], BF16)
        yo = [A.alloc([D], F32) for _ in range(2)]
        wgu_flat = w_gu.rearrange("e k n -> (e k) n"); wdn_flat = w_dn.rearrange("e k n -> (e k) n")
        wgu_v = bass.AP(tensor=w_gu.tensor, offset=0, ap=[[2 * D, NE * D - 896], [128 * 2 * D, 8], [1, 2 * D]])
        wdn_v = bass.AP(tensor=w_dn.tensor, offset=0, ap=[[D, NE * D - 896], [128 * D, 8], [1, D]])
        gs2 = [gs, A.alloc([D], F32)]; sg2 = [sg_, A.alloc([D], F32)]; l12 = [l1, A.alloc([D], F32)]; tt2 = [tt, A.alloc([D], F32)]
        actb2 = [actb, A.alloc([D], BF16)]; aT2 = [aT, A.alloc([8, 128], BF16)]

        def blk_ldgu(blk):
            b = blk % 2
            ib = bass.IndirectOffsetOnAxis(ap=idxb[:, blk:blk + 1], axis=0)
            for k8 in range(8):
                op('gpsimd', lambda e, k8=k8: e.indirect_dma_start(out=wgu[:, k8, :], out_offset=None, in_=wgu_flat,
                                                                   in_offset=bass.IndirectOffsetOnAxis(ap=idxw8[:, k8, blk:blk + 1], axis=0),
                                                                   bounds_check=breg(e, NE * D - 1), oob_is_err=False),
                   reads=['lay'], writes=[f'wgu{k8}'], dma=f'ld_wgu{k8}')
            op('gpsimd', lambda e: e.indirect_dma_start(out=bgu, out_offset=None, in_=b_gu, in_offset=ib, bounds_check=breg(e, NE - 1), oob_is_err=False),
               reads=['lay'], writes=['bgu'], dma='ld_bgu')
            op('sync', lambda e: e.dma_start(out=xe[b], in_=xs_scr[blk * 128:(blk + 1) * 128, :]), writes=[f'xe{b}'], dma=f'ldx{b}')

        def blk_lddn(blk):
            ib = bass.IndirectOffsetOnAxis(ap=idxb[:, blk:blk + 1], axis=0)
            for j4 in range(4):
                op('gpsimd', lambda e, j4=j4: e.indirect_dma_start(out=wdn4[:, j4, :], out_offset=None, in_=wdn_pairs,
                                                                   in_offset=bass.IndirectOffsetOnAxis(ap=idxd[:, j4, blk:blk + 1], axis=0),
                                                                   bounds_check=breg(e, NE * 512 - 1), oob_is_err=False),
                   reads=['lay'], writes=[f'wdn{j4}'], dma=f'ld_wdn{j4}')
            op('gpsimd', lambda e: e.indirect_dma_start(out=bdn, out_offset=None, in_=b_dn, in_offset=ib, bounds_check=breg(e, NE - 1), oob_is_err=False),
               reads=['lay'], writes=['bdn'], dma='ld_bdn')

        def blk_trx(blk):
            b = blk % 2

            def trx(e):
                for k in range(8):
                    r = e.transpose(pbf[0][:, k * 128:(k + 1) * 128], xe[b][:, k * 128:(k + 1) * 128], ident)
                return r
            op('tensor', trx, reads=[f'xe{b}', 'const'], writes=[PB[0]])
            op('scalar', lambda e: e.activation(out=xT[b], in_=pbf[0].rearrange("p (k c) -> p k c", k=8), func=AF.Copy), reads=[PB[0]], writes=[f'xT{b}'])

        def blk_gu(blk):
            b = blk % 2
            g_, s_, l_, t_, a_ = gs2[b], sg2[b], l12[b], tt2[b], actb2[b]

            for k in range(8):
                def mguk(e, k=k):
                    for n in range(4):
                        r = e.matmul(pf[n], lhsT=xT[b][:, k, :], rhs=wgu[:, k, n * 512:(n + 1) * 512], start=(k == 0), stop=False)
                    return r
                op('tensor', mguk, reads=[f'xT{b}', f'wgu{k}'], writes=[PF[0], PF[1], PF[2], PF[3]])

            def mgub(e):
                for n in range(4):
                    r = e.matmul(pf[n], lhsT=ones_bf[0:1, :], rhs=bgu[0:1, n * 512:(n + 1) * 512], start=False, stop=True)
                return r
            op('tensor', mgub, reads=['bgu', 'const'], writes=[PF[0], PF[1], PF[2], PF[3]])
            op('vector', lambda e: e.tensor_scalar(out=g_, in0=pq[0], scalar1=7.0, scalar2=None, op0=ALU.min), reads=[PF[0], PF[1]], writes=[f'gs{b}'])
            op('scalar', lambda e: e.activation(out=s_, in_=g_, func=AF.Sigmoid, scale=1.702), reads=[f'gs{b}'], writes=[f'sg{b}'])
            op('vector', lambda e: e.tensor_scalar(out=l_, in0=pq[1], scalar1=7.0, scalar2=-7.0, op0=ALU.min, op1=ALU.max), reads=[PF[2], PF[3]], writes=[f'l1{b}'])
            op('vector', lambda e: e.tensor_tensor(out=t_, in0=g_, in1=s_, op=ALU.mult), reads=[f'gs{b}', f'sg{b}'], writes=[f'tt{b}'])
            op('vector', lambda e: e.scalar_tensor_tensor(out=a_, in0=l_, scalar=1.0, in1=t_, op0=ALU.add, op1=ALU.mult), reads=[f'l1{b}', f'tt{b}'], writes=[f'actb{b}'])

        def blk_tra(blk):
            b = blk % 2
            a_ = actb2[b]

            def tra(e):
                for k in range(8):
                    r = e.transpose(pbf[1][:, k * 128:(k + 1) * 128], a_.rearrange("t (p k) -> t k p", k=8)[:, k, :], ident)
                return r
            op('tensor', tra, reads=[f'actb{b}', 'const'], writes=[PB[1]])
            op('scalar', lambda e: e.activation(out=aT2[b], in_=pbf[1].rearrange("p (k c) -> p k c", k=8), func=AF.Copy), reads=[PB[1]], writes=[f'aT{b}'])

        def blk_dn(blk):
            b = blk % 2

            for j4 in range(4):
                def mdnj(e, j4=j4):
                    for k in (2 * j4, 2 * j4 + 1):
                        for n in range(2):
                            r = e.matmul(pf[4 + n], lhsT=aT2[b][:, k, :], rhs=wdn_k(k)[:, n * 512:(n + 1) * 512], start=(k == 0), stop=False)
                    return r
                op('tensor', mdnj, reads=[f'aT{b}', f'wdn{j4}'], writes=[PF[4], PF[5]])

            def mdnb(e):
                for n in range(2):
                    r = e.matmul(pf[4 + n], lhsT=ones_bf[0:1, :], rhs=bdn[0:1, n * 512:(n + 1) * 512], start=False, stop=True)
                return r
            op('tensor', mdnb, reads=['bdn', 'const'], writes=[PF[4], PF[5]])
            op('scalar', lambda e: e.activation(out=yo[b], in_=pq[2], func=AF.Copy), reads=[PF[4], PF[5]], writes=[f'yo{b}'])
            op('sync', lambda e: e.dma_start(out=y_scr[blk * 128:(blk + 1) * 128, :], in_=yo[b]), reads=[f'yo{b}'], dma=f'sty{b}')

        blk_ldgu(0); blk_lddn(0); blk_trx(0)
        for sblk in range(NBLK):
            blk_gu(sblk)
            if sblk >= 1:
                blk_tra(sblk - 1)
            if sblk + 1 < NBLK:
                blk_ldgu(sblk + 1)
                blk_trx(sblk + 1)
            if sblk >= 1:
                blk_dn(sblk - 1)
                blk_lddn(sblk)
        blk_tra(NBLK - 1); blk_dn(NBLK - 1)
        S.barrier()

        A.reset(mark_ex)
        gk = [[A.alloc([D], F32) for _ in range(4)] for _ in range(2)]
        acc = A.alloc([D], F32); x1l = [A.alloc([D], F32) for _ in range(2)]; ot = [A.alloc([D], F32) for _ in range(2)]
        jk = A.alloc([D], F32)
        fs = A.alloc([NTR, 2], F32)
        for t in range(NTR):
            b = t % 2
            for k in range(4):
                op('gpsimd', lambda e, t=t, k=k, b=b: e.indirect_dma_start(out=gk[b][k], out_offset=None, in_=y_scr,
                                                                       in_offset=bass.IndirectOffsetOnAxis(ap=sloti[:, 4 * t + k:4 * t + k + 1], axis=0),
                                                                       bounds_check=breg(e, NSLOT - 1), oob_is_err=False),
                   reads=['y_scr'], writes=[f'gk{b}{k}'], dma=f'ga{b}')
            op('sync', lambda e, t=t, b=b: e.dma_start(out=x1l[b], in_=x1_scr[t * 128:(t + 1) * 128, :]), reads=['x1_scr'], writes=[f'x1l{b}'], dma=f'ldx{b}')

            cmb = [lambda e, t=t, b=b: e.tensor_scalar(out=acc, in0=gk[b][0], scalar1=wts[:, t, 0:1], scalar2=None, op0=ALU.mult)]
            for k in range(1, 4):
                cmb.append(lambda e, t=t, b=b, k=k: e.scalar_tensor_tensor(out=acc, in0=gk[b][k], scalar=wts[:, t, k:k + 1], in1=acc, op0=ALU.mult, op1=ALU.add))
            chain('vector', cmb, reads=[f'gk{b}{k}' for k in range(4)], writes=['acc'])
            op('scalar', lambda e, t=t: e.activation(out=jk, in_=acc, func=AF.Square, accum_out=fs[:, t, 0:1]), reads=['acc'], writes=['jk', 'fss'])
            rstd(fs[:, t, 1:2], fs[:, t, 0:1], D, ['fss'], 'fsr')
            op('vector', lambda e, t=t: e.scalar_tensor_tensor(out=acc, in0=acc, scalar=fs[:, t, 1:2], in1=G2, op0=ALU.mult, op1=ALU.mult), reads=['acc', 'fsr'], writes=['acc'])
            op('gpsimd', lambda e, b=b: e.tensor_tensor(out=ot[b], in0=acc, in1=x1l[b], op=ALU.add), reads=['acc', f'x1l{b}'], writes=[f'ot{b}'])
            op('sync', lambda e, t=t, b=b: e.dma_start(out=out[t * 128:(t + 1) * 128, :], in_=ot[b]), reads=[f'ot{b}'], dma=f'sto{b}')
        return finish(nc, S, out, dbg_outs)


def finish(nc, S, out, dbg_outs):
    S.barrier()
    S.emit()
    return nc, dbg_outs


_CACHE = {}


def _host_tables():
    if 'rope' in _CACHE:
        return _CACHE['rope'], _CACHE['tblidx']
    half = 32; nf = 16
    freqs = (10000.0 ** (-np.arange(nf, dtype=np.float32) / nf)).astype(np.float32)
    rope = {}
    for hf in range(2):
        rng_rows = np.arange(28 * hf, 28 * hf + 36)
        rest = np.arange(36, 64) if hf == 0 else np.arange(0, 28)
        rows = np.concatenate([rng_rows, rest])
        tok = (rows[:, None] * 64 + np.arange(64)[None, :]).reshape(-1)
        r = (tok // 64).astype(np.float32); c = (tok % 64).astype(np.float32)
        cosT = np.ones((4352, 64), np.float32); sinT = np.zeros((4352, 64), np.float32)
        for hi, pos in enumerate((r, c)):
            ang = pos[:, None] * freqs[None, :]
            co = np.cos(ang).astype(np.float32); si = np.sin(ang).astype(np.float32)
            cosT[:4096, hi * 32:hi * 32 + 16] = co; cosT[:4096, hi * 32 + 16:hi * 32 + 32] = co
            sinT[:4096, hi * 32:hi * 32 + 16] = -si; sinT[:4096, hi * 32 + 16:hi * 32 + 32] = si
        rope[hf] = (np.ascontiguousarray(np.tile(cosT, (1, 8))), np.ascontiguousarray(np.tile(sinT, (1, 8))), tok)
    qc = np.arange(64)[:, None]; kc = np.arange(64)[None, :]
    c0 = np.clip(qc - 8, 0, 48)
    valid = (kc >= c0) & (kc < c0 + 16)
    off = np.clip(kc - qc + 15, 0, 30)
    _CACHE['rope'] = rope; _CACHE['tblidx'] = (valid, off)
    return rope, (valid, off)


def kernel(x, c, ctx, c_ctx, w_mod, b_mod, g_pre_mix, g_post_mix, g_pre_ffn, g_post_ffn, w_in, rpb, g_qnorm, g_knorm,
           w_out_a, w_out_b, w_o, w_router, b_router, w_gu, b_gu, w_dn, b_dn):
    f = lambda a: np.ascontiguousarray(np.asarray(a, dtype=np.float32))
    x = f(x); ctx = f(ctx); c = f(c); c_ctx = f(c_ctx)
    rope, (valid, off) = _host_tables()
    rp = f(rpb)[0]
    T = rp[:, :, off]
    T = np.where(valid[None, None], T, np.float32(NEG)).astype(np.float32)
    T = T.transpose(0, 2, 1, 3).reshape(4, 2 * 64, 15 * 64)
    w_in0 = f(w_in)[0]
    qb = w_in0[:, 1792:2304].reshape(1024, 2, 4, 64).transpose(0, 2, 1, 3).reshape(1024, 512)
    w_in_p = w_in0.copy(); w_in_p[:, 1792:2304] = qb
    shared = dict(w_mod=f(w_mod)[0], b_mod=f(b_mod)[0], gvec=np.stack([f(g_pre_mix)[0], f(g_post_mix)[0], f(g_pre_ffn)[0], f(g_post_ffn)[0]]),
                  w_in=w_in_p, tbl=np.ascontiguousarray(T), gqk=np.stack([f(g_qnorm)[0], f(g_knorm)[0]]),
                  w_oa=f(w_out_a)[0], w_ob=f(w_out_b)[0], w_o=f(w_o)[0], w_r=f(w_router)[0], b_r=f(b_router)[0],
                  w_gu=f(w_gu)[0], b_gu=f(b_gu)[0], w_dn=f(w_dn)[0], b_dn=f(b_dn)[0])
    in_maps = []
    for core in range(8):
        b, hf = core // 2, core % 2
        cosT, sinT, tok = rope[hf]
        xcore = np.concatenate([x[b][tok], ctx[b]], axis=0)
        m = dict(shared)
        m.update(xc=np.ascontiguousarray(xcore), cvec=np.stack([c[b], c_ctx]), ropec=cosT, ropes=sinT)
        in_maps.append(m)
    key = ('nc', STAGE, tuple(DEBUG))
    if key not in _CACHE:
        _CACHE[key] = build()
    nc, dbg = _CACHE[key]
    res = run_bass_kernel_spmd(nc, in_maps, core_ids=list(range(8)))
    _CACHE['last'] = res
    outp = np.empty((4, 4096, 1024), np.float32)
    for core in range(8):
        b, hf = core // 2, core % 2
        o = res.results[core]["out"]
        if hf == 0:
            outp[b, 0:2048] = o[0:2048]
        else:
            outp[b, 2048:4096] = o[256:2304]
    return outp
```

```python
import numpy as np
from contextlib import ExitStack
import concourse.bass as bass
import concourse.mybir as mybir
from concourse.bass_utils import run_bass_kernel_spmd

F32 = mybir.dt.float32; BF16 = mybir.dt.bfloat16; I32 = mybir.dt.int32; U8 = mybir.dt.uint8
AF = mybir.ActivationFunctionType; ALU = mybir.AluOpType; AX = mybir.AxisListType
ENG = ('tensor', 'vector', 'scalar', 'gpsimd', 'sync')
DSZ = {F32: 4, BF16: 2, I32: 4, U8: 1}

D = 1024; NTR = 18; TOKR = 2304; NTALL = 34; NKEY = 4352; NE = 32
NBLK = 104; NSLOT = NBLK * 128
EPS = 1e-6; NEG = -30000.0; BIG = 1.0e6
STAGE = 99
SAME_ENG_SYNC = True
DEBUG = []


class Sched:
    def __init__(self, nc, stack):
        self.nc = nc; self.stack = stack
        self.ops = {e: [] for e in ENG}
        self.sems = {}; self.cnt = {}
        self.last_write = {}; self.readers = {}
        self.waited = {e: {} for e in ENG}

    def sem(self, name):
        if name not in self.sems:
            self.sems[name] = self.stack.enter_context(self.nc.semaphore(name)); self.cnt[name] = 0
        return self.sems[name]

    def op(self, eng, fn, reads=(), writes=(), dma=None):
        waits = {}
        isdma_op = dma is not None

        def need(tok):
            if tok is None:
                return
            sname, val, teng, isdma = tok
            if teng == eng and not isdma and not isdma_op and (eng == 'tensor' or not SAME_ENG_SYNC):
                return
            if self.waited[eng].get(sname, 0) >= val:
                return
            waits[sname] = max(waits.get(sname, 0), val)
        for b in reads:
            need(self.last_write.get(b))
        for b in writes:
            need(self.last_write.get(b))
            for r in self.readers.get(b, ()):
                need(r)
        for s, v in waits.items():
            self.waited[eng][s] = v
        if isdma_op:
            sname = dma; inc = 16
        else:
            sname = 'e_' + eng; inc = 1
        self.sem(sname); self.cnt[sname] += inc
        tok = (sname, self.cnt[sname], eng, isdma_op)
        for b in writes:
            self.last_write[b] = tok; self.readers[b] = []
        for b in reads:
            self.readers.setdefault(b, []).append(tok)
        self.ops[eng].append((list(waits.items()), fn, sname, inc))
        return tok

    def barrier(self):
        for e in ENG:
            waits = []
            for s, c in self.cnt.items():
                if c > 0 and self.waited[e].get(s, 0) < c and s != 'e_' + e:
                    waits.append((s, c)); self.waited[e][s] = c
            if waits:
                self.ops[e].append((waits, None, None, None))
        self.last_write = {}; self.readers = {}

    def emit(self):
        with self.nc.Block() as block:
            for eng in ENG:
                ops = self.ops[eng]
                if not ops:
                    continue

                def body(e, ops=ops):
                    for waits, fn, sname, inc in ops:
                        for s, v in waits:
                            e.wait_ge(self.sems[s], v)
                        if fn is not None:
                            fn(e).then_inc(self.sems[sname], inc)
                getattr(block, eng)(body)


class Arena:
    def __init__(self, nc, st, name, nbytes):
        self.t = st.enter_context(nc.sbuf_tensor(name, [128, nbytes], U8)); self.off = 0; self.n = nbytes; self.name = name

    def alloc(self, free_shape, dt):
        n = int(np.prod(free_shape)) * DSZ[dt]
        n_al = (n + 63) // 64 * 64
        assert self.off + n_al <= self.n, (self.name, self.off, n_al, self.n)
        ap = self.t[:, self.off:self.off + n].bitcast(dt)
        self.off += n_al
        if len(free_shape) == 2:
            ap = ap.rearrange("p (a b) -> p a b", a=free_shape[0])
        elif len(free_shape) == 3:
            ap = ap.rearrange("p (a b c) -> p a b c", a=free_shape[0], b=free_shape[1])
        return ap

    def reset(self, off=0):
        self.off = off


def build():
    nc = bass.Bass("TRN2", target_bir_lowering=False)
    dt_in = lambda name, shape, dt=F32: nc.dram_tensor(name, shape, dt, kind="ExternalInput").ap()
    xc = dt_in("xc", [NKEY, D]); cvec = dt_in("cvec", [2, D]); w_mod = dt_in("w_mod", [D, 6 * D]); b_mod = dt_in("b_mod", [6 * D])
    gvec = dt_in("gvec", [4, D]); w_in = dt_in("w_in", [D, 4352]); tbl = dt_in("tbl", [4, 128, 960]); gqk = dt_in("gqk", [2, 64])
    ropec = dt_in("ropec", [NKEY, 512]); ropes = dt_in("ropes", [NKEY, 512])
    w_oa = dt_in("w_oa", [512, D]); w_ob = dt_in("w_ob", [512, D]); w_o = dt_in("w_o", [D, D])
    w_r = dt_in("w_r", [D, NE]); b_r = dt_in("b_r", [NE])
    w_gu = dt_in("w_gu", [NE, D, 2 * D]); b_gu = dt_in("b_gu", [NE, 2 * D]); w_dn = dt_in("w_dn", [NE, D, D]); b_dn = dt_in("b_dn", [NE, D])
    out = nc.dram_tensor("out", [TOKR, D], F32, kind="ExternalOutput").ap()
    hT_scr = nc.dram_tensor("hT_scr", [128, 8, NKEY + 128], BF16, kind="Internal").ap()
    x1_scr = nc.dram_tensor("x1_scr", [TOKR, D], F32, kind="Internal").ap()
    h2_scr = nc.dram_tensor("h2_scr", [TOKR, D], BF16, kind="Internal").ap()
    xs_scr = nc.dram_tensor("xs_scr", [NSLOT, D], BF16, kind="Internal").ap()
    y_scr = nc.dram_tensor("y_scr", [NSLOT, D], F32, kind="Internal").ap()
    dbg_outs = {}
    REG = {}

    def breg(e, v):
        if v not in REG:
            REG[v] = e.to_reg(v)
        return REG[v]

    with ExitStack() as st:
        S = Sched(nc, st)
        op = S.op

        def chain(eng, fns, reads=(), writes=()):
            for fn_ in fns:
                op(eng, fn_, reads=list(reads), writes=list(writes))
        A = Arena(nc, st, "arena", 182 * 1024)
        P = Arena(nc, st, "persist", 24 * 1024)
        pq = [st.enter_context(nc.psum_tensor(f"pq{i}", [128, 1024], F32)) for i in range(3)]
        pbf = [st.enter_context(nc.psum_tensor(f"pbf{i}", [128, 1024], BF16)) for i in range(2)]
        pq = [t_[:, :] for t_ in pq]; pbf = [t_[:, :] for t_ in pbf]
        pf = [pq[i // 2][:, (i % 2) * 512:(i % 2) * 512 + 512] for i in range(6)]
        PF = [f"pf{i}" for i in range(6)]; PB = ["pb0", "pb1"]

        def dump(name, ap, shape, dt):
            if name not in DEBUG:
                return
            S.barrier()
            o = nc.dram_tensor("dbg_" + name, shape, dt, kind="ExternalOutput").ap()
            dbg_outs[name] = o
            op('sync', lambda e: e.dma_start(out=o, in_=ap), dma='dbg')

        rows_late = P.alloc([4, D], F32)
        ident = P.alloc([128], BF16)
        identf = P.alloc([128], F32)
        ones_bf = P.alloc([128], BF16)
        ones_f = P.alloc([128], F32)
        G1, A2, B2, G2 = rows_late[:, 0, :], rows_late[:, 1, :], rows_late[:, 2, :], rows_late[:, 3, :]

        chain('gpsimd', [
            lambda e: e.memset(identf, 0.0),
            lambda e: e.affine_select(out=identf, in_=identf, pattern=[[-1, 128]], compare_op=ALU.not_equal, fill=1.0, base=0, channel_multiplier=1),
            lambda e: e.memset(ones_f, 1.0),
            lambda e: e.tensor_copy(out=ones_bf, in_=ones_f),
            lambda e: e.tensor_copy(out=ident, in_=identf)], writes=['const'])

        A.reset()
        rows_early = A.alloc([4, D], F32)
        A1, B1, A1c, B1c = rows_early[:, 0, :], rows_early[:, 1, :], rows_early[:, 2, :], rows_early[:, 3, :]
        mark_p1 = A.off
        modB = A.alloc([6 * D], F32); modC = A.alloc([2 * D], F32)
        gB = A.alloc([4, D], F32)
        bmB = A.alloc([6 * D], F32)
        cT = A.alloc([2, 8], F32); sT = A.alloc([2, 8], F32)
        rep = A.alloc([2, 8, 128], BF16)
        wm = [A.alloc([8, 512], BF16) for _ in range(2)]
        op('sync', lambda e: e.dma_start(out=cT, in_=cvec.rearrange("j (k p) -> p j k", p=128), allow_slow_non_contiguous=True), writes=['cT'], dma='d_cT')
        for i in range(4):
            op('sync', lambda e, i=i: e.dma_start(out=gB[:, i, :], in_=gvec[i, :].partition_broadcast(128)), writes=['gB'], dma='d_gB')
        op('sync', lambda e: e.dma_start(out=bmB, in_=b_mod.partition_broadcast(128)), writes=['bmB'], dma='d_bmB')
        op('scalar', lambda e: e.activation(out=sT, in_=cT, func=AF.Silu), reads=['cT'], writes=['sT'])

        def mk_rep(e):
            for j in range(2):
                for k in range(8):
                    r = e.tensor_scalar(out=rep[:, j, k, :], in0=ones_f, scalar1=sT[:, j, k:k + 1], scalar2=None, op0=ALU.mult)
            return r
        op('vector', mk_rep, reads=['sT', 'const'], writes=['rep'])
        for n in range(12):
            wb = wm[n % 2]
            op('gpsimd', lambda e, n=n, wb=wb: e.dma_start(out=wb, in_=w_mod[:, n * 512:(n + 1) * 512].rearrange("(k p) c -> p k c", p=128)),
               writes=[f'wm{n % 2}'], dma=f'ld_wm{n % 2}')
            for j in range(2 if n < 4 else 1):
                bk = (2 * n + j) % 6

                def mm(e, j=j, wb=wb, bk=bk):
                    for k in range(8):
                        r = e.matmul(pf[bk], lhsT=rep[:, j, k, :], rhs=wb[:, k, :], start=(k == 0), stop=(k == 7))
                    return r
                op('tensor', mm, reads=['rep', f'wm{n % 2}'], writes=[PF[bk]])
                dst = (modB if j == 0 else modC)[:, n * 512:(n + 1) * 512]
                op('vector', lambda e, dst=dst, bk=bk, n=n: e.tensor_tensor(out=dst, in0=pf[bk], in1=bmB[:, n * 512:(n + 1) * 512], op=ALU.add),
                   reads=[PF[bk], 'bmB'], writes=['modB'])

        def mk_rows(e):
            e.scalar_tensor_tensor(out=A1, in0=modB[:, D:2 * D], scalar=1.0, in1=gB[:, 0, :], op0=ALU.add, op1=ALU.mult)
            e.tensor_copy(out=B1, in_=modB[:, 0:D])
            e.scalar_tensor_tensor(out=A1c, in0=modC[:, D:2 * D], scalar=1.0, in1=gB[:, 0, :], op0=ALU.add, op1=ALU.mult)
            e.tensor_copy(out=B1c, in_=modC[:, 0:D])
            e.tensor_tensor(out=G1, in0=modB[:, 2 * D:3 * D], in1=gB[:, 1, :], op=ALU.mult)
            e.scalar_tensor_tensor(out=A2, in0=modB[:, 4 * D:5 * D], scalar=1.0, in1=gB[:, 2, :], op0=ALU.add, op1=ALU.mult)
            e.tensor_copy(out=B2, in_=modB[:, 3 * D:4 * D])
            return e.tensor_tensor(out=G2, in0=modB[:, 5 * D:6 * D], in1=gB[:, 3, :], op=ALU.mult)
        op('vector', mk_rows, reads=['modB', 'gB'], writes=['rows'])
        dump('rows_early', rows_early, [128, 4, D], F32)
        S.barrier()

        A.reset(mark_p1)
        xt = [A.alloc([D], F32) for _ in range(2)]
        hn = [A.alloc([D], F32) for _ in range(2)]
        hb = [A.alloc([D], BF16) for _ in range(2)]
        hTt = [A.alloc([8, 128], BF16) for _ in range(2)]
        junk = A.alloc([D], F32)
        ss = A.alloc([NTALL], F32); rs = A.alloc([NTALL], F32)

        def rstd(dst, src, n, reads, key):
            op('vector', lambda e: e.tensor_scalar(out=dst, in0=src, scalar1=1.0 / n, scalar2=EPS, op0=ALU.mult, op1=ALU.add), reads=reads, writes=[key])
            op('scalar', lambda e: e.activation(out=dst, in_=dst, func=AF.Sqrt), reads=[key], writes=[key])
            op('vector', lambda e: e.reciprocal(out=dst, in_=dst), reads=[key], writes=[key])

        def p1_a(t):
            b = t % 2
            Ar, Br = (A1, B1) if t < 32 else (A1c, B1c)
            op('sync', lambda e, t=t, b=b: e.dma_start(out=xt[b], in_=xc[t * 128:(t + 1) * 128, :]), writes=[f'xt{b}'], dma=f'ldx{b}')
            op('scalar', lambda e, t=t, b=b: e.activation(out=junk, in_=xt[b], func=AF.Square, accum_out=ss[:, t:t + 1]), reads=[f'xt{b}'], writes=['junk', f'ss{t}'])
            rstd(rs[:, t:t + 1], ss[:, t:t + 1], D, [f'ss{t}'], f'rs{t}')
            op('vector', lambda e, t=t, b=b, Ar=Ar: e.scalar_tensor_tensor(out=hn[b], in0=xt[b], scalar=rs[:, t:t + 1], in1=Ar, op0=ALU.mult, op1=ALU.mult),
               reads=[f'xt{b}', f'rs{t}', 'rows'], writes=[f'hn{b}'])
            op('gpsimd', lambda e, b=b, Br=Br: e.tensor_tensor(out=hb[b], in0=hn[b], in1=Br, op=ALU.add), reads=[f'hn{b}', 'rows'], writes=[f'hb{b}'])


        def p1_b(t):
            b = t % 2
            def tr(e, b=b):
                for k in range(8):
                    r = e.transpose(pbf[b][:, k * 128:(k + 1) * 128], hb[b][:, k * 128:(k + 1) * 128], ident)
                return r
            op('tensor', tr, reads=[f'hb{b}', 'const'], writes=[PB[b]])
            op('scalar', lambda e, b=b: e.activation(out=hTt[b], in_=pbf[b].rearrange("p (k c) -> p k c", k=8), func=AF.Copy), reads=[PB[b]], writes=[f'hTt{b}'])
            op('sync', lambda e, t=t, b=b: e.dma_start(out=hT_scr[:, :, t * 128:(t + 1) * 128], in_=hTt[b]), reads=[f'hTt{b}'], dma=f'sth{b}')

        p1_a(0)
        for t in range(NTALL):
            if t + 1 < NTALL:
                p1_a(t + 1)
            p1_b(t)
        S.barrier()
        if STAGE <= 1:
            return finish(nc, S, out, dbg_outs)

        A.reset()
        o_aT = A.alloc([4, TOKR], BF16)
        mark_oa = A.off
        wA = A.alloc([8, 1536], BF16)
        QaT = A.alloc([4, TOKR], BF16); KaT = A.alloc([4, TOKR], BF16)
        Va_e = A.alloc([18, 512], BF16); Va_o = A.alloc([17, 512], BF16)
        KcaT = A.alloc([4, 256], BF16); Vca = A.alloc([2, 512], BF16)
        tblS = A.alloc([4, 960], F32)
        hTg = [A.alloc([8, 576], BF16) for _ in range(2)]
        sbt = [A.alloc([768], F32) for _ in range(2)]
        pbt = [A.alloc([768], BF16) for _ in range(2)]
        pnt = [A.alloc([768], BF16) for _ in range(2)]
        pTt = [A.alloc([768], BF16) for _ in range(2)]
        sm = A.alloc([2, 4], F32)
        for i, (c0, nm) in enumerate(((0, 'ka'), (512, 'va'), (1280, 'qa'))):
            op('gpsimd', lambda e, i=i, c0=c0: e.dma_start(out=wA[:, :, i * 512:(i + 1) * 512], in_=w_in[:, c0:c0 + 512].rearrange("(k p) c -> p k c", p=128)),
               writes=['wA'], dma='d_wA')
        for p in range(4):
            op('sync', lambda e, p=p: e.dma_start(out=tblS[:, p, :], in_=tbl[p]), writes=['tbl'], dma='d_tbl')
        bkc = [0]

        def nbk():
            bkc[0] = (bkc[0] + 1) % 6
            return bkc[0]

        def proj_fm(lhs_cols, rhs_ap, ntok, dst, scale=None):
            bk = nbk()

            def mm(e):
                for k in range(8):
                    r = e.matmul(pf[bk][:, 0:ntok], lhsT=wA[:, k, lhs_cols[0]:lhs_cols[1]], rhs=rhs_ap(k), start=(k == 0), stop=(k == 7))
                return r
            op('tensor', mm, reads=['wA', 'hTg'], writes=[PF[bk]])
            if scale is None:
                op('scalar', lambda e: e.activation(out=dst, in_=pf[bk][:, 0:ntok], func=AF.Copy), reads=[PF[bk]], writes=['naprep'])
            else:
                op('scalar', lambda e: e.activation(out=dst, in_=pf[bk][:, 0:ntok], func=AF.Copy, scale=scale), reads=[PF[bk]], writes=['naprep'])

        def proj_tm(lhs_ap, dst):
            bk = nbk()

            def mm(e):
                for k in range(8):
                    r = e.matmul(pf[bk], lhsT=lhs_ap(k), rhs=wA[:, k, 512:1024], start=(k == 0), stop=(k == 7))
                return r
            op('tensor', mm, reads=['wA', 'hTg'], writes=[PF[bk]])
            op('vector', lambda e: e.tensor_copy(out=dst, in_=pf[bk]), reads=[PF[bk]], writes=['naprep'])

        for g in range(5):
            hg = hTg[g % 2]
            ntok = 512 if g < 4 else 256
            op('sync', lambda e, g=g, hg=hg: e.dma_start(out=hg, in_=hT_scr[:, :, g * 512:g * 512 + 576]), reads=['hT_scr'], writes=['hTg'], dma=f'ldh{g % 2}')
            for c in range(4):
                proj_fm((c * 128, (c + 1) * 128), lambda k, hg=hg, ntok=ntok: hg[:, k, 0:ntok], ntok, KaT[:, c, g * 512:g * 512 + ntok])
                proj_fm((1024 + c * 128, 1024 + (c + 1) * 128), lambda k, hg=hg, ntok=ntok: hg[:, k, 0:ntok], ntok, QaT[:, c, g * 512:g * 512 + ntok], scale=0.125)
            for j in range(ntok // 128):
                proj_tm(lambda k, hg=hg, j=j: hg[:, k, j * 128:(j + 1) * 128], Va_e[:, 4 * g + j, :])
                if 4 * g + j <= 16:
                    proj_tm(lambda k, hg=hg, j=j: hg[:, k, 64 + j * 128:64 + (j + 1) * 128], Va_o[:, 4 * g + j, :])
        hg = hTg[1]
        op('sync', lambda e, hg=hg: e.dma_start(out=hg[:, :, 0:256], in_=hT_scr[:, :, 4096:4352]), reads=['hT_scr'], writes=['hTg'], dma='ldh1')
        for c in range(4):
            proj_fm((c * 128, (c + 1) * 128), lambda k, hg=hg: hg[:, k, 0:256], 256, KcaT[:, c, :])
        for j in range(2):
            proj_tm(lambda k, hg=hg, j=j: hg[:, k, j * 128:(j + 1) * 128], Vca[:, j, :])
        dump('QaT', QaT, [128, 4, TOKR], BF16); dump('KaT', KaT, [128, 4, TOKR], BF16); dump('Va_e', Va_e, [128, 18, 512], BF16)

        na_its = [(l, p) for l in range(36) for p in range(4)]

        def na_ctx(it):
            l, p = na_its[it]
            start = min(max(l - 4, 0), 28); u0 = start - l + 7; tok0 = start * 64
            b = it % 2
            return l, p, start, u0, tok0, b

        def na_stage1(it):
            l, p, start, u0, tok0, b = na_ctx(it)
            sl, sc, po = pf[b], pf[2 + b], pf[4 + b]
            sb_, pb_, pn_, pT_ = sbt[b], pbt[b], pnt[b], pTt[b]

            def qk(e, l=l, p=p, tok0=tok0, sl=sl, sc=sc):
                for hh in range(2):
                    ps_ = slice(hh * 64, hh * 64 + 64)
                    e.matmul(sl[ps_, :], lhsT=QaT[ps_, p, l * 64:(l + 1) * 64], rhs=KaT[ps_, p, tok0:tok0 + 512], start=True, stop=True, tile_position=(hh * 64, hh * 64))
                    r = e.matmul(sc[ps_, 0:256], lhsT=QaT[ps_, p, l * 64:(l + 1) * 64], rhs=KcaT[ps_, p, :], start=True, stop=True, tile_position=(hh * 64, hh * 64))
                return r
            op('tensor', qk, reads=['naprep'], writes=[PF[b], PF[2 + b]])
            op('vector', lambda e, sb_=sb_, sl=sl, p=p, u0=u0: e.tensor_tensor(out=sb_[:, 0:512], in0=sl, in1=tblS[:, p, u0 * 64:u0 * 64 + 512], op=ALU.add),
               reads=[PF[b], 'tbl'], writes=[f'sbA{b}'])
            op('scalar', lambda e, sb_=sb_, sc=sc: e.activation(out=sb_[:, 512:768], in_=sc[:, 0:256], func=AF.Copy), reads=[PF[2 + b]], writes=[f'sbB{b}'])

            chain('vector', [
                lambda e, sb_=sb_, b=b: e.tensor_reduce(out=sm[:, b, 0:1], in_=sb_, axis=AX.X, op=ALU.max),
                lambda e, b=b: e.tensor_scalar(out=sm[:, b, 1:2], in0=sm[:, b, 0:1], scalar1=-1.0, scalar2=None, op0=ALU.mult)],
                reads=[f'sbA{b}', f'sbB{b}'], writes=[f'negm{b}'])
            op('scalar', lambda e, sb_=sb_, pb_=pb_, b=b: e.activation(out=pb_, in_=sb_, func=AF.Exp, bias=sm[:, b, 1:2], scale=1.0, accum_out=sm[:, b, 2:3]),
               reads=[f'sbA{b}', f'sbB{b}', f'negm{b}'], writes=[f'pb{b}', f'sum{b}'])
            op('vector', lambda e, b=b: e.reciprocal(out=sm[:, b, 3:4], in_=sm[:, b, 2:3]), reads=[f'sum{b}'], writes=[f'rsum{b}'])
            op('gpsimd', lambda e, pn_=pn_, pb_=pb_, b=b: e.tensor_scalar(out=pn_, in0=pb_, scalar1=sm[:, b, 3:4], scalar2=None, op0=ALU.mult),
               reads=[f'pb{b}', f'rsum{b}'], writes=[f'pn{b}'])


        def na_stage2(it):
            l, p, start, u0, tok0, b = na_ctx(it)
            sl, sc, po = pf[b], pf[2 + b], pf[4 + b]
            sb_, pb_, pn_, pT_ = sbt[b], pbt[b], pnt[b], pTt[b]
            def trp(e, pn_=pn_, b=b):
                for c in range(6):
                    r = e.transpose(pbf[b][:, c * 128:(c + 1) * 128], pn_[:, c * 128:(c + 1) * 128], ident)
                return r
            op('tensor', trp, reads=[f'pn{b}', 'const'], writes=[PB[b]])
            op('scalar', lambda e, pT_=pT_, b=b: e.activation(out=pT_, in_=pbf[b][:, 0:768], func=AF.Copy), reads=[PB[b]], writes=[f'pT{b}'])

            def pv(e, pT_=pT_, po=po, p=p, start=start):
                for hh in range(2):
                    for c in range(6):
                        if c < 4:
                            V = Va_e[:, start // 2 + c, :] if start % 2 == 0 else Va_o[:, (start - 1) // 2 + c, :]
                        else:
                            V = Vca[:, c - 4, :]
                        r = e.matmul(po[hh * 64:hh * 64 + 64, 0:64], lhsT=V[:, p * 128 + hh * 64:p * 128 + hh * 64 + 64],
                                     rhs=pT_[:, c * 128 + hh * 64:c * 128 + hh * 64 + 64], start=(c == 0), stop=(c == 5), tile_position=(0, hh * 64))
                return r
            op('tensor', pv, reads=[f'pT{b}', 'naprep'], writes=[PF[4 + b]])
            op('vector', lambda e, po=po, p=p, l=l: e.tensor_copy(out=o_aT[:, p, l * 64:(l + 1) * 64], in_=po[:, 0:64]), reads=[PF[4 + b]], writes=['o_aT'])

        for step in range(len(na_its) + 1):
            if step < len(na_its):
                na_stage1(step)
            if step >= 1:
                na_stage2(step - 1)
        dump('o_aT', o_aT, [128, 4, TOKR], BF16)
        S.barrier()
        if STAGE <= 2:
            return finish(nc, S, out, dbg_outs)

        A.reset(mark_oa)
        o_bT = A.alloc([8, TOKR], BF16)
        mark_ob = A.off
        wB = A.alloc([8, 768], BF16)
        QbT = A.alloc([4, TOKR], BF16); KbT = A.alloc([NKEY], BF16)
        Vb = A.alloc([NTALL, 2, 65], BF16)
        gqB = A.alloc([2, 64], F32)
        gtmp = A.alloc([2, 64], F32)
        negC = A.alloc([4], F32)
        hTg = [A.alloc([8, 512], BF16) for _ in range(2)]
        rc = [A.alloc([512], F32) for _ in range(2)]; rsn = [A.alloc([512], F32) for _ in range(2)]
        sq = A.alloc([640], F32); ssh = A.alloc([2, 16], F32)
        qn = A.alloc([640], F32); t1 = A.alloc([640], F32); t2 = A.alloc([640], F32)
        qbb = [A.alloc([640], BF16) for _ in range(2)]
        pTg = [A.alloc([512], BF16) for _ in range(4)]
        osb = [A.alloc([512], F32) for _ in range(2)]
        rec = [A.alloc([512], F32) for _ in range(2)]
        op('gpsimd', lambda e: e.dma_start(out=wB[:, :, 0:256], in_=w_in[:, 1024:1280].rearrange("(k p) c -> p k c", p=128)), writes=['wB'], dma='d_wB')
        op('gpsimd', lambda e: e.dma_start(out=wB[:, :, 256:768], in_=w_in[:, 1792:2304].rearrange("(k p) c -> p k c", p=128)), writes=['wB'], dma='d_wB')
        for i in range(2):
            op('sync', lambda e, i=i: e.dma_start(out=gtmp[:, i, :], in_=gqk[i, :].partition_broadcast(128)), writes=['gtmp'], dma='d_gt')

        qv = qn[:, 0:128].rearrange("p (a b) -> p a b", a=2)
        chain('vector', [
            lambda e: e.tensor_scalar(out=gqB[:, 0, :], in0=gtmp[:, 0, :], scalar1=0.125, scalar2=None, op0=ALU.mult),
            lambda e: e.tensor_copy(out=gqB[:, 1, :], in_=gtmp[:, 1, :]),
            lambda e: e.tensor_scalar(out=qv, in0=gtmp, scalar1=-1.0, scalar2=None, op0=ALU.mult),
            lambda e: e.tensor_tensor(out=gtmp, in0=gtmp, in1=qv, op=ALU.max),
            lambda e: e.tensor_reduce(out=negC[:, 0:1], in_=gtmp[:, 0, :], axis=AX.X, op=ALU.max),
            lambda e: e.tensor_reduce(out=negC[:, 1:2], in_=gtmp[:, 1, :], axis=AX.X, op=ALU.max),
            lambda e: e.tensor_tensor(out=negC[:, 2:3], in0=negC[:, 0:1], in1=negC[:, 1:2], op=ALU.mult),
            lambda e: e.tensor_scalar(out=negC[:, 3:4], in0=negC[:, 2:3], scalar1=-8.0, scalar2=None, op0=ALU.mult),
            lambda e: e.memset(Vb[:, :, :, 64:65], 1.0)], reads=['gtmp'], writes=['gtmp', 'gqB', 'negC', 'Vb', 'qn'])

        def normrope(src, H, gi, b, dst, tagr):
            W = H * 64
            op('scalar', lambda e: e.activation(out=sq[:, 0:W], in_=src, func=AF.Square), reads=tagr, writes=['sq'])

            op('vector', lambda e: e.tensor_reduce(out=ssh[:, 0, 0:H], in_=sq[:, 0:W].rearrange("p (h d) -> p h d", d=64), axis=AX.X, op=ALU.add), reads=['sq'], writes=['ssh0'])
            rstd(ssh[:, 1, 0:H], ssh[:, 0, 0:H], 64, ['ssh0'], 'ssh1')

            def n1(e):
                for h in range(H):
                    r = e.scalar_tensor_tensor(out=qn[:, h * 64:(h + 1) * 64], in0=src[:, h * 64:(h + 1) * 64], scalar=ssh[:, 1, h:h + 1], in1=gqB[:, gi, :],
                                               op0=ALU.mult, op1=ALU.mult)
                return r
            op('vector', n1, reads=['ssh1', 'gqB'] + tagr, writes=['qn'])
            op('vector', lambda e: e.tensor_tensor(out=t1[:, 0:W], in0=qn[:, 0:W], in1=rc[b][:, 0:W], op=ALU.mult), reads=['qn', f'rc{b}'], writes=['t1'])

            def r2(e):
                q4 = qn[:, 0:W].rearrange("p (a s f) -> p a s f", s=2, f=16)
                s4 = rsn[b][:, 0:W].rearrange("p (a s f) -> p a s f", s=2, f=16)
                o4 = t2[:, 0:W].rearrange("p (a s f) -> p a s f", s=2, f=16)
                e.tensor_tensor(out=o4[:, :, 0, :], in0=q4[:, :, 1, :], in1=s4[:, :, 0, :], op=ALU.mult)
                return e.tensor_tensor(out=o4[:, :, 1, :], in0=q4[:, :, 0, :], in1=s4[:, :, 1, :], op=ALU.mult)
            op('vector', r2, reads=['qn', f'rsn{b}'], writes=['t2'])
            op('vector', lambda e: e.tensor_tensor(out=dst, in0=t1[:, 0:W], in1=t2[:, 0:W], op=ALU.add), reads=['t1', 't2'], writes=['qbb'])

        for t in range(NTALL):
            b = t % 2
            g = t // 4
            hg = hTg[g % 2]
            if t % 4 == 0:
                n = min(512, NKEY - g * 512)
                op('sync', lambda e, g=g, hg=hg, n=n: e.dma_start(out=hg[:, :, 0:n], in_=hT_scr[:, :, g * 512:g * 512 + n]), reads=['hT_scr'], writes=[f'hTg{g % 2}'], dma=f'ldh{g % 2}')
            j = t % 4
            op('sync', lambda e, t=t, b=b: e.dma_start(out=rc[b], in_=ropec[t * 128:(t + 1) * 128, :]), writes=[f'rc{b}'], dma=f'ldr{b}')
            op('sync', lambda e, t=t, b=b: e.dma_start(out=rsn[b], in_=ropes[t * 128:(t + 1) * 128, :]), writes=[f'rsn{b}'], dma=f'lds{b}')
            bk = nbk()

            def mmkv(e, hg=hg, j=j, bk=bk):
                for k in range(8):
                    r = e.matmul(pf[bk][:, 0:256], lhsT=hg[:, k, j * 128:(j + 1) * 128], rhs=wB[:, k, 0:256], start=(k == 0), stop=(k == 7))
                return r
            op('tensor', mmkv, reads=['wB', f'hTg{g % 2}'], writes=[PF[bk]])
            op('scalar', lambda e, t=t, bk=bk: e.activation(out=Vb[:, t, :, 0:64], in_=pf[bk][:, 128:256].rearrange("p (h d) -> p h d", d=64), func=AF.Copy),
               reads=[PF[bk]], writes=['Vb'])
            normrope(pf[bk][:, 0:128], 2, 1, b, qbb[b][:, 0:128], [PF[bk]])
            op('tensor', lambda e, b=b: e.transpose(pbf[b][:, 0:128], qbb[b][:, 0:128], ident), reads=['qbb', 'const'], writes=[PB[b]])
            op('scalar', lambda e, t=t, b=b: e.activation(out=KbT[:, t * 128:(t + 1) * 128], in_=pbf[b][:, 0:128], func=AF.Copy), reads=[PB[b]], writes=['KbT'])
            if t < NTR:
                bk2 = nbk()

                def mmq(e, hg=hg, j=j, bk2=bk2):
                    for k in range(8):
                        r = e.matmul(pf[bk2], lhsT=hg[:, k, j * 128:(j + 1) * 128], rhs=wB[:, k, 256:768], start=(k == 0), stop=(k == 7))
                    return r
                op('tensor', mmq, reads=['wB', f'hTg{g % 2}'], writes=[PF[bk2]])
                normrope(pf[bk2], 8, 0, b, qbb[b][:, 0:512], [PF[bk2]])

                def trq(e, b=b):
                    for gg in range(4):
                        r = e.transpose(pbf[b][:, 128 + gg * 128:128 + (gg + 1) * 128], qbb[b][:, gg * 128:(gg + 1) * 128], ident)
                    return r
                op('tensor', trq, reads=['qbb', 'const'], writes=[PB[b]])
                op('scalar', lambda e, t=t, b=b: e.activation(out=QbT[:, :, t * 128:(t + 1) * 128], in_=pbf[b][:, 128:640].rearrange("p (g c) -> p g c", g=4), func=AF.Copy),
                   reads=[PB[b]], writes=['QbT'])
        dump('QbT', QbT, [128, 4, TOKR], BF16); dump('KbT', KbT, [128, NKEY], BF16); dump('Vb', Vb, [128, NTALL, 2, 65], BF16)

        chunks = [(kvh, qt, c) for kvh in range(2) for qt in range(NTR) for c in range(NTALL)]
        LA = 2
        pending = []

        def gq_S(i):
            kvh, qt, c = chunks[i]
            pr = slice(kvh * 64, kvh * 64 + 64)
            sb_ = i % 4
            st_ = pf[sb_]
            op('tensor', lambda e: e.matmul(st_, lhsT=KbT[pr, c * 128:(c + 1) * 128], rhs=QbT[pr, :, qt * 128:(qt + 1) * 128],
                                            start=True, stop=True, tile_position=(kvh * 64, 0)),
               reads=['KbT', 'QbT'], writes=[PF[sb_]])
            op('scalar', lambda e: e.activation(out=pTg[sb_], in_=st_, func=AF.Exp, bias=negC[:, 3:4], scale=1.0), reads=[PF[sb_], 'negC'], writes=[f'pTg{sb_}'])

        def gq_PV(i, step):
            kvh, qt, c = chunks[i]
            sb_ = i % 4
            ob = (kvh * NTR + qt) % 2
            po = pf[4 + ob]
            bk = 4 + ob
            op('tensor', lambda e: e.matmul(po[0:65, :], lhsT=Vb[:, c, kvh, :], rhs=pTg[sb_], start=(c == 0), stop=(c == NTALL - 1)),
               reads=[f'pTg{sb_}', 'Vb'], writes=[PF[bk]])
            if c == NTALL - 1:
                op('scalar', lambda e: e.activation(out=osb[ob][0:65, :], in_=po[0:65, :], func=AF.Copy), reads=[PF[bk]], writes=[f'osb{ob}'])
                op('vector', lambda e: e.reciprocal(out=rec[ob][64:65, :], in_=osb[ob][64:65, :]), reads=[f'osb{ob}'], writes=[f'rec{ob}'])

                def fin():
                    op('tensor', lambda e: e.matmul(po[0:64, :], lhsT=ones_f[64:65, 0:64], rhs=rec[ob][64:65, :], start=True, stop=True), reads=[f'rec{ob}', 'const'], writes=[PF[bk]])
                    op('vector', lambda e: e.tensor_tensor(out=o_bT[0:64, kvh * 4:(kvh + 1) * 4, qt * 128:(qt + 1) * 128],
                                                           in0=osb[ob][0:64, :].rearrange("p (g t) -> p g t", g=4),
                                                           in1=po[0:64, :].rearrange("p (g t) -> p g t", g=4), op=ALU.mult),
                       reads=[f'osb{ob}', PF[bk]], writes=['o_bT'])
                pending.append((step + 4, fin))

        for step in range(len(chunks) + LA + 8):
            if step < len(chunks):
                gq_S(step)
            if LA <= step < len(chunks) + LA:
                gq_PV(step - LA, step)
            for due, fn_ in list(pending):
                if due <= step:
                    fn_(); pending.remove((due, fn_))
        assert not pending
        dump('o_bT', o_bT, [128, 8, TOKR], BF16)
        S.barrier()
        if STAGE <= 3:
            return finish(nc, S, out, dbg_outs)

        A.reset(mark_ob)
        wG = A.alloc([8, 2048], BF16); wOA = A.alloc([4, D], BF16); wOB = A.alloc([8, D], BF16); wO = A.alloc([8, D], BF16)
        wR = A.alloc([8, NE], BF16); bR = A.alloc([NE], BF16)
        hTg = [A.alloc([8, 512], BF16)] * 2
        zT = [A.alloc([8, 512], BF16) for _ in range(2)]
        sga = A.alloc([512], F32); sgb = A.alloc([512], F32)
        xt4 = A.alloc([D], F32); x1t = [A.alloc([D], F32) for _ in range(2)]; tmpf = A.alloc([D], F32)
        h2b = [A.alloc([D], BF16) for _ in range(2)]; h2T = A.alloc([8, 128], BF16)
        lg = P.alloc([NTR, NE], F32); mx8 = P.alloc([NTR, 8], F32); posA = P.alloc([NTR, NE], F32)
        wts = P.alloc([NTR, 4], F32); sloti = P.alloc([NTR * 4], I32)
        mask = A.alloc([NE], F32); maskb = A.alloc([NE], BF16); cntp = P.alloc([NE], F32)
        sms = A.alloc([NTR, 8], F32); e4 = A.alloc([4], F32)
        utri = A.alloc([128], BF16); utf = A.alloc([128], F32)
        mark_route = A.off
        for (dst, src, nm) in ((wG[:, :, 0:1024], w_in[:, 2304:3328], 0), (wG[:, :, 1024:2048], w_in[:, 3328:4352], 1), (wO, w_o, 2)):
            op('gpsimd', lambda e, dst=dst, src=src: e.dma_start(out=dst, in_=src.rearrange("(k p) c -> p k c", p=128)), writes=['wM'], dma='d_wM')
        op('gpsimd', lambda e: e.dma_start(out=wOA, in_=w_oa.rearrange("(k p) c -> p k c", p=128)), writes=['wM'], dma='d_wM')
        op('gpsimd', lambda e: e.dma_start(out=wOB[0:64], in_=w_ob.rearrange("(h d) c -> d h c", d=64)), writes=['wM'], dma='d_wM')
        op('gpsimd', lambda e: e.dma_start(out=wR, in_=w_r.rearrange("(k p) c -> p k c", p=128)), writes=['wM'], dma='d_wM')
        op('gpsimd', lambda e: e.dma_start(out=bR[0:1, :], in_=b_r.rearrange("(o n) -> o n", o=1)), writes=['wM'], dma='d_wM')

        chain('gpsimd', [
            lambda e: e.memset(utf, 1.0),
            lambda e: e.affine_select(out=utf, in_=utf, pattern=[[1, 128]], compare_op=ALU.is_gt, fill=0.0, base=0, channel_multiplier=-1),
            lambda e: e.memset(cntp, 0.0),
            lambda e: e.tensor_copy(out=utri, in_=utf)], writes=['utri', 'route'])

        for g in range(5):
            hg = hTg[g % 2]; z = zT[g % 2]
            ntok = 512 if g < 4 else 256
            tk = slice(g * 512, g * 512 + ntok)
            op('sync', lambda e, g=g, hg=hg, ntok=ntok: e.dma_start(out=hg[:, :, 0:ntok], in_=hT_scr[:, :, g * 512:g * 512 + ntok]), reads=['hT_scr'], writes=[f'hTg{g % 2}'], dma=f'ldh{g % 2}')
            for oc in range(8):
                def mm4(e, hg=hg, oc=oc, ntok=ntok, tk=tk):
                    for k in range(8):
                        e.matmul(pf[0][:, 0:ntok], lhsT=wG[:, k, oc * 128:(oc + 1) * 128], rhs=hg[:, k, 0:ntok], start=(k == 0), stop=(k == 7))
                    for k in range(8):
                        e.matmul(pf[1][:, 0:ntok], lhsT=wG[:, k, 1024 + oc * 128:1024 + (oc + 1) * 128], rhs=hg[:, k, 0:ntok], start=(k == 0), stop=(k == 7))
                    for k in range(4):
                        e.matmul(pf[2][:, 0:ntok], lhsT=wOA[:, k, oc * 128:(oc + 1) * 128], rhs=o_aT[:, k, tk], start=(k == 0), stop=(k == 3))
                    for k in range(8):
                        r = e.matmul(pf[3][:, 0:ntok], lhsT=wOB[0:64, k, oc * 128:(oc + 1) * 128], rhs=o_bT[0:64, k, tk], start=(k == 0), stop=(k == 7))
                    return r
                op('tensor', mm4, reads=['wM', f'hTg{g % 2}', 'o_aT', 'o_bT'], writes=[PF[0], PF[1], PF[2], PF[3]])

                def sg(e, ntok=ntok):
                    e.activation(out=sga[:, 0:ntok], in_=pf[0][:, 0:ntok], func=AF.Sigmoid)
                    return e.activation(out=sgb[:, 0:ntok], in_=pf[1][:, 0:ntok], func=AF.Sigmoid)
                op('scalar', sg, reads=[PF[0], PF[1]], writes=['sg'])

                def zz(e, ntok=ntok):
                    e.tensor_tensor(out=sga[:, 0:ntok], in0=sga[:, 0:ntok], in1=pf[2][:, 0:ntok], op=ALU.mult)
                    return e.tensor_tensor(out=sgb[:, 0:ntok], in0=sgb[:, 0:ntok], in1=pf[3][:, 0:ntok], op=ALU.mult)
                op('vector', zz, reads=['sg', PF[2], PF[3]], writes=['sg2'])
                op('gpsimd', lambda e, z=z, oc=oc, ntok=ntok: e.tensor_tensor(out=z[:, oc, 0:ntok], in0=sga[:, 0:ntok], in1=sgb[:, 0:ntok], op=ALU.add),
                   reads=['sg2'], writes=[f'zT{g % 2}', 'sg'])
            for j in range(ntok // 128):
                t = 4 * g + j
                yb = pq[2]
                b = t % 2

                def mmy(e, z=z, j=j, yb=yb):
                    for n in range(2):
                        for k in range(8):
                            r = e.matmul(yb[:, n * 512:(n + 1) * 512], lhsT=z[:, k, j * 128:(j + 1) * 128], rhs=wO[:, k, n * 512:(n + 1) * 512], start=(k == 0), stop=(k == 7))
                    return r
                op('tensor', mmy, reads=['wM', f'zT{g % 2}'], writes=[PF[4], PF[5]])
                op('sync', lambda e, t=t: e.dma_start(out=xt4, in_=xc[t * 128:(t + 1) * 128, :]), writes=['xt'], dma='ldx0')
                op('scalar', lambda e, yb=yb, t=t: e.activation(out=tmpf, in_=yb, func=AF.Square, accum_out=sms[:, t, 0:1]), reads=[PF[4], PF[5]], writes=['tmpf', 'ssy'])
                rstd(sms[:, t, 1:2], sms[:, t, 0:1], D, ['ssy'], 'rsy')
                op('vector', lambda e, yb=yb, t=t: e.scalar_tensor_tensor(out=tmpf, in0=yb, scalar=sms[:, t, 1:2], in1=G1, op0=ALU.mult, op1=ALU.mult),
                   reads=[PF[4], PF[5], 'rsy', 'rows'], writes=['tmpf'])
                op('gpsimd', lambda e, b=b: e.tensor_tensor(out=x1t[b], in0=tmpf, in1=xt4, op=ALU.add), reads=['tmpf', 'xt'], writes=[f'x1t{b}'])
                op('sync', lambda e, t=t, b=b: e.dma_start(out=x1_scr[t * 128:(t + 1) * 128, :], in_=x1t[b]), reads=[f'x1t{b}'], dma=f'stx{b}')
                op('scalar', lambda e, t=t, b=b: e.activation(out=tmpf, in_=x1t[b], func=AF.Square, accum_out=sms[:, t, 2:3]), reads=[f'x1t{b}'], writes=['tmpf', 'ss2'])
                rstd(sms[:, t, 3:4], sms[:, t, 2:3], D, ['ss2'], 'rs2')
                op('vector', lambda e, t=t, b=b: e.scalar_tensor_tensor(out=tmpf, in0=x1t[b], scalar=sms[:, t, 3:4], in1=A2, op0=ALU.mult, op1=ALU.mult),
                   reads=[f'x1t{b}', 'rs2', 'rows'], writes=['tmpf'])
                op('gpsimd', lambda e, b=b: e.tensor_tensor(out=h2b[b], in0=tmpf, in1=B2, op=ALU.add), reads=['tmpf', 'rows'], writes=[f'h2b{b}'])
                op('sync', lambda e, t=t, b=b: e.dma_start(out=h2_scr[t * 128:(t + 1) * 128, :], in_=h2b[b]), reads=[f'h2b{b}'], dma=f'sth{b}')

                def trh(e, b=b):
                    for k in range(8):
                        r = e.transpose(pbf[b][:, k * 128:(k + 1) * 128], h2b[b][:, k * 128:(k + 1) * 128], ident)
                    return r
                op('tensor', trh, reads=[f'h2b{b}', 'const'], writes=[PB[b]])
                op('scalar', lambda e, b=b: e.activation(out=h2T, in_=pbf[b].rearrange("p (k c) -> p k c", k=8), func=AF.Copy), reads=[PB[b]], writes=['h2T'])

                def mml(e):
                    for k in range(8):
                        e.matmul(pf[0][:, 0:NE], lhsT=h2T[:, k, :], rhs=wR[:, k, :], start=(k == 0), stop=False)
                    return e.matmul(pf[0][:, 0:NE], lhsT=ones_bf[0:1, :], rhs=bR[0:1, :], start=False, stop=True)
                op('tensor', mml, reads=['h2T', 'wM', 'const'], writes=[PF[0]])

                chain('vector', [
                    lambda e, t=t: e.tensor_copy(out=lg[:, t, :], in_=pf[0][:, 0:NE]),
                    lambda e, t=t: e.max(out=mx8[:, t, :], in_=lg[:, t, :]),
                    lambda e, t=t: e.tensor_scalar(out=mask, in0=lg[:, t, :], scalar1=mx8[:, t, 3:4], scalar2=None, op0=ALU.is_ge),
                    lambda e: e.tensor_copy(out=maskb, in_=mask),
                    lambda e, t=t: e.tensor_scalar(out=sms[:, t, 4:5], in0=mx8[:, t, 0:1], scalar1=-1.0, scalar2=None, op0=ALU.mult)],
                    reads=[PF[0]], writes=['lg', 'maskb', 'negmx'])
                op('scalar', lambda e, t=t: e.activation(out=e4, in_=mx8[:, t, 0:4], func=AF.Exp, bias=sms[:, t, 4:5], scale=1.0, accum_out=sms[:, t, 5:6]),
                   reads=['lg', 'negmx'], writes=['e4'])

                def mmc(e):
                    e.matmul(pf[1][:, 0:NE], lhsT=utri, rhs=maskb, start=True, stop=True)
                    return e.matmul(pf[1][:, NE:2 * NE], lhsT=ones_bf, rhs=maskb, start=True, stop=True)
                op('tensor', mmc, reads=['maskb', 'utri', 'const'], writes=[PF[1]])

                chain('vector', [
                    lambda e, t=t: e.reciprocal(out=sms[:, t, 6:7], in_=sms[:, t, 5:6]),
                    lambda e, t=t: e.tensor_scalar(out=wts[:, t, :], in0=e4, scalar1=sms[:, t, 6:7], scalar2=None, op0=ALU.mult),
                    lambda e, t=t: e.tensor_tensor(out=posA[:, t, :], in0=pf[1][:, 0:NE], in1=cntp, op=ALU.add),
                    lambda e: e.tensor_tensor(out=cntp, in0=cntp, in1=pf[1][:, NE:2 * NE], op=ALU.add)],
                    reads=['e4', PF[1]], writes=['route'])
        dump('lg', lg, [128, NTR, NE], F32); dump('posA', posA, [128, NTR, NE], F32); dump('cntp', cntp, [128, NE], F32)
        S.barrier()
        if STAGE <= 4:
            return finish(nc, S, out, dbg_outs)

        A.reset()
        ci = A.alloc([NE], I32); padf = A.alloc([NE], F32); padT = A.alloc([128], F32); ltri = A.alloc([NE], F32)
        basef = A.alloc([NE], F32); pend = A.alloc([NE], F32)
        thr = A.alloc([NBLK], F32); EB = A.alloc([NBLK], F32); skp = A.alloc([NBLK], F32)
        idxw_f = A.alloc([NBLK], F32); idxb_f = A.alloc([NBLK], F32); pidx = A.alloc([1], F32)
        idxw = A.alloc([NBLK], I32); idxb = A.alloc([NBLK], I32)
        idxw8_f = A.alloc([8, NBLK], F32); idxw8 = A.alloc([8, NBLK], I32)
        idxd_f = A.alloc([4, NBLK], F32); idxd = A.alloc([4, NBLK], I32); idxd0 = A.alloc([NBLK], F32); pidx4 = A.alloc([1], F32)
        slot2 = A.alloc([NE], F32); slotf = A.alloc([NTR * 4], F32); tmp32 = A.alloc([NE], F32)
        h2l = [A.alloc([D], BF16) for _ in range(2)]

        chain('vector', [
            lambda e: e.tensor_scalar(out=padf, in0=cntp, scalar1=127.0, scalar2=None, op0=ALU.add),
            lambda e: e.tensor_copy(out=ci, in_=padf),
            lambda e: e.tensor_single_scalar(out=ci, in_=ci, scalar=7, op=ALU.arith_shift_right),
            lambda e: e.tensor_single_scalar(out=ci, in_=ci, scalar=7, op=ALU.logical_shift_left),
            lambda e: e.tensor_copy(out=padf, in_=ci)], reads=['route'], writes=['padf'])

        chain('gpsimd', [
            lambda e: e.memset(ltri, 1.0),
            lambda e: e.affine_select(out=ltri, in_=ltri, pattern=[[1, NE]], compare_op=ALU.is_gt, fill=0.0, base=0, channel_multiplier=-1),
            lambda e: e.iota(thr, pattern=[[128, NBLK]], base=0, channel_multiplier=0, allow_small_or_imprecise_dtypes=True),
            lambda e: e.iota(pidx, pattern=[[0, 1]], base=0, channel_multiplier=1, allow_small_or_imprecise_dtypes=True)], writes=['ltri'])
        op('tensor', lambda e: e.transpose(pq[0][0:NE, 0:128], padf, identf), reads=['padf', 'const'], writes=[PF[0]])
        op('vector', lambda e: e.tensor_copy(out=padT[0:NE, :], in_=pq[0][0:NE, 0:128]), reads=[PF[0]], writes=['padT'])
        op('tensor', lambda e: e.matmul(pf[1][:, 0:NE], lhsT=padT[0:NE, :], rhs=ltri[0:NE, :], start=True, stop=True), reads=['padT', 'ltri'], writes=[PF[1]])

        lay2 = [
            lambda e: e.tensor_copy(out=basef, in_=pf[1][:, 0:NE]),
            lambda e: e.tensor_tensor(out=pend, in0=basef, in1=padf, op=ALU.add),
            lambda e: e.memset(EB, 0.0)]
        for ex in range(NE):
            lay2.append(lambda e, ex=ex: e.scalar_tensor_tensor(out=EB, in0=thr, scalar=pend[:, ex:ex + 1], in1=EB, op0=ALU.is_ge, op1=ALU.add))
        lay2 += [
            lambda e: e.tensor_scalar(out=EB, in0=EB, scalar1=float(NE - 1), scalar2=None, op0=ALU.min),
            lambda e: e.memset(skp, 0.0),
            lambda e: e.tensor_tensor(out=skp[:, 1:NBLK], in0=EB[:, 1:NBLK], in1=EB[:, 0:NBLK - 1], op=ALU.is_equal),
            lambda e: e.tensor_scalar(out=skp, in0=skp, scalar1=BIG, scalar2=None, op0=ALU.mult),
            lambda e: e.scalar_tensor_tensor(out=idxw_f, in0=EB, scalar=1024.0, in1=skp, op0=ALU.mult, op1=ALU.add),
            lambda e: e.tensor_scalar(out=idxw_f, in0=idxw_f, scalar1=pidx[:, 0:1], scalar2=None, op0=ALU.add),
            lambda e: e.tensor_tensor(out=idxb_f, in0=EB, in1=skp, op=ALU.add),
            lambda e: e.tensor_copy(out=idxw, in_=idxw_f)]
        for k8 in range(8):
            lay2.append(lambda e, k8=k8: e.tensor_scalar(out=idxw8_f[:, k8, :], in0=idxw_f, scalar1=128.0 * k8, scalar2=None, op0=ALU.add))
        lay2 += [lambda e: e.tensor_copy(out=idxw8, in_=idxw8_f), lambda e: e.tensor_copy(out=idxb, in_=idxb_f)]
        lay2 += [lambda e: e.tensor_scalar(out=pidx4, in0=pidx, scalar1=4.0, scalar2=None, op0=ALU.mult),
                 lambda e: e.scalar_tensor_tensor(out=idxd0, in0=EB, scalar=512.0, in1=skp, op0=ALU.mult, op1=ALU.add),
                 lambda e: e.tensor_scalar(out=idxd0, in0=idxd0, scalar1=pidx4[:, 0:1], scalar2=None, op0=ALU.add)]
        for j4 in range(4):
            lay2.append(lambda e, j4=j4: e.tensor_scalar(out=idxd_f[:, j4, :], in0=idxd0, scalar1=float(j4), scalar2=None, op0=ALU.add))
        lay2 += [lambda e: e.tensor_copy(out=idxd, in_=idxd_f)]
        chain('vector', lay2, reads=[PF[1], 'padf', 'ltri'], writes=['lay'])
        for t in range(NTR):
            b = t % 2
            op('sync', lambda e, t=t, b=b: e.dma_start(out=h2l[b], in_=h2_scr[t * 128:(t + 1) * 128, :]), reads=['h2_scr'], writes=[f'h2l{b}'], dma=f'ldx{b}')

            slf = [lambda e, t=t: e.tensor_tensor(out=slot2, in0=posA[:, t, :], in1=basef, op=ALU.add)]
            for k in range(4):
                slf.append(lambda e, t=t, k=k: e.scalar_tensor_tensor(out=tmp32, in0=lg[:, t, :], scalar=mx8[:, t, k:k + 1], in1=slot2, op0=ALU.is_equal, op1=ALU.mult,
                                                                      accum_out=slotf[:, 4 * t + k:4 * t + k + 1]))
            slf.append(lambda e, t=t: e.tensor_copy(out=sloti[:, 4 * t:4 * t + 4], in_=slotf[:, 4 * t:4 * t + 4]))
            chain('vector', slf, reads=['lay'], writes=[f'sloti{t}', 'slot2'])
            for k in range(4):
                op('gpsimd', lambda e, t=t, k=k, b=b: e.indirect_dma_start(out=xs_scr, out_offset=bass.IndirectOffsetOnAxis(ap=sloti[:, 4 * t + k:4 * t + k + 1], axis=0),
                                                                       in_=h2l[b], in_offset=None, bounds_check=breg(e, NSLOT - 1), oob_is_err=False),
                   reads=[f'sloti{t}', f'h2l{b}'], dma=f'sc{b}')
        dump('sloti', sloti, [128, NTR * 4], I32); dump('idxw', idxw, [128, NBLK], I32); dump('wts', wts, [128, NTR, 4], F32)
        S.barrier()
        if STAGE <= 5:
            return finish(nc, S, out, dbg_outs)

        mark_ex = A.off
        wgu = A.alloc([8, 2 * D], BF16); wdn4 = A.alloc([4, 2 * D], BF16)
        wdn_k = lambda k: wdn4[:, k // 2, (k % 2) * D:(k % 2 + 1) * D]
        wdn_pairs = w_dn.rearrange("e (q two) n -> (e q) (two n)", two=2)
        bgu = A.alloc([2 * D], BF16); bdn = A.alloc([D], BF16)
        xe = [A.alloc([D], BF16) for _ in range(2)]; xT = [A.alloc([8, 128], BF16) for _ in range(2)]
        gs = A.alloc([D], F32); sg_ = A.alloc([D], F32); l1 = A.alloc([D], F32); tt = A.alloc([D], F32)
        actb = A.alloc([D], BF16); aT = A.alloc([8, 128], BF16)
        yo = [A.alloc([D], F32) for _ in range(2)]
        wgu_flat = w_gu.rearrange("e k n -> (e k) n"); wdn_flat = w_dn.rearrange("e k n -> (e k) n")
        wgu_v = bass.AP(tensor=w_gu.tensor, offset=0, ap=[[2 * D, NE * D - 896], [128 * 2 * D, 8], [1, 2 * D]])
        wdn_v = bass.AP(tensor=w_dn.tensor, offset=0, ap=[[D, NE * D - 896], [128 * D, 8], [1, D]])
        gs2 = [gs, A.alloc([D], F32)]; sg2 = [sg_, A.alloc([D], F32)]; l12 = [l1, A.alloc([D], F32)]; tt2 = [tt, A.alloc([D], F32)]
        actb2 = [actb, A.alloc([D], BF16)]; aT2 = [aT, A.alloc([8, 128], BF16)]

        def blk_ldgu(blk):
            b = blk % 2
            ib = bass.IndirectOffsetOnAxis(ap=idxb[:, blk:blk + 1], axis=0)
            for k8 in range(8):
                op('gpsimd', lambda e, k8=k8: e.indirect_dma_start(out=wgu[:, k8, :], out_offset=None, in_=wgu_flat,
                                                                   in_offset=bass.IndirectOffsetOnAxis(ap=idxw8[:, k8, blk:blk + 1], axis=0),
                                                                   bounds_check=breg(e, NE * D - 1), oob_is_err=False),
                   reads=['lay'], writes=[f'wgu{k8}'], dma=f'ld_wgu{k8}')
            op('gpsimd', lambda e: e.indirect_dma_start(out=bgu, out_offset=None, in_=b_gu, in_offset=ib, bounds_check=breg(e, NE - 1), oob_is_err=False),
               reads=['lay'], writes=['bgu'], dma='ld_bgu')
            op('sync', lambda e: e.dma_start(out=xe[b], in_=xs_scr[blk * 128:(blk + 1) * 128, :]), writes=[f'xe{b}'], dma=f'ldx{b}')

        def blk_lddn(blk):
            ib = bass.IndirectOffsetOnAxis(ap=idxb[:, blk:blk + 1], axis=0)
            for j4 in range(4):
                op('gpsimd', lambda e, j4=j4: e.indirect_dma_start(out=wdn4[:, j4, :], out_offset=None, in_=wdn_pairs,
                                                                   in_offset=bass.IndirectOffsetOnAxis(ap=idxd[:, j4, blk:blk + 1], axis=0),
                                                                   bounds_check=breg(e, NE * 512 - 1), oob_is_err=False),
                   reads=['lay'], writes=[f'wdn{j4}'], dma=f'ld_wdn{j4}')
            op('gpsimd', lambda e: e.indirect_dma_start(out=bdn, out_offset=None, in_=b_dn, in_offset=ib, bounds_check=breg(e, NE - 1), oob_is_err=False),
               reads=['lay'], writes=['bdn'], dma='ld_bdn')

        def blk_trx(blk):
            b = blk % 2

            def trx(e):
                for k in range(8):
                    r = e.transpose(pbf[0][:, k * 128:(k + 1) * 128], xe[b][:, k * 128:(k + 1) * 128], ident)
                return r
            op('tensor', trx, reads=[f'xe{b}', 'const'], writes=[PB[0]])
            op('scalar', lambda e: e.activation(out=xT[b], in_=pbf[0].rearrange("p (k c) -> p k c", k=8), func=AF.Copy), reads=[PB[0]], writes=[f'xT{b}'])

        def blk_gu(blk):
            b = blk % 2
            g_, s_, l_, t_, a_ = gs2[b], sg2[b], l12[b], tt2[b], actb2[b]

            for k in range(8):
                def mguk(e, k=k):
                    for n in range(4):
                        r = e.matmul(pf[n], lhsT=xT[b][:, k, :], rhs=wgu[:, k, n * 512:(n + 1) * 512], start=(k == 0), stop=False)
                    return r
                op('tensor', mguk, reads=[f'xT{b}', f'wgu{k}'], writes=[PF[0], PF[1], PF[2], PF[3]])

            def mgub(e):
                for n in range(4):
                    r = e.matmul(pf[n], lhsT=ones_bf[0:1, :], rhs=bgu[0:1, n * 512:(n + 1) * 512], start=False, stop=True)
                return r
            op('tensor', mgub, reads=['bgu', 'const'], writes=[PF[0], PF[1], PF[2], PF[3]])
            op('vector', lambda e: e.tensor_scalar(out=g_, in0=pq[0], scalar1=7.0, scalar2=None, op0=ALU.min), reads=[PF[0], PF[1]], writes=[f'gs{b}'])
            op('scalar', lambda e: e.activation(out=s_, in_=g_, func=AF.Sigmoid, scale=1.702), reads=[f'gs{b}'], writes=[f'sg{b}'])
            op('vector', lambda e: e.tensor_scalar(out=l_, in0=pq[1], scalar1=7.0, scalar2=-7.0, op0=ALU.min, op1=ALU.max), reads=[PF[2], PF[3]], writes=[f'l1{b}'])
            op('vector', lambda e: e.tensor_tensor(out=t_, in0=g_, in1=s_, op=ALU.mult), reads=[f'gs{b}', f'sg{b}'], writes=[f'tt{b}'])
            op('vector', lambda e: e.scalar_tensor_tensor(out=a_, in0=l_, scalar=1.0, in1=t_, op0=ALU.add, op1=ALU.mult), reads=[f'l1{b}', f'tt{b}'], writes=[f'actb{b}'])

        def blk_tra(blk):
            b = blk % 2
            a_ = actb2[b]

            def tra(e):
                for k in range(8):
                    r = e.transpose(pbf[1][:, k * 128:(k + 1) * 128], a_.rearrange("t (p k) -> t k p", k=8)[:, k, :], ident)
                return r
            op('tensor', tra, reads=[f'actb{b}', 'const'], writes=[PB[1]])
            op('scalar', lambda e: e.activation(out=aT2[b], in_=pbf[1].rearrange("p (k c) -> p k c", k=8), func=AF.Copy), reads=[PB[1]], writes=[f'aT{b}'])

        def blk_dn(blk):
            b = blk % 2

            for j4 in range(4):
                def mdnj(e, j4=j4):
                    for k in (2 * j4, 2 * j4 + 1):
                        for n in range(2):
                            r = e.matmul(pf[4 + n], lhsT=aT2[b][:, k, :], rhs=wdn_k(k)[:, n * 512:(n + 1) * 512], start=(k == 0), stop=False)
                    return r
                op('tensor', mdnj, reads=[f'aT{b}', f'wdn{j4}'], writes=[PF[4], PF[5]])

            def mdnb(e):
                for n in range(2):
                    r = e.matmul(pf[4 + n], lhsT=ones_bf[0:1, :], rhs=bdn[0:1, n * 512:(n + 1) * 512], start=False, stop=True)
                return r
            op('tensor', mdnb, reads=['bdn', 'const'], writes=[PF[4], PF[5]])
            op('scalar', lambda e: e.activation(out=yo[b], in_=pq[2], func=AF.Copy), reads=[PF[4], PF[5]], writes=[f'yo{b}'])
            op('sync', lambda e: e.dma_start(out=y_scr[blk * 128:(blk + 1) * 128, :], in_=yo[b]), reads=[f'yo{b}'], dma=f'sty{b}')

        blk_ldgu(0); blk_lddn(0); blk_trx(0)
        for sblk in range(NBLK):
            blk_gu(sblk)
            if sblk >= 1:
                blk_tra(sblk - 1)
            if sblk + 1 < NBLK:
                blk_ldgu(sblk + 1)
                blk_trx(sblk + 1)
            if sblk >= 1:
                blk_dn(sblk - 1)
                blk_lddn(sblk)
        blk_tra(NBLK - 1); blk_dn(NBLK - 1)
        S.barrier()

        A.reset(mark_ex)
        gk = [[A.alloc([D], F32) for _ in range(4)] for _ in range(2)]
        acc = A.alloc([D], F32); x1l = [A.alloc([D], F32) for _ in range(2)]; ot = [A.alloc([D], F32) for _ in range(2)]
        jk = A.alloc([D], F32)
        fs = A.alloc([NTR, 2], F32)
        for t in range(NTR):
            b = t % 2
            for k in range(4):
                op('gpsimd', lambda e, t=t, k=k, b=b: e.indirect_dma_start(out=gk[b][k], out_offset=None, in_=y_scr,
                                                                       in_offset=bass.IndirectOffsetOnAxis(ap=sloti[:, 4 * t + k:4 * t + k + 1], axis=0),
                                                                       bounds_check=breg(e, NSLOT - 1), oob_is_err=False),
                   reads=['y_scr'], writes=[f'gk{b}{k}'], dma=f'ga{b}')
            op('sync', lambda e, t=t, b=b: e.dma_start(out=x1l[b], in_=x1_scr[t * 128:(t + 1) * 128, :]), reads=['x1_scr'], writes=[f'x1l{b}'], dma=f'ldx{b}')

            cmb = [lambda e, t=t, b=b: e.tensor_scalar(out=acc, in0=gk[b][0], scalar1=wts[:, t, 0:1], scalar2=None, op0=ALU.mult)]
            for k in range(1, 4):
                cmb.append(lambda e, t=t, b=b, k=k: e.scalar_tensor_tensor(out=acc, in0=gk[b][k], scalar=wts[:, t, k:k + 1], in1=acc, op0=ALU.mult, op1=ALU.add))
            chain('vector', cmb, reads=[f'gk{b}{k}' for k in range(4)], writes=['acc'])
            op('scalar', lambda e, t=t: e.activation(out=jk, in_=acc, func=AF.Square, accum_out=fs[:, t, 0:1]), reads=['acc'], writes=['jk', 'fss'])
            rstd(fs[:, t, 1:2], fs[:, t, 0:1], D, ['fss'], 'fsr')
            op('vector', lambda e, t=t: e.scalar_tensor_tensor(out=acc, in0=acc, scalar=fs[:, t, 1:2], in1=G2, op0=ALU.mult, op1=ALU.mult), reads=['acc', 'fsr'], writes=['acc'])
            op('gpsimd', lambda e, b=b: e.tensor_tensor(out=ot[b], in0=acc, in1=x1l[b], op=ALU.add), reads=['acc', f'x1l{b}'], writes=[f'ot{b}'])
            op('sync', lambda e, t=t, b=b: e.dma_start(out=out[t * 128:(t + 1) * 128, :], in_=ot[b]), reads=[f'ot{b}'], dma=f'sto{b}')
        return finish(nc, S, out, dbg_outs)


def finish(nc, S, out, dbg_outs):
    S.barrier()
    S.emit()
    return nc, dbg_outs


_CACHE = {}


def _host_tables():
    if 'rope' in _CACHE:
        return _CACHE['rope'], _CACHE['tblidx']
    half = 32; nf = 16
    freqs = (10000.0 ** (-np.arange(nf, dtype=np.float32) / nf)).astype(np.float32)
    rope = {}
    for hf in range(2):
        rng_rows = np.arange(28 * hf, 28 * hf + 36)
        rest = np.arange(36, 64) if hf == 0 else np.arange(0, 28)
        rows = np.concatenate([rng_rows, rest])
        tok = (rows[:, None] * 64 + np.arange(64)[None, :]).reshape(-1)
        r = (tok // 64).astype(np.float32); c = (tok % 64).astype(np.float32)
        cosT = np.ones((4352, 64), np.float32); sinT = np.zeros((4352, 64), np.float32)
        for hi, pos in enumerate((r, c)):
            ang = pos[:, None] * freqs[None, :]
            co = np.cos(ang).astype(np.float32); si = np.sin(ang).astype(np.float32)
            cosT[:4096, hi * 32:hi * 32 + 16] = co; cosT[:4096, hi * 32 + 16:hi * 32 + 32] = co
            sinT[:4096, hi * 32:hi * 32 + 16] = -si; sinT[:4096, hi * 32 + 16:hi * 32 + 32] = si
        rope[hf] = (np.ascontiguousarray(np.tile(cosT, (1, 8))), np.ascontiguousarray(np.tile(sinT, (1, 8))), tok)
    qc = np.arange(64)[:, None]; kc = np.arange(64)[None, :]
    c0 = np.clip(qc - 8, 0, 48)
    valid = (kc >= c0) & (kc < c0 + 16)
    off = np.clip(kc - qc + 15, 0, 30)
    _CACHE['rope'] = rope; _CACHE['tblidx'] = (valid, off)
    return rope, (valid, off)


def kernel(x, c, ctx, c_ctx, w_mod, b_mod, g_pre_mix, g_post_mix, g_pre_ffn, g_post_ffn, w_in, rpb, g_qnorm, g_knorm,
           w_out_a, w_out_b, w_o, w_router, b_router, w_gu, b_gu, w_dn, b_dn):
    f = lambda a: np.ascontiguousarray(np.asarray(a, dtype=np.float32))
    x = f(x); ctx = f(ctx); c = f(c); c_ctx = f(c_ctx)
    rope, (valid, off) = _host_tables()
    rp = f(rpb)[0]
    T = rp[:, :, off]
    T = np.where(valid[None, None], T, np.float32(NEG)).astype(np.float32)
    T = T.transpose(0, 2, 1, 3).reshape(4, 2 * 64, 15 * 64)
    w_in0 = f(w_in)[0]
    qb = w_in0[:, 1792:2304].reshape(1024, 2, 4, 64).transpose(0, 2, 1, 3).reshape(1024, 512)
    w_in_p = w_in0.copy(); w_in_p[:, 1792:2304] = qb
    shared = dict(w_mod=f(w_mod)[0], b_mod=f(b_mod)[0], gvec=np.stack([f(g_pre_mix)[0], f(g_post_mix)[0], f(g_pre_ffn)[0], f(g_post_ffn)[0]]),
                  w_in=w_in_p, tbl=np.ascontiguousarray(T), gqk=np.stack([f(g_qnorm)[0], f(g_knorm)[0]]),
                  w_oa=f(w_out_a)[0], w_ob=f(w_out_b)[0], w_o=f(w_o)[0], w_r=f(w_router)[0], b_r=f(b_router)[0],
                  w_gu=f(w_gu)[0], b_gu=f(b_gu)[0], w_dn=f(w_dn)[0], b_dn=f(b_dn)[0])
    in_maps = []
    for core in range(8):
        b, hf = core // 2, core % 2
        cosT, sinT, tok = rope[hf]
        xcore = np.concatenate([x[b][tok], ctx[b]], axis=0)
        m = dict(shared)
        m.update(xc=np.ascontiguousarray(xcore), cvec=np.stack([c[b], c_ctx]), ropec=cosT, ropes=sinT)
        in_maps.append(m)
    key = ('nc', STAGE, tuple(DEBUG))
    if key not in _CACHE:
        _CACHE[key] = build()
    nc, dbg = _CACHE[key]
    res = run_bass_kernel_spmd(nc, in_maps, core_ids=list(range(8)))
    _CACHE['last'] = res
    outp = np.empty((4, 4096, 1024), np.float32)
    for core in range(8):
        b, hf = core // 2, core % 2
        o = res.results[core]["out"]
        if hf == 0:
            outp[b, 0:2048] = o[0:2048]
        else:
            outp[b, 2048:4096] = o[256:2304]
    return outp
```

```python
import numpy as np
from contextlib import ExitStack
import concourse.bass as bass
import concourse.mybir as mybir
from concourse.bass_utils import run_bass_kernel_spmd

F32 = mybir.dt.float32; BF16 = mybir.dt.bfloat16; I32 = mybir.dt.int32; U8 = mybir.dt.uint8
AF = mybir.ActivationFunctionType; ALU = mybir.AluOpType; AX = mybir.AxisListType
ENG = ('tensor', 'vector', 'scalar', 'gpsimd', 'sync')
DSZ = {F32: 4, BF16: 2, I32: 4, U8: 1}

D = 1024; NTR = 18; TOKR = 2304; NTALL = 34; NKEY = 4352; NE = 32
NBLK = 104; NSLOT = NBLK * 128
EPS = 1e-6; NEG = -30000.0; BIG = 1.0e6
STAGE = 99
SAME_ENG_SYNC = True
DEBUG = []


class Sched:
    def __init__(self, nc, stack):
        self.nc = nc; self.stack = stack
        self.ops = {e: [] for e in ENG}
        self.sems = {}; self.cnt = {}
        self.last_write = {}; self.readers = {}
        self.waited = {e: {} for e in ENG}

    def sem(self, name):
        if name not in self.sems:
            self.sems[name] = self.stack.enter_context(self.nc.semaphore(name)); self.cnt[name] = 0
        return self.sems[name]

    def op(self, eng, fn, reads=(), writes=(), dma=None):
        waits = {}
        isdma_op = dma is not None

        def need(tok):
            if tok is None:
                return
            sname, val, teng, isdma = tok
            if teng == eng and not isdma and not isdma_op and (eng == 'tensor' or not SAME_ENG_SYNC):
                return
            if self.waited[eng].get(sname, 0) >= val:
                return
            waits[sname] = max(waits.get(sname, 0), val)
        for b in reads:
            need(self.last_write.get(b))
        for b in writes:
            need(self.last_write.get(b))
            for r in self.readers.get(b, ()):
                need(r)
        for s, v in waits.items():
            self.waited[eng][s] = v
        if isdma_op:
            sname = dma; inc = 16
        else:
            sname = 'e_' + eng; inc = 1
        self.sem(sname); self.cnt[sname] += inc
        tok = (sname, self.cnt[sname], eng, isdma_op)
        for b in writes:
            self.last_write[b] = tok; self.readers[b] = []
        for b in reads:
            self.readers.setdefault(b, []).append(tok)
        self.ops[eng].append((list(waits.items()), fn, sname, inc))
        return tok

    def barrier(self):
        for e in ENG:
            waits = []
            for s, c in self.cnt.items():
                if c > 0 and self.waited[e].get(s, 0) < c:
                    waits.append((s, c)); self.waited[e][s] = c
            if waits:
                self.ops[e].append((waits, None, None, None))
        self.last_write = {}; self.readers = {}

    def emit(self):
        with self.nc.Block() as block:
            for eng in ENG:
                ops = self.ops[eng]
                if not ops:
                    continue

                def body(e, ops=ops):
                    for waits, fn, sname, inc in ops:
                        for s, v in waits:
                            e.wait_ge(self.sems[s], v)
                        if fn is not None:
                            fn(e).then_inc(self.sems[sname], inc)
                getattr(block, eng)(body)


class Arena:
    def __init__(self, nc, st, name, nbytes):
        self.t = st.enter_context(nc.sbuf_tensor(name, [128, nbytes], U8)); self.off = 0; self.n = nbytes; self.name = name

    def alloc(self, free_shape, dt):
        n = int(np.prod(free_shape)) * DSZ[dt]
        n_al = (n + 63) // 64 * 64
        assert self.off + n_al <= self.n, (self.name, self.off, n_al, self.n)
        ap = self.t[:, self.off:self.off + n].bitcast(dt)
        self.off += n_al
        if len(free_shape) == 2:
            ap = ap.rearrange("p (a b) -> p a b", a=free_shape[0])
        elif len(free_shape) == 3:
            ap = ap.rearrange("p (a b c) -> p a b c", a=free_shape[0], b=free_shape[1])
        return ap

    def reset(self, off=0):
        self.off = off


def build():
    nc = bass.Bass("TRN2", target_bir_lowering=False)
    dt_in = lambda name, shape, dt=F32: nc.dram_tensor(name, shape, dt, kind="ExternalInput").ap()
    xc = dt_in("xc", [NKEY, D]); cvec = dt_in("cvec", [2, D]); w_mod = dt_in("w_mod", [D, 6 * D]); b_mod = dt_in("b_mod", [6 * D])
    gvec = dt_in("gvec", [4, D]); w_in = dt_in("w_in", [D, 4352]); tbl = dt_in("tbl", [4, 128, 960]); gqk = dt_in("gqk", [2, 64])
    ropec = dt_in("ropec", [NKEY, 512]); ropes = dt_in("ropes", [NKEY, 512])
    w_oa = dt_in("w_oa", [512, D]); w_ob = dt_in("w_ob", [512, D]); w_o = dt_in("w_o", [D, D])
    w_r = dt_in("w_r", [D, NE]); b_r = dt_in("b_r", [NE])
    w_gu = dt_in("w_gu", [NE, D, 2 * D]); b_gu = dt_in("b_gu", [NE, 2 * D]); w_dn = dt_in("w_dn", [NE, D, D]); b_dn = dt_in("b_dn", [NE, D])
    out = nc.dram_tensor("out", [TOKR, D], F32, kind="ExternalOutput").ap()
    hT_scr = nc.dram_tensor("hT_scr", [128, 8, NKEY + 128], BF16, kind="Internal").ap()
    x1_scr = nc.dram_tensor("x1_scr", [TOKR, D], F32, kind="Internal").ap()
    h2_scr = nc.dram_tensor("h2_scr", [TOKR, D], BF16, kind="Internal").ap()
    xs_scr = nc.dram_tensor("xs_scr", [NSLOT, D], BF16, kind="Internal").ap()
    y_scr = nc.dram_tensor("y_scr", [NSLOT, D], F32, kind="Internal").ap()
    dbg_outs = {}
    REG = {}

    def breg(e, v):
        if v not in REG:
            REG[v] = e.to_reg(v)
        return REG[v]

    with ExitStack() as st:
        S = Sched(nc, st)
        op = S.op

        def chain(eng, fns, reads=(), writes=()):
            for fn_ in fns:
                op(eng, fn_, reads=list(reads), writes=list(writes))
        A = Arena(nc, st, "arena", 182 * 1024)
        P = Arena(nc, st, "persist", 24 * 1024)
        pq = [st.enter_context(nc.psum_tensor(f"pq{i}", [128, 1024], F32)) for i in range(3)]
        pbf = [st.enter_context(nc.psum_tensor(f"pbf{i}", [128, 1024], BF16)) for i in range(2)]
        pq = [t_[:, :] for t_ in pq]; pbf = [t_[:, :] for t_ in pbf]
        pf = [pq[i // 2][:, (i % 2) * 512:(i % 2) * 512 + 512] for i in range(6)]
        PF = [f"pf{i}" for i in range(6)]; PB = ["pb0", "pb1"]

        def dump(name, ap, shape, dt):
            if name not in DEBUG:
                return
            S.barrier()
            o = nc.dram_tensor("dbg_" + name, shape, dt, kind="ExternalOutput").ap()
            dbg_outs[name] = o
            op('sync', lambda e: e.dma_start(out=o, in_=ap), dma='dbg')

        rows_late = P.alloc([4, D], F32)
        ident = P.alloc([128], BF16)
        identf = P.alloc([128], F32)
        ones_bf = P.alloc([128], BF16)
        ones_f = P.alloc([128], F32)
        G1, A2, B2, G2 = rows_late[:, 0, :], rows_late[:, 1, :], rows_late[:, 2, :], rows_late[:, 3, :]

        chain('gpsimd', [
            lambda e: e.memset(identf, 0.0),
            lambda e: e.affine_select(out=identf, in_=identf, pattern=[[-1, 128]], compare_op=ALU.not_equal, fill=1.0, base=0, channel_multiplier=1),
            lambda e: e.memset(ones_f, 1.0),
            lambda e: e.tensor_copy(out=ones_bf, in_=ones_f),
            lambda e: e.tensor_copy(out=ident, in_=identf)], writes=['const'])

        A.reset()
        rows_early = A.alloc([4, D], F32)
        A1, B1, A1c, B1c = rows_early[:, 0, :], rows_early[:, 1, :], rows_early[:, 2, :], rows_early[:, 3, :]
        mark_p1 = A.off
        modB = A.alloc([6 * D], F32); modC = A.alloc([2 * D], F32)
        gB = A.alloc([4, D], F32)
        bmB = A.alloc([6 * D], F32)
        cT = A.alloc([2, 8], F32); sT = A.alloc([2, 8], F32)
        rep = A.alloc([2, 8, 128], BF16)
        wm = [A.alloc([8, 512], BF16) for _ in range(2)]
        op('sync', lambda e: e.dma_start(out=cT, in_=cvec.rearrange("j (k p) -> p j k", p=128), allow_slow_non_contiguous=True), writes=['cT'], dma='d_cT')
        for i in range(4):
            op('sync', lambda e, i=i: e.dma_start(out=gB[:, i, :], in_=gvec[i, :].partition_broadcast(128)), writes=['gB'], dma='d_gB')
        op('sync', lambda e: e.dma_start(out=bmB, in_=b_mod.partition_broadcast(128)), writes=['bmB'], dma='d_bmB')
        op('scalar', lambda e: e.activation(out=sT, in_=cT, func=AF.Silu), reads=['cT'], writes=['sT'])

        def mk_rep(e):
            for j in range(2):
                for k in range(8):
                    r = e.tensor_scalar(out=rep[:, j, k, :], in0=ones_f, scalar1=sT[:, j, k:k + 1], scalar2=None, op0=ALU.mult)
            return r
        op('vector', mk_rep, reads=['sT', 'const'], writes=['rep'])
        for n in range(12):
            wb = wm[n % 2]
            op('gpsimd', lambda e, n=n, wb=wb: e.dma_start(out=wb, in_=w_mod[:, n * 512:(n + 1) * 512].rearrange("(k p) c -> p k c", p=128)),
               writes=[f'wm{n % 2}'], dma=f'ld_wm{n % 2}')
            for j in range(2 if n < 4 else 1):
                bk = (2 * n + j) % 6

                def mm(e, j=j, wb=wb, bk=bk):
                    for k in range(8):
                        r = e.matmul(pf[bk], lhsT=rep[:, j, k, :], rhs=wb[:, k, :], start=(k == 0), stop=(k == 7))
                    return r
                op('tensor', mm, reads=['rep', f'wm{n % 2}'], writes=[PF[bk]])
                dst = (modB if j == 0 else modC)[:, n * 512:(n + 1) * 512]
                op('vector', lambda e, dst=dst, bk=bk, n=n: e.tensor_tensor(out=dst, in0=pf[bk], in1=bmB[:, n * 512:(n + 1) * 512], op=ALU.add),
                   reads=[PF[bk], 'bmB'], writes=['modB'])

        def mk_rows(e):
            e.scalar_tensor_tensor(out=A1, in0=modB[:, D:2 * D], scalar=1.0, in1=gB[:, 0, :], op0=ALU.add, op1=ALU.mult)
            e.tensor_copy(out=B1, in_=modB[:, 0:D])
            e.scalar_tensor_tensor(out=A1c, in0=modC[:, D:2 * D], scalar=1.0, in1=gB[:, 0, :], op0=ALU.add, op1=ALU.mult)
            e.tensor_copy(out=B1c, in_=modC[:, 0:D])
            e.tensor_tensor(out=G1, in0=modB[:, 2 * D:3 * D], in1=gB[:, 1, :], op=ALU.mult)
            e.scalar_tensor_tensor(out=A2, in0=modB[:, 4 * D:5 * D], scalar=1.0, in1=gB[:, 2, :], op0=ALU.add, op1=ALU.mult)
            e.tensor_copy(out=B2, in_=modB[:, 3 * D:4 * D])
            return e.tensor_tensor(out=G2, in0=modB[:, 5 * D:6 * D], in1=gB[:, 3, :], op=ALU.mult)
        op('vector', mk_rows, reads=['modB', 'gB'], writes=['rows'])
        dump('rows_early', rows_early, [128, 4, D], F32)
        S.barrier()

        A.reset(mark_p1)
        xt = [A.alloc([D], F32) for _ in range(2)]
        hn = [A.alloc([D], F32) for _ in range(2)]
        hb = [A.alloc([D], BF16) for _ in range(2)]
        hTt = [A.alloc([8, 128], BF16) for _ in range(2)]
        junk = A.alloc([D], F32)
        ss = A.alloc([NTALL], F32); rs = A.alloc([NTALL], F32)

        def rstd(dst, src, n, reads, key):
            op('vector', lambda e: e.tensor_scalar(out=dst, in0=src, scalar1=1.0 / n, scalar2=EPS, op0=ALU.mult, op1=ALU.add), reads=reads, writes=[key])
            op('scalar', lambda e: e.activation(out=dst, in_=dst, func=AF.Sqrt), reads=[key], writes=[key])
            op('vector', lambda e: e.reciprocal(out=dst, in_=dst), reads=[key], writes=[key])

        def p1_a(t):
            b = t % 2
            Ar, Br = (A1, B1) if t < 32 else (A1c, B1c)
            op('sync', lambda e, t=t, b=b: e.dma_start(out=xt[b], in_=xc[t * 128:(t + 1) * 128, :]), writes=[f'xt{b}'], dma=f'ldx{b}')
            op('scalar', lambda e, t=t, b=b: e.activation(out=junk, in_=xt[b], func=AF.Square, accum_out=ss[:, t:t + 1]), reads=[f'xt{b}'], writes=['junk', f'ss{t}'])
            rstd(rs[:, t:t + 1], ss[:, t:t + 1], D, [f'ss{t}'], f'rs{t}')
            op('vector', lambda e, t=t, b=b, Ar=Ar: e.scalar_tensor_tensor(out=hn[b], in0=xt[b], scalar=rs[:, t:t + 1], in1=Ar, op0=ALU.mult, op1=ALU.mult),
               reads=[f'xt{b}', f'rs{t}', 'rows'], writes=[f'hn{b}'])
            op('gpsimd', lambda e, b=b, Br=Br: e.tensor_tensor(out=hb[b], in0=hn[b], in1=Br, op=ALU.add), reads=[f'hn{b}', 'rows'], writes=[f'hb{b}'])


        def p1_b(t):
            b = t % 2
            def tr(e, b=b):
                for k in range(8):
                    r = e.transpose(pbf[b][:, k * 128:(k + 1) * 128], hb[b][:, k * 128:(k + 1) * 128], ident)
                return r
            op('tensor', tr, reads=[f'hb{b}', 'const'], writes=[PB[b]])
            op('scalar', lambda e, b=b: e.activation(out=hTt[b], in_=pbf[b].rearrange("p (k c) -> p k c", k=8), func=AF.Copy), reads=[PB[b]], writes=[f'hTt{b}'])
            op('sync', lambda e, t=t, b=b: e.dma_start(out=hT_scr[:, :, t * 128:(t + 1) * 128], in_=hTt[b]), reads=[f'hTt{b}'], dma=f'sth{b}')

        p1_a(0)
        for t in range(NTALL):
            if t + 1 < NTALL:
                p1_a(t + 1)
            p1_b(t)
        S.barrier()
        if STAGE <= 1:
            return finish(nc, S, out, dbg_outs)

        A.reset()
        o_aT = A.alloc([4, TOKR], BF16)
        mark_oa = A.off
        wA = A.alloc([8, 1536], BF16)
        QaT = A.alloc([4, TOKR], BF16); KaT = A.alloc([4, TOKR], BF16)
        Va_e = A.alloc([18, 512], BF16); Va_o = A.alloc([17, 512], BF16)
        KcaT = A.alloc([4, 256], BF16); Vca = A.alloc([2, 512], BF16)
        tblS = A.alloc([4, 960], F32)
        hTg = [A.alloc([8, 576], BF16) for _ in range(2)]
        sbt = [A.alloc([768], F32) for _ in range(2)]
        pbt = [A.alloc([768], BF16) for _ in range(2)]
        pnt = [A.alloc([768], BF16) for _ in range(2)]
        pTt = [A.alloc([768], BF16) for _ in range(2)]
        sm = A.alloc([2, 4], F32)
        for i, (c0, nm) in enumerate(((0, 'ka'), (512, 'va'), (1280, 'qa'))):
            op('gpsimd', lambda e, i=i, c0=c0: e.dma_start(out=wA[:, :, i * 512:(i + 1) * 512], in_=w_in[:, c0:c0 + 512].rearrange("(k p) c -> p k c", p=128)),
               writes=['wA'], dma='d_wA')
        for p in range(4):
            op('sync', lambda e, p=p: e.dma_start(out=tblS[:, p, :], in_=tbl[p]), writes=['tbl'], dma='d_tbl')
        bkc = [0]

        def nbk():
            bkc[0] = (bkc[0] + 1) % 6
            return bkc[0]

        def proj_fm(lhs_cols, rhs_ap, ntok, dst, scale=None):
            bk = nbk()

            def mm(e):
                for k in range(8):
                    r = e.matmul(pf[bk][:, 0:ntok], lhsT=wA[:, k, lhs_cols[0]:lhs_cols[1]], rhs=rhs_ap(k), start=(k == 0), stop=(k == 7))
                return r
            op('tensor', mm, reads=['wA', 'hTg'], writes=[PF[bk]])
            if scale is None:
                op('scalar', lambda e: e.activation(out=dst, in_=pf[bk][:, 0:ntok], func=AF.Copy), reads=[PF[bk]], writes=['naprep'])
            else:
                op('scalar', lambda e: e.activation(out=dst, in_=pf[bk][:, 0:ntok], func=AF.Copy, scale=scale), reads=[PF[bk]], writes=['naprep'])

        def proj_tm(lhs_ap, dst):
            bk = nbk()

            def mm(e):
                for k in range(8):
                    r = e.matmul(pf[bk], lhsT=lhs_ap(k), rhs=wA[:, k, 512:1024], start=(k == 0), stop=(k == 7))
                return r
            op('tensor', mm, reads=['wA', 'hTg'], writes=[PF[bk]])
            op('vector', lambda e: e.tensor_copy(out=dst, in_=pf[bk]), reads=[PF[bk]], writes=['naprep'])

        for g in range(5):
            hg = hTg[g % 2]
            ntok = 512 if g < 4 else 256
            op('sync', lambda e, g=g, hg=hg: e.dma_start(out=hg, in_=hT_scr[:, :, g * 512:g * 512 + 576]), reads=['hT_scr'], writes=['hTg'], dma=f'ldh{g % 2}')
            for c in range(4):
                proj_fm((c * 128, (c + 1) * 128), lambda k, hg=hg, ntok=ntok: hg[:, k, 0:ntok], ntok, KaT[:, c, g * 512:g * 512 + ntok])
                proj_fm((1024 + c * 128, 1024 + (c + 1) * 128), lambda k, hg=hg, ntok=ntok: hg[:, k, 0:ntok], ntok, QaT[:, c, g * 512:g * 512 + ntok], scale=0.125)
            for j in range(ntok // 128):
                proj_tm(lambda k, hg=hg, j=j: hg[:, k, j * 128:(j + 1) * 128], Va_e[:, 4 * g + j, :])
                if 4 * g + j <= 16:
                    proj_tm(lambda k, hg=hg, j=j: hg[:, k, 64 + j * 128:64 + (j + 1) * 128], Va_o[:, 4 * g + j, :])
        hg = hTg[1]
        op('sync', lambda e, hg=hg: e.dma_start(out=hg[:, :, 0:256], in_=hT_scr[:, :, 4096:4352]), reads=['hT_scr'], writes=['hTg'], dma='ldh1')
        for c in range(4):
            proj_fm((c * 128, (c + 1) * 128), lambda k, hg=hg: hg[:, k, 0:256], 256, KcaT[:, c, :])
        for j in range(2):
            proj_tm(lambda k, hg=hg, j=j: hg[:, k, j * 128:(j + 1) * 128], Vca[:, j, :])
        dump('QaT', QaT, [128, 4, TOKR], BF16); dump('KaT', KaT, [128, 4, TOKR], BF16); dump('Va_e', Va_e, [128, 18, 512], BF16)

        na_its = [(l, p) for l in range(36) for p in range(4)]

        def na_ctx(it):
            l, p = na_its[it]
            start = min(max(l - 4, 0), 28); u0 = start - l + 7; tok0 = start * 64
            b = it % 2
            return l, p, start, u0, tok0, b

        def na_stage1(it):
            l, p, start, u0, tok0, b = na_ctx(it)
            sl, sc, po = pf[b], pf[2 + b], pf[4 + b]
            sb_, pb_, pn_, pT_ = sbt[b], pbt[b], pnt[b], pTt[b]

            def qk(e, l=l, p=p, tok0=tok0, sl=sl, sc=sc):
                for hh in range(2):
                    ps_ = slice(hh * 64, hh * 64 + 64)
                    e.matmul(sl[ps_, :], lhsT=QaT[ps_, p, l * 64:(l + 1) * 64], rhs=KaT[ps_, p, tok0:tok0 + 512], start=True, stop=True, tile_position=(hh * 64, hh * 64))
                    r = e.matmul(sc[ps_, 0:256], lhsT=QaT[ps_, p, l * 64:(l + 1) * 64], rhs=KcaT[ps_, p, :], start=True, stop=True, tile_position=(hh * 64, hh * 64))
                return r
            op('tensor', qk, reads=['naprep'], writes=[PF[b], PF[2 + b]])
            op('vector', lambda e, sb_=sb_, sl=sl, p=p, u0=u0: e.tensor_tensor(out=sb_[:, 0:512], in0=sl, in1=tblS[:, p, u0 * 64:u0 * 64 + 512], op=ALU.add),
               reads=[PF[b], 'tbl'], writes=[f'sbA{b}'])
            op('scalar', lambda e, sb_=sb_, sc=sc: e.activation(out=sb_[:, 512:768], in_=sc[:, 0:256], func=AF.Copy), reads=[PF[2 + b]], writes=[f'sbB{b}'])

            chain('vector', [
                lambda e, sb_=sb_, b=b: e.tensor_reduce(out=sm[:, b, 0:1], in_=sb_, axis=AX.X, op=ALU.max),
                lambda e, b=b: e.tensor_scalar(out=sm[:, b, 1:2], in0=sm[:, b, 0:1], scalar1=-1.0, scalar2=None, op0=ALU.mult)],
                reads=[f'sbA{b}', f'sbB{b}'], writes=[f'negm{b}'])
            op('scalar', lambda e, sb_=sb_, pb_=pb_, b=b: e.activation(out=pb_, in_=sb_, func=AF.Exp, bias=sm[:, b, 1:2], scale=1.0, accum_out=sm[:, b, 2:3]),
               reads=[f'sbA{b}', f'sbB{b}', f'negm{b}'], writes=[f'pb{b}', f'sum{b}'])
            op('vector', lambda e, b=b: e.reciprocal(out=sm[:, b, 3:4], in_=sm[:, b, 2:3]), reads=[f'sum{b}'], writes=[f'rsum{b}'])
            op('gpsimd', lambda e, pn_=pn_, pb_=pb_, b=b: e.tensor_scalar(out=pn_, in0=pb_, scalar1=sm[:, b, 3:4], scalar2=None, op0=ALU.mult),
               reads=[f'pb{b}', f'rsum{b}'], writes=[f'pn{b}'])


        def na_stage2(it):
            l, p, start, u0, tok0, b = na_ctx(it)
            sl, sc, po = pf[b], pf[2 + b], pf[4 + b]
            sb_, pb_, pn_, pT_ = sbt[b], pbt[b], pnt[b], pTt[b]
            def trp(e, pn_=pn_, b=b):
                for c in range(6):
                    r = e.transpose(pbf[b][:, c * 128:(c + 1) * 128], pn_[:, c * 128:(c + 1) * 128], ident)
                return r
            op('tensor', trp, reads=[f'pn{b}', 'const'], writes=[PB[b]])
            op('scalar', lambda e, pT_=pT_, b=b: e.activation(out=pT_, in_=pbf[b][:, 0:768], func=AF.Copy), reads=[PB[b]], writes=[f'pT{b}'])

            def pv(e, pT_=pT_, po=po, p=p, start=start):
                for hh in range(2):
                    for c in range(6):
                        if c < 4:
                            V = Va_e[:, start // 2 + c, :] if start % 2 == 0 else Va_o[:, (start - 1) // 2 + c, :]
                        else:
                            V = Vca[:, c - 4, :]
                        r = e.matmul(po[hh * 64:hh * 64 + 64, 0:64], lhsT=V[:, p * 128 + hh * 64:p * 128 + hh * 64 + 64],
                                     rhs=pT_[:, c * 128 + hh * 64:c * 128 + hh * 64 + 64], start=(c == 0), stop=(c == 5), tile_position=(0, hh * 64))
                return r
            op('tensor', pv, reads=[f'pT{b}', 'naprep'], writes=[PF[4 + b]])
            op('vector', lambda e, po=po, p=p, l=l: e.tensor_copy(out=o_aT[:, p, l * 64:(l + 1) * 64], in_=po[:, 0:64]), reads=[PF[4 + b]], writes=['o_aT'])

        for step in range(len(na_its) + 1):
            if step < len(na_its):
                na_stage1(step)
            if step >= 1:
                na_stage2(step - 1)
        dump('o_aT', o_aT, [128, 4, TOKR], BF16)
        S.barrier()
        if STAGE <= 2:
            return finish(nc, S, out, dbg_outs)

        A.reset(mark_oa)
        o_bT = A.alloc([8, TOKR], BF16)
        mark_ob = A.off
        wB = A.alloc([8, 768], BF16)
        QbT = A.alloc([4, TOKR], BF16); KbT = A.alloc([NKEY], BF16)
        Vb = A.alloc([NTALL, 2, 65], BF16)
        gqB = A.alloc([2, 64], F32)
        gtmp = A.alloc([2, 64], F32)
        negC = A.alloc([4], F32)
        hTg = [A.alloc([8, 512], BF16) for _ in range(2)]
        rc = [A.alloc([512], F32) for _ in range(2)]; rsn = [A.alloc([512], F32) for _ in range(2)]
        sq = A.alloc([640], F32); ssh = A.alloc([2, 16], F32)
        qn = A.alloc([640], F32); t1 = A.alloc([640], F32); t2 = A.alloc([640], F32)
        qbb = [A.alloc([640], BF16) for _ in range(2)]
        pTg = [A.alloc([512], BF16) for _ in range(4)]
        osb = [A.alloc([512], F32) for _ in range(2)]
        rec = [A.alloc([512], F32) for _ in range(2)]
        op('gpsimd', lambda e: e.dma_start(out=wB[:, :, 0:256], in_=w_in[:, 1024:1280].rearrange("(k p) c -> p k c", p=128)), writes=['wB'], dma='d_wB')
        op('gpsimd', lambda e: e.dma_start(out=wB[:, :, 256:768], in_=w_in[:, 1792:2304].rearrange("(k p) c -> p k c", p=128)), writes=['wB'], dma='d_wB')
        for i in range(2):
            op('sync', lambda e, i=i: e.dma_start(out=gtmp[:, i, :], in_=gqk[i, :].partition_broadcast(128)), writes=['gtmp'], dma='d_gt')

        qv = qn[:, 0:128].rearrange("p (a b) -> p a b", a=2)
        chain('vector', [
            lambda e: e.tensor_scalar(out=gqB[:, 0, :], in0=gtmp[:, 0, :], scalar1=0.125, scalar2=None, op0=ALU.mult),
            lambda e: e.tensor_copy(out=gqB[:, 1, :], in_=gtmp[:, 1, :]),
            lambda e: e.tensor_scalar(out=qv, in0=gtmp, scalar1=-1.0, scalar2=None, op0=ALU.mult),
            lambda e: e.tensor_tensor(out=gtmp, in0=gtmp, in1=qv, op=ALU.max),
            lambda e: e.tensor_reduce(out=negC[:, 0:1], in_=gtmp[:, 0, :], axis=AX.X, op=ALU.max),
            lambda e: e.tensor_reduce(out=negC[:, 1:2], in_=gtmp[:, 1, :], axis=AX.X, op=ALU.max),
            lambda e: e.tensor_tensor(out=negC[:, 2:3], in0=negC[:, 0:1], in1=negC[:, 1:2], op=ALU.mult),
            lambda e: e.tensor_scalar(out=negC[:, 3:4], in0=negC[:, 2:3], scalar1=-8.0, scalar2=None, op0=ALU.mult),
            lambda e: e.memset(Vb[:, :, :, 64:65], 1.0)], reads=['gtmp'], writes=['gtmp', 'gqB', 'negC', 'Vb', 'qn'])

        def normrope(src, H, gi, b, dst, tagr):
            W = H * 64
            op('scalar', lambda e: e.activation(out=sq[:, 0:W], in_=src, func=AF.Square), reads=tagr, writes=['sq'])

            op('vector', lambda e: e.tensor_reduce(out=ssh[:, 0, 0:H], in_=sq[:, 0:W].rearrange("p (h d) -> p h d", d=64), axis=AX.X, op=ALU.add), reads=['sq'], writes=['ssh0'])
            rstd(ssh[:, 1, 0:H], ssh[:, 0, 0:H], 64, ['ssh0'], 'ssh1')

            def n1(e):
                for h in range(H):
                    r = e.scalar_tensor_tensor(out=qn[:, h * 64:(h + 1) * 64], in0=src[:, h * 64:(h + 1) * 64], scalar=ssh[:, 1, h:h + 1], in1=gqB[:, gi, :],
                                               op0=ALU.mult, op1=ALU.mult)
                return r
            op('vector', n1, reads=['ssh1', 'gqB'] + tagr, writes=['qn'])
            op('vector', lambda e: e.tensor_tensor(out=t1[:, 0:W], in0=qn[:, 0:W], in1=rc[b][:, 0:W], op=ALU.mult), reads=['qn', f'rc{b}'], writes=['t1'])

            def r2(e):
                q4 = qn[:, 0:W].rearrange("p (a s f) -> p a s f", s=2, f=16)
                s4 = rsn[b][:, 0:W].rearrange("p (a s f) -> p a s f", s=2, f=16)
                o4 = t2[:, 0:W].rearrange("p (a s f) -> p a s f", s=2, f=16)
                e.tensor_tensor(out=o4[:, :, 0, :], in0=q4[:, :, 1, :], in1=s4[:, :, 0, :], op=ALU.mult)
                return e.tensor_tensor(out=o4[:, :, 1, :], in0=q4[:, :, 0, :], in1=s4[:, :, 1, :], op=ALU.mult)
            op('vector', r2, reads=['qn', f'rsn{b}'], writes=['t2'])
            op('vector', lambda e: e.tensor_tensor(out=dst, in0=t1[:, 0:W], in1=t2[:, 0:W], op=ALU.add), reads=['t1', 't2'], writes=['qbb'])

        for t in range(NTALL):
            b = t % 2
            g = t // 4
            hg = hTg[g % 2]
            if t % 4 == 0:
                n = min(512, NKEY - g * 512)
                op('sync', lambda e, g=g, hg=hg, n=n: e.dma_start(out=hg[:, :, 0:n], in_=hT_scr[:, :, g * 512:g * 512 + n]), reads=['hT_scr'], writes=[f'hTg{g % 2}'], dma=f'ldh{g % 2}')
            j = t % 4
            op('sync', lambda e, t=t, b=b: e.dma_start(out=rc[b], in_=ropec[t * 128:(t + 1) * 128, :]), writes=[f'rc{b}'], dma=f'ldr{b}')
            op('sync', lambda e, t=t, b=b: e.dma_start(out=rsn[b], in_=ropes[t * 128:(t + 1) * 128, :]), writes=[f'rsn{b}'], dma=f'lds{b}')
            bk = nbk()

            def mmkv(e, hg=hg, j=j, bk=bk):
                for k in range(8):
                    r = e.matmul(pf[bk][:, 0:256], lhsT=hg[:, k, j * 128:(j + 1) * 128], rhs=wB[:, k, 0:256], start=(k == 0), stop=(k == 7))
                return r
            op('tensor', mmkv, reads=['wB', f'hTg{g % 2}'], writes=[PF[bk]])
            op('scalar', lambda e, t=t, bk=bk: e.activation(out=Vb[:, t, :, 0:64], in_=pf[bk][:, 128:256].rearrange("p (h d) -> p h d", d=64), func=AF.Copy),
               reads=[PF[bk]], writes=['Vb'])
            normrope(pf[bk][:, 0:128], 2, 1, b, qbb[b][:, 0:128], [PF[bk]])
            op('tensor', lambda e, b=b: e.transpose(pbf[b][:, 0:128], qbb[b][:, 0:128], ident), reads=['qbb', 'const'], writes=[PB[b]])
            op('scalar', lambda e, t=t, b=b: e.activation(out=KbT[:, t * 128:(t + 1) * 128], in_=pbf[b][:, 0:128], func=AF.Copy), reads=[PB[b]], writes=['KbT'])
            if t < NTR:
                bk2 = nbk()

                def mmq(e, hg=hg, j=j, bk2=bk2):
                    for k in range(8):
                        r = e.matmul(pf[bk2], lhsT=hg[:, k, j * 128:(j + 1) * 128], rhs=wB[:, k, 256:768], start=(k == 0), stop=(k == 7))
                    return r
                op('tensor', mmq, reads=['wB', f'hTg{g % 2}'], writes=[PF[bk2]])
                normrope(pf[bk2], 8, 0, b, qbb[b][:, 0:512], [PF[bk2]])

                def trq(e, b=b):
                    for gg in range(4):
                        r = e.transpose(pbf[b][:, 128 + gg * 128:128 + (gg + 1) * 128], qbb[b][:, gg * 128:(gg + 1) * 128], ident)
                    return r
                op('tensor', trq, reads=['qbb', 'const'], writes=[PB[b]])
                op('scalar', lambda e, t=t, b=b: e.activation(out=QbT[:, :, t * 128:(t + 1) * 128], in_=pbf[b][:, 128:640].rearrange("p (g c) -> p g c", g=4), func=AF.Copy),
                   reads=[PB[b]], writes=['QbT'])
        dump('QbT', QbT, [128, 4, TOKR], BF16); dump('KbT', KbT, [128, NKEY], BF16); dump('Vb', Vb, [128, NTALL, 2, 65], BF16)

        chunks = [(kvh, qt, c) for kvh in range(2) for qt in range(NTR) for c in range(NTALL)]
        LA = 2
        pending = []

        def gq_S(i):
            kvh, qt, c = chunks[i]
            pr = slice(kvh * 64, kvh * 64 + 64)
            sb_ = i % 4
            st_ = pf[sb_]
            op('tensor', lambda e: e.matmul(st_, lhsT=KbT[pr, c * 128:(c + 1) * 128], rhs=QbT[pr, :, qt * 128:(qt + 1) * 128],
                                            start=True, stop=True, tile_position=(kvh * 64, 0)),
               reads=['KbT', 'QbT'], writes=[PF[sb_]])
            op('scalar', lambda e: e.activation(out=pTg[sb_], in_=st_, func=AF.Exp, bias=negC[:, 3:4], scale=1.0), reads=[PF[sb_], 'negC'], writes=[f'pTg{sb_}'])

        def gq_PV(i, step):
            kvh, qt, c = chunks[i]
            sb_ = i % 4
            ob = (kvh * NTR + qt) % 2
            po = pf[4 + ob]
            bk = 4 + ob
            op('tensor', lambda e: e.matmul(po[0:65, :], lhsT=Vb[:, c, kvh, :], rhs=pTg[sb_], start=(c == 0), stop=(c == NTALL - 1)),
               reads=[f'pTg{sb_}', 'Vb'], writes=[PF[bk]])
            if c == NTALL - 1:
                op('scalar', lambda e: e.activation(out=osb[ob][0:65, :], in_=po[0:65, :], func=AF.Copy), reads=[PF[bk]], writes=[f'osb{ob}'])
                op('vector', lambda e: e.reciprocal(out=rec[ob][64:65, :], in_=osb[ob][64:65, :]), reads=[f'osb{ob}'], writes=[f'rec{ob}'])

                def fin():
                    op('tensor', lambda e: e.matmul(po[0:64, :], lhsT=ones_f[64:65, 0:64], rhs=rec[ob][64:65, :], start=True, stop=True), reads=[f'rec{ob}', 'const'], writes=[PF[bk]])
                    op('vector', lambda e: e.tensor_tensor(out=o_bT[0:64, kvh * 4:(kvh + 1) * 4, qt * 128:(qt + 1) * 128],
                                                           in0=osb[ob][0:64, :].rearrange("p (g t) -> p g t", g=4),
                                                           in1=po[0:64, :].rearrange("p (g t) -> p g t", g=4), op=ALU.mult),
                       reads=[f'osb{ob}', PF[bk]], writes=['o_bT'])
                pending.append((step + 4, fin))

        for step in range(len(chunks) + LA + 8):
            if step < len(chunks):
                gq_S(step)
            if LA <= step < len(chunks) + LA:
                gq_PV(step - LA, step)
            for due, fn_ in list(pending):
                if due <= step:
                    fn_(); pending.remove((due, fn_))
        assert not pending
        dump('o_bT', o_bT, [128, 8, TOKR], BF16)
        S.barrier()
        if STAGE <= 3:
            return finish(nc, S, out, dbg_outs)

        A.reset(mark_ob)
        wG = A.alloc([8, 2048], BF16); wOA = A.alloc([4, D], BF16); wOB = A.alloc([8, D], BF16); wO = A.alloc([8, D], BF16)
        wR = A.alloc([8, NE], BF16); bR = A.alloc([NE], BF16)
        hTg = [A.alloc([8, 512], BF16)] * 2
        zT = [A.alloc([8, 512], BF16) for _ in range(2)]
        sga = A.alloc([512], F32); sgb = A.alloc([512], F32)
        xt4 = A.alloc([D], F32); x1t = [A.alloc([D], F32) for _ in range(2)]; tmpf = A.alloc([D], F32)
        h2b = [A.alloc([D], BF16) for _ in range(2)]; h2T = A.alloc([8, 128], BF16)
        lg = P.alloc([NTR, NE], F32); mx8 = P.alloc([NTR, 8], F32); posA = P.alloc([NTR, NE], F32)
        wts = P.alloc([NTR, 4], F32); sloti = P.alloc([NTR * 4], I32)
        mask = A.alloc([NE], F32); maskb = A.alloc([NE], BF16); cntp = P.alloc([NE], F32)
        sms = A.alloc([NTR, 8], F32); e4 = A.alloc([4], F32)
        utri = A.alloc([128], BF16); utf = A.alloc([128], F32)
        mark_route = A.off
        for (dst, src, nm) in ((wG[:, :, 0:1024], w_in[:, 2304:3328], 0), (wG[:, :, 1024:2048], w_in[:, 3328:4352], 1), (wO, w_o, 2)):
            op('gpsimd', lambda e, dst=dst, src=src: e.dma_start(out=dst, in_=src.rearrange("(k p) c -> p k c", p=128)), writes=['wM'], dma='d_wM')
        op('gpsimd', lambda e: e.dma_start(out=wOA, in_=w_oa.rearrange("(k p) c -> p k c", p=128)), writes=['wM'], dma='d_wM')
        op('gpsimd', lambda e: e.dma_start(out=wOB[0:64], in_=w_ob.rearrange("(h d) c -> d h c", d=64)), writes=['wM'], dma='d_wM')
        op('gpsimd', lambda e: e.dma_start(out=wR, in_=w_r.rearrange("(k p) c -> p k c", p=128)), writes=['wM'], dma='d_wM')
        op('gpsimd', lambda e: e.dma_start(out=bR[0:1, :], in_=b_r.rearrange("(o n) -> o n", o=1)), writes=['wM'], dma='d_wM')

        chain('gpsimd', [
            lambda e: e.memset(utf, 1.0),
            lambda e: e.affine_select(out=utf, in_=utf, pattern=[[1, 128]], compare_op=ALU.is_gt, fill=0.0, base=0, channel_multiplier=-1),
            lambda e: e.memset(cntp, 0.0),
            lambda e: e.tensor_copy(out=utri, in_=utf)], writes=['utri', 'route'])

        for g in range(5):
            hg = hTg[g % 2]; z = zT[g % 2]
            ntok = 512 if g < 4 else 256
            tk = slice(g * 512, g * 512 + ntok)
            op('sync', lambda e, g=g, hg=hg, ntok=ntok: e.dma_start(out=hg[:, :, 0:ntok], in_=hT_scr[:, :, g * 512:g * 512 + ntok]), reads=['hT_scr'], writes=[f'hTg{g % 2}'], dma=f'ldh{g % 2}')
            for oc in range(8):
                def mm4(e, hg=hg, oc=oc, ntok=ntok, tk=tk):
                    for k in range(8):
                        e.matmul(pf[0][:, 0:ntok], lhsT=wG[:, k, oc * 128:(oc + 1) * 128], rhs=hg[:, k, 0:ntok], start=(k == 0), stop=(k == 7))
                    for k in range(8):
                        e.matmul(pf[1][:, 0:ntok], lhsT=wG[:, k, 1024 + oc * 128:1024 + (oc + 1) * 128], rhs=hg[:, k, 0:ntok], start=(k == 0), stop=(k == 7))
                    for k in range(4):
                        e.matmul(pf[2][:, 0:ntok], lhsT=wOA[:, k, oc * 128:(oc + 1) * 128], rhs=o_aT[:, k, tk], start=(k == 0), stop=(k == 3))
                    for k in range(8):
                        r = e.matmul(pf[3][:, 0:ntok], lhsT=wOB[0:64, k, oc * 128:(oc + 1) * 128], rhs=o_bT[0:64, k, tk], start=(k == 0), stop=(k == 7))
                    return r
                op('tensor', mm4, reads=['wM', f'hTg{g % 2}', 'o_aT', 'o_bT'], writes=[PF[0], PF[1], PF[2], PF[3]])

                def sg(e, ntok=ntok):
                    e.activation(out=sga[:, 0:ntok], in_=pf[0][:, 0:ntok], func=AF.Sigmoid)
                    return e.activation(out=sgb[:, 0:ntok], in_=pf[1][:, 0:ntok], func=AF.Sigmoid)
                op('scalar', sg, reads=[PF[0], PF[1]], writes=['sg'])

                def zz(e, ntok=ntok):
                    e.tensor_tensor(out=sga[:, 0:ntok], in0=sga[:, 0:ntok], in1=pf[2][:, 0:ntok], op=ALU.mult)
                    return e.tensor_tensor(out=sgb[:, 0:ntok], in0=sgb[:, 0:ntok], in1=pf[3][:, 0:ntok], op=ALU.mult)
                op('vector', zz, reads=['sg', PF[2], PF[3]], writes=['sg2'])
                op('gpsimd', lambda e, z=z, oc=oc, ntok=ntok: e.tensor_tensor(out=z[:, oc, 0:ntok], in0=sga[:, 0:ntok], in1=sgb[:, 0:ntok], op=ALU.add),
                   reads=['sg2'], writes=[f'zT{g % 2}', 'sg'])
            for j in range(ntok // 128):
                t = 4 * g + j
                yb = pq[2]
                b = t % 2

                def mmy(e, z=z, j=j, yb=yb):
                    for n in range(2):
                        for k in range(8):
                            r = e.matmul(yb[:, n * 512:(n + 1) * 512], lhsT=z[:, k, j * 128:(j + 1) * 128], rhs=wO[:, k, n * 512:(n + 1) * 512], start=(k == 0), stop=(k == 7))
                    return r
                op('tensor', mmy, reads=['wM', f'zT{g % 2}'], writes=[PF[4], PF[5]])
                op('sync', lambda e, t=t: e.dma_start(out=xt4, in_=xc[t * 128:(t + 1) * 128, :]), writes=['xt'], dma='ldx0')
                op('scalar', lambda e, yb=yb, t=t: e.activation(out=tmpf, in_=yb, func=AF.Square, accum_out=sms[:, t, 0:1]), reads=[PF[4], PF[5]], writes=['tmpf', 'ssy'])
                rstd(sms[:, t, 1:2], sms[:, t, 0:1], D, ['ssy'], 'rsy')
                op('vector', lambda e, yb=yb, t=t: e.scalar_tensor_tensor(out=tmpf, in0=yb, scalar=sms[:, t, 1:2], in1=G1, op0=ALU.mult, op1=ALU.mult),
                   reads=[PF[4], PF[5], 'rsy', 'rows'], writes=['tmpf'])
                op('gpsimd', lambda e, b=b: e.tensor_tensor(out=x1t[b], in0=tmpf, in1=xt4, op=ALU.add), reads=['tmpf', 'xt'], writes=[f'x1t{b}'])
                op('sync', lambda e, t=t, b=b: e.dma_start(out=x1_scr[t * 128:(t + 1) * 128, :], in_=x1t[b]), reads=[f'x1t{b}'], dma=f'stx{b}')
                op('scalar', lambda e, t=t, b=b: e.activation(out=tmpf, in_=x1t[b], func=AF.Square, accum_out=sms[:, t, 2:3]), reads=[f'x1t{b}'], writes=['tmpf', 'ss2'])
                rstd(sms[:, t, 3:4], sms[:, t, 2:3], D, ['ss2'], 'rs2')
                op('vector', lambda e, t=t, b=b: e.scalar_tensor_tensor(out=tmpf, in0=x1t[b], scalar=sms[:, t, 3:4], in1=A2, op0=ALU.mult, op1=ALU.mult),
                   reads=[f'x1t{b}', 'rs2', 'rows'], writes=['tmpf'])
                op('gpsimd', lambda e, b=b: e.tensor_tensor(out=h2b[b], in0=tmpf, in1=B2, op=ALU.add), reads=['tmpf', 'rows'], writes=[f'h2b{b}'])
                op('sync', lambda e, t=t, b=b: e.dma_start(out=h2_scr[t * 128:(t + 1) * 128, :], in_=h2b[b]), reads=[f'h2b{b}'], dma=f'sth{b}')

                def trh(e, b=b):
                    for k in range(8):
                        r = e.transpose(pbf[b][:, k * 128:(k + 1) * 128], h2b[b][:, k * 128:(k + 1) * 128], ident)
                    return r
                op('tensor', trh, reads=[f'h2b{b}', 'const'], writes=[PB[b]])
                op('scalar', lambda e, b=b: e.activation(out=h2T, in_=pbf[b].rearrange("p (k c) -> p k c", k=8), func=AF.Copy), reads=[PB[b]], writes=['h2T'])

                def mml(e):
                    for k in range(8):
                        e.matmul(pf[0][:, 0:NE], lhsT=h2T[:, k, :], rhs=wR[:, k, :], start=(k == 0), stop=False)
                    return e.matmul(pf[0][:, 0:NE], lhsT=ones_bf[0:1, :], rhs=bR[0:1, :], start=False, stop=True)
                op('tensor', mml, reads=['h2T', 'wM', 'const'], writes=[PF[0]])

                chain('vector', [
                    lambda e, t=t: e.tensor_copy(out=lg[:, t, :], in_=pf[0][:, 0:NE]),
                    lambda e, t=t: e.max(out=mx8[:, t, :], in_=lg[:, t, :]),
                    lambda e, t=t: e.tensor_scalar(out=mask, in0=lg[:, t, :], scalar1=mx8[:, t, 3:4], scalar2=None, op0=ALU.is_ge),
                    lambda e: e.tensor_copy(out=maskb, in_=mask),
                    lambda e, t=t: e.tensor_scalar(out=sms[:, t, 4:5], in0=mx8[:, t, 0:1], scalar1=-1.0, scalar2=None, op0=ALU.mult)],
                    reads=[PF[0]], writes=['lg', 'maskb', 'negmx'])
                op('scalar', lambda e, t=t: e.activation(out=e4, in_=mx8[:, t, 0:4], func=AF.Exp, bias=sms[:, t, 4:5], scale=1.0, accum_out=sms[:, t, 5:6]),
                   reads=['lg', 'negmx'], writes=['e4'])

                def mmc(e):
                    e.matmul(pf[1][:, 0:NE], lhsT=utri, rhs=maskb, start=True, stop=True)
                    return e.matmul(pf[1][:, NE:2 * NE], lhsT=ones_bf, rhs=maskb, start=True, stop=True)
                op('tensor', mmc, reads=['maskb', 'utri', 'const'], writes=[PF[1]])

                chain('vector', [
                    lambda e, t=t: e.reciprocal(out=sms[:, t, 6:7], in_=sms[:, t, 5:6]),
                    lambda e, t=t: e.tensor_scalar(out=wts[:, t, :], in0=e4, scalar1=sms[:, t, 6:7], scalar2=None, op0=ALU.mult),
                    lambda e, t=t: e.tensor_tensor(out=posA[:, t, :], in0=pf[1][:, 0:NE], in1=cntp, op=ALU.add),
                    lambda e: e.tensor_tensor(out=cntp, in0=cntp, in1=pf[1][:, NE:2 * NE], op=ALU.add)],
                    reads=['e4', PF[1]], writes=['route'])
        dump('lg', lg, [128, NTR, NE], F32); dump('posA', posA, [128, NTR, NE], F32); dump('cntp', cntp, [128, NE], F32)
        S.barrier()
        if STAGE <= 4:
            return finish(nc, S, out, dbg_outs)

        A.reset()
        ci = A.alloc([NE], I32); padf = A.alloc([NE], F32); padT = A.alloc([128], F32); ltri = A.alloc([NE], F32)
        basef = A.alloc([NE], F32); pend = A.alloc([NE], F32)
        thr = A.alloc([NBLK], F32); EB = A.alloc([NBLK], F32); skp = A.alloc([NBLK], F32)
        idxw_f = A.alloc([NBLK], F32); idxb_f = A.alloc([NBLK], F32); pidx = A.alloc([1], F32)
        idxw = A.alloc([NBLK], I32); idxb = A.alloc([NBLK], I32)
        idxw8_f = A.alloc([8, NBLK], F32); idxw8 = A.alloc([8, NBLK], I32)
        idxd_f = A.alloc([4, NBLK], F32); idxd = A.alloc([4, NBLK], I32); idxd0 = A.alloc([NBLK], F32); pidx4 = A.alloc([1], F32)
        slot2 = A.alloc([NE], F32); slotf = A.alloc([NTR * 4], F32); tmp32 = A.alloc([NE], F32)
        h2l = [A.alloc([D], BF16) for _ in range(2)]

        chain('vector', [
            lambda e: e.tensor_scalar(out=padf, in0=cntp, scalar1=127.0, scalar2=None, op0=ALU.add),
            lambda e: e.tensor_copy(out=ci, in_=padf),
            lambda e: e.tensor_single_scalar(out=ci, in_=ci, scalar=7, op=ALU.arith_shift_right),
            lambda e: e.tensor_single_scalar(out=ci, in_=ci, scalar=7, op=ALU.logical_shift_left),
            lambda e: e.tensor_copy(out=padf, in_=ci)], reads=['route'], writes=['padf'])

        chain('gpsimd', [
            lambda e: e.memset(ltri, 1.0),
            lambda e: e.affine_select(out=ltri, in_=ltri, pattern=[[1, NE]], compare_op=ALU.is_gt, fill=0.0, base=0, channel_multiplier=-1),
            lambda e: e.iota(thr, pattern=[[128, NBLK]], base=0, channel_multiplier=0, allow_small_or_imprecise_dtypes=True),
            lambda e: e.iota(pidx, pattern=[[0, 1]], base=0, channel_multiplier=1, allow_small_or_imprecise_dtypes=True)], writes=['ltri'])
        op('tensor', lambda e: e.transpose(pq[0][0:NE, 0:128], padf, identf), reads=['padf', 'const'], writes=[PF[0]])
        op('vector', lambda e: e.tensor_copy(out=padT[0:NE, :], in_=pq[0][0:NE, 0:128]), reads=[PF[0]], writes=['padT'])
        op('tensor', lambda e: e.matmul(pf[1][:, 0:NE], lhsT=padT[0:NE, :], rhs=ltri[0:NE, :], start=True, stop=True), reads=['padT', 'ltri'], writes=[PF[1]])

        lay2 = [
            lambda e: e.tensor_copy(out=basef, in_=pf[1][:, 0:NE]),
            lambda e: e.tensor_tensor(out=pend, in0=basef, in1=padf, op=ALU.add),
            lambda e: e.memset(EB, 0.0)]
        for ex in range(NE):
            lay2.append(lambda e, ex=ex: e.scalar_tensor_tensor(out=EB, in0=thr, scalar=pend[:, ex:ex + 1], in1=EB, op0=ALU.is_ge, op1=ALU.add))
        lay2 += [
            lambda e: e.tensor_scalar(out=EB, in0=EB, scalar1=float(NE - 1), scalar2=None, op0=ALU.min),
            lambda e: e.memset(skp, 0.0),
            lambda e: e.tensor_tensor(out=skp[:, 1:NBLK], in0=EB[:, 1:NBLK], in1=EB[:, 0:NBLK - 1], op=ALU.is_equal),
            lambda e: e.tensor_scalar(out=skp, in0=skp, scalar1=BIG, scalar2=None, op0=ALU.mult),
            lambda e: e.scalar_tensor_tensor(out=idxw_f, in0=EB, scalar=1024.0, in1=skp, op0=ALU.mult, op1=ALU.add),
            lambda e: e.tensor_scalar(out=idxw_f, in0=idxw_f, scalar1=pidx[:, 0:1], scalar2=None, op0=ALU.add),
            lambda e: e.tensor_tensor(out=idxb_f, in0=EB, in1=skp, op=ALU.add),
            lambda e: e.tensor_copy(out=idxw, in_=idxw_f)]
        for k8 in range(8):
            lay2.append(lambda e, k8=k8: e.tensor_scalar(out=idxw8_f[:, k8, :], in0=idxw_f, scalar1=128.0 * k8, scalar2=None, op0=ALU.add))
        lay2 += [lambda e: e.tensor_copy(out=idxw8, in_=idxw8_f), lambda e: e.tensor_copy(out=idxb, in_=idxb_f)]
        lay2 += [lambda e: e.tensor_scalar(out=pidx4, in0=pidx, scalar1=4.0, scalar2=None, op0=ALU.mult),
                 lambda e: e.scalar_tensor_tensor(out=idxd0, in0=EB, scalar=512.0, in1=skp, op0=ALU.mult, op1=ALU.add),
                 lambda e: e.tensor_scalar(out=idxd0, in0=idxd0, scalar1=pidx4[:, 0:1], scalar2=None, op0=ALU.add)]
        for j4 in range(4):
            lay2.append(lambda e, j4=j4: e.tensor_scalar(out=idxd_f[:, j4, :], in0=idxd0, scalar1=float(j4), scalar2=None, op0=ALU.add))
        lay2 += [lambda e: e.tensor_copy(out=idxd, in_=idxd_f)]
        chain('vector', lay2, reads=[PF[1], 'padf', 'ltri'], writes=['lay'])
        for t in range(NTR):
            b = t % 2
            op('sync', lambda e, t=t, b=b: e.dma_start(out=h2l[b], in_=h2_scr[t * 128:(t + 1) * 128, :]), reads=['h2_scr'], writes=[f'h2l{b}'], dma=f'ldx{b}')

            slf = [lambda e, t=t: e.tensor_tensor(out=slot2, in0=posA[:, t, :], in1=basef, op=ALU.add)]
            for k in range(4):
                slf.append(lambda e, t=t, k=k: e.scalar_tensor_tensor(out=tmp32, in0=lg[:, t, :], scalar=mx8[:, t, k:k + 1], in1=slot2, op0=ALU.is_equal, op1=ALU.mult,
                                                                      accum_out=slotf[:, 4 * t + k:4 * t + k + 1]))
            slf.append(lambda e, t=t: e.tensor_copy(out=sloti[:, 4 * t:4 * t + 4], in_=slotf[:, 4 * t:4 * t + 4]))
            chain('vector', slf, reads=['lay'], writes=[f'sloti{t}', 'slot2'])
            for k in range(4):
                op('gpsimd', lambda e, t=t, k=k, b=b: e.indirect_dma_start(out=xs_scr, out_offset=bass.IndirectOffsetOnAxis(ap=sloti[:, 4 * t + k:4 * t + k + 1], axis=0),
                                                                       in_=h2l[b], in_offset=None, bounds_check=breg(e, NSLOT - 1), oob_is_err=False),
                   reads=[f'sloti{t}', f'h2l{b}'], dma=f'sc{b}')
        dump('sloti', sloti, [128, NTR * 4], I32); dump('idxw', idxw, [128, NBLK], I32); dump('wts', wts, [128, NTR, 4], F32)
        S.barrier()
        if STAGE <= 5:
            return finish(nc, S, out, dbg_outs)

        mark_ex = A.off
        wgu = A.alloc([8, 2 * D], BF16); wdn4 = A.alloc([4, 2 * D], BF16)
        wdn_k = lambda k: wdn4[:, k // 2, (k % 2) * D:(k % 2 + 1) * D]
        wdn_pairs = w_dn.rearrange("e (q two) n -> (e q) (two n)", two=2)
        bgu = A.alloc([2 * D], BF16); bdn = A.alloc([D], BF16)
        xe = [A.alloc([D], BF16) for _ in range(2)]; xT = [A.alloc([8, 128], BF16) for _ in range(2)]
        gs = A.alloc([D], F32); sg_ = A.alloc([D], F32); l1 = A.alloc([D], F32); tt = A.alloc([D], F32)
        actb = A.alloc([D], BF16); aT = A.alloc([8, 128], BF16)
        yo = [A.alloc([D], F32) for _ in range(2)]
        wgu_flat = w_gu.rearrange("e k n -> (e k) n"); wdn_flat = w_dn.rearrange("e k n -> (e k) n")
        wgu_v = bass.AP(tensor=w_gu.tensor, offset=0, ap=[[2 * D, NE * D - 896], [128 * 2 * D, 8], [1, 2 * D]])
        wdn_v = bass.AP(tensor=w_dn.tensor, offset=0, ap=[[D, NE * D - 896], [128 * D, 8], [1, D]])
        gs2 = [gs, A.alloc([D], F32)]; sg2 = [sg_, A.alloc([D], F32)]; l12 = [l1, A.alloc([D], F32)]; tt2 = [tt, A.alloc([D], F32)]
        actb2 = [actb, A.alloc([D], BF16)]; aT2 = [aT, A.alloc([8, 128], BF16)]

        def blk_ldgu(blk):
            b = blk % 2
            ib = bass.IndirectOffsetOnAxis(ap=idxb[:, blk:blk + 1], axis=0)
            for k8 in range(8):
                op('gpsimd', lambda e, k8=k8: e.indirect_dma_start(out=wgu[:, k8, :], out_offset=None, in_=wgu_flat,
                                                                   in_offset=bass.IndirectOffsetOnAxis(ap=idxw8[:, k8, blk:blk + 1], axis=0),
                                                                   bounds_check=breg(e, NE * D - 1), oob_is_err=False),
                   reads=['lay'], writes=[f'wgu{k8}'], dma=f'ld_wgu{k8}')
            op('gpsimd', lambda e: e.indirect_dma_start(out=bgu, out_offset=None, in_=b_gu, in_offset=ib, bounds_check=breg(e, NE - 1), oob_is_err=False),
               reads=['lay'], writes=['bgu'], dma='ld_bgu')
            op('sync', lambda e: e.dma_start(out=xe[b], in_=xs_scr[blk * 128:(blk + 1) * 128, :]), writes=[f'xe{b}'], dma=f'ldx{b}')

        def blk_lddn(blk):
            ib = bass.IndirectOffsetOnAxis(ap=idxb[:, blk:blk + 1], axis=0)
            for j4 in range(4):
                op('gpsimd', lambda e, j4=j4: e.indirect_dma_start(out=wdn4[:, j4, :], out_offset=None, in_=wdn_pairs,
                                                                   in_offset=bass.IndirectOffsetOnAxis(ap=idxd[:, j4, blk:blk + 1], axis=0),
                                                                   bounds_check=breg(e, NE * 512 - 1), oob_is_err=False),
                   reads=['lay'], writes=[f'wdn{j4}'], dma=f'ld_wdn{j4}')
            op('gpsimd', lambda e: e.indirect_dma_start(out=bdn, out_offset=None, in_=b_dn, in_offset=ib, bounds_check=breg(e, NE - 1), oob_is_err=False),
               reads=['lay'], writes=['bdn'], dma='ld_bdn')

        def blk_trx(blk):
            b = blk % 2

            def trx(e):
                for k in range(8):
                    r = e.transpose(pbf[0][:, k * 128:(k + 1) * 128], xe[b][:, k * 128:(k + 1) * 128], ident)
                return r
            op('tensor', trx, reads=[f'xe{b}', 'const'], writes=[PB[0]])
            op('scalar', lambda e: e.activation(out=xT[b], in_=pbf[0].rearrange("p (k c) -> p k c", k=8), func=AF.Copy), reads=[PB[0]], writes=[f'xT{b}'])

        def blk_gu(blk):
            b = blk % 2
            g_, s_, l_, t_, a_ = gs2[b], sg2[b], l12[b], tt2[b], actb2[b]

            for k in range(8):
                def mguk(e, k=k):
                    for n in range(4):
                        r = e.matmul(pf[n], lhsT=xT[b][:, k, :], rhs=wgu[:, k, n * 512:(n + 1) * 512], start=(k == 0), stop=False)
                    return r
                op('tensor', mguk, reads=[f'xT{b}', f'wgu{k}'], writes=[PF[0], PF[1], PF[2], PF[3]])

            def mgub(e):
                for n in range(4):
                    r = e.matmul(pf[n], lhsT=ones_bf[0:1, :], rhs=bgu[0:1, n * 512:(n + 1) * 512], start=False, stop=True)
                return r
            op('tensor', mgub, reads=['bgu', 'const'], writes=[PF[0], PF[1], PF[2], PF[3]])
            op('vector', lambda e: e.tensor_scalar(out=g_, in0=pq[0], scalar1=7.0, scalar2=None, op0=ALU.min), reads=[PF[0], PF[1]], writes=[f'gs{b}'])
            op('scalar', lambda e: e.activation(out=s_, in_=g_, func=AF.Sigmoid, scale=1.702), reads=[f'gs{b}'], writes=[f'sg{b}'])
            op('vector', lambda e: e.tensor_scalar(out=l_, in0=pq[1], scalar1=7.0, scalar2=-7.0, op0=ALU.min, op1=ALU.max), reads=[PF[2], PF[3]], writes=[f'l1{b}'])
            op('vector', lambda e: e.tensor_tensor(out=t_, in0=g_, in1=s_, op=ALU.mult), reads=[f'gs{b}', f'sg{b}'], writes=[f'tt{b}'])
            op('vector', lambda e: e.scalar_tensor_tensor(out=a_, in0=l_, scalar=1.0, in1=t_, op0=ALU.add, op1=ALU.mult), reads=[f'l1{b}', f'tt{b}'], writes=[f'actb{b}'])

        def blk_tra(blk):
            b = blk % 2
            a_ = actb2[b]

            def tra(e):
                for k in range(8):
                    r = e.transpose(pbf[1][:, k * 128:(k + 1) * 128], a_.rearrange("t (p k) -> t k p", k=8)[:, k, :], ident)
                return r
            op('tensor', tra, reads=[f'actb{b}', 'const'], writes=[PB[1]])
            op('scalar', lambda e: e.activation(out=aT2[b], in_=pbf[1].rearrange("p (k c) -> p k c", k=8), func=AF.Copy), reads=[PB[1]], writes=[f'aT{b}'])

        def blk_dn(blk):
            b = blk % 2

            for j4 in range(4):
                def mdnj(e, j4=j4):
                    for k in (2 * j4, 2 * j4 + 1):
                        for n in range(2):
                            r = e.matmul(pf[4 + n], lhsT=aT2[b][:, k, :], rhs=wdn_k(k)[:, n * 512:(n + 1) * 512], start=(k == 0), stop=False)
                    return r
                op('tensor', mdnj, reads=[f'aT{b}', f'wdn{j4}'], writes=[PF[4], PF[5]])

            def mdnb(e):
                for n in range(2):
                    r = e.matmul(pf[4 + n], lhsT=ones_bf[0:1, :], rhs=bdn[0:1, n * 512:(n + 1) * 512], start=False, stop=True)
                return r
            op('tensor', mdnb, reads=['bdn', 'const'], writes=[PF[4], PF[5]])
            op('scalar', lambda e: e.activation(out=yo[b], in_=pq[2], func=AF.Copy), reads=[PF[4], PF[5]], writes=[f'yo{b}'])
            op('sync', lambda e: e.dma_start(out=y_scr[blk * 128:(blk + 1) * 128, :], in_=yo[b]), reads=[f'yo{b}'], dma=f'sty{b}')

        blk_ldgu(0); blk_lddn(0); blk_trx(0)
        for sblk in range(NBLK):
            blk_gu(sblk)
            if sblk >= 1:
                blk_tra(sblk - 1)
            if sblk + 1 < NBLK:
                blk_ldgu(sblk + 1)
                blk_trx(sblk + 1)
            if sblk >= 1:
                blk_dn(sblk - 1)
                blk_lddn(sblk)
        blk_tra(NBLK - 1); blk_dn(NBLK - 1)
        S.barrier()

        A.reset(mark_ex)
        gk = [[A.alloc([D], F32) for _ in range(4)] for _ in range(2)]
        acc = A.alloc([D], F32); x1l = [A.alloc([D], F32) for _ in range(2)]; ot = [A.alloc([D], F32) for _ in range(2)]
        jk = A.alloc([D], F32)
        fs = A.alloc([NTR, 2], F32)
        for t in range(NTR):
            b = t % 2
            for k in range(4):
                op('gpsimd', lambda e, t=t, k=k, b=b: e.indirect_dma_start(out=gk[b][k], out_offset=None, in_=y_scr,
                                                                       in_offset=bass.IndirectOffsetOnAxis(ap=sloti[:, 4 * t + k:4 * t + k + 1], axis=0),
                                                                       bounds_check=breg(e, NSLOT - 1), oob_is_err=False),
                   reads=['y_scr'], writes=[f'gk{b}{k}'], dma=f'ga{b}')
            op('sync', lambda e, t=t, b=b: e.dma_start(out=x1l[b], in_=x1_scr[t * 128:(t + 1) * 128, :]), reads=['x1_scr'], writes=[f'x1l{b}'], dma=f'ldx{b}')

            cmb = [lambda e, t=t, b=b: e.tensor_scalar(out=acc, in0=gk[b][0], scalar1=wts[:, t, 0:1], scalar2=None, op0=ALU.mult)]
            for k in range(1, 4):
                cmb.append(lambda e, t=t, b=b, k=k: e.scalar_tensor_tensor(out=acc, in0=gk[b][k], scalar=wts[:, t, k:k + 1], in1=acc, op0=ALU.mult, op1=ALU.add))
            chain('vector', cmb, reads=[f'gk{b}{k}' for k in range(4)], writes=['acc'])
            op('scalar', lambda e, t=t: e.activation(out=jk, in_=acc, func=AF.Square, accum_out=fs[:, t, 0:1]), reads=['acc'], writes=['jk', 'fss'])
            rstd(fs[:, t, 1:2], fs[:, t, 0:1], D, ['fss'], 'fsr')
            op('vector', lambda e, t=t: e.scalar_tensor_tensor(out=acc, in0=acc, scalar=fs[:, t, 1:2], in1=G2, op0=ALU.mult, op1=ALU.mult), reads=['acc', 'fsr'], writes=['acc'])
            op('gpsimd', lambda e, b=b: e.tensor_tensor(out=ot[b], in0=acc, in1=x1l[b], op=ALU.add), reads=['acc', f'x1l{b}'], writes=[f'ot{b}'])
            op('sync', lambda e, t=t, b=b: e.dma_start(out=out[t * 128:(t + 1) * 128, :], in_=ot[b]), reads=[f'ot{b}'], dma=f'sto{b}')
        return finish(nc, S, out, dbg_outs)


def finish(nc, S, out, dbg_outs):
    S.barrier()
    S.emit()
    return nc, dbg_outs


_CACHE = {}


def _host_tables():
    if 'rope' in _CACHE:
        return _CACHE['rope'], _CACHE['tblidx']
    half = 32; nf = 16
    freqs = (10000.0 ** (-np.arange(nf, dtype=np.float32) / nf)).astype(np.float32)
    rope = {}
    for hf in range(2):
        rng_rows = np.arange(28 * hf, 28 * hf + 36)
        rest = np.arange(36, 64) if hf == 0 else np.arange(0, 28)
        rows = np.concatenate([rng_rows, rest])
        tok = (rows[:, None] * 64 + np.arange(64)[None, :]).reshape(-1)
        r = (tok // 64).astype(np.float32); c = (tok % 64).astype(np.float32)
        cosT = np.ones((4352, 64), np.float32); sinT = np.zeros((4352, 64), np.float32)
        for hi, pos in enumerate((r, c)):
            ang = pos[:, None] * freqs[None, :]
            co = np.cos(ang).astype(np.float32); si = np.sin(ang).astype(np.float32)
            cosT[:4096, hi * 32:hi * 32 + 16] = co; cosT[:4096, hi * 32 + 16:hi * 32 + 32] = co
            sinT[:4096, hi * 32:hi * 32 + 16] = -si; sinT[:4096, hi * 32 + 16:hi * 32 + 32] = si
        rope[hf] = (np.ascontiguousarray(np.tile(cosT, (1, 8))), np.ascontiguousarray(np.tile(sinT, (1, 8))), tok)
    qc = np.arange(64)[:, None]; kc = np.arange(64)[None, :]
    c0 = np.clip(qc - 8, 0, 48)
    valid = (kc >= c0) & (kc < c0 + 16)
    off = np.clip(kc - qc + 15, 0, 30)
    _CACHE['rope'] = rope; _CACHE['tblidx'] = (valid, off)
    return rope, (valid, off)


def kernel(x, c, ctx, c_ctx, w_mod, b_mod, g_pre_mix, g_post_mix, g_pre_ffn, g_post_ffn, w_in, rpb, g_qnorm, g_knorm,
           w_out_a, w_out_b, w_o, w_router, b_router, w_gu, b_gu, w_dn, b_dn):
    f = lambda a: np.ascontiguousarray(np.asarray(a, dtype=np.float32))
    x = f(x); ctx = f(ctx); c = f(c); c_ctx = f(c_ctx)
    rope, (valid, off) = _host_tables()
    rp = f(rpb)[0]
    T = rp[:, :, off]
    T = np.where(valid[None, None], T, np.float32(NEG)).astype(np.float32)
    T = T.transpose(0, 2, 1, 3).reshape(4, 2 * 64, 15 * 64)
    w_in0 = f(w_in)[0]
    qb = w_in0[:, 1792:2304].reshape(1024, 2, 4, 64).transpose(0, 2, 1, 3).reshape(1024, 512)
    w_in_p = w_in0.copy(); w_in_p[:, 1792:2304] = qb
    shared = dict(w_mod=f(w_mod)[0], b_mod=f(b_mod)[0], gvec=np.stack([f(g_pre_mix)[0], f(g_post_mix)[0], f(g_pre_ffn)[0], f(g_post_ffn)[0]]),
                  w_in=w_in_p, tbl=np.ascontiguousarray(T), gqk=np.stack([f(g_qnorm)[0], f(g_knorm)[0]]),
                  w_oa=f(w_out_a)[0], w_ob=f(w_out_b)[0], w_o=f(w_o)[0], w_r=f(w_router)[0], b_r=f(b_router)[0],
                  w_gu=f(w_gu)[0], b_gu=f(b_gu)[0], w_dn=f(w_dn)[0], b_dn=f(b_dn)[0])
    in_maps = []
    for core in range(8):
        b, hf = core // 2, core % 2
        cosT, sinT, tok = rope[hf]
        xcore = np.concatenate([x[b][tok], ctx[b]], axis=0)
        m = dict(shared)
        m.update(xc=np.ascontiguousarray(xcore), cvec=np.stack([c[b], c_ctx]), ropec=cosT, ropes=sinT)
        in_maps.append(m)
    key = ('nc', STAGE, tuple(DEBUG))
    if key not in _CACHE:
        _CACHE[key] = build()
    nc, dbg = _CACHE[key]
    res = run_bass_kernel_spmd(nc, in_maps, core_ids=list(range(8)))
    _CACHE['last'] = res
    outp = np.empty((4, 4096, 1024), np.float32)
    for core in range(8):
        b, hf = core // 2, core % 2
        o = res.results[core]["out"]
        if hf == 0:
            outp[b, 0:2048] = o[0:2048]
        else:
            outp[b, 2048:4096] = o[256:2304]
    return outp
```
